# Optimizing a Trainium2 kernel written in Bass

```python
import jax, jax.numpy as jnp
from jax import lax
import numpy as np

D_MODEL = 1024
BATCH = 16
SEQ = 2048
DEPTH = 2

D_MIX = D_MODEL
N_MIXERS = 4
GROUP_W = D_MIX // N_MIXERS
HEAD_DIM = 64
N_HEADS_GROUP = GROUP_W // HEAD_DIM
QK_SCALE = HEAD_DIM ** -0.5
POOL_WINDOWS = (2, 4, 8, 16)
POOL_CH = GROUP_W // len(POOL_WINDOWS)
MAX_WINDOW = 16
RWKV_DECAY_LORA = 64
RWKV_A_LORA = 64
RWKV_GATE_LORA = 128
RWKV_COLS = 3 * GROUP_W + RWKV_DECAY_LORA + RWKV_A_LORA + RWKV_GATE_LORA
RWKV_GN_EPS = 64e-5
GLA_GATE_LORA = 16
GLA_GATE_TAU = 16.0
GLA_COLS = 4 * GROUP_W + GLA_GATE_LORA
CHUNK = 64
D_IN = GROUP_W + 4 * GROUP_W + RWKV_COLS + GLA_COLS
D_FF = 2816
NORM_EPS = 1e-6
GATE_FLOOR = 1e-30

kernel_name = 'hymba_style_pool_hgrn2_rwkv7_gla_macaron'


def rms_norm(x, g, eps=NORM_EPS):
    xf = x.astype(jnp.float32)
    y = xf * lax.rsqrt(jnp.mean(xf * xf, axis=-1, keepdims=True) + eps)
    return (y * g.astype(jnp.float32)).astype(x.dtype)


def swiglu_ffn(x, w_in, w_out):
    gate, up = jnp.split(x @ w_in, 2, axis=-1)
    return (jax.nn.silu(gate) * up) @ w_out


def split_heads(t):
    return t.reshape(t.shape[:-1] + (N_HEADS_GROUP, HEAD_DIM))


def causal_multiscale_pool(p, w_pool, b_pool, scale):
    B, T, _ = p.shape
    cs = jnp.pad(jnp.cumsum(p, axis=1), ((0, 0), (MAX_WINDOW, 0), (0, 0)))
    pos = jnp.arange(T)
    pooled = []
    for gi, w in enumerate(POOL_WINDOWS):
        lo, hi = gi * POOL_CH, (gi + 1) * POOL_CH
        win_sum = cs[:, MAX_WINDOW:, lo:hi] - cs[:, MAX_WINDOW - w:MAX_WINDOW - w + T, lo:hi]
        count = jnp.minimum(pos + 1, w).astype(jnp.float32)[None, :, None]
        pooled.append(win_sum / count)
    pooled = jnp.stack(pooled, axis=2) - p.reshape(B, T, len(POOL_WINDOWS), POOL_CH)
    y = jnp.einsum('btgc,gcd->btgd', pooled, w_pool).reshape(B, T, GROUP_W) + b_pool
    return y * scale


def chunked_gated_linear_attention(q, k, v, log_a):
    B, T, H, K = q.shape
    V = v.shape[-1]
    n = T // CHUNK

    def to_chunks(t):
        return t.reshape(B, n, CHUNK, H, t.shape[-1]).transpose(1, 0, 3, 2, 4)

    causal = jnp.tril(jnp.ones((CHUNK, CHUNK), dtype=bool))[:, :, None]

    def step(S, inp):
        qc, kc, vc, gc = inp
        b = jnp.cumsum(gc, axis=-2)
        diff = b[:, :, :, None, :] - b[:, :, None, :, :]
        decay = jnp.where(causal, jnp.exp(jnp.where(causal, diff, 0.0)), 0.0)
        scores = jnp.einsum('bhtk,bhsk,bhtsk->bhts', qc, kc, decay)
        o = jnp.einsum('bhts,bhsv->bhtv', scores, vc) + jnp.einsum('bhtk,bhkv->bhtv', qc * jnp.exp(b), S)
        b_last = b[:, :, -1:, :]
        S_new = jnp.exp(b_last[:, :, 0, :])[..., None] * S + jnp.einsum('bhsk,bhsv->bhkv', kc * jnp.exp(b_last - b), vc)
        return S_new, o

    S0 = jnp.zeros((B, H, K, V), jnp.float32)
    _, o = lax.scan(step, S0, (to_chunks(q), to_chunks(k), to_chunks(v), to_chunks(log_a)))
    return o.transpose(1, 0, 3, 2, 4).reshape(B, T, H, V)


def rwkv7_scan(r, w, k, v, kk, a):
    B, T, H, N = r.shape

    def step(S, inp):
        r_t, w_t, k_t, v_t, kk_t, a_t = inp
        s_kk = jnp.einsum('bhvk,bhk->bhv', S, kk_t)
        S = (S * w_t[:, :, None, :] - s_kk[..., None] * (kk_t * a_t)[:, :, None, :]
             + v_t[..., :, None] * k_t[:, :, None, :])
        return S, jnp.einsum('bhvk,bhk->bhv', S, r_t)

    xs = tuple(jnp.moveaxis(t, 1, 0) for t in (r, w, k, v, kk, a))
    S0 = jnp.zeros((B, H, N, N), jnp.float32)
    _, out = lax.scan(step, S0, xs)
    return jnp.moveaxis(out, 0, 1)


def token_mixer(u, w_in, w_out, pool_w, pool_b, pool_scale, lb, hgrn_norm,
                rwkv_mu, rwkv_w0, rwkv_w2, rwkv_a0, rwkv_a2, rwkv_g2, rwkv_k_k, rwkv_k_a,
                rwkv_r_k, rwkv_ln_w, rwkv_ln_b, gla_w2, gla_b, gla_norm):
    B, T, _ = u.shape
    G = GROUP_W
    p = (u @ w_in).astype(jnp.float32)
    p_pool, p_hg, p_rw, p_gla = jnp.split(p, [G, 5 * G, 5 * G + RWKV_COLS], axis=-1)

    y_pool = causal_multiscale_pool(p_pool, pool_w, pool_b, pool_scale)

    hq, hf, hi, hg = jnp.split(p_hg, 4, axis=-1)
    f = lb + (1.0 - lb) * jax.nn.sigmoid(hf)
    log_f = jnp.log(jnp.maximum(f, GATE_FLOOR))
    o_h = chunked_gated_linear_attention(split_heads(jax.nn.silu(hq) * QK_SCALE),
                                         split_heads(1.0 - f),
                                         split_heads(hi), split_heads(log_f)).reshape(B, T, G)
    y_hgrn = rms_norm(o_h, hgrn_norm) * jax.nn.sigmoid(hg)

    prev = jnp.pad(p_rw, ((0, 0), (1, 0), (0, 0)))[:, :T]
    p_rw = p_rw + (prev - p_rw) * rwkv_mu
    rr, rk, rv, xw, xa, xg = jnp.split(
        p_rw, [G, 2 * G, 3 * G, 3 * G + RWKV_DECAY_LORA, 3 * G + RWKV_DECAY_LORA + RWKV_A_LORA], axis=-1)
    w_log = -jax.nn.softplus(-(rwkv_w0 + jnp.tanh(xw) @ rwkv_w2)) - 0.5
    decay = jnp.exp(-jnp.exp(w_log))
    a = jax.nn.sigmoid(rwkv_a0 + xa @ rwkv_a2)
    g_r = jax.nn.sigmoid(xg) @ rwkv_g2
    kk = split_heads(rk * rwkv_k_k)
    kk = kk / jnp.maximum(jnp.sqrt(jnp.sum(kk * kk, axis=-1, keepdims=True)), 1e-12)
    rk = rk * (1.0 + (a - 1.0) * rwkv_k_a)
    r_h, k_h, v_h = split_heads(rr), split_heads(rk), split_heads(rv)
    o_r = rwkv7_scan(r_h, split_heads(decay), k_h, v_h, kk, split_heads(a))
    mu = jnp.mean(o_r, axis=-1, keepdims=True)
    var = jnp.mean(jnp.square(o_r - mu), axis=-1, keepdims=True)
    gn = ((o_r - mu) * lax.rsqrt(var + RWKV_GN_EPS)).reshape(B, T, G) * rwkv_ln_w + rwkv_ln_b
    bonus = (jnp.sum(r_h * k_h * split_heads(rwkv_r_k), axis=-1, keepdims=True) * v_h).reshape(B, T, G)
    y_rwkv = (gn + bonus) * g_r

    gq, gk, gv, gg, ga = jnp.split(p_gla, [G, 2 * G, 3 * G, 4 * G], axis=-1)
    log_a = jax.nn.log_sigmoid(ga @ gla_w2 + gla_b) / GLA_GATE_TAU
    o_g = chunked_gated_linear_attention(split_heads(gq * QK_SCALE), split_heads(gk),
                                         split_heads(gv), split_heads(log_a))
    o_g = (o_g * lax.rsqrt(jnp.mean(o_g * o_g, axis=-1, keepdims=True) + NORM_EPS)).reshape(B, T, G)
    y_gla = o_g * gla_norm * jax.nn.silu(gg)

    y = jnp.concatenate([y_pool, y_hgrn, y_rwkv, y_gla], axis=-1).astype(u.dtype)
    return y @ w_out


def setup_inputs(seed: int = 0) -> dict:
    key = jax.random.key(seed)
    ks = jax.random.split(key, 32)
    f32 = jnp.float32
    L, D, G = DEPTH, D_MODEL, GROUP_W

    def dense(k, shape, fan_in):
        return jax.random.normal(k, shape, f32) * fan_in ** -0.5

    def gain(k, shape):
        return 1.0 + 0.1 * jax.random.normal(k, shape, f32)

    def small(k, shape, s):
        return s * jax.random.normal(k, shape, f32)

    return {
        'x': jax.random.normal(ks[0], (BATCH, SEQ, D), f32),
        'norm_ffn1': gain(ks[1], (L, D)),
        'ffn1_w_in': dense(ks[2], (L, D, 2 * D_FF), D),
        'ffn1_w_out': dense(ks[3], (L, D_FF, D), D_FF),
        'norm_mix': gain(ks[4], (L, D)),
        'w_in': dense(ks[5], (L, D, D_IN), D),
        'w_out': dense(ks[6], (L, D_MIX, D), D_MIX),
        'pool_w': dense(ks[7], (L, len(POOL_WINDOWS), POOL_CH, POOL_CH), POOL_CH),
        'pool_b': small(ks[8], (L, G), 0.01),
        'pool_scale': gain(ks[9], (L, G)),
        'hgrn_lb_logits': small(ks[10], (L, G), 0.5),
        'hgrn_norm': gain(ks[11], (L, G)),
        'rwkv_mu': jax.random.uniform(ks[12], (L, RWKV_COLS), f32),
        'rwkv_w0': jax.random.uniform(ks[13], (L, G), f32, -4.0, 0.0),
        'rwkv_w2': dense(ks[14], (L, RWKV_DECAY_LORA, G), RWKV_DECAY_LORA),
        'rwkv_a0': small(ks[15], (L, G), 0.5),
        'rwkv_a2': dense(ks[16], (L, RWKV_A_LORA, G), RWKV_A_LORA),
        'rwkv_g2': dense(ks[17], (L, RWKV_GATE_LORA, G), RWKV_GATE_LORA),
        'rwkv_k_k': 0.85 + small(ks[18], (L, G), 0.1),
        'rwkv_k_a': gain(ks[19], (L, G)),
        'rwkv_r_k': small(ks[20], (L, G), 0.1),
        'rwkv_ln_w': gain(ks[21], (L, G)),
        'rwkv_ln_b': small(ks[22], (L, G), 0.01),
        'gla_w2': dense(ks[23], (L, GLA_GATE_LORA, G), GLA_GATE_LORA),
        'gla_b': small(ks[24], (L, G), 0.5),
        'gla_norm': gain(ks[25], (L, G)),
        'norm_ffn2': gain(ks[26], (L, D)),
        'ffn2_w_in': dense(ks[27], (L, D, 2 * D_FF), D),
        'ffn2_w_out': dense(ks[28], (L, D_FF, D), D_FF),
        'norm_final': gain(ks[29], (D,)),
    }


def reference(x, norm_ffn1, ffn1_w_in, ffn1_w_out, norm_mix, w_in, w_out, pool_w, pool_b,
              pool_scale, hgrn_lb_logits, hgrn_norm, rwkv_mu, rwkv_w0, rwkv_w2, rwkv_a0, rwkv_a2,
              rwkv_g2, rwkv_k_k, rwkv_k_a, rwkv_r_k, rwkv_ln_w, rwkv_ln_b, gla_w2, gla_b, gla_norm,
              norm_ffn2, ffn2_w_in, ffn2_w_out, norm_final):
    sm = jax.nn.softmax(hgrn_lb_logits.astype(jnp.float32), axis=0)
    lower_bounds = jnp.cumsum(sm, axis=0) - sm[0]
    for l in range(DEPTH):
        x = x + 0.5 * swiglu_ffn(rms_norm(x, norm_ffn1[l]), ffn1_w_in[l], ffn1_w_out[l])
        x = x + token_mixer(rms_norm(x, norm_mix[l]), w_in[l], w_out[l], pool_w[l], pool_b[l],
                            pool_scale[l], lower_bounds[l], hgrn_norm[l], rwkv_mu[l], rwkv_w0[l],
                            rwkv_w2[l], rwkv_a0[l], rwkv_a2[l], rwkv_g2[l], rwkv_k_k[l], rwkv_k_a[l],
                            rwkv_r_k[l], rwkv_ln_w[l], rwkv_ln_b[l], gla_w2[l], gla_b[l], gla_norm[l])
        x = x + 0.5 * swiglu_ffn(rms_norm(x, norm_ffn2[l]), ffn2_w_in[l], ffn2_w_out[l])
    return rms_norm(x, norm_final)
```

```python
import numpy as np
from contextlib import ExitStack
import concourse.bass as bass
import concourse.mybir as mybir
from concourse.bass_utils import run_bass_kernel_spmd

F32 = mybir.dt.float32
BF16 = mybir.dt.bfloat16
AF = mybir.ActivationFunctionType
ALU = mybir.AluOpType
ENGS = ['pe', 'act', 'dve', 'pool', 'sp']

D = 1024
KD = 8
FF = 2816
NJ = 22
G = 256
DIN = 3344
NORM_EPS = 1e-6
GN_EPS = 64e-5
QK = 0.125
TB = 512
TBM = 256
CH = 64
NCH = TBM // CH
DEBUG_STAGE = 99

FMG = [(0, 128, 0), (128, 128, 0), (256, 128, 0), (384, 128, 0), (512, 128, 0), (640, 128, 0),
       (1024, 128, 0), (1152, 128, 0),
       (1280, 128, 1), (1408, 128, 1), (1536, 128, 1), (1664, 128, 1), (1792, 128, 1), (1920, 128, 1),
       (2048, 128, 1), (2176, 128, 1),
       (2304, 128, 0), (2432, 128, 0), (2560, 128, 0), (2688, 128, 0), (3072, 128, 0), (3200, 128, 0),
       (3328, 128, 0)]
TMG = [(768, 256, 0), (1792, 256, 1), (2816, 256, 0)]
RW0 = 1280

PV = {}
_o = 0
for _n, _w in [('nf1', 8), ('nmx', 8), ('nf2', 8), ('pool_b', 2), ('pool_s', 2), ('lb0', 2), ('lbl', 2),
               ('hnorm', 2), ('w0', 2), ('a0', 2), ('kk', 2), ('ka', 2), ('rk', 2), ('lnw', 2), ('lnb', 2),
               ('glab', 2), ('gnorm', 2), ('nfin', 8)]:
    PV[_n] = _o
    _o += _w
NPV = _o


class Buf:
    __slots__ = ('name', 'w', 'r', 'const')

    def __init__(self, name, const=False):
        self.name = name
        self.w = None
        self.r = []
        self.const = const


class DSem:
    def __init__(self, h):
        self.h = h
        self.count = 0


class Prog:
    def __init__(self, nc, same_engine_sync=True):
        self.nc = nc
        self.ops = {e: [] for e in ENGS}
        self.cnt = {e: 0 for e in ENGS}
        self.same = same_engine_sync
        self.nops = 0
        self.last_rg = None
        self.last_pe_sig = True

    def op(self, eng, fn, reads=(), writes=(), sig=True, dsem=None, rg=None):
        waits = {}
        if eng == 'pe':
            if rg is not None and self.last_rg is not None and rg != self.last_rg:
                assert self.last_pe_sig
                waits['pe'] = self.cnt['pe']
            self.last_rg = rg
            self.last_pe_sig = sig

        def addw(tok):
            if tok is None:
                return
            k, v = tok
            if k == eng and (eng == 'pe' or not self.same):
                return
            if waits.get(k, 0) < v:
                waits[k] = v
        for b in reads:
            addw(b.w)
        for b in writes:
            addw(b.w)
            for t in b.r:
                addw(t)
        if dsem is not None:
            dsem.count += 16
            tok = (dsem, dsem.count)
            sig = False
        elif sig:
            self.cnt[eng] += 1
            tok = (eng, self.cnt[eng])
        else:
            tok = (eng, self.cnt[eng] + 1)
        for b in reads:
            if not b.const:
                b.r.append(tok)
                if len(b.r) > 48:
                    mx = {}
                    for k, v in b.r:
                        if mx.get(k, 0) < v:
                            mx[k] = v
                    b.r = list(mx.items())
        for b in writes:
            b.w = tok
            b.r = []
        self.ops[eng].append((fn, waits, sig, dsem))
        self.nops += 1
        return tok

    def emit(self, block, esems):
        nc = self.nc
        deco = {'pe': block.tensor, 'act': block.scalar, 'dve': block.vector,
                'pool': block.gpsimd, 'sp': block.sync}
        for e in ENGS:
            ops = self.ops[e]

            def body(eng, ops=ops, e=e):
                known = {}
                for fn, waits, sig, dsem in ops:
                    for k, v in waits.items():
                        if known.get(k, 0) >= v:
                            continue
                        known[k] = v
                        h = k.h if isinstance(k, DSem) else esems[k]
                        eng.wait_ge(h, v)
                    inst = fn(eng)
                    if dsem is not None:
                        inst.then_inc(dsem.h, 16)
                    elif sig:
                        inst.then_inc(esems[e], 1)
            deco[e](body)


def build_program(T, NS, L, mixers=('pool', 'hgrn', 'rwkv', 'gla'), do_ffn=True, do_mix=True):
    NT = T // TB
    nc = bass.Bass("TRN2", target_bir_lowering=False)
    dr = {}

    def din(name, shape):
        dr[name] = nc.dram_tensor(name, list(shape), F32, kind="ExternalInput").ap()
        return dr[name]
    xT = din("xT", [NS, 128, KD, T])
    outT = nc.dram_tensor("outT", [NS, 128, KD, T], F32, kind="ExternalOutput").ap()
    f_win = [[din(f"f{w}_win{l}", [NJ, 128, KD * 256]) for w in (1, 2)] for l in range(L)]
    f_wout = [[din(f"f{w}_wout{l}", [NJ, 128, D]) for w in (1, 2)] for l in range(L)]
    m_fm = [din(f"m_fm{l}", [len(FMG), 128, KD * 128]) for l in range(L)]
    m_tm = [din(f"m_tm{l}", [len(TMG), 128, KD * 256]) for l in range(L)]
    m_wout = [din(f"m_wout{l}", [KD, 128, D]) for l in range(L)]
    pvec_d = [din(f"pvec{l}", [128, NPV]) for l in range(L)]
    mub_d = [din(f"mub{l}", [128, 1024]) for l in range(L)]
    smat_d = [din(f"smat{l}", [128, 5 * 256]) for l in range(L)]
    cst_d = din("cst", [128, 1600])
    es = ExitStack()
    with es:
        def sb(name, shape, dt=F32):
            return es.enter_context(nc.sbuf_tensor("sb_" + name, list(shape), dt))

        def psum(name, shape, dt=F32):
            return es.enter_context(nc.psum_tensor("pp_" + name, list(shape), dt))
        esems = {e: es.enter_context(nc.semaphore("s_" + e)) for e in ENGS}

        def dsem(name):
            return DSem(es.enter_context(nc.semaphore(name)))
        pg = Prog(nc)
        block = es.enter_context(nc.Block())

        X = sb("X", [128, KD, T])
        XB = [[Buf(f"X{k}_{t}") for t in range(NT)] for k in range(KD)]
        xn = sb("xn", [128, KD, TB], BF16)
        xns = sb("xns", [128, KD, TBM], BF16)
        xnsB = Buf("xns")
        xnB = [Buf(f"xn{k}") for k in range(KD)]
        sq = [sb(f"sq{i}", [128, TB], BF16) for i in range(2)]
        sqB = [Buf(f"sq{i}") for i in range(2)]
        rstd = sb("rstd", [128, TB]); rstdB = Buf("rstd")
        ones = sb("ones", [128, 128], BF16); onesB = Buf("ones", const=True)
        bones = sb("bones", [128, 128], BF16)
        ident = sb("ident", [128, 128], BF16)
        cst = sb("cst", [128, 1600])
        cstB = Buf("cst", const=True)
        MI, MSU, MSL, ID4 = 0, 256, 512, 768
        SCM = 1024
        ICN = 1536
        pvec = [sb(f"pvec{l}", [128, NPV]) for l in range(L)]
        pvB = [Buf(f"pvec{l}", const=True) for l in range(L)]
        lbt = sb("lbt", [128, 8])
        mub = sb("mub", [128, 1024]); mubB = Buf("mub")
        smat = sb("smat", [128, 5 * 256]); smatB = Buf("smat")
        smatb = sb("smatb", [128, 2 * 128], BF16)
        wst = [sb(f"wst{i}", [128, KD * 256]) for i in range(2)]
        wstB = [Buf(f"wst{i}") for i in range(2)]
        wstS = [dsem(f"dwst{i}") for i in range(2)]
        wbf = [sb(f"wbf{i}", [128, KD * 256], BF16) for i in range(2)]
        wbfB = [Buf(f"wbf{i}") for i in range(2)]
        wbfB2 = [[Buf(f"wbf{i}a"), Buf(f"wbf{i}b")] for i in range(2)]
        wtmp = sb("wtmp", [128, KD * 256]); wtmpB = Buf("wtmp")
        ost = [sb("ost0", [128, D])] * 2
        ostB = [Buf("ost0")] * 2
        ostS = [dsem("dost0")] * 2
        obf = [sb(f"obf{i}", [128, D], BF16) for i in range(2)]
        obfB = [Buf(f"obf{i}") for i in range(2)]
        hT = sb("hT", [128, NJ, TB], BF16)
        hB = [Buf(f"h{j}") for j in range(NJ)]
        sg = [sb("sg0", [128, TB])] * 2
        sgB = [Buf("sg0")] * 2
        PS = [psum(f"ps{i}", [128, 512]) for i in range(8)]
        PSB = [Buf(f"ps{i}") for i in range(8)]
        xS = [dsem(f"dx{k}") for k in range(KD)]
        oS = [dsem(f"dout{k}") for k in range(KD)]
        cS = dsem("dcst")
        pS = [dsem(f"dpv{l}") for l in range(L)]
        muS = dsem("dmu")
        smS = dsem("dsm")
        counters = {'w': 0, 'o': 0, 'sq': 0, 'sg': 0, 'pp': 0}

        def OP(eng, fn, reads=(), writes=(), sig=True, dsem=None, rg=None):
            return pg.op(eng, fn, reads, writes, sig, dsem, rg)

        def load_w(src_ap, ncols):
            i = counters['w'] % 2
            counters['w'] += 1
            OP('sp', lambda e: e.dma_start(out=wst[i][:, 0:ncols], in_=src_ap), writes=[wstB[i]], dsem=wstS[i])
            return i

        def cast_w(i, ncols, eng='pool'):
            if eng == 'act':
                OP('act', lambda e: e.activation(out=wbf[i][:, 0:ncols], in_=wst[i][:, 0:ncols], func=AF.Copy),
                   reads=[wstB[i]], writes=[wbfB[i], wbfB2[i][0], wbfB2[i][1]])
            else:
                OP(eng, lambda e: e.tensor_copy(out=wbf[i][:, 0:ncols], in_=wst[i][:, 0:ncols]),
                   reads=[wstB[i]], writes=[wbfB[i], wbfB2[i][0], wbfB2[i][1]])

        def cast_w_split(i, ncols):
            c1 = (ncols * 3 // 4) // 128 * 128
            OP('dve', lambda e: e.tensor_copy(out=wbf[i][:, 0:c1], in_=wst[i][:, 0:c1]), reads=[wstB[i]], writes=[wbfB2[i][0], wbfB[i]])
            OP('pool', lambda e: e.tensor_copy(out=wbf[i][:, c1:ncols], in_=wst[i][:, c1:ncols]), reads=[wstB[i]], writes=[wbfB2[i][1]])

        def load_o(src_ap, eng='act'):
            i = counters['o'] % 2
            counters['o'] += 1
            OP('sp', lambda e: e.dma_start(out=ost[i][:], in_=src_ap), writes=[ostB[i]], dsem=ostS[i])
            if eng == 'act':
                OP('act', lambda e: e.activation(out=obf[i][:], in_=ost[i][:], func=AF.Copy),
                   reads=[ostB[i]], writes=[obfB[i]])
            else:
                OP(eng, lambda e: e.tensor_copy(out=obf[i][:], in_=ost[i][:]), reads=[ostB[i]], writes=[obfB[i]])
            return i

        def rstd_from(psb, psbuf, n, scale, eps, dst, dstB):
            OP('act', lambda e: e.activation(out=dst[:, 0:n], in_=psb[:, 0:n], func=AF.Ln, scale=scale, bias=eps),
               reads=[psbuf], writes=[dstB])
            OP('act', lambda e: e.activation(out=dst[:, 0:n], in_=dst[:, 0:n], func=AF.Exp, scale=-0.5),
               reads=[dstB], writes=[dstB])

        def rmsnorm_to_xn(l, gcol, c0, n, bank):
            ti = c0 // TB
            for k in range(KD):
                i = counters['sq'] % 2
                counters['sq'] += 1
                OP('act', lambda e, k=k, i=i: e.activation(out=sq[i][:, 0:n], in_=X[:, k, c0:c0 + n], func=AF.Square),
                   reads=[XB[k][ti]], writes=[sqB[i]])
                OP('pe', lambda e, k=k, i=i: e.matmul(PS[bank][:, 0:n], lhsT=ones[:], rhs=sq[i][:, 0:n], start=(k == 0), stop=(k == KD - 1)),
                   reads=[sqB[i], onesB], writes=[PSB[bank]])
            rstd_from(PS[bank], PSB[bank], n, 1.0 / D, NORM_EPS, rstd, rstdB)
            for k in range(KD):
                OP('dve', lambda e, k=k: e.scalar_tensor_tensor(out=xn[:, k, 0:n], in0=X[:, k, c0:c0 + n],
                                                                 scalar=pvec[l][:, gcol + k:gcol + k + 1], in1=rstd[:, 0:n],
                                                                 op0=ALU.mult, op1=ALU.mult),
                   reads=[XB[k][ti], rstdB, pvB[l]], writes=[xnB[k]])

        def ffn(l, w, ti):
            gcol = PV['nf1'] if w == 0 else PV['nf2']
            c0 = ti * TB
            rmsnorm_to_xn(l, gcol, c0, TB, 0)
            for j in range(NJ):
                i = load_w(f_win[l][w][j], KD * 256)
                cast_w_split(i, KD * 256)
                wv = wbf[i][:].rearrange("p (k c) -> p k c", k=KD)
                pp = counters['pp'] % 2
                counters['pp'] += 1
                bg, bu = 2 * pp, 2 * pp + 1
                for half, bk in ((0, bg), (1, bu)):
                    for k in range(KD):
                        OP('pe', lambda e, k=k, half=half, bk=bk, wv=wv: e.matmul(
                            PS[bk][:, :], lhsT=wv[:, k, half * 128:(half + 1) * 128], rhs=xn[:, k, 0:TB],
                            start=(k == 0), stop=(k == KD - 1)),
                           reads=[wbfB[i], wbfB2[i][0], wbfB2[i][1], xnB[k]], writes=[PSB[bk]], sig=(k == KD - 1))
                si = counters['sg'] % 2
                counters['sg'] += 1
                OP('act', lambda e, si=si, bg=bg: e.activation(out=sg[si][:], in_=PS[bg][:, :], func=AF.Silu),
                   reads=[PSB[bg]], writes=[sgB[si]])
                OP('dve', lambda e, si=si, bu=bu, j=j: e.tensor_tensor(out=hT[:, j, :], in0=sg[si][:], in1=PS[bu][:, :], op=ALU.mult),
                   reads=[sgB[si], PSB[bu]], writes=[hB[j]])
            for j in range(NJ):
                i = load_o(f_wout[l][w][j], 'act')
                for m in range(KD):
                    OP('pe', lambda e, m=m, j=j, i=i: e.matmul(PS[m][:, :], lhsT=obf[i][:, m * 128:(m + 1) * 128], rhs=hT[:, j, :],
                                                           start=(j == 0), stop=(j == NJ - 1)),
                       reads=[obfB[i], hB[j]], writes=[PSB[m]], sig=(m == KD - 1 or j == NJ - 1))
            for m in range(KD):
                OP('dve', lambda e, m=m: e.scalar_tensor_tensor(out=X[:, m, c0:c0 + TB], in0=PS[m][:, :], scalar=0.5,
                                                                 in1=X[:, m, c0:c0 + TB], op0=ALU.mult, op1=ALU.add),
                   reads=[PSB[m], XB[m][ti]], writes=[XB[m][ti]])

        OP('sp', lambda e: e.dma_start(out=cst[:], in_=cst_d), writes=[cstB], dsem=cS)
        for l in range(L):
            OP('sp', lambda e, l=l: e.dma_start(out=pvec[l][:], in_=pvec_d[l]), writes=[pvB[l]], dsem=pS[l])
        OP('pool', lambda e: e.memset(ones[:], 1.0), writes=[onesB])
        bonesB = Buf("bones", const=True)
        OP('pool', lambda e: e.memset(bones[:], 0.0), writes=[bonesB])
        OP('pool', lambda e: e.memset(bones[0:64, 0:64], 1.0), writes=[bonesB])
        OP('pool', lambda e: e.memset(bones[64:128, 64:128], 1.0), writes=[bonesB])
        identB = Buf("ident", const=True)
        OP('pool', lambda e: e.memset(ident[:], 1.0), writes=[identB])
        OP('pool', lambda e: e.affine_select(out=ident[:], in_=ident[:], pattern=[[-1, 128]], compare_op=ALU.is_equal,
                                             fill=0.0, base=0, channel_multiplier=1), reads=[identB], writes=[identB])

        if do_mix:
            YT = sb("YT", [128, KD, TBM], BF16)
            YB = [Buf(f"Y{k}") for k in range(KD)]
            NFA = 10
            FA = [sb(f"fa{i}", [128, TBM]) for i in range(NFA)]
            FAB = [Buf(f"fa{i}") for i in range(NFA)]
            NBA = 4
            BA = [sb(f"ba{i}", [128, TBM], BF16) for i in range(NBA)]
            BAB = [Buf(f"ba{i}") for i in range(NBA)]
            LL = [[sb(f"ll{hp}{i}", [128, TBM]) for i in range(3)] for hp in range(2)]
            LLB_ = [[Buf(f"ll{hp}{i}") for i in range(3)] for hp in range(2)]
            LLX = [sb(f"llx{i}", [128, TBM]) for i in range(2)]
            LLXB = [Buf(f"llx{i}") for i in range(2)]
            LB = [[sb(f"lb{hp}{i}", [128, TBM], BF16) for i in range(5)] for hp in range(2)]
            LBB = [[Buf(f"lb{hp}{i}") for i in range(5)] for hp in range(2)]
            PB16 = [sb(f"pb16{i}", [128, TBM], BF16) for i in range(2)]
            PB16B = [Buf(f"pb16{i}") for i in range(2)]
            Vt = [sb(f"vt{c}", [64, 256], BF16) for c in range(NCH)]
            VtB = [Buf(f"vt{c}") for c in range(NCH)]
            KEt = [sb(f"ket{c}", [64, 256], BF16) for c in range(NCH)]
            KEtB = [Buf(f"ket{c}") for c in range(NCH)]
            AEt = [sb(f"aet{c}", [64, 256], BF16) for c in range(NCH)]
            AEtB = [Buf(f"aet{c}") for c in range(NCH)]
            alias_ctr = [0]

            def mk(name, n):
                ts, bs = [], []
                for c in range(n):
                    idx = alias_ctr[0]
                    alias_ctr[0] += 1
                    j, half = idx // 2, idx % 2
                    ts.append(hT[0:64, j, half * 256:(half + 1) * 256])
                    bs.append(hB[j])
                return ts, bs
            ATs, ATsB = mk("ats", NCH)
            LKs, LKsB = mk("lks", NCH)
            ARs, ARsB = mk("ars", NCH)
            Nn, NnB = mk("nn", NCH)
            NTn, NTnB = mk("ntn", NCH)
            Nn2, Nn2B = mk("nn2", NCH)
            NTn2, NTn2B = mk("ntn2", NCH)
            Pn, PnB = mk("pn", NCH)
            Pn2, Pn2B = mk("pn2", NCH)
            Ysb = sb("ysb", [64, 256], BF16); YsbB = Buf("ysb")
            Usb = sb("usb", [64, 256], BF16); UsbB = Buf("usb")
            S32 = {m: [sb(f"s32{m}{hp}", [128, 64]) for hp in range(2)] for m in ('hgrn', 'gla', 'rwkv')}
            Sbf = {m: [sb(f"sbf{m}{hp}", [128, 64], BF16) for hp in range(2)] for m in ('hgrn', 'gla', 'rwkv')}
            S32B = {m: [Buf(f"s32{m}{hp}") for hp in range(2)] for m in ('hgrn', 'gla', 'rwkv')}
            SbfB = {m: [Buf(f"sbf{m}{hp}") for hp in range(2)] for m in ('hgrn', 'gla', 'rwkv')}
            stmp = [sb(f"stmp{hp}", [128, 64]) for hp in range(2)]
            stmpB = [Buf(f"stmp{hp}") for hp in range(2)]
            pext = sb("pext", [128, 2, 16 + TBM]); pextB = Buf("pext")
            pw = [sb(f"pw{i}", [128, 2, 16 + TBM]) for i in range(2)]
            pwB = [Buf(f"pw{i}") for i in range(2)]
            PST = PS[7][:, :].bitcast(BF16)
            lbtB = Buf("lbt", const=True)
            fa_ctr = [0]
            ba_ctr = [0]

            def fa():
                i = fa_ctr[0] % NFA
                fa_ctr[0] += 1
                return FA[i], FAB[i]

            def ba():
                i = ba_ctr[0] % NBA
                ba_ctr[0] += 1
                return BA[i], BAB[i]

            def pbank():
                b = counters['pp'] % 4
                counters['pp'] += 1
                return b

            def proj_fm(l, g, first_block):
                col0, ncols, shift = FMG[g]
                i = load_w(m_fm[l][g], KD * 128)
                if shift:
                    mc = col0 - RW0
                    mu_b = mub[:, mc:mc + 128].unsqueeze(1).broadcast_to([128, KD, 128])
                    wv32 = wst[i][:, 0:KD * 128].rearrange("p (k c) -> p k c", k=KD)
                    wt = wtmp[:, 0:KD * 128].rearrange("p (k c) -> p k c", k=KD)
                    wbv = wbf[i][:].rearrange("p (k c) -> p k c", k=KD)
                    OP('pool', lambda e: e.tensor_tensor(out=wt, in0=wv32, in1=mu_b, op=ALU.mult),
                       reads=[wstB[i], mubB], writes=[wtmpB])
                    OP('pool', lambda e: e.tensor_tensor(out=wbv[:, :, 0:128], in0=wv32, in1=wt, op=ALU.subtract),
                       reads=[wstB[i], wtmpB], writes=[wbfB[i], wbfB2[i][0], wbfB2[i][1]])
                    OP('pool', lambda e: e.tensor_copy(out=wbv[:, :, 128:256], in_=wt), reads=[wtmpB], writes=[wbfB[i], wbfB2[i][0], wbfB2[i][1]])
                else:
                    cast_w(i, KD * 128, 'act')
                    wbv = wbf[i][:, 0:KD * 128].rearrange("p (k c) -> p k c", k=KD)
                bk = pbank()
                nmm = KD * (2 if shift else 1)
                n = 0
                for k in range(KD):
                    n += 1
                    OP('pe', lambda e, k=k, n=n: e.matmul(PS[bk][0:ncols, 0:TBM], lhsT=wbv[:, k, 0:ncols], rhs=xn[:, k, 0:TBM],
                                                          start=(n == 1), stop=(n == nmm)),
                       reads=[wbfB[i], wbfB2[i][0], wbfB2[i][1], xnB[k]], writes=[PSB[bk]], sig=(n == nmm))
                if shift:
                    for k in range(KD):
                        n += 1
                        OP('pe', lambda e, k=k, n=n: e.matmul(PS[bk][0:ncols, 0:TBM], lhsT=wbv[:, k, 128:128 + ncols], rhs=xns[:, k, 0:TBM],
                                                              start=False, stop=(n == nmm)),
                           reads=[wbfB[i], wbfB2[i][0], wbfB2[i][1], xnsB], writes=[PSB[bk]], sig=(n == nmm))
                return bk

            def proj_tm(l, g):
                col0, ncols, shift = TMG[g]
                i = load_w(m_tm[l][g], KD * 256)
                if shift:
                    i2 = counters['w'] % 2
                    counters['w'] += 1
                    mc = col0 - RW0
                    mu_b = mub[:, mc:mc + 256].unsqueeze(1).broadcast_to([128, KD, 256])
                    wv32 = wst[i][:].rearrange("p (k c) -> p k c", k=KD)
                    wt = wtmp[:].rearrange("p (k c) -> p k c", k=KD)
                    wa = wbf[i][:].rearrange("p (k c) -> p k c", k=KD)
                    wb2 = wbf[i2][:].rearrange("p (k c) -> p k c", k=KD)
                    OP('pool', lambda e: e.tensor_tensor(out=wt, in0=wv32, in1=mu_b, op=ALU.mult),
                       reads=[wstB[i], mubB], writes=[wtmpB])
                    OP('pool', lambda e: e.tensor_tensor(out=wa, in0=wv32, in1=wt, op=ALU.subtract),
                       reads=[wstB[i], wtmpB], writes=[wbfB[i], wbfB2[i][0], wbfB2[i][1]])
                    OP('pool', lambda e: e.tensor_copy(out=wb2, in_=wt), reads=[wtmpB], writes=[wbfB[i2], wbfB2[i2][0], wbfB2[i2][1]])
                else:
                    cast_w(i, KD * 256, 'pool')
                    wa = wbf[i][:].rearrange("p (k c) -> p k c", k=KD)
                for c in range(NCH):
                    bk = pbank()
                    nmm = KD * (2 if shift else 1)
                    n = 0
                    for k in range(KD):
                        n += 1
                        OP('pe', lambda e, k=k, n=n, c=c, bk=bk: e.matmul(PS[bk][0:64, 0:256], lhsT=xn[:, k, c * CH:(c + 1) * CH],
                                                                     rhs=wa[:, k, :], start=(n == 1), stop=(n == nmm)),
                           reads=[wbfB[i], wbfB2[i][0], wbfB2[i][1], xnB[k]], writes=[PSB[bk]], sig=(n == nmm))
                    if shift:
                        for k in range(KD):
                            n += 1
                            OP('pe', lambda e, k=k, n=n, c=c, bk=bk: e.matmul(PS[bk][0:64, 0:256], lhsT=xns[:, k, c * CH:(c + 1) * CH],
                                                                         rhs=wb2[:, k, :], start=False, stop=(n == nmm)),
                               reads=[wbfB[i2], wbfB2[i2][0], wbfB2[i2][1], xnsB], writes=[PSB[bk]], sig=(n == nmm))
                    OP('act', lambda e, c=c, bk=bk: e.activation(out=Vt[c][:], in_=PS[bk][0:64, 0:256], func=AF.Copy),
                       reads=[PSB[bk]], writes=[VtB[c]])

            def scan_decay(g_t, g_b):
                b_t, b_b = fa()
                OP('dve', lambda e: e.tensor_tensor_scan(out=b_t[:], data0=cst[:, SCM:SCM + TBM], data1=g_t[:], initial=0.0,
                                                         op0=ALU.mult, op1=ALU.add), reads=[g_b, cstB], writes=[b_b])
                return b_t, b_b

            def chunk_engine(mname, QE, KE, PC, rw=None):
                isrw = rw is not None
                if DEBUG_STAGE < 1:
                    return
                for c in range(NCH):
                    for (src, dst, dstB_) in ([(KE, KEt, KEtB)] + ([(rw['AE'], AEt, AEtB)] if isrw else [])):
                        for hp in range(2):
                            OP('pe', lambda e, hp=hp, c=c, src=src: e.transpose(PST[0:64, hp * 128:(hp + 1) * 128],
                                                                             src[hp][0][:, c * CH:(c + 1) * CH], ident[:]),
                               reads=[src[hp][1], identB], writes=[PSB[7]])
                        OP('act', lambda e, c=c, dst=dst: e.activation(out=dst[c][:], in_=PST[0:64, 0:256], func=AF.Copy),
                           reads=[PSB[7]], writes=[dstB_[c]])
                if DEBUG_STAGE < 2:
                    return
                for c in range(NCH):
                    cs = slice(c * CH, (c + 1) * CH)
                    def sc(lh, rh, dst_ps, cs=cs):
                        for h in range(4):
                            hp, r = h // 2, (h % 2) * 64
                            OP('pe', lambda e, h=h, hp=hp, r=r: e.matmul(dst_ps[0:64, h * 64:(h + 1) * 64], lhsT=lh[hp][0][r:r + 64, cs],
                                                                         rhs=rh[hp][0][r:r + 64, cs], start=True, stop=True),
                               reads=[lh[hp][1], rh[hp][1]], writes=[PSB[6]], rg=r)
                    sc(KE, QE, PS[6][:, 0:256])
                    OP('dve', lambda e, c=c: e.tensor_tensor(out=ATs[c][:], in0=PS[6][0:64, 0:256], in1=cst[0:64, MI:MI + 256], op=ALU.mult),
                       reads=[PSB[6], cstB], writes=[ATsB[c]])
                    if isrw:
                        sc(KE, rw['BE'], PS[6][:, 256:512])
                        OP('dve', lambda e, c=c: e.tensor_tensor(out=LKs[c][:], in0=PS[6][0:64, 256:512], in1=cst[0:64, MSU:MSU + 256], op=ALU.mult),
                           reads=[PSB[6], cstB], writes=[LKsB[c]])
                        sc(rw['AE'], QE, PS[6][:, 0:256])
                        OP('dve', lambda e, c=c: e.tensor_tensor(out=ARs[c][:], in0=PS[6][0:64, 0:256], in1=cst[0:64, MI:MI + 256], op=ALU.mult),
                           reads=[PSB[6], cstB], writes=[ARsB[c]])
                        sc(rw['AE'], rw['BE'], PS[6][:, 256:512])
                        OP('dve', lambda e, c=c: e.scalar_tensor_tensor(out=NTn[c][:], in0=PS[6][0:64, 256:512], scalar=-1.0,
                                                                         in1=cst[0:64, MSU:MSU + 256], op0=ALU.mult, op1=ALU.mult),
                           reads=[PSB[6], cstB], writes=[NTnB[c]])
                        sc(rw['BE'], rw['AE'], PS[6][:, 0:256])
                        OP('dve', lambda e, c=c: e.scalar_tensor_tensor(out=Nn[c][:], in0=PS[6][0:64, 0:256], scalar=-1.0,
                                                                         in1=cst[0:64, MSL:MSL + 256], op0=ALU.mult, op1=ALU.mult),
                           reads=[PSB[6], cstB], writes=[NnB[c]])
                        OP('pool', lambda e, c=c: e.tensor_tensor(out=Pn[c][:], in0=NTn[c][:], in1=cst[0:64, ID4:ID4 + 256], op=ALU.add),
                           reads=[NTnB[c], cstB], writes=[PnB[c]])
                if isrw:
                    curN, curNB, curNT, curNTB = Nn, NnB, NTn, NTnB
                    nxtN, nxtNB, nxtNT, nxtNTB = Nn2, Nn2B, NTn2, NTn2B
                    curP, curPB, nxtP, nxtPB = Pn, PnB, Pn2, Pn2B
                    for lev in range(1, 6):
                        for c in range(NCH):
                            bk = pbank()
                            for h in range(4):
                                hs = slice(h * 64, (h + 1) * 64)
                                OP('pe', lambda e, c=c, hs=hs, bk=bk, a=curNT, b=curN: e.matmul(PS[bk][0:64, hs], lhsT=a[c][:, hs], rhs=b[c][:, hs],
                                                                                         start=True, stop=True),
                                   reads=[curNTB[c], curNB[c]], writes=[PSB[bk]], rg=0)
                            if lev < 5:
                                for h in range(4):
                                    hs = slice(h * 64, (h + 1) * 64)
                                    hs2 = slice(256 + h * 64, 256 + (h + 1) * 64)
                                    OP('pe', lambda e, c=c, hs=hs, hs2=hs2, bk=bk, a=curN, b=curNT: e.matmul(PS[bk][0:64, hs2], lhsT=a[c][:, hs], rhs=b[c][:, hs],
                                                                                                     start=True, stop=True),
                                       reads=[curNTB[c], curNB[c]], writes=[PSB[bk]], rg=0)
                                OP('act', lambda e, c=c, bk=bk, d=nxtNT: e.activation(out=d[c][:], in_=PS[bk][0:64, 256:512], func=AF.Copy),
                                   reads=[PSB[bk]], writes=[nxtNTB[c]])
                            OP('act', lambda e, c=c, bk=bk, d=nxtN: e.activation(out=d[c][:], in_=PS[bk][0:64, 0:256], func=AF.Copy),
                               reads=[PSB[bk]], writes=[nxtNB[c]])
                        curN, curNB, nxtN, nxtNB = nxtN, nxtNB, curN, curNB
                        curNT, curNTB, nxtNT, nxtNTB = nxtNT, nxtNTB, curNT, curNTB
                        for c in range(NCH):
                            bk = pbank()
                            for h in range(4):
                                hs = slice(h * 64, (h + 1) * 64)
                                OP('pe', lambda e, c=c, hs=hs, bk=bk, a=curN, b=curP: e.matmul(PS[bk][0:64, hs], lhsT=a[c][:, hs], rhs=b[c][:, hs],
                                                                                        start=True, stop=True),
                                   reads=[curNB[c], curPB[c]], writes=[PSB[bk]], rg=0)
                            OP('dve', lambda e, c=c, bk=bk, s=curP, d=nxtP: e.tensor_tensor(out=d[c][:], in0=PS[bk][0:64, 0:256], in1=s[c][:], op=ALU.add),
                               reads=[PSB[bk], curPB[c]], writes=[nxtPB[c]])
                        curP, curPB, nxtP, nxtPB = nxtP, nxtPB, curP, curPB
                    TT, TTB = curP, curPB
                if DEBUG_STAGE < 3:
                    return
                S3, Sb, S3B, SbB = S32[mname], Sbf[mname], S32B[mname], SbfB[mname]
                for c in range(NCH):
                    cs = slice(c * CH, (c + 1) * CH)
                    if isrw:
                        for h in range(4):
                            hp, r = h // 2, (h % 2) * 64
                            hs = slice(h * 64, (h + 1) * 64)
                            OP('pe', lambda e, hp=hp, r=r, hs=hs, cs=cs: e.matmul(PS[6][0:64, hs], lhsT=rw['BE'][hp][0][r:r + 64, cs], rhs=Sb[hp][r:r + 64, :],
                                                                         start=True, stop=False),
                               reads=[rw['BE'][hp][1], SbB[hp]], writes=[PSB[6]], rg=r)
                            OP('pe', lambda e, hs=hs, c=c: e.matmul(PS[6][0:64, hs], lhsT=LKs[c][:, hs], rhs=Vt[c][:, hs], start=False, stop=True),
                               reads=[LKsB[c], VtB[c]], writes=[PSB[6]], rg=0)
                        OP('act', lambda e: e.activation(out=Ysb[:], in_=PS[6][0:64, 0:256], func=AF.Copy), reads=[PSB[6]], writes=[YsbB])
                        for h in range(4):
                            hs = slice(h * 64, (h + 1) * 64)
                            hs2 = slice(256 + h * 64, 256 + (h + 1) * 64)
                            OP('pe', lambda e, hs=hs, hs2=hs2, c=c: e.matmul(PS[6][0:64, hs2], lhsT=TT[c][:, hs], rhs=Ysb[:, hs], start=True, stop=True),
                               reads=[TTB[c], YsbB], writes=[PSB[6]], rg=0)
                        OP('act', lambda e: e.activation(out=Usb[:], in_=PS[6][0:64, 256:512], func=AF.Copy, scale=-1.0),
                           reads=[PSB[6]], writes=[UsbB])
                    for h in range(4):
                        hp, r = h // 2, (h % 2) * 64
                        hs = slice(h * 64, (h + 1) * 64)
                        ob = PS[4 + hp][r:r + 64, cs]
                        OP('pe', lambda e, ob=ob, hs=hs, c=c: e.matmul(ob, lhsT=Vt[c][:, hs], rhs=ATs[c][:, hs], start=True, stop=False),
                           reads=[VtB[c], ATsB[c]], writes=[PSB[4 + hp]], rg=0)
                        if isrw:
                            OP('pe', lambda e, ob=ob, hs=hs, c=c: e.matmul(ob, lhsT=Usb[:, hs], rhs=ARs[c][:, hs], start=False, stop=False),
                               reads=[UsbB, ARsB[c]], writes=[PSB[4 + hp]], rg=0)
                        OP('pe', lambda e, ob=ob, hp=hp, r=r, cs=cs: e.matmul(ob, lhsT=Sb[hp][r:r + 64, :], rhs=QE[hp][0][r:r + 64, cs], start=False, stop=True),
                           reads=[SbB[hp], QE[hp][1]], writes=[PSB[4 + hp]], rg=r)
                    for h in range(4):
                        hp, r = h // 2, (h % 2) * 64
                        hs = slice(h * 64, (h + 1) * 64)
                        sp_ = PS[7][r:r + 64, 256 + hp * 64:256 + (hp + 1) * 64]
                        OP('pe', lambda e, sp_=sp_, hs=hs, c=c: e.matmul(sp_, lhsT=KEt[c][:, hs], rhs=Vt[c][:, hs], start=True, stop=(not isrw)),
                           reads=[KEtB[c], VtB[c]], writes=[PSB[7]], rg=0)
                        if isrw:
                            OP('pe', lambda e, sp_=sp_, hs=hs, c=c: e.matmul(sp_, lhsT=AEt[c][:, hs], rhs=Usb[:, hs], start=False, stop=True),
                               reads=[AEtB[c], UsbB], writes=[PSB[7]], rg=0)
                    for hp in range(2):
                        pc = PC[hp][0][:, (c + 1) * CH - 1:(c + 1) * CH]
                        OP('dve', lambda e, hp=hp: e.tensor_tensor(out=stmp[hp][:], in0=PS[7][:, 256 + hp * 64:256 + (hp + 1) * 64], in1=S3[hp][:], op=ALU.add),
                           reads=[PSB[7], S3B[hp]], writes=[stmpB[hp]])
                        OP('dve', lambda e, hp=hp, pc=pc: e.tensor_scalar(out=S3[hp][:], in0=stmp[hp][:], scalar1=pc, scalar2=None, op0=ALU.mult),
                           reads=[stmpB[hp], PC[hp][1]], writes=[S3B[hp]])
                        OP('dve', lambda e, hp=hp, pc=pc: e.tensor_scalar(out=Sb[hp][:], in0=stmp[hp][:], scalar1=pc, scalar2=None, op0=ALU.mult),
                           reads=[stmpB[hp], PC[hp][1]], writes=[SbB[hp]])

            def evac_fm(bk, func=AF.Copy, scale=1.0, bias=None, dt='f', rows=128, dst=None):
                t, b = dst if dst is not None else (fa() if dt == 'f' else ba())
                kw = {}
                if bias is not None:
                    kw['bias'] = bias
                OP('act', lambda e: e.activation(out=t[0:rows, :], in_=PS[bk][0:rows, 0:TBM], func=func, scale=scale, **kw),
                   reads=[PSB[bk]] + ([pvB[0]] if bias is not None else []), writes=[b])
                return t, b

            def mixer_block(l, s, bi):
                first = (bi == 0)
                c0 = bi * TBM
                ti = c0 // TB
                pv = pvec[l]
                if 'rwkv' in mixers:
                    if first:
                        OP('pool', lambda e: e.memset(xns[:, :, 0:2], 0.0), writes=[xnsB])
                    else:
                        OP('pool', lambda e: e.tensor_copy(out=xns[:, :, 0:1], in_=xn[:, :, TBM - 1:TBM]), reads=xnB, writes=[xnsB])
                rmsnorm_to_xn(l, PV['nmx'], c0, TBM, 0)
                if 'rwkv' in mixers:
                    OP('pool', lambda e: e.tensor_copy(out=xns[:, :, 1:TBM], in_=xn[:, :, 0:TBM - 1]), reads=xnB, writes=[xnsB])
                if 'pool' in mixers:
                    if first:
                        OP('pool', lambda e: e.memset(pext[:, :, 0:16], 0.0), writes=[pextB])
                    else:
                        OP('pool', lambda e: e.tensor_copy(out=pext[:, :, 0:16], in_=pext[:, :, TBM:TBM + 16]), reads=[pextB], writes=[pextB])
                    for ck in range(2):
                        bk = proj_fm(l, ck, first)
                        OP('act', lambda e, ck=ck, bk=bk: e.activation(out=pext[:, ck, 16:16 + TBM], in_=PS[bk][:, 0:TBM], func=AF.Copy),
                           reads=[PSB[bk]], writes=[pextB])
                    W_ = 16 + TBM
                    OP('dve', lambda e: e.tensor_tensor(out=pw[0][:, :, 1:W_], in0=pext[:, :, 1:W_], in1=pext[:, :, 0:W_ - 1], op=ALU.add),
                       reads=[pextB], writes=[pwB[0]])
                    def poolfin(src, ck, r0, wdw, first=first):
                        yt, yb = PB16[ck], PB16B[ck]
                        OP('dve', lambda e: e.scalar_tensor_tensor(out=yt[r0:r0 + 64, :], in0=src[r0:r0 + 64, ck, 16:16 + TBM], scalar=1.0 / wdw,
                                                                   in1=pext[r0:r0 + 64, ck, 16:16 + TBM], op0=ALU.mult, op1=ALU.subtract),
                           reads=[pwB[0], pwB[1], pextB], writes=[yb])
                        if first:
                            t2, b2 = FA[0], FAB[0]
                            OP('dve', lambda e: e.tensor_tensor(out=t2[r0:r0 + 64, 0:16], in0=src[r0:r0 + 64, ck, 16:32],
                                                                in1=cst[r0:r0 + 64, ICN + ck * 16:ICN + ck * 16 + 16], op=ALU.mult),
                               reads=[pwB[0], pwB[1], cstB], writes=[b2])
                            OP('dve', lambda e: e.tensor_tensor(out=yt[r0:r0 + 64, 0:16], in0=t2[r0:r0 + 64, 0:16],
                                                                in1=pext[r0:r0 + 64, ck, 16:32], op=ALU.subtract),
                               reads=[b2, pextB], writes=[yb])
                    poolfin(pw[0], 0, 0, 2)
                    OP('dve', lambda e: e.tensor_tensor(out=pw[1][:, :, 3:W_], in0=pw[0][:, :, 3:W_], in1=pw[0][:, :, 1:W_ - 2], op=ALU.add),
                       reads=[pwB[0]], writes=[pwB[1]])
                    poolfin(pw[1], 0, 64, 4)
                    OP('dve', lambda e: e.tensor_tensor(out=pw[0][:, :, 7:W_], in0=pw[1][:, :, 7:W_], in1=pw[1][:, :, 3:W_ - 4], op=ALU.add),
                       reads=[pwB[1]], writes=[pwB[0]])
                    poolfin(pw[0], 1, 0, 8)
                    OP('dve', lambda e: e.tensor_tensor(out=pw[1][:, :, 15:W_], in0=pw[0][:, :, 15:W_], in1=pw[0][:, :, 7:W_ - 8], op=ALU.add),
                       reads=[pwB[0]], writes=[pwB[1]])
                    poolfin(pw[1], 1, 64, 16)
                    for ck in range(2):
                        bk = pbank()
                        OP('pe', lambda e, ck=ck, bk=bk: e.matmul(PS[bk][:, 0:TBM], lhsT=smatb[:, ck * 128:(ck + 1) * 128], rhs=PB16[ck][:], start=True, stop=True),
                           reads=[PB16B[ck], smatB], writes=[PSB[bk]])
                        OP('dve', lambda e, ck=ck, bk=bk: e.tensor_scalar(out=YT[:, ck, :], in0=PS[bk][:, 0:TBM], scalar1=pv[:, PV['pool_b'] + ck:PV['pool_b'] + ck + 1],
                                                                      scalar2=pv[:, PV['pool_s'] + ck:PV['pool_s'] + ck + 1], op0=ALU.add, op1=ALU.mult),
                           reads=[PSB[bk], pvB[l]], writes=[YB[ck]])
                else:
                    for ck in range(2):
                        OP('pool', lambda e, ck=ck: e.memset(YT[:, ck, :], 0.0), writes=[YB[ck]])

                def out_rstd(Ot, lhs_ones, n_ch, eps):
                    bk = pbank()
                    for hp in range(2):
                        st, sbb = ba()
                        OP('act', lambda e, hp=hp, st=st, Ot=Ot: e.activation(out=st[:], in_=Ot[hp][0][:], func=AF.Square), reads=[Ot[hp][1]], writes=[sbb])
                        if lhs_ones is ones:
                            OP('pe', lambda e, hp=hp, st=st: e.matmul(PS[bk][:, 0:TBM], lhsT=ones[:], rhs=st[:], start=(hp == 0), stop=(hp == 1)),
                               reads=[sbb, onesB], writes=[PSB[bk]])
                        else:
                            bk2 = bk if hp == 0 else pbank()
                            OP('pe', lambda e, hp=hp, st=st, bk2=bk2: e.matmul(PS[bk2][:, 0:TBM], lhsT=bones[:], rhs=st[:], start=True, stop=True),
                               reads=[sbb, bonesB], writes=[PSB[bk2]])
                            if hp == 0:
                                bk0 = bk2
                            else:
                                bk1 = bk2
                    if lhs_ones is ones:
                        rt, rb = fa()
                        rstd_from(PS[bk], PSB[bk], TBM, 1.0 / n_ch, eps, rt, rb)
                        return [(rt, rb), (rt, rb)]
                    res = []
                    for bkx in (bk0, bk1):
                        rt, rb = fa()
                        rstd_from(PS[bkx], PSB[bkx], TBM, 1.0 / n_ch, eps, rt, rb)
                        res.append((rt, rb))
                    return res

                def evac_O():
                    Ot = []
                    for hp in range(2):
                        t, b = fa()
                        OP('act', lambda e, hp=hp, t=t: e.activation(out=t[:], in_=PS[4 + hp][:, 0:TBM], func=AF.Copy), reads=[PSB[4 + hp]], writes=[b])
                        Ot.append((t, b))
                    return Ot

                if 'hgrn' in mixers:
                    QE, KE, PCx, GT = [], [], [], []
                    for hp in range(2):
                        bq = proj_fm(l, 2 + hp, first)
                        qt, qb = evac_fm(bq, AF.Silu)
                        bf_ = proj_fm(l, 4 + hp, first)
                        st_, sb_ = evac_fm(bf_, AF.Sigmoid)
                        ft, fb = fa()
                        OP('dve', lambda e, hp=hp, st_=st_, ft=ft: e.tensor_scalar(out=ft[:], in0=st_[:], scalar1=lbt[:, 2 * l + hp:2 * l + hp + 1],
                                                                             scalar2=lbt[:, 4 + 2 * l + hp:4 + 2 * l + hp + 1], op0=ALU.mult, op1=ALU.add),
                           reads=[sb_, lbtB], writes=[fb])
                        lt, lb_ = fa()
                        OP('dve', lambda e, ft=ft, lt=lt: e.tensor_scalar_max(out=lt[:], in0=ft[:], scalar1=1e-30), reads=[fb], writes=[lb_])
                        OP('act', lambda e, lt=lt: e.activation(out=lt[:], in_=lt[:], func=AF.Ln), reads=[lb_], writes=[lb_])
                        bt, bb = scan_decay(lt, lb_)
                        ebt, ebb = LL[hp][0], LLB_[hp][0]
                        OP('act', lambda e, bt=bt, ebt=ebt: e.activation(out=ebt[:], in_=bt[:], func=AF.Exp), reads=[bb], writes=[ebb])
                        OP('act', lambda e, bt=bt: e.activation(out=bt[:], in_=bt[:], func=AF.Exp, scale=-1.0), reads=[bb], writes=[bb])
                        qe, qeb = LB[hp][0], LBB[hp][0]
                        OP('dve', lambda e, qt=qt, ebt=ebt, qe=qe: e.scalar_tensor_tensor(out=qe[:], in0=qt[:], scalar=QK, in1=ebt[:], op0=ALU.mult, op1=ALU.mult),
                           reads=[qb, ebb], writes=[qeb])
                        OP('dve', lambda e, ft=ft: e.tensor_scalar(out=ft[:], in0=ft[:], scalar1=-1.0, scalar2=1.0, op0=ALU.mult, op1=ALU.add),
                           reads=[fb], writes=[fb])
                        ke, keb = LB[hp][1], LBB[hp][1]
                        OP('dve', lambda e, ft=ft, bt=bt, ke=ke: e.tensor_tensor(out=ke[:], in0=ft[:], in1=bt[:], op=ALU.mult), reads=[fb, bb], writes=[keb])
                        bg_ = proj_fm(l, 6 + hp, first)
                        gt, gb = evac_fm(bg_, AF.Sigmoid, dst=(LL[hp][1], LLB_[hp][1]))
                        QE.append((qe, qeb)); KE.append((ke, keb)); PCx.append((ebt, ebb)); GT.append((gt, gb))
                    proj_tm(l, 0)
                    chunk_engine('hgrn', QE, KE, PCx)
                    Ot = evac_O()
                    rs = out_rstd(Ot, ones, 256, NORM_EPS)
                    for hp in range(2):
                        t1, b1 = fa()
                        OP('dve', lambda e, hp=hp, t1=t1, Ot=Ot, rs=rs: e.scalar_tensor_tensor(out=t1[:], in0=Ot[hp][0][:], scalar=pv[:, PV['hnorm'] + hp:PV['hnorm'] + hp + 1],
                                                                           in1=rs[hp][0][:], op0=ALU.mult, op1=ALU.mult),
                           reads=[Ot[hp][1], rs[hp][1], pvB[l]], writes=[b1])
                        OP('dve', lambda e, hp=hp, t1=t1, GT=GT: e.tensor_tensor(out=YT[:, 2 + hp, :], in0=t1[:], in1=GT[hp][0][:], op=ALU.mult),
                           reads=[b1, GT[hp][1]], writes=[YB[2 + hp]])
                else:
                    for ck in (2, 3):
                        OP('pool', lambda e, ck=ck: e.memset(YT[:, ck, :], 0.0), writes=[YB[ck]])

                if 'gla' in mixers:
                    bga = proj_fm(l, 22, first)
                    gat, gab = LLX[0], LLXB[0]
                    OP('act', lambda e: e.activation(out=gat[:], in_=PS[bga][:, 0:TBM], func=AF.Copy), reads=[PSB[bga]], writes=[gab])
                    QE, KE, PCx, GT = [], [], [], []
                    for hp in range(2):
                        bk = pbank()
                        OP('pe', lambda e, hp=hp, bk=bk: e.matmul(PS[bk][:, 0:TBM], lhsT=smat[:, 1024 + hp * 128:1024 + (hp + 1) * 128], rhs=gat[:], start=True, stop=True),
                           reads=[gab, smatB], writes=[PSB[bk]])
                        lt, lb_ = evac_fm(bk, AF.Sigmoid, bias=pv[:, PV['glab'] + hp:PV['glab'] + hp + 1])
                        OP('act', lambda e, lt=lt: e.activation(out=lt[:], in_=lt[:], func=AF.Ln), reads=[lb_], writes=[lb_])
                        bt, bb = scan_decay(lt, lb_)
                        ebt, ebb = LL[hp][0], LLB_[hp][0]
                        OP('act', lambda e, bt=bt, ebt=ebt: e.activation(out=ebt[:], in_=bt[:], func=AF.Exp, scale=1.0 / 16), reads=[bb], writes=[ebb])
                        OP('act', lambda e, bt=bt: e.activation(out=bt[:], in_=bt[:], func=AF.Exp, scale=-1.0 / 16), reads=[bb], writes=[bb])
                        bq = proj_fm(l, 16 + hp, first)
                        qe, qeb = LB[hp][0], LBB[hp][0]
                        OP('dve', lambda e, bq=bq, ebt=ebt, qe=qe: e.scalar_tensor_tensor(out=qe[:], in0=PS[bq][:, 0:TBM], scalar=QK, in1=ebt[:], op0=ALU.mult, op1=ALU.mult),
                           reads=[PSB[bq], ebb], writes=[qeb])
                        bkk = proj_fm(l, 18 + hp, first)
                        ke, keb = LB[hp][1], LBB[hp][1]
                        OP('dve', lambda e, bkk=bkk, bt=bt, ke=ke: e.tensor_tensor(out=ke[:], in0=PS[bkk][:, 0:TBM], in1=bt[:], op=ALU.mult), reads=[PSB[bkk], bb], writes=[keb])
                        bg_ = proj_fm(l, 20 + hp, first)
                        gt, gb = evac_fm(bg_, AF.Silu, dst=(LL[hp][1], LLB_[hp][1]))
                        QE.append((qe, qeb)); KE.append((ke, keb)); PCx.append((ebt, ebb)); GT.append((gt, gb))
                    proj_tm(l, 2)
                    chunk_engine('gla', QE, KE, PCx)
                    Ot = evac_O()
                    rs = out_rstd(Ot, bones, 64, NORM_EPS)
                    for hp in range(2):
                        t1, b1 = fa()
                        OP('dve', lambda e, hp=hp, t1=t1, Ot=Ot, rs=rs: e.scalar_tensor_tensor(out=t1[:], in0=Ot[hp][0][:], scalar=pv[:, PV['gnorm'] + hp:PV['gnorm'] + hp + 1],
                                                                           in1=rs[hp][0][:], op0=ALU.mult, op1=ALU.mult),
                           reads=[Ot[hp][1], rs[hp][1], pvB[l]], writes=[b1])
                        OP('dve', lambda e, hp=hp, t1=t1, GT=GT: e.tensor_tensor(out=YT[:, 6 + hp, :], in0=t1[:], in1=GT[hp][0][:], op=ALU.mult),
                           reads=[b1, GT[hp][1]], writes=[YB[6 + hp]])
                else:
                    for ck in (6, 7):
                        OP('pool', lambda e, ck=ck: e.memset(YT[:, ck, :], 0.0), writes=[YB[ck]])

                if 'rwkv' in mixers:
                    bwa = proj_fm(l, 14, first)
                    twa, twab = LLX[0], LLXB[0]
                    OP('act', lambda e: e.activation(out=twa[0:64, :], in_=PS[bwa][0:64, 0:TBM], func=AF.Tanh), reads=[PSB[bwa]], writes=[twab])
                    OP('act', lambda e: e.activation(out=twa[64:128, :], in_=PS[bwa][64:128, 0:TBM], func=AF.Copy), reads=[PSB[bwa]], writes=[twab])
                    bxg = proj_fm(l, 15, first)
                    sxg, sxgb = evac_fm(bxg, AF.Sigmoid, dst=(LLX[1], LLXB[1]))
                    QE, KE, AE, BE, PCx, GR, RKR, VF = [], [], [], [], [], [], [], []
                    for hp in range(2):
                        cs_ = slice(hp * 128, (hp + 1) * 128)
                        bk = pbank()
                        OP('pe', lambda e, bk=bk, hp=hp: e.matmul(PS[bk][:, 0:TBM], lhsT=smat[:, 256 + hp * 128:256 + (hp + 1) * 128], rhs=twa[:], start=True, stop=True),
                           reads=[twab, smatB], writes=[PSB[bk]])
                        lw, lwb = evac_fm(bk, AF.Sigmoid, bias=pv[:, PV['w0'] + hp:PV['w0'] + hp + 1])
                        bk = pbank()
                        OP('pe', lambda e, bk=bk, hp=hp: e.matmul(PS[bk][:, 0:TBM], lhsT=smat[:, 768 + hp * 128:768 + (hp + 1) * 128], rhs=twa[:], start=True, stop=True),
                           reads=[twab, smatB], writes=[PSB[bk]])
                        at, ab_ = evac_fm(bk, AF.Sigmoid, bias=pv[:, PV['a0'] + hp:PV['a0'] + hp + 1])
                        bk = pbank()
                        OP('pe', lambda e, bk=bk, hp=hp: e.matmul(PS[bk][:, 0:TBM], lhsT=smat[:, 512 + hp * 128:512 + (hp + 1) * 128], rhs=sxg[:], start=True, stop=True),
                           reads=[sxgb, smatB], writes=[PSB[bk]])
                        grt, grb = evac_fm(bk, AF.Copy, dst=(LL[hp][1], LLB_[hp][1]))
                        bt, bb = scan_decay(lw, lwb)
                        CW = -float(np.exp(-0.5))
                        ebt, ebb = LL[hp][0], LLB_[hp][0]
                        OP('act', lambda e, bt=bt, ebt=ebt: e.activation(out=ebt[:], in_=bt[:], func=AF.Exp, scale=CW), reads=[bb], writes=[ebb])
                        enb, enbb = fa()
                        OP('act', lambda e, bt=bt, enb=enb: e.activation(out=enb[:], in_=bt[:], func=AF.Exp, scale=-CW), reads=[bb], writes=[enbb])
                        OP('dve', lambda e, bt=bt, lw=lw: e.tensor_tensor(out=bt[:], in0=bt[:], in1=lw[:], op=ALU.subtract), reads=[bb, lwb], writes=[bb])
                        OP('act', lambda e, bt=bt: e.activation(out=bt[:], in_=bt[:], func=AF.Exp, scale=CW), reads=[bb], writes=[bb])
                        br = proj_fm(l, 8 + hp, first)
                        rt, rb = evac_fm(br, AF.Copy)
                        bkr = proj_fm(l, 10 + hp, first)
                        kt, kb = evac_fm(bkr, AF.Copy)
                        bv = proj_fm(l, 12 + hp, first)
                        vt, vb = evac_fm(bv, AF.Copy, dst=(LL[hp][2], LLB_[hp][2]))
                        kkt, kkb = fa()
                        OP('dve', lambda e, kt=kt, kkt=kkt, hp=hp: e.tensor_scalar(out=kkt[:], in0=kt[:], scalar1=pv[:, PV['kk'] + hp:PV['kk'] + hp + 1], scalar2=None, op0=ALU.mult),
                           reads=[kb, pvB[l]], writes=[kkb])
                        sq_, sqb_ = ba()
                        OP('act', lambda e, kkt=kkt, sq_=sq_: e.activation(out=sq_[:], in_=kkt[:], func=AF.Square), reads=[kkb], writes=[sqb_])
                        bk = pbank()
                        OP('pe', lambda e, bk=bk, sq_=sq_: e.matmul(PS[bk][:, 0:TBM], lhsT=bones[:], rhs=sq_[:], start=True, stop=True), reads=[sqb_, bonesB], writes=[PSB[bk]])
                        rn, rnb = fa()
                        rstd_from(PS[bk], PSB[bk], TBM, 1.0, 1e-24, rn, rnb)
                        OP('dve', lambda e, kkt=kkt, rn=rn: e.tensor_tensor(out=kkt[:], in0=kkt[:], in1=rn[:], op=ALU.mult), reads=[kkb, rnb], writes=[kkb])
                        fac, facb = fa()
                        OP('dve', lambda e, at=at, fac=fac, hp=hp: e.tensor_scalar(out=fac[:], in0=at[:], scalar1=-1.0, scalar2=pv[:, PV['ka'] + hp:PV['ka'] + hp + 1], op0=ALU.add, op1=ALU.mult),
                           reads=[ab_, pvB[l]], writes=[facb])
                        OP('dve', lambda e, fac=fac, kt=kt: e.scalar_tensor_tensor(out=kt[:], in0=fac[:], scalar=1.0, in1=kt[:], op0=ALU.add, op1=ALU.mult),
                           reads=[facb, kb], writes=[kb])
                        rk_, rkb_ = LB[hp][4], LBB[hp][4]
                        OP('dve', lambda e, rt=rt, kt=kt, rk_=rk_, hp=hp: e.scalar_tensor_tensor(out=rk_[:], in0=rt[:], scalar=pv[:, PV['rk'] + hp:PV['rk'] + hp + 1], in1=kt[:], op0=ALU.mult, op1=ALU.mult),
                           reads=[rb, kb, pvB[l]], writes=[rkb_])
                        qe, qeb = LB[hp][0], LBB[hp][0]
                        OP('dve', lambda e, rt=rt, ebt=ebt, qe=qe: e.tensor_tensor(out=qe[:], in0=rt[:], in1=ebt[:], op=ALU.mult), reads=[rb, ebb], writes=[qeb])
                        ke, keb = LB[hp][1], LBB[hp][1]
                        OP('dve', lambda e, kt=kt, enb=enb, ke=ke: e.tensor_tensor(out=ke[:], in0=kt[:], in1=enb[:], op=ALU.mult), reads=[kb, enbb], writes=[keb])
                        be, beb = LB[hp][3], LBB[hp][3]
                        OP('dve', lambda e, kkt=kkt, bt=bt, be=be: e.tensor_tensor(out=be[:], in0=kkt[:], in1=bt[:], op=ALU.mult), reads=[kkb, bb], writes=[beb])
                        OP('dve', lambda e, kkt=kkt, at=at: e.tensor_tensor(out=kkt[:], in0=kkt[:], in1=at[:], op=ALU.mult), reads=[kkb, ab_], writes=[kkb])
                        ae, aeb = LB[hp][2], LBB[hp][2]
                        OP('dve', lambda e, kkt=kkt, enb=enb, ae=ae: e.tensor_tensor(out=ae[:], in0=kkt[:], in1=enb[:], op=ALU.mult), reads=[kkb, enbb], writes=[aeb])
                        QE.append((qe, qeb)); KE.append((ke, keb)); AE.append((ae, aeb)); BE.append((be, beb)); PCx.append((ebt, ebb))
                        GR.append((grt, grb)); RKR.append((rk_, rkb_)); VF.append((vt, vb))
                    proj_tm(l, 1)
                    chunk_engine('rwkv', QE, KE, PCx, rw={'AE': AE, 'BE': BE})
                    Ot = evac_O()
                    for hp in range(2):
                        ob16, ob16b = ba()
                        OP('dve', lambda e, hp=hp, ob16=ob16, Ot=Ot: e.tensor_copy(out=ob16[:], in_=Ot[hp][0][:]), reads=[Ot[hp][1]], writes=[ob16b])
                        bk = pbank()
                        OP('pe', lambda e, bk=bk, ob16=ob16: e.matmul(PS[bk][:, 0:TBM], lhsT=bones[:], rhs=ob16[:], start=True, stop=True), reads=[ob16b, bonesB], writes=[PSB[bk]])
                        ct, cb = fa()
                        OP('dve', lambda e, hp=hp, bk=bk, ct=ct, Ot=Ot: e.scalar_tensor_tensor(out=ct[:], in0=PS[bk][:, 0:TBM], scalar=-1.0 / 64, in1=Ot[hp][0][:], op0=ALU.mult, op1=ALU.add),
                           reads=[PSB[bk], Ot[hp][1]], writes=[cb])
                        s2, s2b = ba()
                        OP('act', lambda e, ct=ct, s2=s2: e.activation(out=s2[:], in_=ct[:], func=AF.Square), reads=[cb], writes=[s2b])
                        bk = pbank()
                        OP('pe', lambda e, bk=bk, s2=s2: e.matmul(PS[bk][:, 0:TBM], lhsT=bones[:], rhs=s2[:], start=True, stop=True), reads=[s2b, bonesB], writes=[PSB[bk]])
                        rn, rnb = fa()
                        rstd_from(PS[bk], PSB[bk], TBM, 1.0 / 64, GN_EPS, rn, rnb)
                        OP('dve', lambda e, hp=hp, ct=ct, rn=rn: e.scalar_tensor_tensor(out=ct[:], in0=ct[:], scalar=pv[:, PV['lnw'] + hp:PV['lnw'] + hp + 1], in1=rn[:], op0=ALU.mult, op1=ALU.mult),
                           reads=[cb, rnb, pvB[l]], writes=[cb])
                        bk = pbank()
                        OP('pe', lambda e, bk=bk, hp=hp, RKR=RKR: e.matmul(PS[bk][:, 0:TBM], lhsT=bones[:], rhs=RKR[hp][0][:], start=True, stop=True), reads=[RKR[hp][1], bonesB], writes=[PSB[bk]])
                        bo, bob = fa()
                        OP('dve', lambda e, bk=bk, hp=hp, bo=bo, VF=VF: e.tensor_tensor(out=bo[:], in0=PS[bk][:, 0:TBM], in1=VF[hp][0][:], op=ALU.mult), reads=[PSB[bk], VF[hp][1]], writes=[bob])
                        OP('dve', lambda e, hp=hp, ct=ct, bo=bo: e.scalar_tensor_tensor(out=ct[:], in0=ct[:], scalar=pv[:, PV['lnb'] + hp:PV['lnb'] + hp + 1], in1=bo[:], op0=ALU.add, op1=ALU.add),
                           reads=[cb, bob, pvB[l]], writes=[cb])
                        OP('dve', lambda e, hp=hp, ct=ct, GR=GR: e.tensor_tensor(out=YT[:, 4 + hp, :], in0=ct[:], in1=GR[hp][0][:], op=ALU.mult),
                           reads=[cb, GR[hp][1]], writes=[YB[4 + hp]])
                else:
                    for ck in (4, 5):
                        OP('pool', lambda e, ck=ck: e.memset(YT[:, ck, :], 0.0), writes=[YB[ck]])

                for j in range(KD):
                    i = load_o(m_wout[l][j], 'act')
                    for m in range(KD):
                        OP('pe', lambda e, m=m, j=j, i=i: e.matmul(PS[m][:, 0:TBM], lhsT=obf[i][:, m * 128:(m + 1) * 128], rhs=YT[:, j, :],
                                                               start=(j == 0), stop=(j == KD - 1)),
                           reads=[obfB[i], YB[j]], writes=[PSB[m]], sig=(m == KD - 1 or j == KD - 1))
                for m in range(KD):
                    OP('dve', lambda e, m=m: e.tensor_tensor(out=X[:, m, c0:c0 + TBM], in0=PS[m][:, 0:TBM], in1=X[:, m, c0:c0 + TBM], op=ALU.add),
                       reads=[PSB[m], XB[m][ti]], writes=[XB[m][ti]])

            def mixer_setup(l, s):
                OP('sp', lambda e: e.dma_start(out=mub[:], in_=mub_d[l]), writes=[mubB], dsem=muS)
                OP('sp', lambda e: e.dma_start(out=smat[:], in_=smat_d[l]), writes=[smatB], dsem=smS)
                OP('pool', lambda e: e.tensor_copy(out=smatb[:], in_=smat[:, 0:256]), reads=[smatB], writes=[smatB])
                for m in ('hgrn', 'gla', 'rwkv'):
                    for hp in range(2):
                        OP('pool', lambda e, m=m, hp=hp: e.memset(S32[m][hp][:], 0.0), writes=[S32B[m][hp]])
                        OP('pool', lambda e, m=m, hp=hp: e.memset(Sbf[m][hp][:], 0.0), writes=[SbfB[m][hp]])

        if do_mix:
            OP('act', lambda e: e.activation(out=lbt[:, 0:2], in_=pvec[0][:, PV['lb0']:PV['lb0'] + 2], func=AF.Exp), reads=[pvB[0]], writes=[lbtB])
            OP('act', lambda e: e.activation(out=lbt[:, 2:4], in_=pvec[L - 1][:, PV['lbl']:PV['lbl'] + 2], func=AF.Exp), reads=[pvB[L - 1], lbtB], writes=[lbtB])
            OP('dve', lambda e: e.tensor_tensor(out=lbt[:, 4:6], in0=lbt[:, 0:2], in1=lbt[:, 2:4], op=ALU.add), reads=[lbtB], writes=[lbtB])
            OP('dve', lambda e: e.reciprocal(out=lbt[:, 4:6], in_=lbt[:, 4:6]), reads=[lbtB], writes=[lbtB])
            OP('dve', lambda e: e.tensor_tensor(out=lbt[:, 6:8], in0=lbt[:, 2:4], in1=lbt[:, 4:6], op=ALU.mult), reads=[lbtB], writes=[lbtB])
            OP('dve', lambda e: e.memset(lbt[:, 4:6], 0.0), reads=[lbtB], writes=[lbtB])
            OP('dve', lambda e: e.tensor_scalar(out=lbt[:, 0:4], in0=lbt[:, 4:8], scalar1=-1.0, scalar2=1.0, op0=ALU.mult, op1=ALU.add), reads=[lbtB], writes=[lbtB])

        for s in range(NS):
            for k in range(KD):
                OP('sp', lambda e, k=k, s=s: e.dma_start(out=X[:, k, :], in_=xT[s, :, k, :]), writes=XB[k], dsem=xS[k])
            for l in range(L):
                if do_ffn:
                    for ti in range(NT):
                        ffn(l, 0, ti)
                if do_mix:
                    mixer_setup(l, s)
                    for bi in range(T // TBM):
                        mixer_block(l, s, bi)
                if do_ffn:
                    for ti in range(NT):
                        ffn(l, 1, ti)
            for ti in range(NT):
                c0 = ti * TB
                for k in range(KD):
                    i = counters['sq'] % 2
                    counters['sq'] += 1
                    OP('act', lambda e, k=k, i=i, c0=c0: e.activation(out=sq[i][:], in_=X[:, k, c0:c0 + TB], func=AF.Square),
                       reads=[XB[k][ti]], writes=[sqB[i]])
                    OP('pe', lambda e, k=k, i=i: e.matmul(PS[0][:, :], lhsT=ones[:], rhs=sq[i][:], start=(k == 0), stop=(k == KD - 1)),
                       reads=[sqB[i], onesB], writes=[PSB[0]])
                rstd_from(PS[0], PSB[0], TB, 1.0 / D, NORM_EPS, rstd, rstdB)
                for k in range(KD):
                    OP('dve', lambda e, k=k, c0=c0: e.scalar_tensor_tensor(out=X[:, k, c0:c0 + TB], in0=X[:, k, c0:c0 + TB],
                                                                        scalar=pvec[0][:, PV['nfin'] + k:PV['nfin'] + k + 1], in1=rstd[:],
                                                                        op0=ALU.mult, op1=ALU.mult),
                       reads=[XB[k][ti], rstdB, pvB[0]], writes=[XB[k][ti]])
            for k in range(KD):
                OP('sp', lambda e, k=k, s=s: e.dma_start(out=outT[s, :, k, :], in_=X[:, k, :]), reads=XB[k], writes=XB[k], dsem=oS[k])
        pg.ops['sp'].append((lambda e: e.nop(), {o_: o_.count for o_ in oS}, False, None))
        pg.emit(block, esems)
    return nc, pg


def _col(v, n):
    return np.ascontiguousarray(np.asarray(v, np.float32).reshape(n, 128).T)


def make_consts():
    cst = np.zeros((128, 1600), np.float32)
    p = np.arange(64)[:, None]
    f = np.arange(64)[None, :]
    for h in range(4):
        cst[0:64, 0 + h * 64:0 + (h + 1) * 64] = (p <= f)
        cst[0:64, 256 + h * 64:256 + (h + 1) * 64] = (p < f)
        cst[0:64, 512 + h * 64:512 + (h + 1) * 64] = (f < p)
        cst[0:64, 768 + h * 64:768 + (h + 1) * 64] = (p == f)
    scm = np.ones(512, np.float32)
    scm[::64] = 0.0
    cst[:, 1024:1536] = scm[None, :]
    wins = {(0, 0): 2, (0, 1): 4, (1, 0): 8, (1, 1): 16}
    for ck in range(2):
        for half in range(2):
            w = wins[(ck, half)]
            t = np.arange(16)
            cst[half * 64:(half + 1) * 64, 1536 + ck * 16:1536 + ck * 16 + 16] = (1.0 / np.minimum(t + 1, w))[None, :]
    return cst


def prep_weights(inp, L):
    out = {}
    f32 = np.float32
    for l in range(L):
        for w, (wi, wo) in enumerate((('ffn1_w_in', 'ffn1_w_out'), ('ffn2_w_in', 'ffn2_w_out'))):
            W = np.asarray(inp[wi][l], f32)
            Wk = W.reshape(KD, 128, 2 * FF)
            g = Wk[:, :, :FF].reshape(KD, 128, NJ, 128)
            u = Wk[:, :, FF:].reshape(KD, 128, NJ, 128)
            blk = np.concatenate([g, u], axis=3)
            out[f"f{w + 1}_win{l}"] = np.ascontiguousarray(blk.transpose(2, 1, 0, 3)).reshape(NJ, 128, KD * 256)
            out[f"f{w + 1}_wout{l}"] = np.ascontiguousarray(np.asarray(inp[wo][l], f32).reshape(NJ, 128, D))
        W = np.asarray(inp['w_in'][l], f32).reshape(KD, 128, DIN)
        fm = np.zeros((len(FMG), 128, KD, 128), f32)
        for gi, (c0, nc_, _) in enumerate(FMG):
            nc_ = min(nc_, DIN - c0)
            fm[gi, :, :, :nc_] = W[:, :, c0:c0 + nc_].transpose(1, 0, 2)
        out[f"m_fm{l}"] = fm.reshape(len(FMG), 128, KD * 128)
        tm = np.zeros((len(TMG), 128, KD, 256), f32)
        for gi, (c0, nc_, _) in enumerate(TMG):
            tm[gi] = W[:, :, c0:c0 + nc_].transpose(1, 0, 2)
        out[f"m_tm{l}"] = tm.reshape(len(TMG), 128, KD * 256)
        out[f"m_wout{l}"] = np.ascontiguousarray(np.asarray(inp['w_out'][l], f32).reshape(KD, 128, D))
        pv = np.zeros((128, NPV), f32)
        pv[:, PV['nf1']:PV['nf1'] + 8] = _col(inp['norm_ffn1'][l], 8)
        pv[:, PV['nmx']:PV['nmx'] + 8] = _col(inp['norm_mix'][l], 8)
        pv[:, PV['nf2']:PV['nf2'] + 8] = _col(inp['norm_ffn2'][l], 8)
        pv[:, PV['nfin']:PV['nfin'] + 8] = _col(inp['norm_final'], 8)
        pv[:, PV['lb0']:PV['lb0'] + 2] = _col(inp['hgrn_lb_logits'][0], 2)
        pv[:, PV['lbl']:PV['lbl'] + 2] = _col(inp['hgrn_lb_logits'][l], 2)
        for nm, key in (('pool_b', 'pool_b'), ('pool_s', 'pool_scale'), ('hnorm', 'hgrn_norm'), ('w0', 'rwkv_w0'), ('a0', 'rwkv_a0'),
                        ('kk', 'rwkv_k_k'), ('ka', 'rwkv_k_a'), ('rk', 'rwkv_r_k'), ('lnw', 'rwkv_ln_w'), ('lnb', 'rwkv_ln_b'),
                        ('glab', 'gla_b'), ('gnorm', 'gla_norm')):
            pv[:, PV[nm]:PV[nm] + 2] = _col(inp[key][l], 2)
        out[f"pvec{l}"] = pv
        out[f"mub{l}"] = np.ascontiguousarray(np.broadcast_to(np.asarray(inp['rwkv_mu'][l], f32)[None, :], (128, 1024)))
        sm = np.zeros((128, 5 * 256), f32)
        pw_ = np.asarray(inp['pool_w'][l], f32)
        for ck in range(2):
            sm[0:64, ck * 128:ck * 128 + 64] = pw_[2 * ck]
            sm[64:128, ck * 128 + 64:ck * 128 + 128] = pw_[2 * ck + 1]
        sm[0:64, 256:512] = np.asarray(inp['rwkv_w2'][l], f32)
        sm[64:128, 768:1024] = np.asarray(inp['rwkv_a2'][l], f32)
        sm[:, 512:768] = np.asarray(inp['rwkv_g2'][l], f32)
        sm[0:16, 1024:1280] = np.asarray(inp['gla_w2'][l], f32)
        out[f"smat{l}"] = sm
    out["cst"] = make_consts()
    return out


def prep_x(xc):
    NS, T, _ = xc.shape
    return np.ascontiguousarray(xc.reshape(NS, T, KD, 128).transpose(0, 3, 2, 1))


def unprep_out(o):
    NS, _, _, T = o.shape
    return np.ascontiguousarray(o.transpose(0, 3, 2, 1)).reshape(NS, T, D)


def kernel(**inputs):
    x = np.asarray(inputs['x'], np.float32)
    B, T, _ = x.shape
    NCORES = 8
    NS = B // NCORES
    L = 2
    nc, pg = build_program(T, NS, L)
    wts = prep_weights(inputs, L)
    in_maps = []
    for c in range(NCORES):
        m = dict(wts)
        m["xT"] = prep_x(x[c * NS:(c + 1) * NS])
        in_maps.append(m)
    res = run_bass_kernel_spmd(nc, in_maps, core_ids=list(range(NCORES)))
    outs = [unprep_out(np.asarray(r["outT"])) for r in res.results]
    return np.concatenate(outs, axis=0).astype(np.float32)
```

```python
import numpy as np
from contextlib import ExitStack
import concourse.bass as bass
import concourse.mybir as mybir
from concourse.bass_utils import run_bass_kernel_spmd

F32 = mybir.dt.float32
BF16 = mybir.dt.bfloat16
AF = mybir.ActivationFunctionType
ALU = mybir.AluOpType
ENGS = ['pe', 'act', 'dve', 'pool', 'sp']

D = 1024
KD = 8
FF = 2816
NJ = 22
G = 256
DIN = 3344
NORM_EPS = 1e-6
GN_EPS = 64e-5
QK = 0.125
TB = 512
TBM = 256
CH = 64
NCH = TBM // CH
DEBUG_STAGE = 99

FMG = [(0, 128, 0), (128, 128, 0), (256, 128, 0), (384, 128, 0), (512, 128, 0), (640, 128, 0),
       (1024, 128, 0), (1152, 128, 0),
       (1280, 128, 1), (1408, 128, 1), (1536, 128, 1), (1664, 128, 1), (1792, 128, 1), (1920, 128, 1),
       (2048, 128, 1), (2176, 128, 1),
       (2304, 128, 0), (2432, 128, 0), (2560, 128, 0), (2688, 128, 0), (3072, 128, 0), (3200, 128, 0),
       (3328, 128, 0)]
TMG = [(768, 256, 0), (1792, 256, 1), (2816, 256, 0)]
RW0 = 1280

PV = {}
_o = 0
for _n, _w in [('nf1', 8), ('nmx', 8), ('nf2', 8), ('pool_b', 2), ('pool_s', 2), ('lb0', 2), ('lbl', 2),
               ('hnorm', 2), ('w0', 2), ('a0', 2), ('kk', 2), ('ka', 2), ('rk', 2), ('lnw', 2), ('lnb', 2),
               ('glab', 2), ('gnorm', 2), ('nfin', 8)]:
    PV[_n] = _o
    _o += _w
NPV = _o


class Buf:
    __slots__ = ('name', 'w', 'r', 'const')

    def __init__(self, name, const=False):
        self.name = name
        self.w = None
        self.r = []
        self.const = const


class DSem:
    def __init__(self, h):
        self.h = h
        self.count = 0


class Prog:
    def __init__(self, nc, same_engine_sync=True):
        self.nc = nc
        self.ops = {e: [] for e in ENGS}
        self.cnt = {e: 0 for e in ENGS}
        self.same = same_engine_sync
        self.nops = 0
        self.last_rg = None
        self.last_pe_sig = True

    def op(self, eng, fn, reads=(), writes=(), sig=True, dsem=None, rg=None):
        waits = {}
        if eng == 'pe':
            if rg is not None and self.last_rg is not None and rg != self.last_rg:
                assert self.last_pe_sig
                waits['pe'] = self.cnt['pe']
            self.last_rg = rg
            self.last_pe_sig = sig

        def addw(tok):
            if tok is None:
                return
            k, v = tok
            if k == eng and (eng == 'pe' or not self.same):
                return
            if waits.get(k, 0) < v:
                waits[k] = v
        for b in reads:
            addw(b.w)
        for b in writes:
            addw(b.w)
            for t in b.r:
                addw(t)
        if dsem is not None:
            dsem.count += 16
            tok = (dsem, dsem.count)
            sig = False
        elif sig:
            self.cnt[eng] += 1
            tok = (eng, self.cnt[eng])
        else:
            tok = (eng, self.cnt[eng] + 1)
        for b in reads:
            if not b.const:
                b.r.append(tok)
                if len(b.r) > 48:
                    mx = {}
                    for k, v in b.r:
                        if mx.get(k, 0) < v:
                            mx[k] = v
                    b.r = list(mx.items())
        for b in writes:
            b.w = tok
            b.r = []
        self.ops[eng].append((fn, waits, sig, dsem))
        self.nops += 1
        return tok

    def emit(self, block, esems):
        nc = self.nc
        deco = {'pe': block.tensor, 'act': block.scalar, 'dve': block.vector,
                'pool': block.gpsimd, 'sp': block.sync}
        for e in ENGS:
            ops = self.ops[e]

            def body(eng, ops=ops, e=e):
                known = {}
                for fn, waits, sig, dsem in ops:
                    for k, v in waits.items():
                        if known.get(k, 0) >= v:
                            continue
                        known[k] = v
                        h = k.h if isinstance(k, DSem) else esems[k]
                        eng.wait_ge(h, v)
                    inst = fn(eng)
                    if dsem is not None:
                        inst.then_inc(dsem.h, 16)
                    elif sig:
                        inst.then_inc(esems[e], 1)
            deco[e](body)


def build_program(T, NS, L, mixers=('pool', 'hgrn', 'rwkv', 'gla'), do_ffn=True, do_mix=True):
    NT = T // TB
    nc = bass.Bass("TRN2", target_bir_lowering=False)
    dr = {}

    def din(name, shape):
        dr[name] = nc.dram_tensor(name, list(shape), F32, kind="ExternalInput").ap()
        return dr[name]
    xT = din("xT", [NS, 128, KD, T])
    outT = nc.dram_tensor("outT", [NS, 128, KD, T], F32, kind="ExternalOutput").ap()
    f_win = [[din(f"f{w}_win{l}", [NJ, 128, KD * 256]) for w in (1, 2)] for l in range(L)]
    f_wout = [[din(f"f{w}_wout{l}", [NJ, 128, D]) for w in (1, 2)] for l in range(L)]
    m_fm = [din(f"m_fm{l}", [len(FMG), 128, KD * 128]) for l in range(L)]
    m_tm = [din(f"m_tm{l}", [len(TMG), 128, KD * 256]) for l in range(L)]
    m_wout = [din(f"m_wout{l}", [KD, 128, D]) for l in range(L)]
    pvec_d = [din(f"pvec{l}", [128, NPV]) for l in range(L)]
    mub_d = [din(f"mub{l}", [128, 1024]) for l in range(L)]
    smat_d = [din(f"smat{l}", [128, 5 * 256]) for l in range(L)]
    cst_d = din("cst", [128, 1600])
    es = ExitStack()
    with es:
        def sb(name, shape, dt=F32):
            return es.enter_context(nc.sbuf_tensor("sb_" + name, list(shape), dt))

        def psum(name, shape, dt=F32):
            return es.enter_context(nc.psum_tensor("pp_" + name, list(shape), dt))
        esems = {e: es.enter_context(nc.semaphore("s_" + e)) for e in ENGS}

        def dsem(name):
            return DSem(es.enter_context(nc.semaphore(name)))
        pg = Prog(nc)
        block = es.enter_context(nc.Block())

        X = sb("X", [128, KD, T])
        XB = [[Buf(f"X{k}_{t}") for t in range(NT)] for k in range(KD)]
        xn = sb("xn", [128, KD, TB], BF16)
        xns = sb("xns", [128, KD, TBM], BF16)
        xnsB = Buf("xns")
        xnB = [Buf(f"xn{k}") for k in range(KD)]
        sq = [sb(f"sq{i}", [128, TB], BF16) for i in range(2)]
        sqB = [Buf(f"sq{i}") for i in range(2)]
        rstd = sb("rstd", [128, TB]); rstdB = Buf("rstd")
        ones = sb("ones", [128, 128], BF16); onesB = Buf("ones", const=True)
        bones = sb("bones", [128, 128], BF16)
        ident = sb("ident", [128, 128], BF16)
        cst = sb("cst", [128, 1600])
        cstB = Buf("cst", const=True)
        MI, MSU, MSL, ID4 = 0, 256, 512, 768
        SCM = 1024
        ICN = 1536
        pvec = [sb(f"pvec{l}", [128, NPV]) for l in range(L)]
        pvB = [Buf(f"pvec{l}", const=True) for l in range(L)]
        lbt = sb("lbt", [128, 8])
        mub = sb("mub", [128, 1024]); mubB = Buf("mub")
        smat = sb("smat", [128, 5 * 256]); smatB = Buf("smat")
        smatb = sb("smatb", [128, 2 * 128], BF16)
        wst = [sb(f"wst{i}", [128, KD * 256]) for i in range(2)]
        wstB = [Buf(f"wst{i}") for i in range(2)]
        wstS = [dsem(f"dwst{i}") for i in range(2)]
        wbf = [sb(f"wbf{i}", [128, KD * 256], BF16) for i in range(2)]
        wbfB = [Buf(f"wbf{i}") for i in range(2)]
        wbfB2 = [[Buf(f"wbf{i}a"), Buf(f"wbf{i}b")] for i in range(2)]
        wtmp = sb("wtmp", [128, KD * 256]); wtmpB = Buf("wtmp")
        ost = [sb("ost0", [128, D])] * 2
        ostB = [Buf("ost0")] * 2
        ostS = [dsem("dost0")] * 2
        obf = [sb(f"obf{i}", [128, D], BF16) for i in range(2)]
        obfB = [Buf(f"obf{i}") for i in range(2)]
        hT = sb("hT", [128, NJ, TB], BF16)
        hB = [Buf(f"h{j}") for j in range(NJ)]
        sg = [sb("sg0", [128, TB])] * 2
        sgB = [Buf("sg0")] * 2
        PS = [psum(f"ps{i}", [128, 512]) for i in range(8)]
        PSB = [Buf(f"ps{i}") for i in range(8)]
        xS = [dsem(f"dx{k}") for k in range(KD)]
        oS = [dsem(f"dout{k}") for k in range(KD)]
        cS = dsem("dcst")
        pS = [dsem(f"dpv{l}") for l in range(L)]
        muS = dsem("dmu")
        smS = dsem("dsm")
        counters = {'w': 0, 'o': 0, 'sq': 0, 'sg': 0, 'pp': 0}

        def OP(eng, fn, reads=(), writes=(), sig=True, dsem=None, rg=None):
            return pg.op(eng, fn, reads, writes, sig, dsem, rg)

        def load_w(src_ap, ncols):
            i = counters['w'] % 2
            counters['w'] += 1
            OP('sp', lambda e: e.dma_start(out=wst[i][:, 0:ncols], in_=src_ap), writes=[wstB[i]], dsem=wstS[i])
            return i

        def cast_w(i, ncols, eng='pool'):
            if eng == 'act':
                OP('act', lambda e: e.activation(out=wbf[i][:, 0:ncols], in_=wst[i][:, 0:ncols], func=AF.Copy),
                   reads=[wstB[i]], writes=[wbfB[i], wbfB2[i][0], wbfB2[i][1]])
            else:
                OP(eng, lambda e: e.tensor_copy(out=wbf[i][:, 0:ncols], in_=wst[i][:, 0:ncols]),
                   reads=[wstB[i]], writes=[wbfB[i], wbfB2[i][0], wbfB2[i][1]])

        def cast_w_split(i, ncols):
            c1 = (ncols * 3 // 4) // 128 * 128
            OP('dve', lambda e: e.tensor_copy(out=wbf[i][:, 0:c1], in_=wst[i][:, 0:c1]), reads=[wstB[i]], writes=[wbfB2[i][0], wbfB[i]])
            OP('pool', lambda e: e.tensor_copy(out=wbf[i][:, c1:ncols], in_=wst[i][:, c1:ncols]), reads=[wstB[i]], writes=[wbfB2[i][1]])

        def load_o(src_ap, eng='act'):
            i = counters['o'] % 2
            counters['o'] += 1
            OP('sp', lambda e: e.dma_start(out=ost[i][:], in_=src_ap), writes=[ostB[i]], dsem=ostS[i])
            if eng == 'act':
                OP('act', lambda e: e.activation(out=obf[i][:], in_=ost[i][:], func=AF.Copy),
                   reads=[ostB[i]], writes=[obfB[i]])
            else:
                OP(eng, lambda e: e.tensor_copy(out=obf[i][:], in_=ost[i][:]), reads=[ostB[i]], writes=[obfB[i]])
            return i

        def rstd_from(psb, psbuf, n, scale, eps, dst, dstB):
            OP('act', lambda e: e.activation(out=dst[:, 0:n], in_=psb[:, 0:n], func=AF.Ln, scale=scale, bias=eps),
               reads=[psbuf], writes=[dstB])
            OP('act', lambda e: e.activation(out=dst[:, 0:n], in_=dst[:, 0:n], func=AF.Exp, scale=-0.5),
               reads=[dstB], writes=[dstB])

        def rmsnorm_to_xn(l, gcol, c0, n, bank):
            ti = c0 // TB
            for k in range(KD):
                i = counters['sq'] % 2
                counters['sq'] += 1
                OP('act', lambda e, k=k, i=i: e.activation(out=sq[i][:, 0:n], in_=X[:, k, c0:c0 + n], func=AF.Square),
                   reads=[XB[k][ti]], writes=[sqB[i]])
                OP('pe', lambda e, k=k, i=i: e.matmul(PS[bank][:, 0:n], lhsT=ones[:], rhs=sq[i][:, 0:n], start=(k == 0), stop=(k == KD - 1)),
                   reads=[sqB[i], onesB], writes=[PSB[bank]])
            rstd_from(PS[bank], PSB[bank], n, 1.0 / D, NORM_EPS, rstd, rstdB)
            for k in range(KD):
                OP('dve', lambda e, k=k: e.scalar_tensor_tensor(out=xn[:, k, 0:n], in0=X[:, k, c0:c0 + n],
                                                                 scalar=pvec[l][:, gcol + k:gcol + k + 1], in1=rstd[:, 0:n],
                                                                 op0=ALU.mult, op1=ALU.mult),
                   reads=[XB[k][ti], rstdB, pvB[l]], writes=[xnB[k]])

        def ffn(l, w, ti):
            gcol = PV['nf1'] if w == 0 else PV['nf2']
            c0 = ti * TB
            blocks = [('in', j) for j in range(NJ)] + [('out', j) for j in range(NJ)]

            def issue(bd):
                kind, j = bd
                if kind == 'in':
                    i = load_w(f_win[l][w][j], KD * 256)
                    cast_w_split(i, KD * 256)
                    return i
                return load_o(f_wout[l][w][j], 'act')
            slot = issue(blocks[0])
            rmsnorm_to_xn(l, gcol, c0, TB, 0)
            for t, (kind, j) in enumerate(blocks):
                i = slot
                slot = issue(blocks[t + 1]) if t + 1 < len(blocks) else None
                if kind == 'in':
                    wv = wbf[i][:].rearrange("p (k c) -> p k c", k=KD)
                    pp = counters['pp'] % 2
                    counters['pp'] += 1
                    bg, bu = 2 * pp, 2 * pp + 1
                    for half, bk in ((0, bg), (1, bu)):
                        for k in range(KD):
                            OP('pe', lambda e, k=k, half=half, bk=bk, wv=wv: e.matmul(
                                PS[bk][:, :], lhsT=wv[:, k, half * 128:(half + 1) * 128], rhs=xn[:, k, 0:TB],
                                start=(k == 0), stop=(k == KD - 1)),
                               reads=[wbfB[i], wbfB2[i][0], wbfB2[i][1], xnB[k]], writes=[PSB[bk]], sig=(k == KD - 1))
                    si = counters['sg'] % 2
                    counters['sg'] += 1
                    OP('act', lambda e, si=si, bg=bg: e.activation(out=sg[si][:], in_=PS[bg][:, :], func=AF.Silu),
                       reads=[PSB[bg]], writes=[sgB[si]])
                    OP('dve', lambda e, si=si, bu=bu, j=j: e.tensor_tensor(out=hT[:, j, :], in0=sg[si][:], in1=PS[bu][:, :], op=ALU.mult),
                       reads=[sgB[si], PSB[bu]], writes=[hB[j]])
                else:
                    for m in range(KD):
                        OP('pe', lambda e, m=m, j=j, i=i: e.matmul(PS[m][:, :], lhsT=obf[i][:, m * 128:(m + 1) * 128], rhs=hT[:, j, :],
                                                               start=(j == 0), stop=(j == NJ - 1)),
                           reads=[obfB[i], hB[j]], writes=[PSB[m]], sig=(m == KD - 1 or j == NJ - 1))
            for m in range(KD):
                OP('dve', lambda e, m=m: e.scalar_tensor_tensor(out=X[:, m, c0:c0 + TB], in0=PS[m][:, :], scalar=0.5,
                                                                 in1=X[:, m, c0:c0 + TB], op0=ALU.mult, op1=ALU.add),
                   reads=[PSB[m], XB[m][ti]], writes=[XB[m][ti]])

        OP('sp', lambda e: e.dma_start(out=cst[:], in_=cst_d), writes=[cstB], dsem=cS)
        for l in range(L):
            OP('sp', lambda e, l=l: e.dma_start(out=pvec[l][:], in_=pvec_d[l]), writes=[pvB[l]], dsem=pS[l])
        OP('pool', lambda e: e.memset(ones[:], 1.0), writes=[onesB])
        bonesB = Buf("bones", const=True)
        OP('pool', lambda e: e.memset(bones[:], 0.0), writes=[bonesB])
        OP('pool', lambda e: e.memset(bones[0:64, 0:64], 1.0), writes=[bonesB])
        OP('pool', lambda e: e.memset(bones[64:128, 64:128], 1.0), writes=[bonesB])
        identB = Buf("ident", const=True)
        OP('pool', lambda e: e.memset(ident[:], 1.0), writes=[identB])
        OP('pool', lambda e: e.affine_select(out=ident[:], in_=ident[:], pattern=[[-1, 128]], compare_op=ALU.is_equal,
                                             fill=0.0, base=0, channel_multiplier=1), reads=[identB], writes=[identB])

        if do_mix:
            YT = sb("YT", [128, KD, TBM], BF16)
            YB = [Buf(f"Y{k}") for k in range(KD)]
            NFA = 10
            FA = [sb(f"fa{i}", [128, TBM]) for i in range(NFA)]
            FAB = [Buf(f"fa{i}") for i in range(NFA)]
            NBA = 4
            BA = [sb(f"ba{i}", [128, TBM], BF16) for i in range(NBA)]
            BAB = [Buf(f"ba{i}") for i in range(NBA)]
            LL = [[sb(f"ll{hp}{i}", [128, TBM]) for i in range(3)] for hp in range(2)]
            LLB_ = [[Buf(f"ll{hp}{i}") for i in range(3)] for hp in range(2)]
            LLX = [sb(f"llx{i}", [128, TBM]) for i in range(2)]
            LLXB = [Buf(f"llx{i}") for i in range(2)]
            LB = [[sb(f"lb{hp}{i}", [128, TBM], BF16) for i in range(5)] for hp in range(2)]
            LBB = [[Buf(f"lb{hp}{i}") for i in range(5)] for hp in range(2)]
            PB16 = [sb(f"pb16{i}", [128, TBM], BF16) for i in range(2)]
            PB16B = [Buf(f"pb16{i}") for i in range(2)]
            Vt = [sb(f"vt{c}", [64, 256], BF16) for c in range(NCH)]
            VtB = [Buf(f"vt{c}") for c in range(NCH)]
            KEt = [sb(f"ket{c}", [64, 256], BF16) for c in range(NCH)]
            KEtB = [Buf(f"ket{c}") for c in range(NCH)]
            AEt = [sb(f"aet{c}", [64, 256], BF16) for c in range(NCH)]
            AEtB = [Buf(f"aet{c}") for c in range(NCH)]
            alias_ctr = [0]

            def mk(name, n):
                ts, bs = [], []
                for c in range(n):
                    idx = alias_ctr[0]
                    alias_ctr[0] += 1
                    j, half = idx // 2, idx % 2
                    ts.append(hT[0:64, j, half * 256:(half + 1) * 256])
                    bs.append(hB[j])
                return ts, bs
            ATs, ATsB = mk("ats", NCH)
            LKs, LKsB = mk("lks", NCH)
            ARs, ARsB = mk("ars", NCH)
            Nn, NnB = mk("nn", NCH)
            NTn, NTnB = mk("ntn", NCH)
            Nn2, Nn2B = mk("nn2", NCH)
            NTn2, NTn2B = mk("ntn2", NCH)
            Pn, PnB = mk("pn", NCH)
            Pn2, Pn2B = mk("pn2", NCH)
            Ysb = sb("ysb", [64, 256], BF16); YsbB = Buf("ysb")
            Usb = sb("usb", [64, 256], BF16); UsbB = Buf("usb")
            S32 = {m: [sb(f"s32{m}{hp}", [128, 64]) for hp in range(2)] for m in ('hgrn', 'gla', 'rwkv')}
            Sbf = {m: [sb(f"sbf{m}{hp}", [128, 64], BF16) for hp in range(2)] for m in ('hgrn', 'gla', 'rwkv')}
            S32B = {m: [Buf(f"s32{m}{hp}") for hp in range(2)] for m in ('hgrn', 'gla', 'rwkv')}
            SbfB = {m: [Buf(f"sbf{m}{hp}") for hp in range(2)] for m in ('hgrn', 'gla', 'rwkv')}
            stmp = [sb(f"stmp{hp}", [128, 64]) for hp in range(2)]
            stmpB = [Buf(f"stmp{hp}") for hp in range(2)]
            pext = sb("pext", [128, 2, 16 + TBM]); pextB = Buf("pext")
            pw = [sb(f"pw{i}", [128, 2, 16 + TBM]) for i in range(2)]
            pwB = [Buf(f"pw{i}") for i in range(2)]
            PST = PS[7][:, :].bitcast(BF16)
            lbtB = Buf("lbt", const=True)
            fa_ctr = [0]
            ba_ctr = [0]

            def fa():
                i = fa_ctr[0] % NFA
                fa_ctr[0] += 1
                return FA[i], FAB[i]

            def ba():
                i = ba_ctr[0] % NBA
                ba_ctr[0] += 1
                return BA[i], BAB[i]

            def pbank():
                b = counters['pp'] % 4
                counters['pp'] += 1
                return b

            wplan = {'plan': [], 'pos': 0, 'issued': {}, 'l': 0}

            def issue_desc(l, d):
                kind, g = d
                if kind == 'fm':
                    col0, ncols, shift = FMG[g]
                    i = load_w(m_fm[l][g], KD * 128)
                    if shift:
                        mc = col0 - RW0
                        mu_b = mub[:, mc:mc + 128].unsqueeze(1).broadcast_to([128, KD, 128])
                        wv32 = wst[i][:, 0:KD * 128].rearrange("p (k c) -> p k c", k=KD)
                        wt = wtmp[:, 0:KD * 128].rearrange("p (k c) -> p k c", k=KD)
                        wbv = wbf[i][:].rearrange("p (k c) -> p k c", k=KD)
                        OP('pool', lambda e: e.tensor_tensor(out=wt, in0=wv32, in1=mu_b, op=ALU.mult),
                           reads=[wstB[i], mubB], writes=[wtmpB])
                        OP('pool', lambda e: e.tensor_tensor(out=wbv[:, :, 0:128], in0=wv32, in1=wt, op=ALU.subtract),
                           reads=[wstB[i], wtmpB], writes=[wbfB[i], wbfB2[i][0], wbfB2[i][1]])
                        OP('pool', lambda e: e.tensor_copy(out=wbv[:, :, 128:256], in_=wt), reads=[wtmpB], writes=[wbfB[i], wbfB2[i][0], wbfB2[i][1]])
                    else:
                        cast_w(i, KD * 128, 'act')
                        wbv = wbf[i][:, 0:KD * 128].rearrange("p (k c) -> p k c", k=KD)
                    return (i, wbv)
                col0, ncols, shift = TMG[g]
                i = load_w(m_tm[l][g], KD * 256)
                i2 = None
                wb2 = None
                if shift:
                    i2 = counters['w'] % 2
                    counters['w'] += 1
                    mc = col0 - RW0
                    mu_b = mub[:, mc:mc + 256].unsqueeze(1).broadcast_to([128, KD, 256])
                    wv32 = wst[i][:].rearrange("p (k c) -> p k c", k=KD)
                    wt = wtmp[:].rearrange("p (k c) -> p k c", k=KD)
                    wa = wbf[i][:].rearrange("p (k c) -> p k c", k=KD)
                    wb2 = wbf[i2][:].rearrange("p (k c) -> p k c", k=KD)
                    OP('pool', lambda e: e.tensor_tensor(out=wt, in0=wv32, in1=mu_b, op=ALU.mult),
                       reads=[wstB[i], mubB], writes=[wtmpB])
                    OP('pool', lambda e: e.tensor_tensor(out=wa, in0=wv32, in1=wt, op=ALU.subtract),
                       reads=[wstB[i], wtmpB], writes=[wbfB[i], wbfB2[i][0], wbfB2[i][1]])
                    OP('pool', lambda e: e.tensor_copy(out=wb2, in_=wt), reads=[wtmpB], writes=[wbfB[i2], wbfB2[i2][0], wbfB2[i2][1]])
                else:
                    cast_w_split(i, KD * 256)
                    wa = wbf[i][:].rearrange("p (k c) -> p k c", k=KD)
                return (i, i2, wa, wb2)

            def acq(l, d):
                pos = wplan['pos']
                assert wplan['plan'][pos] == d, (wplan['plan'][pos], d)
                if pos not in wplan['issued']:
                    wplan['issued'][pos] = issue_desc(l, d)
                info = wplan['issued'].pop(pos)
                pos += 1
                wplan['pos'] = pos
                if pos < len(wplan['plan']):
                    nd = wplan['plan'][pos]
                    two_slot = (nd[0] == 'tm' and TMG[nd[1]][2])
                    if not two_slot:
                        wplan['issued'][pos] = issue_desc(l, nd)
                return info

            def proj_fm(l, g, first_block):
                col0, ncols, shift = FMG[g]
                i, wbv = acq(l, ('fm', g))
                bk = pbank()
                nmm = KD * (2 if shift else 1)
                n = 0
                for k in range(KD):
                    n += 1
                    OP('pe', lambda e, k=k, n=n: e.matmul(PS[bk][0:ncols, 0:TBM], lhsT=wbv[:, k, 0:ncols], rhs=xn[:, k, 0:TBM],
                                                          start=(n == 1), stop=(n == nmm)),
                       reads=[wbfB[i], wbfB2[i][0], wbfB2[i][1], xnB[k]], writes=[PSB[bk]], sig=(n == nmm))
                if shift:
                    for k in range(KD):
                        n += 1
                        OP('pe', lambda e, k=k, n=n: e.matmul(PS[bk][0:ncols, 0:TBM], lhsT=wbv[:, k, 128:128 + ncols], rhs=xns[:, k, 0:TBM],
                                                              start=False, stop=(n == nmm)),
                           reads=[wbfB[i], wbfB2[i][0], wbfB2[i][1], xnsB], writes=[PSB[bk]], sig=(n == nmm))
                return bk

            def proj_tm(l, g):
                col0, ncols, shift = TMG[g]
                i, i2, wa, wb2 = acq(l, ('tm', g))
                for c in range(NCH):
                    bk = pbank()
                    nmm = KD * (2 if shift else 1)
                    n = 0
                    for k in range(KD):
                        n += 1
                        OP('pe', lambda e, k=k, n=n, c=c, bk=bk: e.matmul(PS[bk][0:64, 0:256], lhsT=xn[:, k, c * CH:(c + 1) * CH],
                                                                     rhs=wa[:, k, :], start=(n == 1), stop=(n == nmm)),
                           reads=[wbfB[i], wbfB2[i][0], wbfB2[i][1], xnB[k]], writes=[PSB[bk]], sig=(n == nmm))
                    if shift:
                        for k in range(KD):
                            n += 1
                            OP('pe', lambda e, k=k, n=n, c=c, bk=bk: e.matmul(PS[bk][0:64, 0:256], lhsT=xns[:, k, c * CH:(c + 1) * CH],
                                                                         rhs=wb2[:, k, :], start=False, stop=(n == nmm)),
                               reads=[wbfB[i2], wbfB2[i2][0], wbfB2[i2][1], xnsB], writes=[PSB[bk]], sig=(n == nmm))
                    OP('act', lambda e, c=c, bk=bk: e.activation(out=Vt[c][:], in_=PS[bk][0:64, 0:256], func=AF.Copy),
                       reads=[PSB[bk]], writes=[VtB[c]])

            def scan_decay(g_t, g_b):
                b_t, b_b = fa()
                OP('dve', lambda e: e.tensor_tensor_scan(out=b_t[:], data0=cst[:, SCM:SCM + TBM], data1=g_t[:], initial=0.0,
                                                         op0=ALU.mult, op1=ALU.add), reads=[g_b, cstB], writes=[b_b])
                return b_t, b_b

            def chunk_engine(mname, QE, KE, PC, rw=None):
                isrw = rw is not None
                if DEBUG_STAGE < 1:
                    return
                for c in range(NCH):
                    for (src, dst, dstB_) in ([(KE, KEt, KEtB)] + ([(rw['AE'], AEt, AEtB)] if isrw else [])):
                        for hp in range(2):
                            OP('pe', lambda e, hp=hp, c=c, src=src: e.transpose(PST[0:64, hp * 128:(hp + 1) * 128],
                                                                             src[hp][0][:, c * CH:(c + 1) * CH], ident[:]),
                               reads=[src[hp][1], identB], writes=[PSB[7]])
                        OP('act', lambda e, c=c, dst=dst: e.activation(out=dst[c][:], in_=PST[0:64, 0:256], func=AF.Copy),
                           reads=[PSB[7]], writes=[dstB_[c]])
                if DEBUG_STAGE < 2:
                    return
                for c in range(NCH):
                    cs = slice(c * CH, (c + 1) * CH)
                    def sc(lh, rh, dst_ps, cs=cs):
                        for h in range(4):
                            hp, r = h // 2, (h % 2) * 64
                            OP('pe', lambda e, h=h, hp=hp, r=r: e.matmul(dst_ps[0:64, h * 64:(h + 1) * 64], lhsT=lh[hp][0][r:r + 64, cs],
                                                                         rhs=rh[hp][0][r:r + 64, cs], start=True, stop=True),
                               reads=[lh[hp][1], rh[hp][1]], writes=[PSB[6]], rg=r)
                    sc(KE, QE, PS[6][:, 0:256])
                    OP('dve', lambda e, c=c: e.tensor_tensor(out=ATs[c][:], in0=PS[6][0:64, 0:256], in1=cst[0:64, MI:MI + 256], op=ALU.mult),
                       reads=[PSB[6], cstB], writes=[ATsB[c]])
                    if isrw:
                        sc(KE, rw['BE'], PS[6][:, 256:512])
                        OP('dve', lambda e, c=c: e.tensor_tensor(out=LKs[c][:], in0=PS[6][0:64, 256:512], in1=cst[0:64, MSU:MSU + 256], op=ALU.mult),
                           reads=[PSB[6], cstB], writes=[LKsB[c]])
                        sc(rw['AE'], QE, PS[6][:, 0:256])
                        OP('dve', lambda e, c=c: e.tensor_tensor(out=ARs[c][:], in0=PS[6][0:64, 0:256], in1=cst[0:64, MI:MI + 256], op=ALU.mult),
                           reads=[PSB[6], cstB], writes=[ARsB[c]])
                        sc(rw['AE'], rw['BE'], PS[6][:, 256:512])
                        OP('dve', lambda e, c=c: e.scalar_tensor_tensor(out=NTn[c][:], in0=PS[6][0:64, 256:512], scalar=-1.0,
                                                                         in1=cst[0:64, MSU:MSU + 256], op0=ALU.mult, op1=ALU.mult),
                           reads=[PSB[6], cstB], writes=[NTnB[c]])
                        sc(rw['BE'], rw['AE'], PS[6][:, 0:256])
                        OP('dve', lambda e, c=c: e.scalar_tensor_tensor(out=Nn[c][:], in0=PS[6][0:64, 0:256], scalar=-1.0,
                                                                         in1=cst[0:64, MSL:MSL + 256], op0=ALU.mult, op1=ALU.mult),
                           reads=[PSB[6], cstB], writes=[NnB[c]])
                        OP('pool', lambda e, c=c: e.tensor_tensor(out=Pn[c][:], in0=NTn[c][:], in1=cst[0:64, ID4:ID4 + 256], op=ALU.add),
                           reads=[NTnB[c], cstB], writes=[PnB[c]])
                if isrw:
                    curN, curNB, curNT, curNTB = Nn, NnB, NTn, NTnB
                    nxtN, nxtNB, nxtNT, nxtNTB = Nn2, Nn2B, NTn2, NTn2B
                    curP, curPB, nxtP, nxtPB = Pn, PnB, Pn2, Pn2B
                    for lev in range(1, 6):
                        for c in range(NCH):
                            bk = pbank()
                            for h in range(4):
                                hs = slice(h * 64, (h + 1) * 64)
                                OP('pe', lambda e, c=c, hs=hs, bk=bk, a=curNT, b=curN: e.matmul(PS[bk][0:64, hs], lhsT=a[c][:, hs], rhs=b[c][:, hs],
                                                                                         start=True, stop=True),
                                   reads=[curNTB[c], curNB[c]], writes=[PSB[bk]], rg=0)
                            if lev < 5:
                                for h in range(4):
                                    hs = slice(h * 64, (h + 1) * 64)
                                    hs2 = slice(256 + h * 64, 256 + (h + 1) * 64)
                                    OP('pe', lambda e, c=c, hs=hs, hs2=hs2, bk=bk, a=curN, b=curNT: e.matmul(PS[bk][0:64, hs2], lhsT=a[c][:, hs], rhs=b[c][:, hs],
                                                                                                     start=True, stop=True),
                                       reads=[curNTB[c], curNB[c]], writes=[PSB[bk]], rg=0)
                                OP('act', lambda e, c=c, bk=bk, d=nxtNT: e.activation(out=d[c][:], in_=PS[bk][0:64, 256:512], func=AF.Copy),
                                   reads=[PSB[bk]], writes=[nxtNTB[c]])
                            OP('act', lambda e, c=c, bk=bk, d=nxtN: e.activation(out=d[c][:], in_=PS[bk][0:64, 0:256], func=AF.Copy),
                               reads=[PSB[bk]], writes=[nxtNB[c]])
                        curN, curNB, nxtN, nxtNB = nxtN, nxtNB, curN, curNB
                        curNT, curNTB, nxtNT, nxtNTB = nxtNT, nxtNTB, curNT, curNTB
                        for c in range(NCH):
                            bk = pbank()
                            for h in range(4):
                                hs = slice(h * 64, (h + 1) * 64)
                                OP('pe', lambda e, c=c, hs=hs, bk=bk, a=curN, b=curP: e.matmul(PS[bk][0:64, hs], lhsT=a[c][:, hs], rhs=b[c][:, hs],
                                                                                        start=True, stop=True),
                                   reads=[curNB[c], curPB[c]], writes=[PSB[bk]], rg=0)
                            OP('dve', lambda e, c=c, bk=bk, s=curP, d=nxtP: e.tensor_tensor(out=d[c][:], in0=PS[bk][0:64, 0:256], in1=s[c][:], op=ALU.add),
                               reads=[PSB[bk], curPB[c]], writes=[nxtPB[c]])
                        curP, curPB, nxtP, nxtPB = nxtP, nxtPB, curP, curPB
                    TT, TTB = curP, curPB
                if DEBUG_STAGE < 3:
                    return
                S3, Sb, S3B, SbB = S32[mname], Sbf[mname], S32B[mname], SbfB[mname]
                for c in range(NCH):
                    cs = slice(c * CH, (c + 1) * CH)
                    if isrw:
                        for h in range(4):
                            hp, r = h // 2, (h % 2) * 64
                            hs = slice(h * 64, (h + 1) * 64)
                            OP('pe', lambda e, hp=hp, r=r, hs=hs, cs=cs: e.matmul(PS[6][0:64, hs], lhsT=rw['BE'][hp][0][r:r + 64, cs], rhs=Sb[hp][r:r + 64, :],
                                                                         start=True, stop=False),
                               reads=[rw['BE'][hp][1], SbB[hp]], writes=[PSB[6]], rg=r)
                            OP('pe', lambda e, hs=hs, c=c: e.matmul(PS[6][0:64, hs], lhsT=LKs[c][:, hs], rhs=Vt[c][:, hs], start=False, stop=True),
                               reads=[LKsB[c], VtB[c]], writes=[PSB[6]], rg=0)
                        OP('act', lambda e: e.activation(out=Ysb[:], in_=PS[6][0:64, 0:256], func=AF.Copy), reads=[PSB[6]], writes=[YsbB])
                        for h in range(4):
                            hs = slice(h * 64, (h + 1) * 64)
                            hs2 = slice(256 + h * 64, 256 + (h + 1) * 64)
                            OP('pe', lambda e, hs=hs, hs2=hs2, c=c: e.matmul(PS[6][0:64, hs2], lhsT=TT[c][:, hs], rhs=Ysb[:, hs], start=True, stop=True),
                               reads=[TTB[c], YsbB], writes=[PSB[6]], rg=0)
                        OP('act', lambda e: e.activation(out=Usb[:], in_=PS[6][0:64, 256:512], func=AF.Copy, scale=-1.0),
                           reads=[PSB[6]], writes=[UsbB])
                    for h in range(4):
                        hp, r = h // 2, (h % 2) * 64
                        hs = slice(h * 64, (h + 1) * 64)
                        ob = PS[4 + hp][r:r + 64, cs]
                        OP('pe', lambda e, ob=ob, hs=hs, c=c: e.matmul(ob, lhsT=Vt[c][:, hs], rhs=ATs[c][:, hs], start=True, stop=False),
                           reads=[VtB[c], ATsB[c]], writes=[PSB[4 + hp]], rg=0)
                        if isrw:
                            OP('pe', lambda e, ob=ob, hs=hs, c=c: e.matmul(ob, lhsT=Usb[:, hs], rhs=ARs[c][:, hs], start=False, stop=False),
                               reads=[UsbB, ARsB[c]], writes=[PSB[4 + hp]], rg=0)
                        OP('pe', lambda e, ob=ob, hp=hp, r=r, cs=cs: e.matmul(ob, lhsT=Sb[hp][r:r + 64, :], rhs=QE[hp][0][r:r + 64, cs], start=False, stop=True),
                           reads=[SbB[hp], QE[hp][1]], writes=[PSB[4 + hp]], rg=r)
                    for h in range(4):
                        hp, r = h // 2, (h % 2) * 64
                        hs = slice(h * 64, (h + 1) * 64)
                        sp_ = PS[7][r:r + 64, 256 + hp * 64:256 + (hp + 1) * 64]
                        OP('pe', lambda e, sp_=sp_, hs=hs, c=c: e.matmul(sp_, lhsT=KEt[c][:, hs], rhs=Vt[c][:, hs], start=True, stop=(not isrw)),
                           reads=[KEtB[c], VtB[c]], writes=[PSB[7]], rg=0)
                        if isrw:
                            OP('pe', lambda e, sp_=sp_, hs=hs, c=c: e.matmul(sp_, lhsT=AEt[c][:, hs], rhs=Usb[:, hs], start=False, stop=True),
                               reads=[AEtB[c], UsbB], writes=[PSB[7]], rg=0)
                    for hp in range(2):
                        pc = PC[hp][0][:, (c + 1) * CH - 1:(c + 1) * CH]
                        OP('dve', lambda e, hp=hp: e.tensor_tensor(out=stmp[hp][:], in0=PS[7][:, 256 + hp * 64:256 + (hp + 1) * 64], in1=S3[hp][:], op=ALU.add),
                           reads=[PSB[7], S3B[hp]], writes=[stmpB[hp]])
                        OP('dve', lambda e, hp=hp, pc=pc: e.tensor_scalar(out=S3[hp][:], in0=stmp[hp][:], scalar1=pc, scalar2=None, op0=ALU.mult),
                           reads=[stmpB[hp], PC[hp][1]], writes=[S3B[hp]])
                        OP('dve', lambda e, hp=hp, pc=pc: e.tensor_scalar(out=Sb[hp][:], in0=stmp[hp][:], scalar1=pc, scalar2=None, op0=ALU.mult),
                           reads=[stmpB[hp], PC[hp][1]], writes=[SbB[hp]])

            def evac_fm(bk, func=AF.Copy, scale=1.0, bias=None, dt='f', rows=128, dst=None):
                t, b = dst if dst is not None else (fa() if dt == 'f' else ba())
                kw = {}
                if bias is not None:
                    kw['bias'] = bias
                OP('act', lambda e: e.activation(out=t[0:rows, :], in_=PS[bk][0:rows, 0:TBM], func=func, scale=scale, **kw),
                   reads=[PSB[bk]] + ([pvB[0]] if bias is not None else []), writes=[b])
                return t, b

            def mixer_block(l, s, bi):
                first = (bi == 0)
                c0 = bi * TBM
                ti = c0 // TB
                plan = []
                if 'pool' in mixers:
                    plan += [('fm', 0), ('fm', 1)]
                if 'hgrn' in mixers:
                    for hp_ in range(2):
                        plan += [('fm', 2 + hp_), ('fm', 4 + hp_), ('fm', 6 + hp_)]
                    plan += [('tm', 0)]
                if 'gla' in mixers:
                    plan += [('fm', 22)]
                    for hp_ in range(2):
                        plan += [('fm', 16 + hp_), ('fm', 18 + hp_), ('fm', 20 + hp_)]
                    plan += [('tm', 2)]
                if 'rwkv' in mixers:
                    plan += [('fm', 14), ('fm', 15)]
                    for hp_ in range(2):
                        plan += [('fm', 8 + hp_), ('fm', 10 + hp_), ('fm', 12 + hp_)]
                    plan += [('tm', 1)]
                wplan['plan'] = plan
                wplan['pos'] = 0
                wplan['issued'] = {}
                if plan:
                    wplan['issued'][0] = issue_desc(l, plan[0])
                pv = pvec[l]
                if 'rwkv' in mixers:
                    if first:
                        OP('pool', lambda e: e.memset(xns[:, :, 0:2], 0.0), writes=[xnsB])
                    else:
                        OP('pool', lambda e: e.tensor_copy(out=xns[:, :, 0:1], in_=xn[:, :, TBM - 1:TBM]), reads=xnB, writes=[xnsB])
                rmsnorm_to_xn(l, PV['nmx'], c0, TBM, 0)
                if 'rwkv' in mixers:
                    OP('pool', lambda e: e.tensor_copy(out=xns[:, :, 1:TBM], in_=xn[:, :, 0:TBM - 1]), reads=xnB, writes=[xnsB])
                if 'pool' in mixers:
                    if first:
                        OP('pool', lambda e: e.memset(pext[:, :, 0:16], 0.0), writes=[pextB])
                    else:
                        OP('pool', lambda e: e.tensor_copy(out=pext[:, :, 0:16], in_=pext[:, :, TBM:TBM + 16]), reads=[pextB], writes=[pextB])
                    for ck in range(2):
                        bk = proj_fm(l, ck, first)
                        OP('act', lambda e, ck=ck, bk=bk: e.activation(out=pext[:, ck, 16:16 + TBM], in_=PS[bk][:, 0:TBM], func=AF.Copy),
                           reads=[PSB[bk]], writes=[pextB])
                    W_ = 16 + TBM
                    OP('dve', lambda e: e.tensor_tensor(out=pw[0][:, :, 1:W_], in0=pext[:, :, 1:W_], in1=pext[:, :, 0:W_ - 1], op=ALU.add),
                       reads=[pextB], writes=[pwB[0]])
                    def poolfin(src, ck, r0, wdw, first=first):
                        yt, yb = PB16[ck], PB16B[ck]
                        OP('dve', lambda e: e.scalar_tensor_tensor(out=yt[r0:r0 + 64, :], in0=src[r0:r0 + 64, ck, 16:16 + TBM], scalar=1.0 / wdw,
                                                                   in1=pext[r0:r0 + 64, ck, 16:16 + TBM], op0=ALU.mult, op1=ALU.subtract),
                           reads=[pwB[0], pwB[1], pextB], writes=[yb])
                        if first:
                            t2, b2 = FA[0], FAB[0]
                            OP('dve', lambda e: e.tensor_tensor(out=t2[r0:r0 + 64, 0:16], in0=src[r0:r0 + 64, ck, 16:32],
                                                                in1=cst[r0:r0 + 64, ICN + ck * 16:ICN + ck * 16 + 16], op=ALU.mult),
                               reads=[pwB[0], pwB[1], cstB], writes=[b2])
                            OP('dve', lambda e: e.tensor_tensor(out=yt[r0:r0 + 64, 0:16], in0=t2[r0:r0 + 64, 0:16],
                                                                in1=pext[r0:r0 + 64, ck, 16:32], op=ALU.subtract),
                               reads=[b2, pextB], writes=[yb])
                    poolfin(pw[0], 0, 0, 2)
                    OP('dve', lambda e: e.tensor_tensor(out=pw[1][:, :, 3:W_], in0=pw[0][:, :, 3:W_], in1=pw[0][:, :, 1:W_ - 2], op=ALU.add),
                       reads=[pwB[0]], writes=[pwB[1]])
                    poolfin(pw[1], 0, 64, 4)
                    OP('dve', lambda e: e.tensor_tensor(out=pw[0][:, :, 7:W_], in0=pw[1][:, :, 7:W_], in1=pw[1][:, :, 3:W_ - 4], op=ALU.add),
                       reads=[pwB[1]], writes=[pwB[0]])
                    poolfin(pw[0], 1, 0, 8)
                    OP('dve', lambda e: e.tensor_tensor(out=pw[1][:, :, 15:W_], in0=pw[0][:, :, 15:W_], in1=pw[0][:, :, 7:W_ - 8], op=ALU.add),
                       reads=[pwB[0]], writes=[pwB[1]])
                    poolfin(pw[1], 1, 64, 16)
                    for ck in range(2):
                        bk = pbank()
                        OP('pe', lambda e, ck=ck, bk=bk: e.matmul(PS[bk][:, 0:TBM], lhsT=smatb[:, ck * 128:(ck + 1) * 128], rhs=PB16[ck][:], start=True, stop=True),
                           reads=[PB16B[ck], smatB], writes=[PSB[bk]])
                        OP('dve', lambda e, ck=ck, bk=bk: e.tensor_scalar(out=YT[:, ck, :], in0=PS[bk][:, 0:TBM], scalar1=pv[:, PV['pool_b'] + ck:PV['pool_b'] + ck + 1],
                                                                      scalar2=pv[:, PV['pool_s'] + ck:PV['pool_s'] + ck + 1], op0=ALU.add, op1=ALU.mult),
                           reads=[PSB[bk], pvB[l]], writes=[YB[ck]])
                else:
                    for ck in range(2):
                        OP('pool', lambda e, ck=ck: e.memset(YT[:, ck, :], 0.0), writes=[YB[ck]])

                def out_rstd(Ot, lhs_ones, n_ch, eps):
                    bk = pbank()
                    for hp in range(2):
                        st, sbb = ba()
                        OP('act', lambda e, hp=hp, st=st, Ot=Ot: e.activation(out=st[:], in_=Ot[hp][0][:], func=AF.Square), reads=[Ot[hp][1]], writes=[sbb])
                        if lhs_ones is ones:
                            OP('pe', lambda e, hp=hp, st=st: e.matmul(PS[bk][:, 0:TBM], lhsT=ones[:], rhs=st[:], start=(hp == 0), stop=(hp == 1)),
                               reads=[sbb, onesB], writes=[PSB[bk]])
                        else:
                            bk2 = bk if hp == 0 else pbank()
                            OP('pe', lambda e, hp=hp, st=st, bk2=bk2: e.matmul(PS[bk2][:, 0:TBM], lhsT=bones[:], rhs=st[:], start=True, stop=True),
                               reads=[sbb, bonesB], writes=[PSB[bk2]])
                            if hp == 0:
                                bk0 = bk2
                            else:
                                bk1 = bk2
                    if lhs_ones is ones:
                        rt, rb = fa()
                        rstd_from(PS[bk], PSB[bk], TBM, 1.0 / n_ch, eps, rt, rb)
                        return [(rt, rb), (rt, rb)]
                    res = []
                    for bkx in (bk0, bk1):
                        rt, rb = fa()
                        rstd_from(PS[bkx], PSB[bkx], TBM, 1.0 / n_ch, eps, rt, rb)
                        res.append((rt, rb))
                    return res

                def evac_O():
                    Ot = []
                    for hp in range(2):
                        t, b = fa()
                        OP('act', lambda e, hp=hp, t=t: e.activation(out=t[:], in_=PS[4 + hp][:, 0:TBM], func=AF.Copy), reads=[PSB[4 + hp]], writes=[b])
                        Ot.append((t, b))
                    return Ot

                if 'hgrn' in mixers:
                    QE, KE, PCx, GT = [], [], [], []
                    for hp in range(2):
                        bq = proj_fm(l, 2 + hp, first)
                        qt, qb = evac_fm(bq, AF.Silu)
                        bf_ = proj_fm(l, 4 + hp, first)
                        st_, sb_ = evac_fm(bf_, AF.Sigmoid)
                        ft, fb = fa()
                        OP('dve', lambda e, hp=hp, st_=st_, ft=ft: e.tensor_scalar(out=ft[:], in0=st_[:], scalar1=lbt[:, 2 * l + hp:2 * l + hp + 1],
                                                                             scalar2=lbt[:, 4 + 2 * l + hp:4 + 2 * l + hp + 1], op0=ALU.mult, op1=ALU.add),
                           reads=[sb_, lbtB], writes=[fb])
                        lt, lb_ = fa()
                        OP('dve', lambda e, ft=ft, lt=lt: e.tensor_scalar_max(out=lt[:], in0=ft[:], scalar1=1e-30), reads=[fb], writes=[lb_])
                        OP('act', lambda e, lt=lt: e.activation(out=lt[:], in_=lt[:], func=AF.Ln), reads=[lb_], writes=[lb_])
                        bt, bb = scan_decay(lt, lb_)
                        ebt, ebb = LL[hp][0], LLB_[hp][0]
                        OP('act', lambda e, bt=bt, ebt=ebt: e.activation(out=ebt[:], in_=bt[:], func=AF.Exp), reads=[bb], writes=[ebb])
                        OP('act', lambda e, bt=bt: e.activation(out=bt[:], in_=bt[:], func=AF.Exp, scale=-1.0), reads=[bb], writes=[bb])
                        qe, qeb = LB[hp][0], LBB[hp][0]
                        OP('dve', lambda e, qt=qt, ebt=ebt, qe=qe: e.scalar_tensor_tensor(out=qe[:], in0=qt[:], scalar=QK, in1=ebt[:], op0=ALU.mult, op1=ALU.mult),
                           reads=[qb, ebb], writes=[qeb])
                        OP('dve', lambda e, ft=ft: e.tensor_scalar(out=ft[:], in0=ft[:], scalar1=-1.0, scalar2=1.0, op0=ALU.mult, op1=ALU.add),
                           reads=[fb], writes=[fb])
                        ke, keb = LB[hp][1], LBB[hp][1]
                        OP('dve', lambda e, ft=ft, bt=bt, ke=ke: e.tensor_tensor(out=ke[:], in0=ft[:], in1=bt[:], op=ALU.mult), reads=[fb, bb], writes=[keb])
                        bg_ = proj_fm(l, 6 + hp, first)
                        gt, gb = evac_fm(bg_, AF.Sigmoid, dst=(LL[hp][1], LLB_[hp][1]))
                        QE.append((qe, qeb)); KE.append((ke, keb)); PCx.append((ebt, ebb)); GT.append((gt, gb))
                    proj_tm(l, 0)
                    chunk_engine('hgrn', QE, KE, PCx)
                    Ot = evac_O()
                    rs = out_rstd(Ot, ones, 256, NORM_EPS)
                    for hp in range(2):
                        t1, b1 = fa()
                        OP('dve', lambda e, hp=hp, t1=t1, Ot=Ot, rs=rs: e.scalar_tensor_tensor(out=t1[:], in0=Ot[hp][0][:], scalar=pv[:, PV['hnorm'] + hp:PV['hnorm'] + hp + 1],
                                                                           in1=rs[hp][0][:], op0=ALU.mult, op1=ALU.mult),
                           reads=[Ot[hp][1], rs[hp][1], pvB[l]], writes=[b1])
                        OP('dve', lambda e, hp=hp, t1=t1, GT=GT: e.tensor_tensor(out=YT[:, 2 + hp, :], in0=t1[:], in1=GT[hp][0][:], op=ALU.mult),
                           reads=[b1, GT[hp][1]], writes=[YB[2 + hp]])
                else:
                    for ck in (2, 3):
                        OP('pool', lambda e, ck=ck: e.memset(YT[:, ck, :], 0.0), writes=[YB[ck]])

                if 'gla' in mixers:
                    bga = proj_fm(l, 22, first)
                    gat, gab = LLX[0], LLXB[0]
                    OP('act', lambda e: e.activation(out=gat[:], in_=PS[bga][:, 0:TBM], func=AF.Copy), reads=[PSB[bga]], writes=[gab])
                    QE, KE, PCx, GT = [], [], [], []
                    for hp in range(2):
                        bk = pbank()
                        OP('pe', lambda e, hp=hp, bk=bk: e.matmul(PS[bk][:, 0:TBM], lhsT=smat[:, 1024 + hp * 128:1024 + (hp + 1) * 128], rhs=gat[:], start=True, stop=True),
                           reads=[gab, smatB], writes=[PSB[bk]])
                        lt, lb_ = evac_fm(bk, AF.Sigmoid, bias=pv[:, PV['glab'] + hp:PV['glab'] + hp + 1])
                        OP('act', lambda e, lt=lt: e.activation(out=lt[:], in_=lt[:], func=AF.Ln), reads=[lb_], writes=[lb_])
                        bt, bb = scan_decay(lt, lb_)
                        ebt, ebb = LL[hp][0], LLB_[hp][0]
                        OP('act', lambda e, bt=bt, ebt=ebt: e.activation(out=ebt[:], in_=bt[:], func=AF.Exp, scale=1.0 / 16), reads=[bb], writes=[ebb])
                        OP('act', lambda e, bt=bt: e.activation(out=bt[:], in_=bt[:], func=AF.Exp, scale=-1.0 / 16), reads=[bb], writes=[bb])
                        bq = proj_fm(l, 16 + hp, first)
                        qe, qeb = LB[hp][0], LBB[hp][0]
                        OP('dve', lambda e, bq=bq, ebt=ebt, qe=qe: e.scalar_tensor_tensor(out=qe[:], in0=PS[bq][:, 0:TBM], scalar=QK, in1=ebt[:], op0=ALU.mult, op1=ALU.mult),
                           reads=[PSB[bq], ebb], writes=[qeb])
                        bkk = proj_fm(l, 18 + hp, first)
                        ke, keb = LB[hp][1], LBB[hp][1]
                        OP('dve', lambda e, bkk=bkk, bt=bt, ke=ke: e.tensor_tensor(out=ke[:], in0=PS[bkk][:, 0:TBM], in1=bt[:], op=ALU.mult), reads=[PSB[bkk], bb], writes=[keb])
                        bg_ = proj_fm(l, 20 + hp, first)
                        gt, gb = evac_fm(bg_, AF.Silu, dst=(LL[hp][1], LLB_[hp][1]))
                        QE.append((qe, qeb)); KE.append((ke, keb)); PCx.append((ebt, ebb)); GT.append((gt, gb))
                    proj_tm(l, 2)
                    chunk_engine('gla', QE, KE, PCx)
                    Ot = evac_O()
                    rs = out_rstd(Ot, bones, 64, NORM_EPS)
                    for hp in range(2):
                        t1, b1 = fa()
                        OP('dve', lambda e, hp=hp, t1=t1, Ot=Ot, rs=rs: e.scalar_tensor_tensor(out=t1[:], in0=Ot[hp][0][:], scalar=pv[:, PV['gnorm'] + hp:PV['gnorm'] + hp + 1],
                                                                           in1=rs[hp][0][:], op0=ALU.mult, op1=ALU.mult),
                           reads=[Ot[hp][1], rs[hp][1], pvB[l]], writes=[b1])
                        OP('dve', lambda e, hp=hp, t1=t1, GT=GT: e.tensor_tensor(out=YT[:, 6 + hp, :], in0=t1[:], in1=GT[hp][0][:], op=ALU.mult),
                           reads=[b1, GT[hp][1]], writes=[YB[6 + hp]])
                else:
                    for ck in (6, 7):
                        OP('pool', lambda e, ck=ck: e.memset(YT[:, ck, :], 0.0), writes=[YB[ck]])

                if 'rwkv' in mixers:
                    bwa = proj_fm(l, 14, first)
                    twa, twab = LLX[0], LLXB[0]
                    OP('act', lambda e: e.activation(out=twa[0:64, :], in_=PS[bwa][0:64, 0:TBM], func=AF.Tanh), reads=[PSB[bwa]], writes=[twab])
                    OP('act', lambda e: e.activation(out=twa[64:128, :], in_=PS[bwa][64:128, 0:TBM], func=AF.Copy), reads=[PSB[bwa]], writes=[twab])
                    bxg = proj_fm(l, 15, first)
                    sxg, sxgb = evac_fm(bxg, AF.Sigmoid, dst=(LLX[1], LLXB[1]))
                    QE, KE, AE, BE, PCx, GR, RKR, VF = [], [], [], [], [], [], [], []
                    for hp in range(2):
                        cs_ = slice(hp * 128, (hp + 1) * 128)
                        bk = pbank()
                        OP('pe', lambda e, bk=bk, hp=hp: e.matmul(PS[bk][:, 0:TBM], lhsT=smat[:, 256 + hp * 128:256 + (hp + 1) * 128], rhs=twa[:], start=True, stop=True),
                           reads=[twab, smatB], writes=[PSB[bk]])
                        lw, lwb = evac_fm(bk, AF.Sigmoid, bias=pv[:, PV['w0'] + hp:PV['w0'] + hp + 1])
                        bk = pbank()
                        OP('pe', lambda e, bk=bk, hp=hp: e.matmul(PS[bk][:, 0:TBM], lhsT=smat[:, 768 + hp * 128:768 + (hp + 1) * 128], rhs=twa[:], start=True, stop=True),
                           reads=[twab, smatB], writes=[PSB[bk]])
                        at, ab_ = evac_fm(bk, AF.Sigmoid, bias=pv[:, PV['a0'] + hp:PV['a0'] + hp + 1])
                        bk = pbank()
                        OP('pe', lambda e, bk=bk, hp=hp: e.matmul(PS[bk][:, 0:TBM], lhsT=smat[:, 512 + hp * 128:512 + (hp + 1) * 128], rhs=sxg[:], start=True, stop=True),
                           reads=[sxgb, smatB], writes=[PSB[bk]])
                        grt, grb = evac_fm(bk, AF.Copy, dst=(LL[hp][1], LLB_[hp][1]))
                        bt, bb = scan_decay(lw, lwb)
                        CW = -float(np.exp(-0.5))
                        ebt, ebb = LL[hp][0], LLB_[hp][0]
                        OP('act', lambda e, bt=bt, ebt=ebt: e.activation(out=ebt[:], in_=bt[:], func=AF.Exp, scale=CW), reads=[bb], writes=[ebb])
                        enb, enbb = fa()
                        OP('act', lambda e, bt=bt, enb=enb: e.activation(out=enb[:], in_=bt[:], func=AF.Exp, scale=-CW), reads=[bb], writes=[enbb])
                        OP('dve', lambda e, bt=bt, lw=lw: e.tensor_tensor(out=bt[:], in0=bt[:], in1=lw[:], op=ALU.subtract), reads=[bb, lwb], writes=[bb])
                        OP('act', lambda e, bt=bt: e.activation(out=bt[:], in_=bt[:], func=AF.Exp, scale=CW), reads=[bb], writes=[bb])
                        br = proj_fm(l, 8 + hp, first)
                        rt, rb = evac_fm(br, AF.Copy)
                        bkr = proj_fm(l, 10 + hp, first)
                        kt, kb = evac_fm(bkr, AF.Copy)
                        bv = proj_fm(l, 12 + hp, first)
                        vt, vb = evac_fm(bv, AF.Copy, dst=(LL[hp][2], LLB_[hp][2]))
                        kkt, kkb = fa()
                        OP('dve', lambda e, kt=kt, kkt=kkt, hp=hp: e.tensor_scalar(out=kkt[:], in0=kt[:], scalar1=pv[:, PV['kk'] + hp:PV['kk'] + hp + 1], scalar2=None, op0=ALU.mult),
                           reads=[kb, pvB[l]], writes=[kkb])
                        sq_, sqb_ = ba()
                        OP('act', lambda e, kkt=kkt, sq_=sq_: e.activation(out=sq_[:], in_=kkt[:], func=AF.Square), reads=[kkb], writes=[sqb_])
                        bk = pbank()
                        OP('pe', lambda e, bk=bk, sq_=sq_: e.matmul(PS[bk][:, 0:TBM], lhsT=bones[:], rhs=sq_[:], start=True, stop=True), reads=[sqb_, bonesB], writes=[PSB[bk]])
                        rn, rnb = fa()
                        rstd_from(PS[bk], PSB[bk], TBM, 1.0, 1e-24, rn, rnb)
                        OP('dve', lambda e, kkt=kkt, rn=rn: e.tensor_tensor(out=kkt[:], in0=kkt[:], in1=rn[:], op=ALU.mult), reads=[kkb, rnb], writes=[kkb])
                        fac, facb = fa()
                        OP('dve', lambda e, at=at, fac=fac, hp=hp: e.tensor_scalar(out=fac[:], in0=at[:], scalar1=-1.0, scalar2=pv[:, PV['ka'] + hp:PV['ka'] + hp + 1], op0=ALU.add, op1=ALU.mult),
                           reads=[ab_, pvB[l]], writes=[facb])
                        OP('dve', lambda e, fac=fac, kt=kt: e.scalar_tensor_tensor(out=kt[:], in0=fac[:], scalar=1.0, in1=kt[:], op0=ALU.add, op1=ALU.mult),
                           reads=[facb, kb], writes=[kb])
                        rk_, rkb_ = LB[hp][4], LBB[hp][4]
                        OP('dve', lambda e, rt=rt, kt=kt, rk_=rk_, hp=hp: e.scalar_tensor_tensor(out=rk_[:], in0=rt[:], scalar=pv[:, PV['rk'] + hp:PV['rk'] + hp + 1], in1=kt[:], op0=ALU.mult, op1=ALU.mult),
                           reads=[rb, kb, pvB[l]], writes=[rkb_])
                        qe, qeb = LB[hp][0], LBB[hp][0]
                        OP('dve', lambda e, rt=rt, ebt=ebt, qe=qe: e.tensor_tensor(out=qe[:], in0=rt[:], in1=ebt[:], op=ALU.mult), reads=[rb, ebb], writes=[qeb])
                        ke, keb = LB[hp][1], LBB[hp][1]
                        OP('dve', lambda e, kt=kt, enb=enb, ke=ke: e.tensor_tensor(out=ke[:], in0=kt[:], in1=enb[:], op=ALU.mult), reads=[kb, enbb], writes=[keb])
                        be, beb = LB[hp][3], LBB[hp][3]
                        OP('dve', lambda e, kkt=kkt, bt=bt, be=be: e.tensor_tensor(out=be[:], in0=kkt[:], in1=bt[:], op=ALU.mult), reads=[kkb, bb], writes=[beb])
                        OP('dve', lambda e, kkt=kkt, at=at: e.tensor_tensor(out=kkt[:], in0=kkt[:], in1=at[:], op=ALU.mult), reads=[kkb, ab_], writes=[kkb])
                        ae, aeb = LB[hp][2], LBB[hp][2]
                        OP('dve', lambda e, kkt=kkt, enb=enb, ae=ae: e.tensor_tensor(out=ae[:], in0=kkt[:], in1=enb[:], op=ALU.mult), reads=[kkb, enbb], writes=[aeb])
                        QE.append((qe, qeb)); KE.append((ke, keb)); AE.append((ae, aeb)); BE.append((be, beb)); PCx.append((ebt, ebb))
                        GR.append((grt, grb)); RKR.append((rk_, rkb_)); VF.append((vt, vb))
                    proj_tm(l, 1)
                    chunk_engine('rwkv', QE, KE, PCx, rw={'AE': AE, 'BE': BE})
                    Ot = evac_O()
                    for hp in range(2):
                        ob16, ob16b = ba()
                        OP('dve', lambda e, hp=hp, ob16=ob16, Ot=Ot: e.tensor_copy(out=ob16[:], in_=Ot[hp][0][:]), reads=[Ot[hp][1]], writes=[ob16b])
                        bk = pbank()
                        OP('pe', lambda e, bk=bk, ob16=ob16: e.matmul(PS[bk][:, 0:TBM], lhsT=bones[:], rhs=ob16[:], start=True, stop=True), reads=[ob16b, bonesB], writes=[PSB[bk]])
                        ct, cb = fa()
                        OP('dve', lambda e, hp=hp, bk=bk, ct=ct, Ot=Ot: e.scalar_tensor_tensor(out=ct[:], in0=PS[bk][:, 0:TBM], scalar=-1.0 / 64, in1=Ot[hp][0][:], op0=ALU.mult, op1=ALU.add),
                           reads=[PSB[bk], Ot[hp][1]], writes=[cb])
                        s2, s2b = ba()
                        OP('act', lambda e, ct=ct, s2=s2: e.activation(out=s2[:], in_=ct[:], func=AF.Square), reads=[cb], writes=[s2b])
                        bk = pbank()
                        OP('pe', lambda e, bk=bk, s2=s2: e.matmul(PS[bk][:, 0:TBM], lhsT=bones[:], rhs=s2[:], start=True, stop=True), reads=[s2b, bonesB], writes=[PSB[bk]])
                        rn, rnb = fa()
                        rstd_from(PS[bk], PSB[bk], TBM, 1.0 / 64, GN_EPS, rn, rnb)
                        OP('dve', lambda e, hp=hp, ct=ct, rn=rn: e.scalar_tensor_tensor(out=ct[:], in0=ct[:], scalar=pv[:, PV['lnw'] + hp:PV['lnw'] + hp + 1], in1=rn[:], op0=ALU.mult, op1=ALU.mult),
                           reads=[cb, rnb, pvB[l]], writes=[cb])
                        bk = pbank()
                        OP('pe', lambda e, bk=bk, hp=hp, RKR=RKR: e.matmul(PS[bk][:, 0:TBM], lhsT=bones[:], rhs=RKR[hp][0][:], start=True, stop=True), reads=[RKR[hp][1], bonesB], writes=[PSB[bk]])
                        bo, bob = fa()
                        OP('dve', lambda e, bk=bk, hp=hp, bo=bo, VF=VF: e.tensor_tensor(out=bo[:], in0=PS[bk][:, 0:TBM], in1=VF[hp][0][:], op=ALU.mult), reads=[PSB[bk], VF[hp][1]], writes=[bob])
                        OP('dve', lambda e, hp=hp, ct=ct, bo=bo: e.scalar_tensor_tensor(out=ct[:], in0=ct[:], scalar=pv[:, PV['lnb'] + hp:PV['lnb'] + hp + 1], in1=bo[:], op0=ALU.add, op1=ALU.add),
                           reads=[cb, bob, pvB[l]], writes=[cb])
                        OP('dve', lambda e, hp=hp, ct=ct, GR=GR: e.tensor_tensor(out=YT[:, 4 + hp, :], in0=ct[:], in1=GR[hp][0][:], op=ALU.mult),
                           reads=[cb, GR[hp][1]], writes=[YB[4 + hp]])
                else:
                    for ck in (4, 5):
                        OP('pool', lambda e, ck=ck: e.memset(YT[:, ck, :], 0.0), writes=[YB[ck]])

                nxt_o = load_o(m_wout[l][0], 'act')
                for j in range(KD):
                    i = nxt_o
                    if j + 1 < KD:
                        nxt_o = load_o(m_wout[l][j + 1], 'act')
                    for m in range(KD):
                        OP('pe', lambda e, m=m, j=j, i=i: e.matmul(PS[m][:, 0:TBM], lhsT=obf[i][:, m * 128:(m + 1) * 128], rhs=YT[:, j, :],
                                                               start=(j == 0), stop=(j == KD - 1)),
                           reads=[obfB[i], YB[j]], writes=[PSB[m]], sig=(m == KD - 1 or j == KD - 1))
                for m in range(KD):
                    OP('dve', lambda e, m=m: e.tensor_tensor(out=X[:, m, c0:c0 + TBM], in0=PS[m][:, 0:TBM], in1=X[:, m, c0:c0 + TBM], op=ALU.add),
                       reads=[PSB[m], XB[m][ti]], writes=[XB[m][ti]])

            def mixer_setup(l, s):
                OP('sp', lambda e: e.dma_start(out=mub[:], in_=mub_d[l]), writes=[mubB], dsem=muS)
                OP('sp', lambda e: e.dma_start(out=smat[:], in_=smat_d[l]), writes=[smatB], dsem=smS)
                OP('pool', lambda e: e.tensor_copy(out=smatb[:], in_=smat[:, 0:256]), reads=[smatB], writes=[smatB])
                for m in ('hgrn', 'gla', 'rwkv'):
                    for hp in range(2):
                        OP('pool', lambda e, m=m, hp=hp: e.memset(S32[m][hp][:], 0.0), writes=[S32B[m][hp]])
                        OP('pool', lambda e, m=m, hp=hp: e.memset(Sbf[m][hp][:], 0.0), writes=[SbfB[m][hp]])

        if do_mix:
            OP('act', lambda e: e.activation(out=lbt[:, 0:2], in_=pvec[0][:, PV['lb0']:PV['lb0'] + 2], func=AF.Exp), reads=[pvB[0]], writes=[lbtB])
            OP('act', lambda e: e.activation(out=lbt[:, 2:4], in_=pvec[L - 1][:, PV['lbl']:PV['lbl'] + 2], func=AF.Exp), reads=[pvB[L - 1], lbtB], writes=[lbtB])
            OP('dve', lambda e: e.tensor_tensor(out=lbt[:, 4:6], in0=lbt[:, 0:2], in1=lbt[:, 2:4], op=ALU.add), reads=[lbtB], writes=[lbtB])
            OP('dve', lambda e: e.reciprocal(out=lbt[:, 4:6], in_=lbt[:, 4:6]), reads=[lbtB], writes=[lbtB])
            OP('dve', lambda e: e.tensor_tensor(out=lbt[:, 6:8], in0=lbt[:, 2:4], in1=lbt[:, 4:6], op=ALU.mult), reads=[lbtB], writes=[lbtB])
            OP('dve', lambda e: e.memset(lbt[:, 4:6], 0.0), reads=[lbtB], writes=[lbtB])
            OP('dve', lambda e: e.tensor_scalar(out=lbt[:, 0:4], in0=lbt[:, 4:8], scalar1=-1.0, scalar2=1.0, op0=ALU.mult, op1=ALU.add), reads=[lbtB], writes=[lbtB])

        for s in range(NS):
            for k in range(KD):
                OP('sp', lambda e, k=k, s=s: e.dma_start(out=X[:, k, :], in_=xT[s, :, k, :]), writes=XB[k], dsem=xS[k])
            for l in range(L):
                if do_ffn:
                    for ti in range(NT):
                        ffn(l, 0, ti)
                if do_mix:
                    mixer_setup(l, s)
                    for bi in range(T // TBM):
                        mixer_block(l, s, bi)
                if do_ffn:
                    for ti in range(NT):
                        ffn(l, 1, ti)
            for ti in range(NT):
                c0 = ti * TB
                for k in range(KD):
                    i = counters['sq'] % 2
                    counters['sq'] += 1
                    OP('act', lambda e, k=k, i=i, c0=c0: e.activation(out=sq[i][:], in_=X[:, k, c0:c0 + TB], func=AF.Square),
                       reads=[XB[k][ti]], writes=[sqB[i]])
                    OP('pe', lambda e, k=k, i=i: e.matmul(PS[0][:, :], lhsT=ones[:], rhs=sq[i][:], start=(k == 0), stop=(k == KD - 1)),
                       reads=[sqB[i], onesB], writes=[PSB[0]])
                rstd_from(PS[0], PSB[0], TB, 1.0 / D, NORM_EPS, rstd, rstdB)
                for k in range(KD):
                    OP('dve', lambda e, k=k, c0=c0: e.scalar_tensor_tensor(out=X[:, k, c0:c0 + TB], in0=X[:, k, c0:c0 + TB],
                                                                        scalar=pvec[0][:, PV['nfin'] + k:PV['nfin'] + k + 1], in1=rstd[:],
                                                                        op0=ALU.mult, op1=ALU.mult),
                       reads=[XB[k][ti], rstdB, pvB[0]], writes=[XB[k][ti]])
            for k in range(KD):
                OP('sp', lambda e, k=k, s=s: e.dma_start(out=outT[s, :, k, :], in_=X[:, k, :]), reads=XB[k], writes=XB[k], dsem=oS[k])
        pg.ops['sp'].append((lambda e: e.nop(), {o_: o_.count for o_ in oS}, False, None))
        pg.emit(block, esems)
    return nc, pg


def _col(v, n):
    return np.ascontiguousarray(np.asarray(v, np.float32).reshape(n, 128).T)


def make_consts():
    cst = np.zeros((128, 1600), np.float32)
    p = np.arange(64)[:, None]
    f = np.arange(64)[None, :]
    for h in range(4):
        cst[0:64, 0 + h * 64:0 + (h + 1) * 64] = (p <= f)
        cst[0:64, 256 + h * 64:256 + (h + 1) * 64] = (p < f)
        cst[0:64, 512 + h * 64:512 + (h + 1) * 64] = (f < p)
        cst[0:64, 768 + h * 64:768 + (h + 1) * 64] = (p == f)
    scm = np.ones(512, np.float32)
    scm[::64] = 0.0
    cst[:, 1024:1536] = scm[None, :]
    wins = {(0, 0): 2, (0, 1): 4, (1, 0): 8, (1, 1): 16}
    for ck in range(2):
        for half in range(2):
            w = wins[(ck, half)]
            t = np.arange(16)
            cst[half * 64:(half + 1) * 64, 1536 + ck * 16:1536 + ck * 16 + 16] = (1.0 / np.minimum(t + 1, w))[None, :]
    return cst


def prep_weights(inp, L):
    out = {}
    f32 = np.float32
    for l in range(L):
        for w, (wi, wo) in enumerate((('ffn1_w_in', 'ffn1_w_out'), ('ffn2_w_in', 'ffn2_w_out'))):
            W = np.asarray(inp[wi][l], f32)
            Wk = W.reshape(KD, 128, 2 * FF)
            g = Wk[:, :, :FF].reshape(KD, 128, NJ, 128)
            u = Wk[:, :, FF:].reshape(KD, 128, NJ, 128)
            blk = np.concatenate([g, u], axis=3)
            out[f"f{w + 1}_win{l}"] = np.ascontiguousarray(blk.transpose(2, 1, 0, 3)).reshape(NJ, 128, KD * 256)
            out[f"f{w + 1}_wout{l}"] = np.ascontiguousarray(np.asarray(inp[wo][l], f32).reshape(NJ, 128, D))
        W = np.asarray(inp['w_in'][l], f32).reshape(KD, 128, DIN)
        fm = np.zeros((len(FMG), 128, KD, 128), f32)
        for gi, (c0, nc_, _) in enumerate(FMG):
            nc_ = min(nc_, DIN - c0)
            fm[gi, :, :, :nc_] = W[:, :, c0:c0 + nc_].transpose(1, 0, 2)
        out[f"m_fm{l}"] = fm.reshape(len(FMG), 128, KD * 128)
        tm = np.zeros((len(TMG), 128, KD, 256), f32)
        for gi, (c0, nc_, _) in enumerate(TMG):
            tm[gi] = W[:, :, c0:c0 + nc_].transpose(1, 0, 2)
        out[f"m_tm{l}"] = tm.reshape(len(TMG), 128, KD * 256)
        out[f"m_wout{l}"] = np.ascontiguousarray(np.asarray(inp['w_out'][l], f32).reshape(KD, 128, D))
        pv = np.zeros((128, NPV), f32)
        pv[:, PV['nf1']:PV['nf1'] + 8] = _col(inp['norm_ffn1'][l], 8)
        pv[:, PV['nmx']:PV['nmx'] + 8] = _col(inp['norm_mix'][l], 8)
        pv[:, PV['nf2']:PV['nf2'] + 8] = _col(inp['norm_ffn2'][l], 8)
        pv[:, PV['nfin']:PV['nfin'] + 8] = _col(inp['norm_final'], 8)
        pv[:, PV['lb0']:PV['lb0'] + 2] = _col(inp['hgrn_lb_logits'][0], 2)
        pv[:, PV['lbl']:PV['lbl'] + 2] = _col(inp['hgrn_lb_logits'][l], 2)
        for nm, key in (('pool_b', 'pool_b'), ('pool_s', 'pool_scale'), ('hnorm', 'hgrn_norm'), ('w0', 'rwkv_w0'), ('a0', 'rwkv_a0'),
                        ('kk', 'rwkv_k_k'), ('ka', 'rwkv_k_a'), ('rk', 'rwkv_r_k'), ('lnw', 'rwkv_ln_w'), ('lnb', 'rwkv_ln_b'),
                        ('glab', 'gla_b'), ('gnorm', 'gla_norm')):
            pv[:, PV[nm]:PV[nm] + 2] = _col(inp[key][l], 2)
        out[f"pvec{l}"] = pv
        out[f"mub{l}"] = np.ascontiguousarray(np.broadcast_to(np.asarray(inp['rwkv_mu'][l], f32)[None, :], (128, 1024)))
        sm = np.zeros((128, 5 * 256), f32)
        pw_ = np.asarray(inp['pool_w'][l], f32)
        for ck in range(2):
            sm[0:64, ck * 128:ck * 128 + 64] = pw_[2 * ck]
            sm[64:128, ck * 128 + 64:ck * 128 + 128] = pw_[2 * ck + 1]
        sm[0:64, 256:512] = np.asarray(inp['rwkv_w2'][l], f32)
        sm[64:128, 768:1024] = np.asarray(inp['rwkv_a2'][l], f32)
        sm[:, 512:768] = np.asarray(inp['rwkv_g2'][l], f32)
        sm[0:16, 1024:1280] = np.asarray(inp['gla_w2'][l], f32)
        out[f"smat{l}"] = sm
    out["cst"] = make_consts()
    return out


def prep_x(xc):
    NS, T, _ = xc.shape
    return np.ascontiguousarray(xc.reshape(NS, T, KD, 128).transpose(0, 3, 2, 1))


def unprep_out(o):
    NS, _, _, T = o.shape
    return np.ascontiguousarray(o.transpose(0, 3, 2, 1)).reshape(NS, T, D)


def kernel(**inputs):
    x = np.asarray(inputs['x'], np.float32)
    B, T, _ = x.shape
    NCORES = 8
    NS = B // NCORES
    L = 2
    nc, pg = build_program(T, NS, L)
    wts = prep_weights(inputs, L)
    in_maps = []
    for c in range(NCORES):
        m = dict(wts)
        m["xT"] = prep_x(x[c * NS:(c + 1) * NS])
        in_maps.append(m)
    res = run_bass_kernel_spmd(nc, in_maps, core_ids=list(range(NCORES)))
    outs = [unprep_out(np.asarray(r["outT"])) for r in res.results]
    return np.concatenate(outs, axis=0).astype(np.float32)
```

```python
import numpy as np
from contextlib import ExitStack
import concourse.bass as bass
import concourse.mybir as mybir
from concourse.bass_utils import run_bass_kernel_spmd

F32 = mybir.dt.float32
BF16 = mybir.dt.bfloat16
AF = mybir.ActivationFunctionType
ALU = mybir.AluOpType
ENGS = ['pe', 'act', 'dve', 'pool', 'sp']

D = 1024
KD = 8
FF = 2816
NJ = 22
G = 256
DIN = 3344
NORM_EPS = 1e-6
GN_EPS = 64e-5
QK = 0.125
TB = 512
TBM = 256
CH = 64
NCH = TBM // CH
DEBUG_STAGE = 99

FMG = [(0, 128, 0), (128, 128, 0), (256, 128, 0), (384, 128, 0), (512, 128, 0), (640, 128, 0),
       (1024, 128, 0), (1152, 128, 0),
       (1280, 128, 1), (1408, 128, 1), (1536, 128, 1), (1664, 128, 1), (1792, 128, 1), (1920, 128, 1),
       (2048, 128, 1), (2176, 128, 1),
       (2304, 128, 0), (2432, 128, 0), (2560, 128, 0), (2688, 128, 0), (3072, 128, 0), (3200, 128, 0),
       (3328, 128, 0)]
TMG = [(768, 256, 0), (1792, 256, 1), (2816, 256, 0)]
RW0 = 1280

PV = {}
_o = 0
for _n, _w in [('nf1', 8), ('nmx', 8), ('nf2', 8), ('pool_b', 2), ('pool_s', 2), ('lb0', 2), ('lbl', 2),
               ('hnorm', 2), ('w0', 2), ('a0', 2), ('kk', 2), ('ka', 2), ('rk', 2), ('lnw', 2), ('lnb', 2),
               ('glab', 2), ('gnorm', 2), ('nfin', 8)]:
    PV[_n] = _o
    _o += _w
NPV = _o


class Buf:
    __slots__ = ('name', 'w', 'r', 'const')

    def __init__(self, name, const=False):
        self.name = name
        self.w = None
        self.r = []
        self.const = const


class DSem:
    def __init__(self, h):
        self.h = h
        self.count = 0


class Prog:
    def __init__(self, nc, same_engine_sync=True):
        self.nc = nc
        self.ops = {e: [] for e in ENGS}
        self.cnt = {e: 0 for e in ENGS}
        self.same = same_engine_sync
        self.nops = 0
        self.last_rg = None
        self.last_pe_sig = True

    def op(self, eng, fn, reads=(), writes=(), sig=True, dsem=None, rg=None):
        waits = {}
        if eng == 'pe':
            if rg is not None and self.last_rg is not None and rg != self.last_rg:
                assert self.last_pe_sig
                waits['pe'] = self.cnt['pe']
            self.last_rg = rg
            self.last_pe_sig = sig

        def addw(tok):
            if tok is None:
                return
            k, v = tok
            if k == eng and (eng == 'pe' or not self.same):
                return
            if waits.get(k, 0) < v:
                waits[k] = v
        for b in reads:
            addw(b.w)
        for b in writes:
            addw(b.w)
            for t in b.r:
                addw(t)
        if dsem is not None:
            dsem.count += 16
            tok = (dsem, dsem.count)
            sig = False
        elif sig:
            self.cnt[eng] += 1
            tok = (eng, self.cnt[eng])
        else:
            tok = (eng, self.cnt[eng] + 1)
        for b in reads:
            if not b.const:
                b.r.append(tok)
                if len(b.r) > 48:
                    mx = {}
                    for k, v in b.r:
                        if mx.get(k, 0) < v:
                            mx[k] = v
                    b.r = list(mx.items())
        for b in writes:
            b.w = tok
            b.r = []
        self.ops[eng].append((fn, waits, sig, dsem))
        self.nops += 1
        return tok

    def emit(self, block, esems):
        nc = self.nc
        deco = {'pe': block.tensor, 'act': block.scalar, 'dve': block.vector,
                'pool': block.gpsimd, 'sp': block.sync}
        for e in ENGS:
            ops = self.ops[e]

            def body(eng, ops=ops, e=e):
                known = {}
                for fn, waits, sig, dsem in ops:
                    for k, v in waits.items():
                        if known.get(k, 0) >= v:
                            continue
                        known[k] = v
                        h = k.h if isinstance(k, DSem) else esems[k]
                        eng.wait_ge(h, v)
                    inst = fn(eng)
                    if dsem is not None:
                        inst.then_inc(dsem.h, 16)
                    elif sig:
                        inst.then_inc(esems[e], 1)
            deco[e](body)


def build_program(T, NS, L, mixers=('pool', 'hgrn', 'rwkv', 'gla'), do_ffn=True, do_mix=True):
    NT = T // TB
    nc = bass.Bass("TRN2", target_bir_lowering=False)
    dr = {}

    def din(name, shape):
        dr[name] = nc.dram_tensor(name, list(shape), F32, kind="ExternalInput").ap()
        return dr[name]
    xT = din("xT", [NS, 128, KD, T])
    outT = nc.dram_tensor("outT", [NS, 128, KD, T], F32, kind="ExternalOutput").ap()
    f_win = [[din(f"f{w}_win{l}", [NJ, 128, KD * 256]) for w in (1, 2)] for l in range(L)]
    f_wout = [[din(f"f{w}_wout{l}", [NJ, 128, D]) for w in (1, 2)] for l in range(L)]
    m_fm = [din(f"m_fm{l}", [len(FMG), 128, KD * 128]) for l in range(L)]
    m_tm = [din(f"m_tm{l}", [len(TMG), 128, KD * 256]) for l in range(L)]
    m_wout = [din(f"m_wout{l}", [KD, 128, D]) for l in range(L)]
    pvec_d = [din(f"pvec{l}", [128, NPV]) for l in range(L)]
    mub_d = [din(f"mub{l}", [128, 1024]) for l in range(L)]
    smat_d = [din(f"smat{l}", [128, 5 * 256]) for l in range(L)]
    cst_d = din("cst", [128, 1600])
    es = ExitStack()
    with es:
        def sb(name, shape, dt=F32):
            return es.enter_context(nc.sbuf_tensor("sb_" + name, list(shape), dt))

        def psum(name, shape, dt=F32):
            return es.enter_context(nc.psum_tensor("pp_" + name, list(shape), dt))
        esems = {e: es.enter_context(nc.semaphore("s_" + e)) for e in ENGS}

        def dsem(name):
            return DSem(es.enter_context(nc.semaphore(name)))
        pg = Prog(nc)
        block = es.enter_context(nc.Block())

        X = sb("X", [128, KD, T])
        XB = [[Buf(f"X{k}_{t}") for t in range(NT)] for k in range(KD)]
        xn = sb("xn", [128, KD, TB], BF16)
        xns = sb("xns", [128, KD, TBM], BF16)
        xnsB = Buf("xns")
        xnB = [Buf(f"xn{k}") for k in range(KD)]
        sq = [sb(f"sq{i}", [128, TB], BF16) for i in range(2)]
        sqB = [Buf(f"sq{i}") for i in range(2)]
        rstd = sb("rstd", [128, TB]); rstdB = Buf("rstd")
        ones = sb("ones", [128, 128], BF16); onesB = Buf("ones", const=True)
        bones = sb("bones", [128, 128], BF16)
        ident = sb("ident", [128, 128], BF16)
        cst = sb("cst", [128, 1600])
        cstB = Buf("cst", const=True)
        MI, MSU, MSL, ID4 = 0, 256, 512, 768
        SCM = 1024
        ICN = 1536
        pvec = [sb(f"pvec{l}", [128, NPV]) for l in range(L)]
        pvB = [Buf(f"pvec{l}", const=True) for l in range(L)]
        lbt = sb("lbt", [128, 8])
        mub = sb("mub", [128, 1024]); mubB = Buf("mub")
        smat = sb("smat", [128, 5 * 256]); smatB = Buf("smat")
        smatb = sb("smatb", [128, 2 * 128], BF16)
        wst = [sb(f"wst{i}", [128, KD * 256]) for i in range(2)]
        wstB = [Buf(f"wst{i}") for i in range(2)]
        wstS = [dsem(f"dwst{i}") for i in range(2)]
        wbf = [sb(f"wbf{i}", [128, KD * 256], BF16) for i in range(2)]
        wbfB = [Buf(f"wbf{i}") for i in range(2)]
        wbfB2 = [[Buf(f"wbf{i}a"), Buf(f"wbf{i}b")] for i in range(2)]
        wtmp = sb("wtmp", [128, KD * 256]); wtmpB = Buf("wtmp")
        ost = [sb("ost0", [128, D])] * 2
        ostB = [Buf("ost0")] * 2
        ostS = [dsem("dost0")] * 2
        obf = [sb(f"obf{i}", [128, D], BF16) for i in range(2)]
        obfB = [Buf(f"obf{i}") for i in range(2)]
        hT = sb("hT", [128, NJ, TB], BF16)
        hB = [Buf(f"h{j}") for j in range(NJ)]
        sg = [sb("sg0", [128, TB])] * 2
        sgB = [Buf("sg0")] * 2
        PS = [psum(f"ps{i}", [128, 512]) for i in range(8)]
        PSB = [Buf(f"ps{i}") for i in range(8)]
        xS = [dsem(f"dx{k}") for k in range(KD)]
        oS = [dsem(f"dout{k}") for k in range(KD)]
        cS = dsem("dcst")
        pS = [dsem(f"dpv{l}") for l in range(L)]
        muS = dsem("dmu")
        smS = dsem("dsm")
        counters = {'w': 0, 'o': 0, 'sq': 0, 'sg': 0, 'pp': 0}

        def OP(eng, fn, reads=(), writes=(), sig=True, dsem=None, rg=None):
            return pg.op(eng, fn, reads, writes, sig, dsem, rg)

        def load_w(src_ap, ncols):
            i = counters['w'] % 2
            counters['w'] += 1
            OP('sp', lambda e: e.dma_start(out=wst[i][:, 0:ncols], in_=src_ap), writes=[wstB[i]], dsem=wstS[i])
            return i

        def cast_w(i, ncols, eng='pool'):
            if eng == 'act':
                OP('act', lambda e: e.activation(out=wbf[i][:, 0:ncols], in_=wst[i][:, 0:ncols], func=AF.Copy),
                   reads=[wstB[i]], writes=[wbfB[i], wbfB2[i][0], wbfB2[i][1]])
            else:
                OP(eng, lambda e: e.tensor_copy(out=wbf[i][:, 0:ncols], in_=wst[i][:, 0:ncols]),
                   reads=[wstB[i]], writes=[wbfB[i], wbfB2[i][0], wbfB2[i][1]])

        def cast_w_split(i, ncols):
            c1 = (ncols * 3 // 4) // 128 * 128
            OP('dve', lambda e: e.tensor_copy(out=wbf[i][:, 0:c1], in_=wst[i][:, 0:c1]), reads=[wstB[i]], writes=[wbfB2[i][0], wbfB[i]])
            OP('pool', lambda e: e.tensor_copy(out=wbf[i][:, c1:ncols], in_=wst[i][:, c1:ncols]), reads=[wstB[i]], writes=[wbfB2[i][1]])

        def load_o(src_ap, eng='act'):
            i = counters['o'] % 2
            counters['o'] += 1
            OP('sp', lambda e: e.dma_start(out=ost[i][:], in_=src_ap), writes=[ostB[i]], dsem=ostS[i])
            if eng == 'act':
                OP('act', lambda e: e.activation(out=obf[i][:], in_=ost[i][:], func=AF.Copy),
                   reads=[ostB[i]], writes=[obfB[i]])
            else:
                OP(eng, lambda e: e.tensor_copy(out=obf[i][:], in_=ost[i][:]), reads=[ostB[i]], writes=[obfB[i]])
            return i

        def rstd_from(psb, psbuf, n, scale, eps, dst, dstB):
            OP('act', lambda e: e.activation(out=dst[:, 0:n], in_=psb[:, 0:n], func=AF.Ln, scale=scale, bias=eps),
               reads=[psbuf], writes=[dstB])
            OP('act', lambda e: e.activation(out=dst[:, 0:n], in_=dst[:, 0:n], func=AF.Exp, scale=-0.5),
               reads=[dstB], writes=[dstB])

        def rmsnorm_to_xn(l, gcol, c0, n, bank):
            ti = c0 // TB
            for k in range(KD):
                i = counters['sq'] % 2
                counters['sq'] += 1
                OP('act', lambda e, k=k, i=i: e.activation(out=sq[i][:, 0:n], in_=X[:, k, c0:c0 + n], func=AF.Square),
                   reads=[XB[k][ti]], writes=[sqB[i]])
                OP('pe', lambda e, k=k, i=i: e.matmul(PS[bank][:, 0:n], lhsT=ones[:], rhs=sq[i][:, 0:n], start=(k == 0), stop=(k == KD - 1)),
                   reads=[sqB[i], onesB], writes=[PSB[bank]])
            rstd_from(PS[bank], PSB[bank], n, 1.0 / D, NORM_EPS, rstd, rstdB)
            for k in range(KD):
                OP('dve', lambda e, k=k: e.scalar_tensor_tensor(out=xn[:, k, 0:n], in0=X[:, k, c0:c0 + n],
                                                                 scalar=pvec[l][:, gcol + k:gcol + k + 1], in1=rstd[:, 0:n],
                                                                 op0=ALU.mult, op1=ALU.mult),
                   reads=[XB[k][ti], rstdB, pvB[l]], writes=[xnB[k]])

        def ffn(l, w, ti):
            gcol = PV['nf1'] if w == 0 else PV['nf2']
            c0 = ti * TB
            blocks = [('in', j) for j in range(NJ)] + [('out', j) for j in range(NJ)]

            slots = {}

            def do_load(t):
                if t < len(blocks) and blocks[t][0] == 'in' and t not in slots:
                    slots[t] = load_w(f_win[l][w][blocks[t][1]], KD * 256)

            def do_ready(t):
                if t >= len(blocks):
                    return
                if blocks[t][0] == 'in':
                    do_load(t)
                    cast_w_split(slots[t], KD * 256)
                else:
                    slots[t] = load_o(f_wout[l][w][blocks[t][1]], 'act')
            do_load(0)
            do_load(1)
            do_ready(0)
            rmsnorm_to_xn(l, gcol, c0, TB, 0)
            for t, (kind, j) in enumerate(blocks):
                i = slots[t]
                do_ready(t + 1)
                do_load(t + 2)
                if kind == 'in':
                    wv = wbf[i][:].rearrange("p (k c) -> p k c", k=KD)
                    pp = counters['pp'] % 2
                    counters['pp'] += 1
                    bg, bu = 2 * pp, 2 * pp + 1
                    for half, bk in ((0, bg), (1, bu)):
                        for k in range(KD):
                            OP('pe', lambda e, k=k, half=half, bk=bk, wv=wv: e.matmul(
                                PS[bk][:, :], lhsT=wv[:, k, half * 128:(half + 1) * 128], rhs=xn[:, k, 0:TB],
                                start=(k == 0), stop=(k == KD - 1)),
                               reads=[wbfB[i], wbfB2[i][0], wbfB2[i][1], xnB[k]], writes=[PSB[bk]], sig=(k == KD - 1))
                    si = counters['sg'] % 2
                    counters['sg'] += 1
                    OP('act', lambda e, si=si, bg=bg: e.activation(out=sg[si][:], in_=PS[bg][:, :], func=AF.Silu),
                       reads=[PSB[bg]], writes=[sgB[si]])
                    OP('dve', lambda e, si=si, bu=bu, j=j: e.tensor_tensor(out=hT[:, j, :], in0=sg[si][:], in1=PS[bu][:, :], op=ALU.mult),
                       reads=[sgB[si], PSB[bu]], writes=[hB[j]])
                else:
                    for m in range(KD):
                        OP('pe', lambda e, m=m, j=j, i=i: e.matmul(PS[m][:, :], lhsT=obf[i][:, m * 128:(m + 1) * 128], rhs=hT[:, j, :],
                                                               start=(j == 0), stop=(j == NJ - 1)),
                           reads=[obfB[i], hB[j]], writes=[PSB[m]], sig=(m == KD - 1 or j == NJ - 1))
            for m in range(KD):
                OP('dve', lambda e, m=m: e.scalar_tensor_tensor(out=X[:, m, c0:c0 + TB], in0=PS[m][:, :], scalar=0.5,
                                                                 in1=X[:, m, c0:c0 + TB], op0=ALU.mult, op1=ALU.add),
                   reads=[PSB[m], XB[m][ti]], writes=[XB[m][ti]])

        OP('sp', lambda e: e.dma_start(out=cst[:], in_=cst_d), writes=[cstB], dsem=cS)
        for l in range(L):
            OP('sp', lambda e, l=l: e.dma_start(out=pvec[l][:], in_=pvec_d[l]), writes=[pvB[l]], dsem=pS[l])
        OP('pool', lambda e: e.memset(ones[:], 1.0), writes=[onesB])
        bonesB = Buf("bones", const=True)
        OP('pool', lambda e: e.memset(bones[:], 0.0), writes=[bonesB])
        OP('pool', lambda e: e.memset(bones[0:64, 0:64], 1.0), writes=[bonesB])
        OP('pool', lambda e: e.memset(bones[64:128, 64:128], 1.0), writes=[bonesB])
        identB = Buf("ident", const=True)
        OP('pool', lambda e: e.memset(ident[:], 1.0), writes=[identB])
        OP('pool', lambda e: e.affine_select(out=ident[:], in_=ident[:], pattern=[[-1, 128]], compare_op=ALU.is_equal,
                                             fill=0.0, base=0, channel_multiplier=1), reads=[identB], writes=[identB])

        if do_mix:
            YT = sb("YT", [128, KD, TBM], BF16)
            YB = [Buf(f"Y{k}") for k in range(KD)]
            NFA = 10
            FA = [sb(f"fa{i}", [128, TBM]) for i in range(NFA)]
            FAB = [Buf(f"fa{i}") for i in range(NFA)]
            NBA = 4
            BA = [sb(f"ba{i}", [128, TBM], BF16) for i in range(NBA)]
            BAB = [Buf(f"ba{i}") for i in range(NBA)]
            LL = [[sb(f"ll{hp}{i}", [128, TBM]) for i in range(3)] for hp in range(2)]
            LLB_ = [[Buf(f"ll{hp}{i}") for i in range(3)] for hp in range(2)]
            LLX = [sb(f"llx{i}", [128, TBM]) for i in range(2)]
            LLXB = [Buf(f"llx{i}") for i in range(2)]
            LB = [[sb(f"lb{hp}{i}", [128, TBM], BF16) for i in range(5)] for hp in range(2)]
            LBB = [[Buf(f"lb{hp}{i}") for i in range(5)] for hp in range(2)]
            PB16 = [sb(f"pb16{i}", [128, TBM], BF16) for i in range(2)]
            PB16B = [Buf(f"pb16{i}") for i in range(2)]
            Vt = [sb(f"vt{c}", [64, 256], BF16) for c in range(NCH)]
            VtB = [Buf(f"vt{c}") for c in range(NCH)]
            KEt = [sb(f"ket{c}", [64, 256], BF16) for c in range(NCH)]
            KEtB = [Buf(f"ket{c}") for c in range(NCH)]
            AEt = [sb(f"aet{c}", [64, 256], BF16) for c in range(NCH)]
            AEtB = [Buf(f"aet{c}") for c in range(NCH)]
            alias_ctr = [0]

            def mk(name, n):
                ts, bs = [], []
                for c in range(n):
                    idx = alias_ctr[0]
                    alias_ctr[0] += 1
                    j, half = idx // 2, idx % 2
                    ts.append(hT[0:64, j, half * 256:(half + 1) * 256])
                    bs.append(hB[j])
                return ts, bs
            ATs, ATsB = mk("ats", NCH)
            LKs, LKsB = mk("lks", NCH)
            ARs, ARsB = mk("ars", NCH)
            Nn, NnB = mk("nn", NCH)
            NTn, NTnB = mk("ntn", NCH)
            Nn2, Nn2B = mk("nn2", NCH)
            NTn2, NTn2B = mk("ntn2", NCH)
            Pn, PnB = mk("pn", NCH)
            Pn2, Pn2B = mk("pn2", NCH)
            Ysb = sb("ysb", [64, 256], BF16); YsbB = Buf("ysb")
            Usb = sb("usb", [64, 256], BF16); UsbB = Buf("usb")
            S32 = {m: [sb(f"s32{m}{hp}", [128, 64]) for hp in range(2)] for m in ('hgrn', 'gla', 'rwkv')}
            Sbf = {m: [sb(f"sbf{m}{hp}", [128, 64], BF16) for hp in range(2)] for m in ('hgrn', 'gla', 'rwkv')}
            S32B = {m: [Buf(f"s32{m}{hp}") for hp in range(2)] for m in ('hgrn', 'gla', 'rwkv')}
            SbfB = {m: [Buf(f"sbf{m}{hp}") for hp in range(2)] for m in ('hgrn', 'gla', 'rwkv')}
            stmp = [sb(f"stmp{hp}", [128, 64]) for hp in range(2)]
            stmpB = [Buf(f"stmp{hp}") for hp in range(2)]
            pext = sb("pext", [128, 2, 16 + TBM]); pextB = Buf("pext")
            pw = [sb(f"pw{i}", [128, 2, 16 + TBM]) for i in range(2)]
            pwB = [Buf(f"pw{i}") for i in range(2)]
            PST = PS[7][:, :].bitcast(BF16)
            lbtB = Buf("lbt", const=True)
            fa_ctr = [0]
            ba_ctr = [0]

            def fa():
                i = fa_ctr[0] % NFA
                fa_ctr[0] += 1
                return FA[i], FAB[i]

            def ba():
                i = ba_ctr[0] % NBA
                ba_ctr[0] += 1
                return BA[i], BAB[i]

            def pbank():
                b = counters['pp'] % 4
                counters['pp'] += 1
                return b

            wplan = {'plan': [], 'pos': 0, 'issued': {}, 'l': 0}

            def issue_load(l, d):
                kind, g = d
                if kind == 'fm':
                    return load_w(m_fm[l][g], KD * 128)
                return load_w(m_tm[l][g], KD * 256)

            def issue_cast(l, d, i):
                kind, g = d
                if kind == 'fm':
                    col0, ncols, shift = FMG[g]
                    if shift:
                        mc = col0 - RW0
                        mu_b = mub[:, mc:mc + 128].unsqueeze(1).broadcast_to([128, KD, 128])
                        wv32 = wst[i][:, 0:KD * 128].rearrange("p (k c) -> p k c", k=KD)
                        wt = wtmp[:, 0:KD * 128].rearrange("p (k c) -> p k c", k=KD)
                        wbv = wbf[i][:].rearrange("p (k c) -> p k c", k=KD)
                        OP('pool', lambda e: e.tensor_tensor(out=wt, in0=wv32, in1=mu_b, op=ALU.mult),
                           reads=[wstB[i], mubB], writes=[wtmpB])
                        OP('pool', lambda e: e.tensor_tensor(out=wbv[:, :, 0:128], in0=wv32, in1=wt, op=ALU.subtract),
                           reads=[wstB[i], wtmpB], writes=[wbfB[i], wbfB2[i][0], wbfB2[i][1]])
                        OP('pool', lambda e: e.tensor_copy(out=wbv[:, :, 128:256], in_=wt), reads=[wtmpB], writes=[wbfB[i], wbfB2[i][0], wbfB2[i][1]])
                    else:
                        cast_w(i, KD * 128, 'act')
                        wbv = wbf[i][:, 0:KD * 128].rearrange("p (k c) -> p k c", k=KD)
                    return (i, wbv)
                col0, ncols, shift = TMG[g]
                i2 = None
                wb2 = None
                if shift:
                    i2 = counters['w'] % 2
                    counters['w'] += 1
                    mc = col0 - RW0
                    mu_b = mub[:, mc:mc + 256].unsqueeze(1).broadcast_to([128, KD, 256])
                    wv32 = wst[i][:].rearrange("p (k c) -> p k c", k=KD)
                    wt = wtmp[:].rearrange("p (k c) -> p k c", k=KD)
                    wa = wbf[i][:].rearrange("p (k c) -> p k c", k=KD)
                    wb2 = wbf[i2][:].rearrange("p (k c) -> p k c", k=KD)
                    OP('pool', lambda e: e.tensor_tensor(out=wt, in0=wv32, in1=mu_b, op=ALU.mult),
                       reads=[wstB[i], mubB], writes=[wtmpB])
                    OP('pool', lambda e: e.tensor_tensor(out=wa, in0=wv32, in1=wt, op=ALU.subtract),
                       reads=[wstB[i], wtmpB], writes=[wbfB[i], wbfB2[i][0], wbfB2[i][1]])
                    OP('pool', lambda e: e.tensor_copy(out=wb2, in_=wt), reads=[wtmpB], writes=[wbfB[i2], wbfB2[i2][0], wbfB2[i2][1]])
                else:
                    cast_w_split(i, KD * 256)
                    wa = wbf[i][:].rearrange("p (k c) -> p k c", k=KD)
                return (i, i2, wa, wb2)

            def two_slot(d):
                return d[0] == 'tm' and bool(TMG[d[1]][2])

            def ensure_load(l, pos):
                plan = wplan['plan']
                if pos < len(plan) and pos not in wplan['loaded'] and not two_slot(plan[pos]):
                    wplan['loaded'][pos] = issue_load(l, plan[pos])

            def ensure_cast(l, pos):
                plan = wplan['plan']
                if pos < len(plan) and pos not in wplan['issued'] and not two_slot(plan[pos]):
                    ensure_load(l, pos)
                    wplan['issued'][pos] = issue_cast(l, plan[pos], wplan['loaded'].pop(pos))

            def acq(l, d):
                pos = wplan['pos']
                plan = wplan['plan']
                assert plan[pos] == d, (plan[pos], d)
                if pos not in wplan['issued']:
                    if pos not in wplan['loaded']:
                        wplan['loaded'][pos] = issue_load(l, d)
                    wplan['issued'][pos] = issue_cast(l, d, wplan['loaded'].pop(pos))
                info = wplan['issued'].pop(pos)
                wplan['pos'] = pos + 1
                if not (pos + 1 < len(plan) and two_slot(plan[pos + 1])):
                    ensure_cast(l, pos + 1)
                    if not (pos + 2 < len(plan) and two_slot(plan[pos + 2])):
                        ensure_load(l, pos + 2)
                return info

            def proj_fm(l, g, first_block):
                col0, ncols, shift = FMG[g]
                i, wbv = acq(l, ('fm', g))
                bk = pbank()
                nmm = KD * (2 if shift else 1)
                n = 0
                for k in range(KD):
                    n += 1
                    OP('pe', lambda e, k=k, n=n: e.matmul(PS[bk][0:ncols, 0:TBM], lhsT=wbv[:, k, 0:ncols], rhs=xn[:, k, 0:TBM],
                                                          start=(n == 1), stop=(n == nmm)),
                       reads=[wbfB[i], wbfB2[i][0], wbfB2[i][1], xnB[k]], writes=[PSB[bk]], sig=(n == nmm))
                if shift:
                    for k in range(KD):
                        n += 1
                        OP('pe', lambda e, k=k, n=n: e.matmul(PS[bk][0:ncols, 0:TBM], lhsT=wbv[:, k, 128:128 + ncols], rhs=xns[:, k, 0:TBM],
                                                              start=False, stop=(n == nmm)),
                           reads=[wbfB[i], wbfB2[i][0], wbfB2[i][1], xnsB], writes=[PSB[bk]], sig=(n == nmm))
                return bk

            def proj_tm(l, g):
                col0, ncols, shift = TMG[g]
                i, i2, wa, wb2 = acq(l, ('tm', g))
                for c in range(NCH):
                    bk = pbank()
                    nmm = KD * (2 if shift else 1)
                    n = 0
                    for k in range(KD):
                        n += 1
                        OP('pe', lambda e, k=k, n=n, c=c, bk=bk: e.matmul(PS[bk][0:64, 0:256], lhsT=xn[:, k, c * CH:(c + 1) * CH],
                                                                     rhs=wa[:, k, :], start=(n == 1), stop=(n == nmm)),
                           reads=[wbfB[i], wbfB2[i][0], wbfB2[i][1], xnB[k]], writes=[PSB[bk]], sig=(n == nmm))
                    if shift:
                        for k in range(KD):
                            n += 1
                            OP('pe', lambda e, k=k, n=n, c=c, bk=bk: e.matmul(PS[bk][0:64, 0:256], lhsT=xns[:, k, c * CH:(c + 1) * CH],
                                                                         rhs=wb2[:, k, :], start=False, stop=(n == nmm)),
                               reads=[wbfB[i2], wbfB2[i2][0], wbfB2[i2][1], xnsB], writes=[PSB[bk]], sig=(n == nmm))
                    OP('act', lambda e, c=c, bk=bk: e.activation(out=Vt[c][:], in_=PS[bk][0:64, 0:256], func=AF.Copy),
                       reads=[PSB[bk]], writes=[VtB[c]])

            def scan_decay(g_t, g_b):
                b_t, b_b = fa()
                OP('dve', lambda e: e.tensor_tensor_scan(out=b_t[:], data0=cst[:, SCM:SCM + TBM], data1=g_t[:], initial=0.0,
                                                         op0=ALU.mult, op1=ALU.add), reads=[g_b, cstB], writes=[b_b])
                return b_t, b_b

            def chunk_engine(mname, QE, KE, PC, rw=None):
                isrw = rw is not None
                if DEBUG_STAGE < 1:
                    return
                for c in range(NCH):
                    for (src, dst, dstB_) in ([(KE, KEt, KEtB)] + ([(rw['AE'], AEt, AEtB)] if isrw else [])):
                        for hp in range(2):
                            OP('pe', lambda e, hp=hp, c=c, src=src: e.transpose(PST[0:64, hp * 128:(hp + 1) * 128],
                                                                             src[hp][0][:, c * CH:(c + 1) * CH], ident[:]),
                               reads=[src[hp][1], identB], writes=[PSB[7]])
                        OP('act', lambda e, c=c, dst=dst: e.activation(out=dst[c][:], in_=PST[0:64, 0:256], func=AF.Copy),
                           reads=[PSB[7]], writes=[dstB_[c]])
                if DEBUG_STAGE < 2:
                    return
                for c in range(NCH):
                    cs = slice(c * CH, (c + 1) * CH)
                    def sc(lh, rh, dst_ps, cs=cs):
                        for h in range(4):
                            hp, r = h // 2, (h % 2) * 64
                            OP('pe', lambda e, h=h, hp=hp, r=r: e.matmul(dst_ps[0:64, h * 64:(h + 1) * 64], lhsT=lh[hp][0][r:r + 64, cs],
                                                                         rhs=rh[hp][0][r:r + 64, cs], start=True, stop=True),
                               reads=[lh[hp][1], rh[hp][1]], writes=[PSB[6]], rg=r)
                    sc(KE, QE, PS[6][:, 0:256])
                    OP('dve', lambda e, c=c: e.tensor_tensor(out=ATs[c][:], in0=PS[6][0:64, 0:256], in1=cst[0:64, MI:MI + 256], op=ALU.mult),
                       reads=[PSB[6], cstB], writes=[ATsB[c]])
                    if isrw:
                        sc(KE, rw['BE'], PS[6][:, 256:512])
                        OP('dve', lambda e, c=c: e.tensor_tensor(out=LKs[c][:], in0=PS[6][0:64, 256:512], in1=cst[0:64, MSU:MSU + 256], op=ALU.mult),
                           reads=[PSB[6], cstB], writes=[LKsB[c]])
                        sc(rw['AE'], QE, PS[6][:, 0:256])
                        OP('dve', lambda e, c=c: e.tensor_tensor(out=ARs[c][:], in0=PS[6][0:64, 0:256], in1=cst[0:64, MI:MI + 256], op=ALU.mult),
                           reads=[PSB[6], cstB], writes=[ARsB[c]])
                        sc(rw['AE'], rw['BE'], PS[6][:, 256:512])
                        OP('dve', lambda e, c=c: e.scalar_tensor_tensor(out=NTn[c][:], in0=PS[6][0:64, 256:512], scalar=-1.0,
                                                                         in1=cst[0:64, MSU:MSU + 256], op0=ALU.mult, op1=ALU.mult),
                           reads=[PSB[6], cstB], writes=[NTnB[c]])
                        sc(rw['BE'], rw['AE'], PS[6][:, 0:256])
                        OP('dve', lambda e, c=c: e.scalar_tensor_tensor(out=Nn[c][:], in0=PS[6][0:64, 0:256], scalar=-1.0,
                                                                         in1=cst[0:64, MSL:MSL + 256], op0=ALU.mult, op1=ALU.mult),
                           reads=[PSB[6], cstB], writes=[NnB[c]])
                        OP('pool', lambda e, c=c: e.tensor_tensor(out=Pn[c][:], in0=NTn[c][:], in1=cst[0:64, ID4:ID4 + 256], op=ALU.add),
                           reads=[NTnB[c], cstB], writes=[PnB[c]])
                if isrw:
                    curN, curNB, curNT, curNTB = Nn, NnB, NTn, NTnB
                    nxtN, nxtNB, nxtNT, nxtNTB = Nn2, Nn2B, NTn2, NTn2B
                    curP, curPB, nxtP, nxtPB = Pn, PnB, Pn2, Pn2B
                    for lev in range(1, 6):
                        for c in range(NCH):
                            bk = pbank()
                            for h in range(4):
                                hs = slice(h * 64, (h + 1) * 64)
                                OP('pe', lambda e, c=c, hs=hs, bk=bk, a=curNT, b=curN: e.matmul(PS[bk][0:64, hs], lhsT=a[c][:, hs], rhs=b[c][:, hs],
                                                                                         start=True, stop=True),
                                   reads=[curNTB[c], curNB[c]], writes=[PSB[bk]], rg=0)
                            if lev < 5:
                                for h in range(4):
                                    hs = slice(h * 64, (h + 1) * 64)
                                    hs2 = slice(256 + h * 64, 256 + (h + 1) * 64)
                                    OP('pe', lambda e, c=c, hs=hs, hs2=hs2, bk=bk, a=curN, b=curNT: e.matmul(PS[bk][0:64, hs2], lhsT=a[c][:, hs], rhs=b[c][:, hs],
                                                                                                     start=True, stop=True),
                                       reads=[curNTB[c], curNB[c]], writes=[PSB[bk]], rg=0)
                                OP('act', lambda e, c=c, bk=bk, d=nxtNT: e.activation(out=d[c][:], in_=PS[bk][0:64, 256:512], func=AF.Copy),
                                   reads=[PSB[bk]], writes=[nxtNTB[c]])
                            OP('act', lambda e, c=c, bk=bk, d=nxtN: e.activation(out=d[c][:], in_=PS[bk][0:64, 0:256], func=AF.Copy),
                               reads=[PSB[bk]], writes=[nxtNB[c]])
                        curN, curNB, nxtN, nxtNB = nxtN, nxtNB, curN, curNB
                        curNT, curNTB, nxtNT, nxtNTB = nxtNT, nxtNTB, curNT, curNTB
                        for c in range(NCH):
                            bk = pbank()
                            for h in range(4):
                                hs = slice(h * 64, (h + 1) * 64)
                                OP('pe', lambda e, c=c, hs=hs, bk=bk, a=curN, b=curP: e.matmul(PS[bk][0:64, hs], lhsT=a[c][:, hs], rhs=b[c][:, hs],
                                                                                        start=True, stop=True),
                                   reads=[curNB[c], curPB[c]], writes=[PSB[bk]], rg=0)
                            OP('dve', lambda e, c=c, bk=bk, s=curP, d=nxtP: e.tensor_tensor(out=d[c][:], in0=PS[bk][0:64, 0:256], in1=s[c][:], op=ALU.add),
                               reads=[PSB[bk], curPB[c]], writes=[nxtPB[c]])
                        curP, curPB, nxtP, nxtPB = nxtP, nxtPB, curP, curPB
                    TT, TTB = curP, curPB
                if DEBUG_STAGE < 3:
                    return
                S3, Sb, S3B, SbB = S32[mname], Sbf[mname], S32B[mname], SbfB[mname]
                for c in range(NCH):
                    cs = slice(c * CH, (c + 1) * CH)
                    if isrw:
                        for h in range(4):
                            hp, r = h // 2, (h % 2) * 64
                            hs = slice(h * 64, (h + 1) * 64)
                            OP('pe', lambda e, hp=hp, r=r, hs=hs, cs=cs: e.matmul(PS[6][0:64, hs], lhsT=rw['BE'][hp][0][r:r + 64, cs], rhs=Sb[hp][r:r + 64, :],
                                                                         start=True, stop=False),
                               reads=[rw['BE'][hp][1], SbB[hp]], writes=[PSB[6]], rg=r)
                            OP('pe', lambda e, hs=hs, c=c: e.matmul(PS[6][0:64, hs], lhsT=LKs[c][:, hs], rhs=Vt[c][:, hs], start=False, stop=True),
                               reads=[LKsB[c], VtB[c]], writes=[PSB[6]], rg=0)
                        OP('act', lambda e: e.activation(out=Ysb[:], in_=PS[6][0:64, 0:256], func=AF.Copy), reads=[PSB[6]], writes=[YsbB])
                        for h in range(4):
                            hs = slice(h * 64, (h + 1) * 64)
                            hs2 = slice(256 + h * 64, 256 + (h + 1) * 64)
                            OP('pe', lambda e, hs=hs, hs2=hs2, c=c: e.matmul(PS[6][0:64, hs2], lhsT=TT[c][:, hs], rhs=Ysb[:, hs], start=True, stop=True),
                               reads=[TTB[c], YsbB], writes=[PSB[6]], rg=0)
                        OP('act', lambda e: e.activation(out=Usb[:], in_=PS[6][0:64, 256:512], func=AF.Copy, scale=-1.0),
                           reads=[PSB[6]], writes=[UsbB])
                    for h in range(4):
                        hp, r = h // 2, (h % 2) * 64
                        hs = slice(h * 64, (h + 1) * 64)
                        ob = PS[4 + hp][r:r + 64, cs]
                        OP('pe', lambda e, ob=ob, hs=hs, c=c: e.matmul(ob, lhsT=Vt[c][:, hs], rhs=ATs[c][:, hs], start=True, stop=False),
                           reads=[VtB[c], ATsB[c]], writes=[PSB[4 + hp]], rg=0)
                        if isrw:
                            OP('pe', lambda e, ob=ob, hs=hs, c=c: e.matmul(ob, lhsT=Usb[:, hs], rhs=ARs[c][:, hs], start=False, stop=False),
                               reads=[UsbB, ARsB[c]], writes=[PSB[4 + hp]], rg=0)
                        OP('pe', lambda e, ob=ob, hp=hp, r=r, cs=cs: e.matmul(ob, lhsT=Sb[hp][r:r + 64, :], rhs=QE[hp][0][r:r + 64, cs], start=False, stop=True),
                           reads=[SbB[hp], QE[hp][1]], writes=[PSB[4 + hp]], rg=r)
                    for h in range(4):
                        hp, r = h // 2, (h % 2) * 64
                        hs = slice(h * 64, (h + 1) * 64)
                        sp_ = PS[7][r:r + 64, 256 + hp * 64:256 + (hp + 1) * 64]
                        OP('pe', lambda e, sp_=sp_, hs=hs, c=c: e.matmul(sp_, lhsT=KEt[c][:, hs], rhs=Vt[c][:, hs], start=True, stop=(not isrw)),
                           reads=[KEtB[c], VtB[c]], writes=[PSB[7]], rg=0)
                        if isrw:
                            OP('pe', lambda e, sp_=sp_, hs=hs, c=c: e.matmul(sp_, lhsT=AEt[c][:, hs], rhs=Usb[:, hs], start=False, stop=True),
                               reads=[AEtB[c], UsbB], writes=[PSB[7]], rg=0)
                    for hp in range(2):
                        pc = PC[hp][0][:, (c + 1) * CH - 1:(c + 1) * CH]
                        OP('dve', lambda e, hp=hp: e.tensor_tensor(out=stmp[hp][:], in0=PS[7][:, 256 + hp * 64:256 + (hp + 1) * 64], in1=S3[hp][:], op=ALU.add),
                           reads=[PSB[7], S3B[hp]], writes=[stmpB[hp]])
                        OP('dve', lambda e, hp=hp, pc=pc: e.tensor_scalar(out=S3[hp][:], in0=stmp[hp][:], scalar1=pc, scalar2=None, op0=ALU.mult),
                           reads=[stmpB[hp], PC[hp][1]], writes=[S3B[hp]])
                        OP('dve', lambda e, hp=hp, pc=pc: e.tensor_scalar(out=Sb[hp][:], in0=stmp[hp][:], scalar1=pc, scalar2=None, op0=ALU.mult),
                           reads=[stmpB[hp], PC[hp][1]], writes=[SbB[hp]])

            def evac_fm(bk, func=AF.Copy, scale=1.0, bias=None, dt='f', rows=128, dst=None):
                t, b = dst if dst is not None else (fa() if dt == 'f' else ba())
                kw = {}
                if bias is not None:
                    kw['bias'] = bias
                OP('act', lambda e: e.activation(out=t[0:rows, :], in_=PS[bk][0:rows, 0:TBM], func=func, scale=scale, **kw),
                   reads=[PSB[bk]] + ([pvB[0]] if bias is not None else []), writes=[b])
                return t, b

            def mixer_block(l, s, bi):
                first = (bi == 0)
                c0 = bi * TBM
                ti = c0 // TB
                plan = []
                if 'pool' in mixers:
                    plan += [('fm', 0), ('fm', 1)]
                if 'hgrn' in mixers:
                    for hp_ in range(2):
                        plan += [('fm', 2 + hp_), ('fm', 4 + hp_), ('fm', 6 + hp_)]
                    plan += [('tm', 0)]
                if 'gla' in mixers:
                    plan += [('fm', 22)]
                    for hp_ in range(2):
                        plan += [('fm', 16 + hp_), ('fm', 18 + hp_), ('fm', 20 + hp_)]
                    plan += [('tm', 2)]
                if 'rwkv' in mixers:
                    plan += [('fm', 14), ('fm', 15)]
                    for hp_ in range(2):
                        plan += [('fm', 8 + hp_), ('fm', 10 + hp_), ('fm', 12 + hp_)]
                    plan += [('tm', 1)]
                wplan['plan'] = plan
                wplan['pos'] = 0
                wplan['issued'] = {}
                wplan['loaded'] = {}
                if plan:
                    ensure_load(l, 0)
                    ensure_load(l, 1)
                    ensure_cast(l, 0)
                pv = pvec[l]
                if 'rwkv' in mixers:
                    if first:
                        OP('pool', lambda e: e.memset(xns[:, :, 0:2], 0.0), writes=[xnsB])
                    else:
                        OP('pool', lambda e: e.tensor_copy(out=xns[:, :, 0:1], in_=xn[:, :, TBM - 1:TBM]), reads=xnB, writes=[xnsB])
                rmsnorm_to_xn(l, PV['nmx'], c0, TBM, 0)
                if 'rwkv' in mixers:
                    OP('pool', lambda e: e.tensor_copy(out=xns[:, :, 1:TBM], in_=xn[:, :, 0:TBM - 1]), reads=xnB, writes=[xnsB])
                if 'pool' in mixers:
                    if first:
                        OP('pool', lambda e: e.memset(pext[:, :, 0:16], 0.0), writes=[pextB])
                    else:
                        OP('pool', lambda e: e.tensor_copy(out=pext[:, :, 0:16], in_=pext[:, :, TBM:TBM + 16]), reads=[pextB], writes=[pextB])
                    for ck in range(2):
                        bk = proj_fm(l, ck, first)
                        OP('act', lambda e, ck=ck, bk=bk: e.activation(out=pext[:, ck, 16:16 + TBM], in_=PS[bk][:, 0:TBM], func=AF.Copy),
                           reads=[PSB[bk]], writes=[pextB])
                    W_ = 16 + TBM
                    OP('dve', lambda e: e.tensor_tensor(out=pw[0][:, :, 1:W_], in0=pext[:, :, 1:W_], in1=pext[:, :, 0:W_ - 1], op=ALU.add),
                       reads=[pextB], writes=[pwB[0]])
                    def poolfin(src, ck, r0, wdw, first=first):
                        yt, yb = PB16[ck], PB16B[ck]
                        OP('dve', lambda e: e.scalar_tensor_tensor(out=yt[r0:r0 + 64, :], in0=src[r0:r0 + 64, ck, 16:16 + TBM], scalar=1.0 / wdw,
                                                                   in1=pext[r0:r0 + 64, ck, 16:16 + TBM], op0=ALU.mult, op1=ALU.subtract),
                           reads=[pwB[0], pwB[1], pextB], writes=[yb])
                        if first:
                            t2, b2 = FA[0], FAB[0]
                            OP('dve', lambda e: e.tensor_tensor(out=t2[r0:r0 + 64, 0:16], in0=src[r0:r0 + 64, ck, 16:32],
                                                                in1=cst[r0:r0 + 64, ICN + ck * 16:ICN + ck * 16 + 16], op=ALU.mult),
                               reads=[pwB[0], pwB[1], cstB], writes=[b2])
                            OP('dve', lambda e: e.tensor_tensor(out=yt[r0:r0 + 64, 0:16], in0=t2[r0:r0 + 64, 0:16],
                                                                in1=pext[r0:r0 + 64, ck, 16:32], op=ALU.subtract),
                               reads=[b2, pextB], writes=[yb])
                    poolfin(pw[0], 0, 0, 2)
                    OP('dve', lambda e: e.tensor_tensor(out=pw[1][:, :, 3:W_], in0=pw[0][:, :, 3:W_], in1=pw[0][:, :, 1:W_ - 2], op=ALU.add),
                       reads=[pwB[0]], writes=[pwB[1]])
                    poolfin(pw[1], 0, 64, 4)
                    OP('dve', lambda e: e.tensor_tensor(out=pw[0][:, :, 7:W_], in0=pw[1][:, :, 7:W_], in1=pw[1][:, :, 3:W_ - 4], op=ALU.add),
                       reads=[pwB[1]], writes=[pwB[0]])
                    poolfin(pw[0], 1, 0, 8)
                    OP('dve', lambda e: e.tensor_tensor(out=pw[1][:, :, 15:W_], in0=pw[0][:, :, 15:W_], in1=pw[0][:, :, 7:W_ - 8], op=ALU.add),
                       reads=[pwB[0]], writes=[pwB[1]])
                    poolfin(pw[1], 1, 64, 16)
                    for ck in range(2):
                        bk = pbank()
                        OP('pe', lambda e, ck=ck, bk=bk: e.matmul(PS[bk][:, 0:TBM], lhsT=smatb[:, ck * 128:(ck + 1) * 128], rhs=PB16[ck][:], start=True, stop=True),
                           reads=[PB16B[ck], smatB], writes=[PSB[bk]])
                        OP('dve', lambda e, ck=ck, bk=bk: e.tensor_scalar(out=YT[:, ck, :], in0=PS[bk][:, 0:TBM], scalar1=pv[:, PV['pool_b'] + ck:PV['pool_b'] + ck + 1],
                                                                      scalar2=pv[:, PV['pool_s'] + ck:PV['pool_s'] + ck + 1], op0=ALU.add, op1=ALU.mult),
                           reads=[PSB[bk], pvB[l]], writes=[YB[ck]])
                else:
                    for ck in range(2):
                        OP('pool', lambda e, ck=ck: e.memset(YT[:, ck, :], 0.0), writes=[YB[ck]])

                def out_rstd(Ot, lhs_ones, n_ch, eps):
                    bk = pbank()
                    for hp in range(2):
                        st, sbb = ba()
                        OP('act', lambda e, hp=hp, st=st, Ot=Ot: e.activation(out=st[:], in_=Ot[hp][0][:], func=AF.Square), reads=[Ot[hp][1]], writes=[sbb])
                        if lhs_ones is ones:
                            OP('pe', lambda e, hp=hp, st=st: e.matmul(PS[bk][:, 0:TBM], lhsT=ones[:], rhs=st[:], start=(hp == 0), stop=(hp == 1)),
                               reads=[sbb, onesB], writes=[PSB[bk]])
                        else:
                            bk2 = bk if hp == 0 else pbank()
                            OP('pe', lambda e, hp=hp, st=st, bk2=bk2: e.matmul(PS[bk2][:, 0:TBM], lhsT=bones[:], rhs=st[:], start=True, stop=True),
                               reads=[sbb, bonesB], writes=[PSB[bk2]])
                            if hp == 0:
                                bk0 = bk2
                            else:
                                bk1 = bk2
                    if lhs_ones is ones:
                        rt, rb = fa()
                        rstd_from(PS[bk], PSB[bk], TBM, 1.0 / n_ch, eps, rt, rb)
                        return [(rt, rb), (rt, rb)]
                    res = []
                    for bkx in (bk0, bk1):
                        rt, rb = fa()
                        rstd_from(PS[bkx], PSB[bkx], TBM, 1.0 / n_ch, eps, rt, rb)
                        res.append((rt, rb))
                    return res

                def evac_O():
                    Ot = []
                    for hp in range(2):
                        t, b = fa()
                        OP('act', lambda e, hp=hp, t=t: e.activation(out=t[:], in_=PS[4 + hp][:, 0:TBM], func=AF.Copy), reads=[PSB[4 + hp]], writes=[b])
                        Ot.append((t, b))
                    return Ot

                if 'hgrn' in mixers:
                    QE, KE, PCx, GT = [], [], [], []
                    for hp in range(2):
                        bq = proj_fm(l, 2 + hp, first)
                        qt, qb = evac_fm(bq, AF.Silu)
                        bf_ = proj_fm(l, 4 + hp, first)
                        st_, sb_ = evac_fm(bf_, AF.Sigmoid)
                        ft, fb = fa()
                        OP('dve', lambda e, hp=hp, st_=st_, ft=ft: e.tensor_scalar(out=ft[:], in0=st_[:], scalar1=lbt[:, 2 * l + hp:2 * l + hp + 1],
                                                                             scalar2=lbt[:, 4 + 2 * l + hp:4 + 2 * l + hp + 1], op0=ALU.mult, op1=ALU.add),
                           reads=[sb_, lbtB], writes=[fb])
                        lt, lb_ = fa()
                        OP('dve', lambda e, ft=ft, lt=lt: e.tensor_scalar_max(out=lt[:], in0=ft[:], scalar1=1e-30), reads=[fb], writes=[lb_])
                        OP('act', lambda e, lt=lt: e.activation(out=lt[:], in_=lt[:], func=AF.Ln), reads=[lb_], writes=[lb_])
                        bt, bb = scan_decay(lt, lb_)
                        ebt, ebb = LL[hp][0], LLB_[hp][0]
                        OP('act', lambda e, bt=bt, ebt=ebt: e.activation(out=ebt[:], in_=bt[:], func=AF.Exp), reads=[bb], writes=[ebb])
                        OP('act', lambda e, bt=bt: e.activation(out=bt[:], in_=bt[:], func=AF.Exp, scale=-1.0), reads=[bb], writes=[bb])
                        qe, qeb = LB[hp][0], LBB[hp][0]
                        OP('dve', lambda e, qt=qt, ebt=ebt, qe=qe: e.scalar_tensor_tensor(out=qe[:], in0=qt[:], scalar=QK, in1=ebt[:], op0=ALU.mult, op1=ALU.mult),
                           reads=[qb, ebb], writes=[qeb])
                        OP('dve', lambda e, ft=ft: e.tensor_scalar(out=ft[:], in0=ft[:], scalar1=-1.0, scalar2=1.0, op0=ALU.mult, op1=ALU.add),
                           reads=[fb], writes=[fb])
                        ke, keb = LB[hp][1], LBB[hp][1]
                        OP('dve', lambda e, ft=ft, bt=bt, ke=ke: e.tensor_tensor(out=ke[:], in0=ft[:], in1=bt[:], op=ALU.mult), reads=[fb, bb], writes=[keb])
                        bg_ = proj_fm(l, 6 + hp, first)
                        gt, gb = evac_fm(bg_, AF.Sigmoid, dst=(LL[hp][1], LLB_[hp][1]))
                        QE.append((qe, qeb)); KE.append((ke, keb)); PCx.append((ebt, ebb)); GT.append((gt, gb))
                    proj_tm(l, 0)
                    chunk_engine('hgrn', QE, KE, PCx)
                    Ot = evac_O()
                    rs = out_rstd(Ot, ones, 256, NORM_EPS)
                    for hp in range(2):
                        t1, b1 = fa()
                        OP('dve', lambda e, hp=hp, t1=t1, Ot=Ot, rs=rs: e.scalar_tensor_tensor(out=t1[:], in0=Ot[hp][0][:], scalar=pv[:, PV['hnorm'] + hp:PV['hnorm'] + hp + 1],
                                                                           in1=rs[hp][0][:], op0=ALU.mult, op1=ALU.mult),
                           reads=[Ot[hp][1], rs[hp][1], pvB[l]], writes=[b1])
                        OP('dve', lambda e, hp=hp, t1=t1, GT=GT: e.tensor_tensor(out=YT[:, 2 + hp, :], in0=t1[:], in1=GT[hp][0][:], op=ALU.mult),
                           reads=[b1, GT[hp][1]], writes=[YB[2 + hp]])
                else:
                    for ck in (2, 3):
                        OP('pool', lambda e, ck=ck: e.memset(YT[:, ck, :], 0.0), writes=[YB[ck]])

                if 'gla' in mixers:
                    bga = proj_fm(l, 22, first)
                    gat, gab = LLX[0], LLXB[0]
                    OP('act', lambda e: e.activation(out=gat[:], in_=PS[bga][:, 0:TBM], func=AF.Copy), reads=[PSB[bga]], writes=[gab])
                    QE, KE, PCx, GT = [], [], [], []
                    for hp in range(2):
                        bk = pbank()
                        OP('pe', lambda e, hp=hp, bk=bk: e.matmul(PS[bk][:, 0:TBM], lhsT=smat[:, 1024 + hp * 128:1024 + (hp + 1) * 128], rhs=gat[:], start=True, stop=True),
                           reads=[gab, smatB], writes=[PSB[bk]])
                        lt, lb_ = evac_fm(bk, AF.Sigmoid, bias=pv[:, PV['glab'] + hp:PV['glab'] + hp + 1])
                        OP('act', lambda e, lt=lt: e.activation(out=lt[:], in_=lt[:], func=AF.Ln), reads=[lb_], writes=[lb_])
                        bt, bb = scan_decay(lt, lb_)
                        ebt, ebb = LL[hp][0], LLB_[hp][0]
                        OP('act', lambda e, bt=bt, ebt=ebt: e.activation(out=ebt[:], in_=bt[:], func=AF.Exp, scale=1.0 / 16), reads=[bb], writes=[ebb])
                        OP('act', lambda e, bt=bt: e.activation(out=bt[:], in_=bt[:], func=AF.Exp, scale=-1.0 / 16), reads=[bb], writes=[bb])
                        bq = proj_fm(l, 16 + hp, first)
                        qe, qeb = LB[hp][0], LBB[hp][0]
                        OP('dve', lambda e, bq=bq, ebt=ebt, qe=qe: e.scalar_tensor_tensor(out=qe[:], in0=PS[bq][:, 0:TBM], scalar=QK, in1=ebt[:], op0=ALU.mult, op1=ALU.mult),
                           reads=[PSB[bq], ebb], writes=[qeb])
                        bkk = proj_fm(l, 18 + hp, first)
                        ke, keb = LB[hp][1], LBB[hp][1]
                        OP('dve', lambda e, bkk=bkk, bt=bt, ke=ke: e.tensor_tensor(out=ke[:], in0=PS[bkk][:, 0:TBM], in1=bt[:], op=ALU.mult), reads=[PSB[bkk], bb], writes=[keb])
                        bg_ = proj_fm(l, 20 + hp, first)
                        gt, gb = evac_fm(bg_, AF.Silu, dst=(LL[hp][1], LLB_[hp][1]))
                        QE.append((qe, qeb)); KE.append((ke, keb)); PCx.append((ebt, ebb)); GT.append((gt, gb))
                    proj_tm(l, 2)
                    chunk_engine('gla', QE, KE, PCx)
                    Ot = evac_O()
                    rs = out_rstd(Ot, bones, 64, NORM_EPS)
                    for hp in range(2):
                        t1, b1 = fa()
                        OP('dve', lambda e, hp=hp, t1=t1, Ot=Ot, rs=rs: e.scalar_tensor_tensor(out=t1[:], in0=Ot[hp][0][:], scalar=pv[:, PV['gnorm'] + hp:PV['gnorm'] + hp + 1],
                                                                           in1=rs[hp][0][:], op0=ALU.mult, op1=ALU.mult),
                           reads=[Ot[hp][1], rs[hp][1], pvB[l]], writes=[b1])
                        OP('dve', lambda e, hp=hp, t1=t1, GT=GT: e.tensor_tensor(out=YT[:, 6 + hp, :], in0=t1[:], in1=GT[hp][0][:], op=ALU.mult),
                           reads=[b1, GT[hp][1]], writes=[YB[6 + hp]])
                else:
                    for ck in (6, 7):
                        OP('pool', lambda e, ck=ck: e.memset(YT[:, ck, :], 0.0), writes=[YB[ck]])

                if 'rwkv' in mixers:
                    bwa = proj_fm(l, 14, first)
                    twa, twab = LLX[0], LLXB[0]
                    OP('act', lambda e: e.activation(out=twa[0:64, :], in_=PS[bwa][0:64, 0:TBM], func=AF.Tanh), reads=[PSB[bwa]], writes=[twab])
                    OP('act', lambda e: e.activation(out=twa[64:128, :], in_=PS[bwa][64:128, 0:TBM], func=AF.Copy), reads=[PSB[bwa]], writes=[twab])
                    bxg = proj_fm(l, 15, first)
                    sxg, sxgb = evac_fm(bxg, AF.Sigmoid, dst=(LLX[1], LLXB[1]))
                    QE, KE, AE, BE, PCx, GR, RKR, VF = [], [], [], [], [], [], [], []
                    for hp in range(2):
                        cs_ = slice(hp * 128, (hp + 1) * 128)
                        bk = pbank()
                        OP('pe', lambda e, bk=bk, hp=hp: e.matmul(PS[bk][:, 0:TBM], lhsT=smat[:, 256 + hp * 128:256 + (hp + 1) * 128], rhs=twa[:], start=True, stop=True),
                           reads=[twab, smatB], writes=[PSB[bk]])
                        lw, lwb = evac_fm(bk, AF.Sigmoid, bias=pv[:, PV['w0'] + hp:PV['w0'] + hp + 1])
                        bk = pbank()
                        OP('pe', lambda e, bk=bk, hp=hp: e.matmul(PS[bk][:, 0:TBM], lhsT=smat[:, 768 + hp * 128:768 + (hp + 1) * 128], rhs=twa[:], start=True, stop=True),
                           reads=[twab, smatB], writes=[PSB[bk]])
                        at, ab_ = evac_fm(bk, AF.Sigmoid, bias=pv[:, PV['a0'] + hp:PV['a0'] + hp + 1])
                        bk = pbank()
                        OP('pe', lambda e, bk=bk, hp=hp: e.matmul(PS[bk][:, 0:TBM], lhsT=smat[:, 512 + hp * 128:512 + (hp + 1) * 128], rhs=sxg[:], start=True, stop=True),
                           reads=[sxgb, smatB], writes=[PSB[bk]])
                        grt, grb = evac_fm(bk, AF.Copy, dst=(LL[hp][1], LLB_[hp][1]))
                        bt, bb = scan_decay(lw, lwb)
                        CW = -float(np.exp(-0.5))
                        ebt, ebb = LL[hp][0], LLB_[hp][0]
                        OP('act', lambda e, bt=bt, ebt=ebt: e.activation(out=ebt[:], in_=bt[:], func=AF.Exp, scale=CW), reads=[bb], writes=[ebb])
                        enb, enbb = fa()
                        OP('act', lambda e, bt=bt, enb=enb: e.activation(out=enb[:], in_=bt[:], func=AF.Exp, scale=-CW), reads=[bb], writes=[enbb])
                        OP('dve', lambda e, bt=bt, lw=lw: e.tensor_tensor(out=bt[:], in0=bt[:], in1=lw[:], op=ALU.subtract), reads=[bb, lwb], writes=[bb])
                        OP('act', lambda e, bt=bt: e.activation(out=bt[:], in_=bt[:], func=AF.Exp, scale=CW), reads=[bb], writes=[bb])
                        br = proj_fm(l, 8 + hp, first)
                        rt, rb = evac_fm(br, AF.Copy)
                        bkr = proj_fm(l, 10 + hp, first)
                        kt, kb = evac_fm(bkr, AF.Copy)
                        bv = proj_fm(l, 12 + hp, first)
                        vt, vb = evac_fm(bv, AF.Copy, dst=(LL[hp][2], LLB_[hp][2]))
                        kkt, kkb = fa()
                        OP('dve', lambda e, kt=kt, kkt=kkt, hp=hp: e.tensor_scalar(out=kkt[:], in0=kt[:], scalar1=pv[:, PV['kk'] + hp:PV['kk'] + hp + 1], scalar2=None, op0=ALU.mult),
                           reads=[kb, pvB[l]], writes=[kkb])
                        sq_, sqb_ = ba()
                        OP('act', lambda e, kkt=kkt, sq_=sq_: e.activation(out=sq_[:], in_=kkt[:], func=AF.Square), reads=[kkb], writes=[sqb_])
                        bk = pbank()
                        OP('pe', lambda e, bk=bk, sq_=sq_: e.matmul(PS[bk][:, 0:TBM], lhsT=bones[:], rhs=sq_[:], start=True, stop=True), reads=[sqb_, bonesB], writes=[PSB[bk]])
                        rn, rnb = fa()
                        rstd_from(PS[bk], PSB[bk], TBM, 1.0, 1e-24, rn, rnb)
                        OP('dve', lambda e, kkt=kkt, rn=rn: e.tensor_tensor(out=kkt[:], in0=kkt[:], in1=rn[:], op=ALU.mult), reads=[kkb, rnb], writes=[kkb])
                        fac, facb = fa()
                        OP('dve', lambda e, at=at, fac=fac, hp=hp: e.tensor_scalar(out=fac[:], in0=at[:], scalar1=-1.0, scalar2=pv[:, PV['ka'] + hp:PV['ka'] + hp + 1], op0=ALU.add, op1=ALU.mult),
                           reads=[ab_, pvB[l]], writes=[facb])
                        OP('dve', lambda e, fac=fac, kt=kt: e.scalar_tensor_tensor(out=kt[:], in0=fac[:], scalar=1.0, in1=kt[:], op0=ALU.add, op1=ALU.mult),
                           reads=[facb, kb], writes=[kb])
                        rk_, rkb_ = LB[hp][4], LBB[hp][4]
                        OP('dve', lambda e, rt=rt, kt=kt, rk_=rk_, hp=hp: e.scalar_tensor_tensor(out=rk_[:], in0=rt[:], scalar=pv[:, PV['rk'] + hp:PV['rk'] + hp + 1], in1=kt[:], op0=ALU.mult, op1=ALU.mult),
                           reads=[rb, kb, pvB[l]], writes=[rkb_])
                        qe, qeb = LB[hp][0], LBB[hp][0]
                        OP('dve', lambda e, rt=rt, ebt=ebt, qe=qe: e.tensor_tensor(out=qe[:], in0=rt[:], in1=ebt[:], op=ALU.mult), reads=[rb, ebb], writes=[qeb])
                        ke, keb = LB[hp][1], LBB[hp][1]
                        OP('dve', lambda e, kt=kt, enb=enb, ke=ke: e.tensor_tensor(out=ke[:], in0=kt[:], in1=enb[:], op=ALU.mult), reads=[kb, enbb], writes=[keb])
                        be, beb = LB[hp][3], LBB[hp][3]
                        OP('dve', lambda e, kkt=kkt, bt=bt, be=be: e.tensor_tensor(out=be[:], in0=kkt[:], in1=bt[:], op=ALU.mult), reads=[kkb, bb], writes=[beb])
                        OP('dve', lambda e, kkt=kkt, at=at: e.tensor_tensor(out=kkt[:], in0=kkt[:], in1=at[:], op=ALU.mult), reads=[kkb, ab_], writes=[kkb])
                        ae, aeb = LB[hp][2], LBB[hp][2]
                        OP('dve', lambda e, kkt=kkt, enb=enb, ae=ae: e.tensor_tensor(out=ae[:], in0=kkt[:], in1=enb[:], op=ALU.mult), reads=[kkb, enbb], writes=[aeb])
                        QE.append((qe, qeb)); KE.append((ke, keb)); AE.append((ae, aeb)); BE.append((be, beb)); PCx.append((ebt, ebb))
                        GR.append((grt, grb)); RKR.append((rk_, rkb_)); VF.append((vt, vb))
                    proj_tm(l, 1)
                    chunk_engine('rwkv', QE, KE, PCx, rw={'AE': AE, 'BE': BE})
                    Ot = evac_O()
                    for hp in range(2):
                        ob16, ob16b = ba()
                        OP('dve', lambda e, hp=hp, ob16=ob16, Ot=Ot: e.tensor_copy(out=ob16[:], in_=Ot[hp][0][:]), reads=[Ot[hp][1]], writes=[ob16b])
                        bk = pbank()
                        OP('pe', lambda e, bk=bk, ob16=ob16: e.matmul(PS[bk][:, 0:TBM], lhsT=bones[:], rhs=ob16[:], start=True, stop=True), reads=[ob16b, bonesB], writes=[PSB[bk]])
                        ct, cb = fa()
                        OP('dve', lambda e, hp=hp, bk=bk, ct=ct, Ot=Ot: e.scalar_tensor_tensor(out=ct[:], in0=PS[bk][:, 0:TBM], scalar=-1.0 / 64, in1=Ot[hp][0][:], op0=ALU.mult, op1=ALU.add),
                           reads=[PSB[bk], Ot[hp][1]], writes=[cb])
                        s2, s2b = ba()
                        OP('act', lambda e, ct=ct, s2=s2: e.activation(out=s2[:], in_=ct[:], func=AF.Square), reads=[cb], writes=[s2b])
                        bk = pbank()
                        OP('pe', lambda e, bk=bk, s2=s2: e.matmul(PS[bk][:, 0:TBM], lhsT=bones[:], rhs=s2[:], start=True, stop=True), reads=[s2b, bonesB], writes=[PSB[bk]])
                        rn, rnb = fa()
                        rstd_from(PS[bk], PSB[bk], TBM, 1.0 / 64, GN_EPS, rn, rnb)
                        OP('dve', lambda e, hp=hp, ct=ct, rn=rn: e.scalar_tensor_tensor(out=ct[:], in0=ct[:], scalar=pv[:, PV['lnw'] + hp:PV['lnw'] + hp + 1], in1=rn[:], op0=ALU.mult, op1=ALU.mult),
                           reads=[cb, rnb, pvB[l]], writes=[cb])
                        bk = pbank()
                        OP('pe', lambda e, bk=bk, hp=hp, RKR=RKR: e.matmul(PS[bk][:, 0:TBM], lhsT=bones[:], rhs=RKR[hp][0][:], start=True, stop=True), reads=[RKR[hp][1], bonesB], writes=[PSB[bk]])
                        bo, bob = fa()
                        OP('dve', lambda e, bk=bk, hp=hp, bo=bo, VF=VF: e.tensor_tensor(out=bo[:], in0=PS[bk][:, 0:TBM], in1=VF[hp][0][:], op=ALU.mult), reads=[PSB[bk], VF[hp][1]], writes=[bob])
                        OP('dve', lambda e, hp=hp, ct=ct, bo=bo: e.scalar_tensor_tensor(out=ct[:], in0=ct[:], scalar=pv[:, PV['lnb'] + hp:PV['lnb'] + hp + 1], in1=bo[:], op0=ALU.add, op1=ALU.add),
                           reads=[cb, bob, pvB[l]], writes=[cb])
                        OP('dve', lambda e, hp=hp, ct=ct, GR=GR: e.tensor_tensor(out=YT[:, 4 + hp, :], in0=ct[:], in1=GR[hp][0][:], op=ALU.mult),
                           reads=[cb, GR[hp][1]], writes=[YB[4 + hp]])
                else:
                    for ck in (4, 5):
                        OP('pool', lambda e, ck=ck: e.memset(YT[:, ck, :], 0.0), writes=[YB[ck]])

                nxt_o = load_o(m_wout[l][0], 'act')
                for j in range(KD):
                    i = nxt_o
                    if j + 1 < KD:
                        nxt_o = load_o(m_wout[l][j + 1], 'act')
                    for m in range(KD):
                        OP('pe', lambda e, m=m, j=j, i=i: e.matmul(PS[m][:, 0:TBM], lhsT=obf[i][:, m * 128:(m + 1) * 128], rhs=YT[:, j, :],
                                                               start=(j == 0), stop=(j == KD - 1)),
                           reads=[obfB[i], YB[j]], writes=[PSB[m]], sig=(m == KD - 1 or j == KD - 1))
                for m in range(KD):
                    OP('dve', lambda e, m=m: e.tensor_tensor(out=X[:, m, c0:c0 + TBM], in0=PS[m][:, 0:TBM], in1=X[:, m, c0:c0 + TBM], op=ALU.add),
                       reads=[PSB[m], XB[m][ti]], writes=[XB[m][ti]])

            def mixer_setup(l, s):
                OP('sp', lambda e: e.dma_start(out=mub[:], in_=mub_d[l]), writes=[mubB], dsem=muS)
                OP('sp', lambda e: e.dma_start(out=smat[:], in_=smat_d[l]), writes=[smatB], dsem=smS)
                OP('pool', lambda e: e.tensor_copy(out=smatb[:], in_=smat[:, 0:256]), reads=[smatB], writes=[smatB])
                for m in ('hgrn', 'gla', 'rwkv'):
                    for hp in range(2):
                        OP('pool', lambda e, m=m, hp=hp: e.memset(S32[m][hp][:], 0.0), writes=[S32B[m][hp]])
                        OP('pool', lambda e, m=m, hp=hp: e.memset(Sbf[m][hp][:], 0.0), writes=[SbfB[m][hp]])

        if do_mix:
            OP('act', lambda e: e.activation(out=lbt[:, 0:2], in_=pvec[0][:, PV['lb0']:PV['lb0'] + 2], func=AF.Exp), reads=[pvB[0]], writes=[lbtB])
            OP('act', lambda e: e.activation(out=lbt[:, 2:4], in_=pvec[L - 1][:, PV['lbl']:PV['lbl'] + 2], func=AF.Exp), reads=[pvB[L - 1], lbtB], writes=[lbtB])
            OP('dve', lambda e: e.tensor_tensor(out=lbt[:, 4:6], in0=lbt[:, 0:2], in1=lbt[:, 2:4], op=ALU.add), reads=[lbtB], writes=[lbtB])
            OP('dve', lambda e: e.reciprocal(out=lbt[:, 4:6], in_=lbt[:, 4:6]), reads=[lbtB], writes=[lbtB])
            OP('dve', lambda e: e.tensor_tensor(out=lbt[:, 6:8], in0=lbt[:, 2:4], in1=lbt[:, 4:6], op=ALU.mult), reads=[lbtB], writes=[lbtB])
            OP('dve', lambda e: e.memset(lbt[:, 4:6], 0.0), reads=[lbtB], writes=[lbtB])
            OP('dve', lambda e: e.tensor_scalar(out=lbt[:, 0:4], in0=lbt[:, 4:8], scalar1=-1.0, scalar2=1.0, op0=ALU.mult, op1=ALU.add), reads=[lbtB], writes=[lbtB])

        for s in range(NS):
            for k in range(KD):
                OP('sp', lambda e, k=k, s=s: e.dma_start(out=X[:, k, :], in_=xT[s, :, k, :]), writes=XB[k], dsem=xS[k])
            for l in range(L):
                if do_ffn:
                    for ti in range(NT):
                        ffn(l, 0, ti)
                if do_mix:
                    mixer_setup(l, s)
                    for bi in range(T // TBM):
                        mixer_block(l, s, bi)
                if do_ffn:
                    for ti in range(NT):
                        ffn(l, 1, ti)
            for ti in range(NT):
                c0 = ti * TB
                for k in range(KD):
                    i = counters['sq'] % 2
                    counters['sq'] += 1
                    OP('act', lambda e, k=k, i=i, c0=c0: e.activation(out=sq[i][:], in_=X[:, k, c0:c0 + TB], func=AF.Square),
                       reads=[XB[k][ti]], writes=[sqB[i]])
                    OP('pe', lambda e, k=k, i=i: e.matmul(PS[0][:, :], lhsT=ones[:], rhs=sq[i][:], start=(k == 0), stop=(k == KD - 1)),
                       reads=[sqB[i], onesB], writes=[PSB[0]])
                rstd_from(PS[0], PSB[0], TB, 1.0 / D, NORM_EPS, rstd, rstdB)
                for k in range(KD):
                    OP('dve', lambda e, k=k, c0=c0: e.scalar_tensor_tensor(out=X[:, k, c0:c0 + TB], in0=X[:, k, c0:c0 + TB],
                                                                        scalar=pvec[0][:, PV['nfin'] + k:PV['nfin'] + k + 1], in1=rstd[:],
                                                                        op0=ALU.mult, op1=ALU.mult),
                       reads=[XB[k][ti], rstdB, pvB[0]], writes=[XB[k][ti]])
            for k in range(KD):
                OP('sp', lambda e, k=k, s=s: e.dma_start(out=outT[s, :, k, :], in_=X[:, k, :]), reads=XB[k], writes=XB[k], dsem=oS[k])
        pg.ops['sp'].append((lambda e: e.nop(), {o_: o_.count for o_ in oS}, False, None))
        pg.emit(block, esems)
    return nc, pg


def _col(v, n):
    return np.ascontiguousarray(np.asarray(v, np.float32).reshape(n, 128).T)


def make_consts():
    cst = np.zeros((128, 1600), np.float32)
    p = np.arange(64)[:, None]
    f = np.arange(64)[None, :]
    for h in range(4):
        cst[0:64, 0 + h * 64:0 + (h + 1) * 64] = (p <= f)
        cst[0:64, 256 + h * 64:256 + (h + 1) * 64] = (p < f)
        cst[0:64, 512 + h * 64:512 + (h + 1) * 64] = (f < p)
        cst[0:64, 768 + h * 64:768 + (h + 1) * 64] = (p == f)
    scm = np.ones(512, np.float32)
    scm[::64] = 0.0
    cst[:, 1024:1536] = scm[None, :]
    wins = {(0, 0): 2, (0, 1): 4, (1, 0): 8, (1, 1): 16}
    for ck in range(2):
        for half in range(2):
            w = wins[(ck, half)]
            t = np.arange(16)
            cst[half * 64:(half + 1) * 64, 1536 + ck * 16:1536 + ck * 16 + 16] = (1.0 / np.minimum(t + 1, w))[None, :]
    return cst


def prep_weights(inp, L):
    out = {}
    f32 = np.float32
    for l in range(L):
        for w, (wi, wo) in enumerate((('ffn1_w_in', 'ffn1_w_out'), ('ffn2_w_in', 'ffn2_w_out'))):
            W = np.asarray(inp[wi][l], f32)
            Wk = W.reshape(KD, 128, 2 * FF)
            g = Wk[:, :, :FF].reshape(KD, 128, NJ, 128)
            u = Wk[:, :, FF:].reshape(KD, 128, NJ, 128)
            blk = np.concatenate([g, u], axis=3)
            out[f"f{w + 1}_win{l}"] = np.ascontiguousarray(blk.transpose(2, 1, 0, 3)).reshape(NJ, 128, KD * 256)
            out[f"f{w + 1}_wout{l}"] = np.ascontiguousarray(np.asarray(inp[wo][l], f32).reshape(NJ, 128, D))
        W = np.asarray(inp['w_in'][l], f32).reshape(KD, 128, DIN)
        fm = np.zeros((len(FMG), 128, KD, 128), f32)
        for gi, (c0, nc_, _) in enumerate(FMG):
            nc_ = min(nc_, DIN - c0)
            fm[gi, :, :, :nc_] = W[:, :, c0:c0 + nc_].transpose(1, 0, 2)
        out[f"m_fm{l}"] = fm.reshape(len(FMG), 128, KD * 128)
        tm = np.zeros((len(TMG), 128, KD, 256), f32)
        for gi, (c0, nc_, _) in enumerate(TMG):
            tm[gi] = W[:, :, c0:c0 + nc_].transpose(1, 0, 2)
        out[f"m_tm{l}"] = tm.reshape(len(TMG), 128, KD * 256)
        out[f"m_wout{l}"] = np.ascontiguousarray(np.asarray(inp['w_out'][l], f32).reshape(KD, 128, D))
        pv = np.zeros((128, NPV), f32)
        pv[:, PV['nf1']:PV['nf1'] + 8] = _col(inp['norm_ffn1'][l], 8)
        pv[:, PV['nmx']:PV['nmx'] + 8] = _col(inp['norm_mix'][l], 8)
        pv[:, PV['nf2']:PV['nf2'] + 8] = _col(inp['norm_ffn2'][l], 8)
        pv[:, PV['nfin']:PV['nfin'] + 8] = _col(inp['norm_final'], 8)
        pv[:, PV['lb0']:PV['lb0'] + 2] = _col(inp['hgrn_lb_logits'][0], 2)
        pv[:, PV['lbl']:PV['lbl'] + 2] = _col(inp['hgrn_lb_logits'][l], 2)
        for nm, key in (('pool_b', 'pool_b'), ('pool_s', 'pool_scale'), ('hnorm', 'hgrn_norm'), ('w0', 'rwkv_w0'), ('a0', 'rwkv_a0'),
                        ('kk', 'rwkv_k_k'), ('ka', 'rwkv_k_a'), ('rk', 'rwkv_r_k'), ('lnw', 'rwkv_ln_w'), ('lnb', 'rwkv_ln_b'),
                        ('glab', 'gla_b'), ('gnorm', 'gla_norm')):
            pv[:, PV[nm]:PV[nm] + 2] = _col(inp[key][l], 2)
        out[f"pvec{l}"] = pv
        out[f"mub{l}"] = np.ascontiguousarray(np.broadcast_to(np.asarray(inp['rwkv_mu'][l], f32)[None, :], (128, 1024)))
        sm = np.zeros((128, 5 * 256), f32)
        pw_ = np.asarray(inp['pool_w'][l], f32)
        for ck in range(2):
            sm[0:64, ck * 128:ck * 128 + 64] = pw_[2 * ck]
            sm[64:128, ck * 128 + 64:ck * 128 + 128] = pw_[2 * ck + 1]
        sm[0:64, 256:512] = np.asarray(inp['rwkv_w2'][l], f32)
        sm[64:128, 768:1024] = np.asarray(inp['rwkv_a2'][l], f32)
        sm[:, 512:768] = np.asarray(inp['rwkv_g2'][l], f32)
        sm[0:16, 1024:1280] = np.asarray(inp['gla_w2'][l], f32)
        out[f"smat{l}"] = sm
    out["cst"] = make_consts()
    return out


def prep_x(xc):
    NS, T, _ = xc.shape
    return np.ascontiguousarray(xc.reshape(NS, T, KD, 128).transpose(0, 3, 2, 1))


def unprep_out(o):
    NS, _, _, T = o.shape
    return np.ascontiguousarray(o.transpose(0, 3, 2, 1)).reshape(NS, T, D)


def kernel(**inputs):
    x = np.asarray(inputs['x'], np.float32)
    B, T, _ = x.shape
    NCORES = 8
    NS = B // NCORES
    L = 2
    nc, pg = build_program(T, NS, L)
    wts = prep_weights(inputs, L)
    in_maps = []
    for c in range(NCORES):
        m = dict(wts)
        m["xT"] = prep_x(x[c * NS:(c + 1) * NS])
        in_maps.append(m)
    res = run_bass_kernel_spmd(nc, in_maps, core_ids=list(range(NCORES)))
    outs = [unprep_out(np.asarray(r["outT"])) for r in res.results]
    return np.concatenate(outs, axis=0).astype(np.float32)
```

```python
import numpy as np
from contextlib import ExitStack
import concourse.bass as bass
import concourse.mybir as mybir
from concourse.bass_utils import run_bass_kernel_spmd

F32 = mybir.dt.float32
BF16 = mybir.dt.bfloat16
AF = mybir.ActivationFunctionType
ALU = mybir.AluOpType
ENGS = ['pe', 'act', 'dve', 'pool', 'sp']

D = 1024
KD = 8
FF = 2816
NJ = 22
G = 256
DIN = 3344
NORM_EPS = 1e-6
GN_EPS = 64e-5
QK = 0.125
TB = 512
TBM = 256
CH = 64
NCH = TBM // CH
DEBUG_STAGE = 99

FMG = [(0, 128, 0), (128, 128, 0), (256, 128, 0), (384, 128, 0), (512, 128, 0), (640, 128, 0),
       (1024, 128, 0), (1152, 128, 0),
       (1280, 128, 1), (1408, 128, 1), (1536, 128, 1), (1664, 128, 1), (1792, 128, 1), (1920, 128, 1),
       (2048, 128, 1), (2176, 128, 1),
       (2304, 128, 0), (2432, 128, 0), (2560, 128, 0), (2688, 128, 0), (3072, 128, 0), (3200, 128, 0),
       (3328, 128, 0)]
TMG = [(768, 256, 0), (1792, 256, 1), (2816, 256, 0)]
RW0 = 1280

PV = {}
_o = 0
for _n, _w in [('nf1', 8), ('nmx', 8), ('nf2', 8), ('pool_b', 2), ('pool_s', 2), ('lb0', 2), ('lbl', 2),
               ('hnorm', 2), ('w0', 2), ('a0', 2), ('kk', 2), ('ka', 2), ('rk', 2), ('lnw', 2), ('lnb', 2),
               ('glab', 2), ('gnorm', 2), ('nfin', 8)]:
    PV[_n] = _o
    _o += _w
NPV = _o


class Buf:
    __slots__ = ('name', 'w', 'r', 'const')

    def __init__(self, name, const=False):
        self.name = name
        self.w = None
        self.r = []
        self.const = const


class DSem:
    def __init__(self, h):
        self.h = h
        self.count = 0


class Prog:
    def __init__(self, nc, same_engine_sync=True):
        self.nc = nc
        self.ops = {e: [] for e in ENGS}
        self.cnt = {e: 0 for e in ENGS}
        self.same = same_engine_sync
        self.nops = 0
        self.last_rg = None
        self.last_pe_sig = True

    def op(self, eng, fn, reads=(), writes=(), sig=True, dsem=None, rg=None):
        waits = {}
        if eng == 'pe':
            if rg is not None and self.last_rg is not None and rg != self.last_rg:
                assert self.last_pe_sig
                waits['pe'] = self.cnt['pe']
            self.last_rg = rg
            self.last_pe_sig = sig

        def addw(tok):
            if tok is None:
                return
            k, v = tok
            if k == eng and (eng == 'pe' or not self.same):
                return
            if waits.get(k, 0) < v:
                waits[k] = v
        for b in reads:
            addw(b.w)
        for b in writes:
            addw(b.w)
            for t in b.r:
                addw(t)
        if dsem is not None:
            dsem.count += 16
            tok = (dsem, dsem.count)
            sig = False
        elif sig:
            self.cnt[eng] += 1
            tok = (eng, self.cnt[eng])
        else:
            tok = (eng, self.cnt[eng] + 1)
        for b in reads:
            if not b.const:
                b.r.append(tok)
                if len(b.r) > 48:
                    mx = {}
                    for k, v in b.r:
                        if mx.get(k, 0) < v:
                            mx[k] = v
                    b.r = list(mx.items())
        for b in writes:
            b.w = tok
            b.r = []
        self.ops[eng].append((fn, waits, sig, dsem))
        self.nops += 1
        return tok

    def emit(self, block, esems):
        nc = self.nc
        deco = {'pe': block.tensor, 'act': block.scalar, 'dve': block.vector,
                'pool': block.gpsimd, 'sp': block.sync}
        for e in ENGS:
            ops = self.ops[e]

            def body(eng, ops=ops, e=e):
                known = {}
                for fn, waits, sig, dsem in ops:
                    for k, v in waits.items():
                        if known.get(k, 0) >= v:
                            continue
                        known[k] = v
                        h = k.h if isinstance(k, DSem) else esems[k]
                        eng.wait_ge(h, v)
                    inst = fn(eng)
                    if dsem is not None:
                        inst.then_inc(dsem.h, 16)
                    elif sig:
                        inst.then_inc(esems[e], 1)
            deco[e](body)


def build_program(T, NS, L, mixers=('pool', 'hgrn', 'rwkv', 'gla'), do_ffn=True, do_mix=True):
    NT = T // TB
    nc = bass.Bass("TRN2", target_bir_lowering=False)
    dr = {}

    def din(name, shape):
        dr[name] = nc.dram_tensor(name, list(shape), F32, kind="ExternalInput").ap()
        return dr[name]
    xT = din("xT", [NS, 128, KD, T])
    outT = nc.dram_tensor("outT", [NS, 128, KD, T], F32, kind="ExternalOutput").ap()
    f_win = [[din(f"f{w}_win{l}", [NJ, 128, KD * 256]) for w in (1, 2)] for l in range(L)]
    f_wout = [[din(f"f{w}_wout{l}", [NJ, 128, D]) for w in (1, 2)] for l in range(L)]
    m_fm = [din(f"m_fm{l}", [len(FMG), 128, KD * 128]) for l in range(L)]
    m_tm = [din(f"m_tm{l}", [len(TMG), 128, KD * 256]) for l in range(L)]
    m_wout = [din(f"m_wout{l}", [KD, 128, D]) for l in range(L)]
    pvec_d = [din(f"pvec{l}", [128, NPV]) for l in range(L)]
    mub_d = [din(f"mub{l}", [128, 1024]) for l in range(L)]
    smat_d = [din(f"smat{l}", [128, 5 * 256]) for l in range(L)]
    cst_d = din("cst", [128, 1600])
    es = ExitStack()
    with es:
        def sb(name, shape, dt=F32):
            return es.enter_context(nc.sbuf_tensor("sb_" + name, list(shape), dt))

        def psum(name, shape, dt=F32):
            return es.enter_context(nc.psum_tensor("pp_" + name, list(shape), dt))
        esems = {e: es.enter_context(nc.semaphore("s_" + e)) for e in ENGS}

        def dsem(name):
            return DSem(es.enter_context(nc.semaphore(name)))
        pg = Prog(nc)
        block = es.enter_context(nc.Block())

        X = sb("X", [128, KD, T])
        XB = [[Buf(f"X{k}_{t}") for t in range(NT)] for k in range(KD)]
        xn = sb("xn", [128, KD, TB], BF16)
        xns = sb("xns", [128, KD, TBM], BF16)
        xnsB = Buf("xns")
        xnB = [Buf(f"xn{k}") for k in range(KD)]
        sq = [sb(f"sq{i}", [128, TB], BF16) for i in range(2)]
        sqB = [Buf(f"sq{i}") for i in range(2)]
        rstd = sb("rstd", [128, TB]); rstdB = Buf("rstd")
        ones = sb("ones", [128, 128], BF16); onesB = Buf("ones", const=True)
        bones = sb("bones", [128, 128], BF16)
        ident = sb("ident", [128, 128], BF16)
        cst = sb("cst", [128, 1600])
        cstB = Buf("cst", const=True)
        MI, MSU, MSL, ID4 = 0, 256, 512, 768
        SCM = 1024
        ICN = 1536
        pvec = [sb(f"pvec{l}", [128, NPV]) for l in range(L)]
        pvB = [Buf(f"pvec{l}", const=True) for l in range(L)]
        lbt = sb("lbt", [128, 8])
        mub = sb("mub", [128, 1024]); mubB = Buf("mub")
        smat = sb("smat", [128, 5 * 256]); smatB = Buf("smat")
        smatb = sb("smatb", [128, 2 * 128], BF16)
        wst = [sb(f"wst{i}", [128, KD * 256]) for i in range(2)]
        wstB = [Buf(f"wst{i}") for i in range(2)]
        wstS = [dsem(f"dwst{i}") for i in range(2)]
        wbf = [sb(f"wbf{i}", [128, KD * 256], BF16) for i in range(2)]
        wbfB = [Buf(f"wbf{i}") for i in range(2)]
        wbfB2 = [[Buf(f"wbf{i}a"), Buf(f"wbf{i}b")] for i in range(2)]
        wtmp = sb("wtmp", [128, KD * 256]); wtmpB = Buf("wtmp")
        ost = [sb("ost0", [128, D])] * 2
        ostB = [Buf("ost0")] * 2
        ostS = [dsem("dost0")] * 2
        obf = [sb(f"obf{i}", [128, D], BF16) for i in range(2)]
        obfB = [Buf(f"obf{i}") for i in range(2)]
        hT = sb("hT", [128, NJ, TB], BF16)
        hB = [Buf(f"h{j}") for j in range(NJ)]
        sg = [sb("sg0", [128, TB])] * 2
        sgB = [Buf("sg0")] * 2
        PS = [psum(f"ps{i}", [128, 512]) for i in range(8)]
        PSB = [Buf(f"ps{i}") for i in range(8)]
        xS = [dsem(f"dx{k}") for k in range(KD)]
        oS = [dsem(f"dout{k}") for k in range(KD)]
        cS = dsem("dcst")
        pS = [dsem(f"dpv{l}") for l in range(L)]
        muS = dsem("dmu")
        smS = dsem("dsm")
        counters = {'w': 0, 'o': 0, 'sq': 0, 'sg': 0, 'pp': 0}

        def OP(eng, fn, reads=(), writes=(), sig=True, dsem=None, rg=None):
            return pg.op(eng, fn, reads, writes, sig, dsem, rg)

        def load_w(src_ap, ncols):
            i = counters['w'] % 2
            counters['w'] += 1
            OP('sp', lambda e: e.dma_start(out=wst[i][:, 0:ncols], in_=src_ap), writes=[wstB[i]], dsem=wstS[i])
            return i

        def cast_w(i, ncols, eng='pool'):
            if eng == 'act':
                OP('act', lambda e: e.activation(out=wbf[i][:, 0:ncols], in_=wst[i][:, 0:ncols], func=AF.Copy),
                   reads=[wstB[i]], writes=[wbfB[i], wbfB2[i][0], wbfB2[i][1]])
            else:
                OP(eng, lambda e: e.tensor_copy(out=wbf[i][:, 0:ncols], in_=wst[i][:, 0:ncols]),
                   reads=[wstB[i]], writes=[wbfB[i], wbfB2[i][0], wbfB2[i][1]])

        def cast_w_split(i, ncols):
            c1 = (ncols * 3 // 4) // 128 * 128
            OP('dve', lambda e: e.tensor_copy(out=wbf[i][:, 0:c1], in_=wst[i][:, 0:c1]), reads=[wstB[i]], writes=[wbfB2[i][0], wbfB[i]])
            OP('pool', lambda e: e.tensor_copy(out=wbf[i][:, c1:ncols], in_=wst[i][:, c1:ncols]), reads=[wstB[i]], writes=[wbfB2[i][1]])

        def load_o(src_ap, eng='act'):
            i = counters['o'] % 2
            counters['o'] += 1
            OP('sp', lambda e: e.dma_start(out=ost[i][:], in_=src_ap), writes=[ostB[i]], dsem=ostS[i])
            if eng == 'act':
                OP('act', lambda e: e.activation(out=obf[i][:], in_=ost[i][:], func=AF.Copy),
                   reads=[ostB[i]], writes=[obfB[i]])
            else:
                OP(eng, lambda e: e.tensor_copy(out=obf[i][:], in_=ost[i][:]), reads=[ostB[i]], writes=[obfB[i]])
            return i

        def rstd_from(psb, psbuf, n, scale, eps, dst, dstB):
            OP('act', lambda e: e.activation(out=dst[:, 0:n], in_=psb[:, 0:n], func=AF.Ln, scale=scale, bias=eps),
               reads=[psbuf], writes=[dstB])
            OP('act', lambda e: e.activation(out=dst[:, 0:n], in_=dst[:, 0:n], func=AF.Exp, scale=-0.5),
               reads=[dstB], writes=[dstB])

        def rmsnorm_to_xn(l, gcol, c0, n, bank):
            ti = c0 // TB
            for k in range(KD):
                i = counters['sq'] % 2
                counters['sq'] += 1
                OP('act', lambda e, k=k, i=i: e.activation(out=sq[i][:, 0:n], in_=X[:, k, c0:c0 + n], func=AF.Square),
                   reads=[XB[k][ti]], writes=[sqB[i]])
                OP('pe', lambda e, k=k, i=i: e.matmul(PS[bank][:, 0:n], lhsT=ones[:], rhs=sq[i][:, 0:n], start=(k == 0), stop=(k == KD - 1)),
                   reads=[sqB[i], onesB], writes=[PSB[bank]])
            rstd_from(PS[bank], PSB[bank], n, 1.0 / D, NORM_EPS, rstd, rstdB)
            for k in range(KD):
                OP('dve', lambda e, k=k: e.scalar_tensor_tensor(out=xn[:, k, 0:n], in0=X[:, k, c0:c0 + n],
                                                                 scalar=pvec[l][:, gcol + k:gcol + k + 1], in1=rstd[:, 0:n],
                                                                 op0=ALU.mult, op1=ALU.mult),
                   reads=[XB[k][ti], rstdB, pvB[l]], writes=[xnB[k]])

        def ffn(l, w, ti):
            gcol = PV['nf1'] if w == 0 else PV['nf2']
            c0 = ti * TB
            blocks = [('in', j) for j in range(NJ)] + [('out', j) for j in range(NJ)]

            slots = {}

            def do_load(t):
                if t < len(blocks) and blocks[t][0] == 'in' and t not in slots:
                    slots[t] = load_w(f_win[l][w][blocks[t][1]], KD * 256)

            def do_ready(t):
                if t >= len(blocks):
                    return
                if blocks[t][0] == 'in':
                    do_load(t)
                    cast_w_split(slots[t], KD * 256)
                else:
                    slots[t] = load_o(f_wout[l][w][blocks[t][1]], 'act')
            do_load(0)
            do_load(1)
            do_ready(0)
            rmsnorm_to_xn(l, gcol, c0, TB, 0)
            for t, (kind, j) in enumerate(blocks):
                i = slots[t]
                do_ready(t + 1)
                do_load(t + 2)
                if kind == 'in':
                    wv = wbf[i][:].rearrange("p (k c) -> p k c", k=KD)
                    pp = counters['pp'] % 2
                    counters['pp'] += 1
                    bg, bu = 2 * pp, 2 * pp + 1
                    for half, bk in ((0, bg), (1, bu)):
                        for k in range(KD):
                            OP('pe', lambda e, k=k, half=half, bk=bk, wv=wv: e.matmul(
                                PS[bk][:, :], lhsT=wv[:, k, half * 128:(half + 1) * 128], rhs=xn[:, k, 0:TB],
                                start=(k == 0), stop=(k == KD - 1)),
                               reads=[wbfB[i], wbfB2[i][0], wbfB2[i][1], xnB[k]], writes=[PSB[bk]], sig=(k == KD - 1))
                    si = counters['sg'] % 2
                    counters['sg'] += 1
                    OP('act', lambda e, si=si, bg=bg: e.activation(out=sg[si][:], in_=PS[bg][:, :], func=AF.Silu),
                       reads=[PSB[bg]], writes=[sgB[si]])
                    OP('dve', lambda e, si=si, bu=bu, j=j: e.tensor_tensor(out=hT[:, j, :], in0=sg[si][:], in1=PS[bu][:, :], op=ALU.mult),
                       reads=[sgB[si], PSB[bu]], writes=[hB[j]])
                else:
                    for m in range(KD):
                        OP('pe', lambda e, m=m, j=j, i=i: e.matmul(PS[m][:, :], lhsT=obf[i][:, m * 128:(m + 1) * 128], rhs=hT[:, j, :],
                                                               start=(j == 0), stop=(j == NJ - 1)),
                           reads=[obfB[i], hB[j]], writes=[PSB[m]], sig=(m == KD - 1 or j == NJ - 1))
            for m in range(KD):
                OP('dve', lambda e, m=m: e.scalar_tensor_tensor(out=X[:, m, c0:c0 + TB], in0=PS[m][:, :], scalar=0.5,
                                                                 in1=X[:, m, c0:c0 + TB], op0=ALU.mult, op1=ALU.add),
                   reads=[PSB[m], XB[m][ti]], writes=[XB[m][ti]])

        OP('sp', lambda e: e.dma_start(out=cst[:], in_=cst_d), writes=[cstB], dsem=cS)
        for l in range(L):
            OP('sp', lambda e, l=l: e.dma_start(out=pvec[l][:], in_=pvec_d[l]), writes=[pvB[l]], dsem=pS[l])
        OP('pool', lambda e: e.memset(ones[:], 1.0), writes=[onesB])
        bonesB = Buf("bones", const=True)
        OP('pool', lambda e: e.memset(bones[:], 0.0), writes=[bonesB])
        OP('pool', lambda e: e.memset(bones[0:64, 0:64], 1.0), writes=[bonesB])
        OP('pool', lambda e: e.memset(bones[64:128, 64:128], 1.0), writes=[bonesB])
        identB = Buf("ident", const=True)
        OP('pool', lambda e: e.memset(ident[:], 1.0), writes=[identB])
        OP('pool', lambda e: e.affine_select(out=ident[:], in_=ident[:], pattern=[[-1, 128]], compare_op=ALU.is_equal,
                                             fill=0.0, base=0, channel_multiplier=1), reads=[identB], writes=[identB])

        if do_mix:
            YT = sb("YT", [128, KD, TBM], BF16)
            YB = [Buf(f"Y{k}") for k in range(KD)]
            NFA = 10
            FA = [sb(f"fa{i}", [128, TBM]) for i in range(NFA)]
            FAB = [Buf(f"fa{i}") for i in range(NFA)]
            NBA = 4
            BA = [sb(f"ba{i}", [128, TBM], BF16) for i in range(NBA)]
            BAB = [Buf(f"ba{i}") for i in range(NBA)]
            LL = [[sb(f"ll{hp}{i}", [128, TBM]) for i in range(3)] for hp in range(2)]
            LLB_ = [[Buf(f"ll{hp}{i}") for i in range(3)] for hp in range(2)]
            LLX = [sb(f"llx{i}", [128, TBM]) for i in range(2)]
            LLXB = [Buf(f"llx{i}") for i in range(2)]
            LB = [[sb(f"lb{hp}{i}", [128, TBM], BF16) for i in range(5)] for hp in range(2)]
            LBB = [[Buf(f"lb{hp}{i}") for i in range(5)] for hp in range(2)]
            PB16 = [sb(f"pb16{i}", [128, TBM], BF16) for i in range(2)]
            PB16B = [Buf(f"pb16{i}") for i in range(2)]
            Vt = [sb(f"vt{c}", [64, 256], BF16) for c in range(NCH)]
            VtB = [Buf(f"vt{c}") for c in range(NCH)]
            KEt = [sb(f"ket{c}", [64, 256], BF16) for c in range(NCH)]
            KEtB = [Buf(f"ket{c}") for c in range(NCH)]
            AEt = [sb(f"aet{c}", [64, 256], BF16) for c in range(NCH)]
            AEtB = [Buf(f"aet{c}") for c in range(NCH)]
            alias_ctr = [0]

            def mk(name, n):
                ts, bs = [], []
                for c in range(n):
                    idx = alias_ctr[0]
                    alias_ctr[0] += 1
                    j, half = idx // 2, idx % 2
                    ts.append(hT[0:64, j, half * 256:(half + 1) * 256])
                    bs.append(hB[j])
                return ts, bs
            ATs, ATsB = mk("ats", NCH)
            LKs, LKsB = mk("lks", NCH)
            ARs, ARsB = mk("ars", NCH)
            Nn, NnB = mk("nn", NCH)
            NTn, NTnB = mk("ntn", NCH)
            Nn2, Nn2B = mk("nn2", NCH)
            NTn2, NTn2B = mk("ntn2", NCH)
            Pn, PnB = mk("pn", NCH)
            Pn2, Pn2B = mk("pn2", NCH)
            Ysb = sb("ysb", [64, 256], BF16); YsbB = Buf("ysb")
            Usb = sb("usb", [64, 256], BF16); UsbB = Buf("usb")
            S32 = {m: [sb(f"s32{m}{hp}", [128, 64]) for hp in range(2)] for m in ('hgrn', 'gla', 'rwkv')}
            Sbf = {m: [sb(f"sbf{m}{hp}", [128, 64], BF16) for hp in range(2)] for m in ('hgrn', 'gla', 'rwkv')}
            S32B = {m: [Buf(f"s32{m}{hp}") for hp in range(2)] for m in ('hgrn', 'gla', 'rwkv')}
            SbfB = {m: [Buf(f"sbf{m}{hp}") for hp in range(2)] for m in ('hgrn', 'gla', 'rwkv')}
            stmp = [sb(f"stmp{hp}", [128, 64]) for hp in range(2)]
            stmpB = [Buf(f"stmp{hp}") for hp in range(2)]
            pext = sb("pext", [128, 2, 16 + TBM]); pextB = Buf("pext")
            pw = [sb(f"pw{i}", [128, 2, 16 + TBM]) for i in range(2)]
            pwB = [Buf(f"pw{i}") for i in range(2)]
            PST = PS[7][:, :].bitcast(BF16)
            lbtB = Buf("lbt", const=True)
            fa_ctr = [0]
            ba_ctr = [0]

            def fa():
                i = fa_ctr[0] % NFA
                fa_ctr[0] += 1
                return FA[i], FAB[i]

            def ba():
                i = ba_ctr[0] % NBA
                ba_ctr[0] += 1
                return BA[i], BAB[i]

            def pbank():
                b = counters['pp'] % 4
                counters['pp'] += 1
                return b

            wplan = {'plan': [], 'pos': 0, 'issued': {}, 'l': 0}

            def issue_load(l, d):
                kind, g = d
                if kind == 'fm':
                    return load_w(m_fm[l][g], KD * 128)
                return load_w(m_tm[l][g], KD * 256)

            def issue_cast(l, d, i):
                kind, g = d
                if kind == 'fm':
                    col0, ncols, shift = FMG[g]
                    if shift:
                        mc = col0 - RW0
                        mu_b = mub[:, mc:mc + 128].unsqueeze(1).broadcast_to([128, KD, 128])
                        wv32 = wst[i][:, 0:KD * 128].rearrange("p (k c) -> p k c", k=KD)
                        wt = wtmp[:, 0:KD * 128].rearrange("p (k c) -> p k c", k=KD)
                        wbv = wbf[i][:].rearrange("p (k c) -> p k c", k=KD)
                        OP('pool', lambda e: e.tensor_tensor(out=wt, in0=wv32, in1=mu_b, op=ALU.mult),
                           reads=[wstB[i], mubB], writes=[wtmpB])
                        OP('pool', lambda e: e.tensor_tensor(out=wbv[:, :, 0:128], in0=wv32, in1=wt, op=ALU.subtract),
                           reads=[wstB[i], wtmpB], writes=[wbfB[i], wbfB2[i][0], wbfB2[i][1]])
                        OP('pool', lambda e: e.tensor_copy(out=wbv[:, :, 128:256], in_=wt), reads=[wtmpB], writes=[wbfB[i], wbfB2[i][0], wbfB2[i][1]])
                    else:
                        cast_w(i, KD * 128, 'act')
                        wbv = wbf[i][:, 0:KD * 128].rearrange("p (k c) -> p k c", k=KD)
                    return (i, wbv)
                col0, ncols, shift = TMG[g]
                i2 = None
                wb2 = None
                if shift:
                    i2 = counters['w'] % 2
                    counters['w'] += 1
                    mc = col0 - RW0
                    mu_b = mub[:, mc:mc + 256].unsqueeze(1).broadcast_to([128, KD, 256])
                    wv32 = wst[i][:].rearrange("p (k c) -> p k c", k=KD)
                    wt = wtmp[:].rearrange("p (k c) -> p k c", k=KD)
                    wa = wbf[i][:].rearrange("p (k c) -> p k c", k=KD)
                    wb2 = wbf[i2][:].rearrange("p (k c) -> p k c", k=KD)
                    OP('pool', lambda e: e.tensor_tensor(out=wt, in0=wv32, in1=mu_b, op=ALU.mult),
                       reads=[wstB[i], mubB], writes=[wtmpB])
                    OP('pool', lambda e: e.tensor_tensor(out=wa, in0=wv32, in1=wt, op=ALU.subtract),
                       reads=[wstB[i], wtmpB], writes=[wbfB[i], wbfB2[i][0], wbfB2[i][1]])
                    OP('pool', lambda e: e.tensor_copy(out=wb2, in_=wt), reads=[wtmpB], writes=[wbfB[i2], wbfB2[i2][0], wbfB2[i2][1]])
                else:
                    cast_w_split(i, KD * 256)
                    wa = wbf[i][:].rearrange("p (k c) -> p k c", k=KD)
                return (i, i2, wa, wb2)

            def two_slot(d):
                return d[0] == 'tm' and bool(TMG[d[1]][2])

            def ensure_load(l, pos):
                plan = wplan['plan']
                if pos < len(plan) and pos not in wplan['loaded'] and not two_slot(plan[pos]):
                    wplan['loaded'][pos] = issue_load(l, plan[pos])

            def ensure_cast(l, pos):
                plan = wplan['plan']
                if pos < len(plan) and pos not in wplan['issued'] and not two_slot(plan[pos]):
                    ensure_load(l, pos)
                    wplan['issued'][pos] = issue_cast(l, plan[pos], wplan['loaded'].pop(pos))

            def acq(l, d):
                pos = wplan['pos']
                plan = wplan['plan']
                assert plan[pos] == d, (plan[pos], d)
                if pos not in wplan['issued']:
                    if pos not in wplan['loaded']:
                        wplan['loaded'][pos] = issue_load(l, d)
                    wplan['issued'][pos] = issue_cast(l, d, wplan['loaded'].pop(pos))
                info = wplan['issued'].pop(pos)
                wplan['pos'] = pos + 1
                if not (pos + 1 < len(plan) and two_slot(plan[pos + 1])):
                    ensure_cast(l, pos + 1)
                    if not (pos + 2 < len(plan) and two_slot(plan[pos + 2])):
                        ensure_load(l, pos + 2)
                return info

            def proj_fm(l, g, first_block):
                col0, ncols, shift = FMG[g]
                i, wbv = acq(l, ('fm', g))
                bk = pbank()
                nmm = KD * (2 if shift else 1)
                n = 0
                for k in range(KD):
                    n += 1
                    OP('pe', lambda e, k=k, n=n: e.matmul(PS[bk][0:ncols, 0:TBM], lhsT=wbv[:, k, 0:ncols], rhs=xn[:, k, 0:TBM],
                                                          start=(n == 1), stop=(n == nmm)),
                       reads=[wbfB[i], wbfB2[i][0], wbfB2[i][1], xnB[k]], writes=[PSB[bk]], sig=(n == nmm))
                if shift:
                    for k in range(KD):
                        n += 1
                        OP('pe', lambda e, k=k, n=n: e.matmul(PS[bk][0:ncols, 0:TBM], lhsT=wbv[:, k, 128:128 + ncols], rhs=xns[:, k, 0:TBM],
                                                              start=False, stop=(n == nmm)),
                           reads=[wbfB[i], wbfB2[i][0], wbfB2[i][1], xnsB], writes=[PSB[bk]], sig=(n == nmm))
                return bk

            def proj_tm(l, g):
                col0, ncols, shift = TMG[g]
                i, i2, wa, wb2 = acq(l, ('tm', g))
                for c in range(NCH):
                    bk = pbank()
                    nmm = KD * (2 if shift else 1)
                    n = 0
                    for k in range(KD):
                        n += 1
                        OP('pe', lambda e, k=k, n=n, c=c, bk=bk: e.matmul(PS[bk][0:64, 0:256], lhsT=xn[:, k, c * CH:(c + 1) * CH],
                                                                     rhs=wa[:, k, :], start=(n == 1), stop=(n == nmm)),
                           reads=[wbfB[i], wbfB2[i][0], wbfB2[i][1], xnB[k]], writes=[PSB[bk]], sig=(n == nmm))
                    if shift:
                        for k in range(KD):
                            n += 1
                            OP('pe', lambda e, k=k, n=n, c=c, bk=bk: e.matmul(PS[bk][0:64, 0:256], lhsT=xns[:, k, c * CH:(c + 1) * CH],
                                                                         rhs=wb2[:, k, :], start=False, stop=(n == nmm)),
                               reads=[wbfB[i2], wbfB2[i2][0], wbfB2[i2][1], xnsB], writes=[PSB[bk]], sig=(n == nmm))
                    OP('act', lambda e, c=c, bk=bk: e.activation(out=Vt[c][:], in_=PS[bk][0:64, 0:256], func=AF.Copy),
                       reads=[PSB[bk]], writes=[VtB[c]])

            def scan_decay(g_t, g_b):
                b_t, b_b = fa()
                OP('dve', lambda e: e.tensor_tensor_scan(out=b_t[:], data0=cst[:, SCM:SCM + TBM], data1=g_t[:], initial=0.0,
                                                         op0=ALU.mult, op1=ALU.add), reads=[g_b, cstB], writes=[b_b])
                return b_t, b_b

            def chunk_engine(mname, QE, KE, PC, rw=None):
                isrw = rw is not None
                if DEBUG_STAGE < 1:
                    return
                for c in range(NCH):
                    for (src, dst, dstB_) in ([(KE, KEt, KEtB)] + ([(rw['AE'], AEt, AEtB)] if isrw else [])):
                        for hp in range(2):
                            OP('pe', lambda e, hp=hp, c=c, src=src: e.transpose(PST[0:64, hp * 128:(hp + 1) * 128],
                                                                             src[hp][0][:, c * CH:(c + 1) * CH], ident[:]),
                               reads=[src[hp][1], identB], writes=[PSB[7]])
                        OP('act', lambda e, c=c, dst=dst: e.activation(out=dst[c][:], in_=PST[0:64, 0:256], func=AF.Copy),
                           reads=[PSB[7]], writes=[dstB_[c]])
                if DEBUG_STAGE < 2:
                    return
                for c in range(NCH):
                    cs = slice(c * CH, (c + 1) * CH)
                    def sc(lh, rh, cs=cs):
                        bk = pbank()
                        dst_ps = PS[bk][:, 0:256]
                        for h in range(4):
                            hp, r = h // 2, (h % 2) * 64
                            OP('pe', lambda e, h=h, hp=hp, r=r: e.matmul(dst_ps[0:64, h * 64:(h + 1) * 64], lhsT=lh[hp][0][r:r + 64, cs],
                                                                         rhs=rh[hp][0][r:r + 64, cs], start=True, stop=True),
                               reads=[lh[hp][1], rh[hp][1]], writes=[PSB[bk]], rg=r)
                        return bk
                    bk = sc(KE, QE)
                    OP('dve', lambda e, c=c, bk=bk: e.tensor_tensor(out=ATs[c][:], in0=PS[bk][0:64, 0:256], in1=cst[0:64, MI:MI + 256], op=ALU.mult),
                       reads=[PSB[bk], cstB], writes=[ATsB[c]])
                    if isrw:
                        bk = sc(KE, rw['BE'])
                        OP('dve', lambda e, c=c, bk=bk: e.tensor_tensor(out=LKs[c][:], in0=PS[bk][0:64, 0:256], in1=cst[0:64, MSU:MSU + 256], op=ALU.mult),
                           reads=[PSB[bk], cstB], writes=[LKsB[c]])
                        bk = sc(rw['AE'], QE)
                        OP('dve', lambda e, c=c, bk=bk: e.tensor_tensor(out=ARs[c][:], in0=PS[bk][0:64, 0:256], in1=cst[0:64, MI:MI + 256], op=ALU.mult),
                           reads=[PSB[bk], cstB], writes=[ARsB[c]])
                        bk = sc(rw['AE'], rw['BE'])
                        OP('dve', lambda e, c=c, bk=bk: e.scalar_tensor_tensor(out=NTn[c][:], in0=PS[bk][0:64, 0:256], scalar=-1.0,
                                                                         in1=cst[0:64, MSU:MSU + 256], op0=ALU.mult, op1=ALU.mult),
                           reads=[PSB[bk], cstB], writes=[NTnB[c]])
                        bk = sc(rw['BE'], rw['AE'])
                        OP('dve', lambda e, c=c, bk=bk: e.scalar_tensor_tensor(out=Nn[c][:], in0=PS[bk][0:64, 0:256], scalar=-1.0,
                                                                         in1=cst[0:64, MSL:MSL + 256], op0=ALU.mult, op1=ALU.mult),
                           reads=[PSB[bk], cstB], writes=[NnB[c]])
                        OP('pool', lambda e, c=c: e.tensor_tensor(out=Pn[c][:], in0=NTn[c][:], in1=cst[0:64, ID4:ID4 + 256], op=ALU.add),
                           reads=[NTnB[c], cstB], writes=[PnB[c]])
                if isrw:
                    curN, curNB, curNT, curNTB = Nn, NnB, NTn, NTnB
                    nxtN, nxtNB, nxtNT, nxtNTB = Nn2, Nn2B, NTn2, NTn2B
                    curP, curPB, nxtP, nxtPB = Pn, PnB, Pn2, Pn2B
                    for lev in range(1, 6):
                        for c in range(NCH):
                            bk = pbank()
                            for h in range(4):
                                hs = slice(h * 64, (h + 1) * 64)
                                OP('pe', lambda e, c=c, hs=hs, bk=bk, a=curNT, b=curN: e.matmul(PS[bk][0:64, hs], lhsT=a[c][:, hs], rhs=b[c][:, hs],
                                                                                         start=True, stop=True),
                                   reads=[curNTB[c], curNB[c]], writes=[PSB[bk]], rg=0)
                            if lev < 5:
                                for h in range(4):
                                    hs = slice(h * 64, (h + 1) * 64)
                                    hs2 = slice(256 + h * 64, 256 + (h + 1) * 64)
                                    OP('pe', lambda e, c=c, hs=hs, hs2=hs2, bk=bk, a=curN, b=curNT: e.matmul(PS[bk][0:64, hs2], lhsT=a[c][:, hs], rhs=b[c][:, hs],
                                                                                                     start=True, stop=True),
                                       reads=[curNTB[c], curNB[c]], writes=[PSB[bk]], rg=0)
                                OP('act', lambda e, c=c, bk=bk, d=nxtNT: e.activation(out=d[c][:], in_=PS[bk][0:64, 256:512], func=AF.Copy),
                                   reads=[PSB[bk]], writes=[nxtNTB[c]])
                            OP('act', lambda e, c=c, bk=bk, d=nxtN: e.activation(out=d[c][:], in_=PS[bk][0:64, 0:256], func=AF.Copy),
                               reads=[PSB[bk]], writes=[nxtNB[c]])
                        curN, curNB, nxtN, nxtNB = nxtN, nxtNB, curN, curNB
                        curNT, curNTB, nxtNT, nxtNTB = nxtNT, nxtNTB, curNT, curNTB
                        for c in range(NCH):
                            bk = pbank()
                            for h in range(4):
                                hs = slice(h * 64, (h + 1) * 64)
                                OP('pe', lambda e, c=c, hs=hs, bk=bk, a=curN, b=curP: e.matmul(PS[bk][0:64, hs], lhsT=a[c][:, hs], rhs=b[c][:, hs],
                                                                                        start=True, stop=True),
                                   reads=[curNB[c], curPB[c]], writes=[PSB[bk]], rg=0)
                            OP('dve', lambda e, c=c, bk=bk, s=curP, d=nxtP: e.tensor_tensor(out=d[c][:], in0=PS[bk][0:64, 0:256], in1=s[c][:], op=ALU.add),
                               reads=[PSB[bk], curPB[c]], writes=[nxtPB[c]])
                        curP, curPB, nxtP, nxtPB = nxtP, nxtPB, curP, curPB
                    TT, TTB = curP, curPB
                if DEBUG_STAGE < 3:
                    return
                S3, Sb, S3B, SbB = S32[mname], Sbf[mname], S32B[mname], SbfB[mname]
                for c in range(NCH):
                    cs = slice(c * CH, (c + 1) * CH)
                    if isrw:
                        for h in range(4):
                            hp, r = h // 2, (h % 2) * 64
                            hs = slice(h * 64, (h + 1) * 64)
                            OP('pe', lambda e, hp=hp, r=r, hs=hs, cs=cs: e.matmul(PS[6][0:64, hs], lhsT=rw['BE'][hp][0][r:r + 64, cs], rhs=Sb[hp][r:r + 64, :],
                                                                         start=True, stop=False),
                               reads=[rw['BE'][hp][1], SbB[hp]], writes=[PSB[6]], rg=r)
                            OP('pe', lambda e, hs=hs, c=c: e.matmul(PS[6][0:64, hs], lhsT=LKs[c][:, hs], rhs=Vt[c][:, hs], start=False, stop=True),
                               reads=[LKsB[c], VtB[c]], writes=[PSB[6]], rg=0)
                        OP('act', lambda e: e.activation(out=Ysb[:], in_=PS[6][0:64, 0:256], func=AF.Copy), reads=[PSB[6]], writes=[YsbB])
                        for h in range(4):
                            hs = slice(h * 64, (h + 1) * 64)
                            hs2 = slice(256 + h * 64, 256 + (h + 1) * 64)
                            OP('pe', lambda e, hs=hs, hs2=hs2, c=c: e.matmul(PS[6][0:64, hs2], lhsT=TT[c][:, hs], rhs=Ysb[:, hs], start=True, stop=True),
                               reads=[TTB[c], YsbB], writes=[PSB[6]], rg=0)
                        OP('act', lambda e: e.activation(out=Usb[:], in_=PS[6][0:64, 256:512], func=AF.Copy, scale=-1.0),
                           reads=[PSB[6]], writes=[UsbB])
                    for h in range(4):
                        hp, r = h // 2, (h % 2) * 64
                        hs = slice(h * 64, (h + 1) * 64)
                        ob = PS[4 + hp][r:r + 64, cs]
                        OP('pe', lambda e, ob=ob, hs=hs, c=c: e.matmul(ob, lhsT=Vt[c][:, hs], rhs=ATs[c][:, hs], start=True, stop=False),
                           reads=[VtB[c], ATsB[c]], writes=[PSB[4 + hp]], rg=0)
                        if isrw:
                            OP('pe', lambda e, ob=ob, hs=hs, c=c: e.matmul(ob, lhsT=Usb[:, hs], rhs=ARs[c][:, hs], start=False, stop=False),
                               reads=[UsbB, ARsB[c]], writes=[PSB[4 + hp]], rg=0)
                        OP('pe', lambda e, ob=ob, hp=hp, r=r, cs=cs: e.matmul(ob, lhsT=Sb[hp][r:r + 64, :], rhs=QE[hp][0][r:r + 64, cs], start=False, stop=True),
                           reads=[SbB[hp], QE[hp][1]], writes=[PSB[4 + hp]], rg=r)
                    for h in range(4):
                        hp, r = h // 2, (h % 2) * 64
                        hs = slice(h * 64, (h + 1) * 64)
                        sp_ = PS[7][r:r + 64, 256 + hp * 64:256 + (hp + 1) * 64]
                        OP('pe', lambda e, sp_=sp_, hs=hs, c=c: e.matmul(sp_, lhsT=KEt[c][:, hs], rhs=Vt[c][:, hs], start=True, stop=(not isrw)),
                           reads=[KEtB[c], VtB[c]], writes=[PSB[7]], rg=0)
                        if isrw:
                            OP('pe', lambda e, sp_=sp_, hs=hs, c=c: e.matmul(sp_, lhsT=AEt[c][:, hs], rhs=Usb[:, hs], start=False, stop=True),
                               reads=[AEtB[c], UsbB], writes=[PSB[7]], rg=0)
                    for hp in range(2):
                        pc = PC[hp][0][:, (c + 1) * CH - 1:(c + 1) * CH]
                        OP('dve', lambda e, hp=hp: e.tensor_tensor(out=stmp[hp][:], in0=PS[7][:, 256 + hp * 64:256 + (hp + 1) * 64], in1=S3[hp][:], op=ALU.add),
                           reads=[PSB[7], S3B[hp]], writes=[stmpB[hp]])
                        OP('dve', lambda e, hp=hp, pc=pc: e.tensor_scalar(out=S3[hp][:], in0=stmp[hp][:], scalar1=pc, scalar2=None, op0=ALU.mult),
                           reads=[stmpB[hp], PC[hp][1]], writes=[S3B[hp]])
                        OP('dve', lambda e, hp=hp, pc=pc: e.tensor_scalar(out=Sb[hp][:], in0=stmp[hp][:], scalar1=pc, scalar2=None, op0=ALU.mult),
                           reads=[stmpB[hp], PC[hp][1]], writes=[SbB[hp]])

            def evac_fm(bk, func=AF.Copy, scale=1.0, bias=None, dt='f', rows=128, dst=None):
                t, b = dst if dst is not None else (fa() if dt == 'f' else ba())
                kw = {}
                if bias is not None:
                    kw['bias'] = bias
                OP('act', lambda e: e.activation(out=t[0:rows, :], in_=PS[bk][0:rows, 0:TBM], func=func, scale=scale, **kw),
                   reads=[PSB[bk]] + ([pvB[0]] if bias is not None else []), writes=[b])
                return t, b

            def mixer_block(l, s, bi):
                first = (bi == 0)
                c0 = bi * TBM
                ti = c0 // TB
                plan = []
                if 'pool' in mixers:
                    plan += [('fm', 0), ('fm', 1)]
                if 'hgrn' in mixers:
                    for hp_ in range(2):
                        plan += [('fm', 2 + hp_), ('fm', 4 + hp_), ('fm', 6 + hp_)]
                    plan += [('tm', 0)]
                if 'gla' in mixers:
                    plan += [('fm', 22)]
                    for hp_ in range(2):
                        plan += [('fm', 16 + hp_), ('fm', 18 + hp_), ('fm', 20 + hp_)]
                    plan += [('tm', 2)]
                if 'rwkv' in mixers:
                    plan += [('fm', 14), ('fm', 15)]
                    for hp_ in range(2):
                        plan += [('fm', 8 + hp_), ('fm', 10 + hp_), ('fm', 12 + hp_)]
                    plan += [('tm', 1)]
                wplan['plan'] = plan
                wplan['pos'] = 0
                wplan['issued'] = {}
                wplan['loaded'] = {}
                if plan:
                    ensure_load(l, 0)
                    ensure_load(l, 1)
                    ensure_cast(l, 0)
                pv = pvec[l]
                if 'rwkv' in mixers:
                    if first:
                        OP('pool', lambda e: e.memset(xns[:, :, 0:2], 0.0), writes=[xnsB])
                    else:
                        OP('pool', lambda e: e.tensor_copy(out=xns[:, :, 0:1], in_=xn[:, :, TBM - 1:TBM]), reads=xnB, writes=[xnsB])
                rmsnorm_to_xn(l, PV['nmx'], c0, TBM, 0)
                if 'rwkv' in mixers:
                    OP('pool', lambda e: e.tensor_copy(out=xns[:, :, 1:TBM], in_=xn[:, :, 0:TBM - 1]), reads=xnB, writes=[xnsB])
                if 'pool' in mixers:
                    if first:
                        OP('pool', lambda e: e.memset(pext[:, :, 0:16], 0.0), writes=[pextB])
                    else:
                        OP('pool', lambda e: e.tensor_copy(out=pext[:, :, 0:16], in_=pext[:, :, TBM:TBM + 16]), reads=[pextB], writes=[pextB])
                    for ck in range(2):
                        bk = proj_fm(l, ck, first)
                        OP('act', lambda e, ck=ck, bk=bk: e.activation(out=pext[:, ck, 16:16 + TBM], in_=PS[bk][:, 0:TBM], func=AF.Copy),
                           reads=[PSB[bk]], writes=[pextB])
                    W_ = 16 + TBM
                    OP('dve', lambda e: e.tensor_tensor(out=pw[0][:, :, 1:W_], in0=pext[:, :, 1:W_], in1=pext[:, :, 0:W_ - 1], op=ALU.add),
                       reads=[pextB], writes=[pwB[0]])
                    def poolfin(src, ck, r0, wdw, first=first):
                        yt, yb = PB16[ck], PB16B[ck]
                        OP('dve', lambda e: e.scalar_tensor_tensor(out=yt[r0:r0 + 64, :], in0=src[r0:r0 + 64, ck, 16:16 + TBM], scalar=1.0 / wdw,
                                                                   in1=pext[r0:r0 + 64, ck, 16:16 + TBM], op0=ALU.mult, op1=ALU.subtract),
                           reads=[pwB[0], pwB[1], pextB], writes=[yb])
                        if first:
                            t2, b2 = FA[0], FAB[0]
                            OP('dve', lambda e: e.tensor_tensor(out=t2[r0:r0 + 64, 0:16], in0=src[r0:r0 + 64, ck, 16:32],
                                                                in1=cst[r0:r0 + 64, ICN + ck * 16:ICN + ck * 16 + 16], op=ALU.mult),
                               reads=[pwB[0], pwB[1], cstB], writes=[b2])
                            OP('dve', lambda e: e.tensor_tensor(out=yt[r0:r0 + 64, 0:16], in0=t2[r0:r0 + 64, 0:16],
                                                                in1=pext[r0:r0 + 64, ck, 16:32], op=ALU.subtract),
                               reads=[b2, pextB], writes=[yb])
                    poolfin(pw[0], 0, 0, 2)
                    OP('dve', lambda e: e.tensor_tensor(out=pw[1][:, :, 3:W_], in0=pw[0][:, :, 3:W_], in1=pw[0][:, :, 1:W_ - 2], op=ALU.add),
                       reads=[pwB[0]], writes=[pwB[1]])
                    poolfin(pw[1], 0, 64, 4)
                    OP('dve', lambda e: e.tensor_tensor(out=pw[0][:, :, 7:W_], in0=pw[1][:, :, 7:W_], in1=pw[1][:, :, 3:W_ - 4], op=ALU.add),
                       reads=[pwB[1]], writes=[pwB[0]])
                    poolfin(pw[0], 1, 0, 8)
                    OP('dve', lambda e: e.tensor_tensor(out=pw[1][:, :, 15:W_], in0=pw[0][:, :, 15:W_], in1=pw[0][:, :, 7:W_ - 8], op=ALU.add),
                       reads=[pwB[0]], writes=[pwB[1]])
                    poolfin(pw[1], 1, 64, 16)
                    for ck in range(2):
                        bk = pbank()
                        OP('pe', lambda e, ck=ck, bk=bk: e.matmul(PS[bk][:, 0:TBM], lhsT=smatb[:, ck * 128:(ck + 1) * 128], rhs=PB16[ck][:], start=True, stop=True),
                           reads=[PB16B[ck], smatB], writes=[PSB[bk]])
                        OP('dve', lambda e, ck=ck, bk=bk: e.tensor_scalar(out=YT[:, ck, :], in0=PS[bk][:, 0:TBM], scalar1=pv[:, PV['pool_b'] + ck:PV['pool_b'] + ck + 1],
                                                                      scalar2=pv[:, PV['pool_s'] + ck:PV['pool_s'] + ck + 1], op0=ALU.add, op1=ALU.mult),
                           reads=[PSB[bk], pvB[l]], writes=[YB[ck]])
                else:
                    for ck in range(2):
                        OP('pool', lambda e, ck=ck: e.memset(YT[:, ck, :], 0.0), writes=[YB[ck]])

                def out_rstd(Ot, lhs_ones, n_ch, eps):
                    bk = pbank()
                    for hp in range(2):
                        st, sbb = ba()
                        OP('act', lambda e, hp=hp, st=st, Ot=Ot: e.activation(out=st[:], in_=Ot[hp][0][:], func=AF.Square), reads=[Ot[hp][1]], writes=[sbb])
                        if lhs_ones is ones:
                            OP('pe', lambda e, hp=hp, st=st: e.matmul(PS[bk][:, 0:TBM], lhsT=ones[:], rhs=st[:], start=(hp == 0), stop=(hp == 1)),
                               reads=[sbb, onesB], writes=[PSB[bk]])
                        else:
                            bk2 = bk if hp == 0 else pbank()
                            OP('pe', lambda e, hp=hp, st=st, bk2=bk2: e.matmul(PS[bk2][:, 0:TBM], lhsT=bones[:], rhs=st[:], start=True, stop=True),
                               reads=[sbb, bonesB], writes=[PSB[bk2]])
                            if hp == 0:
                                bk0 = bk2
                            else:
                                bk1 = bk2
                    if lhs_ones is ones:
                        rt, rb = fa()
                        rstd_from(PS[bk], PSB[bk], TBM, 1.0 / n_ch, eps, rt, rb)
                        return [(rt, rb), (rt, rb)]
                    res = []
                    for bkx in (bk0, bk1):
                        rt, rb = fa()
                        rstd_from(PS[bkx], PSB[bkx], TBM, 1.0 / n_ch, eps, rt, rb)
                        res.append((rt, rb))
                    return res

                def evac_O():
                    Ot = []
                    for hp in range(2):
                        t, b = fa()
                        OP('act', lambda e, hp=hp, t=t: e.activation(out=t[:], in_=PS[4 + hp][:, 0:TBM], func=AF.Copy), reads=[PSB[4 + hp]], writes=[b])
                        Ot.append((t, b))
                    return Ot

                if 'hgrn' in mixers:
                    QE, KE, PCx, GT = [], [], [], []
                    for hp in range(2):
                        bq = proj_fm(l, 2 + hp, first)
                        qt, qb = evac_fm(bq, AF.Silu)
                        bf_ = proj_fm(l, 4 + hp, first)
                        st_, sb_ = evac_fm(bf_, AF.Sigmoid)
                        ft, fb = fa()
                        OP('dve', lambda e, hp=hp, st_=st_, ft=ft: e.tensor_scalar(out=ft[:], in0=st_[:], scalar1=lbt[:, 2 * l + hp:2 * l + hp + 1],
                                                                             scalar2=lbt[:, 4 + 2 * l + hp:4 + 2 * l + hp + 1], op0=ALU.mult, op1=ALU.add),
                           reads=[sb_, lbtB], writes=[fb])
                        lt, lb_ = fa()
                        OP('dve', lambda e, ft=ft, lt=lt: e.tensor_scalar_max(out=lt[:], in0=ft[:], scalar1=1e-30), reads=[fb], writes=[lb_])
                        OP('act', lambda e, lt=lt: e.activation(out=lt[:], in_=lt[:], func=AF.Ln), reads=[lb_], writes=[lb_])
                        bt, bb = scan_decay(lt, lb_)
                        ebt, ebb = LL[hp][0], LLB_[hp][0]
                        OP('act', lambda e, bt=bt, ebt=ebt: e.activation(out=ebt[:], in_=bt[:], func=AF.Exp), reads=[bb], writes=[ebb])
                        OP('act', lambda e, bt=bt: e.activation(out=bt[:], in_=bt[:], func=AF.Exp, scale=-1.0), reads=[bb], writes=[bb])
                        qe, qeb = LB[hp][0], LBB[hp][0]
                        OP('dve', lambda e, qt=qt, ebt=ebt, qe=qe: e.scalar_tensor_tensor(out=qe[:], in0=qt[:], scalar=QK, in1=ebt[:], op0=ALU.mult, op1=ALU.mult),
                           reads=[qb, ebb], writes=[qeb])
                        OP('dve', lambda e, ft=ft: e.tensor_scalar(out=ft[:], in0=ft[:], scalar1=-1.0, scalar2=1.0, op0=ALU.mult, op1=ALU.add),
                           reads=[fb], writes=[fb])
                        ke, keb = LB[hp][1], LBB[hp][1]
                        OP('dve', lambda e, ft=ft, bt=bt, ke=ke: e.tensor_tensor(out=ke[:], in0=ft[:], in1=bt[:], op=ALU.mult), reads=[fb, bb], writes=[keb])
                        bg_ = proj_fm(l, 6 + hp, first)
                        gt, gb = evac_fm(bg_, AF.Sigmoid, dst=(LL[hp][1], LLB_[hp][1]))
                        QE.append((qe, qeb)); KE.append((ke, keb)); PCx.append((ebt, ebb)); GT.append((gt, gb))
                    proj_tm(l, 0)
                    chunk_engine('hgrn', QE, KE, PCx)
                    Ot = evac_O()
                    rs = out_rstd(Ot, ones, 256, NORM_EPS)
                    for hp in range(2):
                        t1, b1 = fa()
                        OP('dve', lambda e, hp=hp, t1=t1, Ot=Ot, rs=rs: e.scalar_tensor_tensor(out=t1[:], in0=Ot[hp][0][:], scalar=pv[:, PV['hnorm'] + hp:PV['hnorm'] + hp + 1],
                                                                           in1=rs[hp][0][:], op0=ALU.mult, op1=ALU.mult),
                           reads=[Ot[hp][1], rs[hp][1], pvB[l]], writes=[b1])
                        OP('dve', lambda e, hp=hp, t1=t1, GT=GT: e.tensor_tensor(out=YT[:, 2 + hp, :], in0=t1[:], in1=GT[hp][0][:], op=ALU.mult),
                           reads=[b1, GT[hp][1]], writes=[YB[2 + hp]])
                else:
                    for ck in (2, 3):
                        OP('pool', lambda e, ck=ck: e.memset(YT[:, ck, :], 0.0), writes=[YB[ck]])

                if 'gla' in mixers:
                    bga = proj_fm(l, 22, first)
                    gat, gab = LLX[0], LLXB[0]
                    OP('act', lambda e: e.activation(out=gat[:], in_=PS[bga][:, 0:TBM], func=AF.Copy), reads=[PSB[bga]], writes=[gab])
                    QE, KE, PCx, GT = [], [], [], []
                    for hp in range(2):
                        bk = pbank()
                        OP('pe', lambda e, hp=hp, bk=bk: e.matmul(PS[bk][:, 0:TBM], lhsT=smat[:, 1024 + hp * 128:1024 + (hp + 1) * 128], rhs=gat[:], start=True, stop=True),
                           reads=[gab, smatB], writes=[PSB[bk]])
                        lt, lb_ = evac_fm(bk, AF.Sigmoid, bias=pv[:, PV['glab'] + hp:PV['glab'] + hp + 1])
                        OP('act', lambda e, lt=lt: e.activation(out=lt[:], in_=lt[:], func=AF.Ln), reads=[lb_], writes=[lb_])
                        bt, bb = scan_decay(lt, lb_)
                        ebt, ebb = LL[hp][0], LLB_[hp][0]
                        OP('act', lambda e, bt=bt, ebt=ebt: e.activation(out=ebt[:], in_=bt[:], func=AF.Exp, scale=1.0 / 16), reads=[bb], writes=[ebb])
                        OP('act', lambda e, bt=bt: e.activation(out=bt[:], in_=bt[:], func=AF.Exp, scale=-1.0 / 16), reads=[bb], writes=[bb])
                        bq = proj_fm(l, 16 + hp, first)
                        qe, qeb = LB[hp][0], LBB[hp][0]
                        OP('dve', lambda e, bq=bq, ebt=ebt, qe=qe: e.scalar_tensor_tensor(out=qe[:], in0=PS[bq][:, 0:TBM], scalar=QK, in1=ebt[:], op0=ALU.mult, op1=ALU.mult),
                           reads=[PSB[bq], ebb], writes=[qeb])
                        bkk = proj_fm(l, 18 + hp, first)
                        ke, keb = LB[hp][1], LBB[hp][1]
                        OP('dve', lambda e, bkk=bkk, bt=bt, ke=ke: e.tensor_tensor(out=ke[:], in0=PS[bkk][:, 0:TBM], in1=bt[:], op=ALU.mult), reads=[PSB[bkk], bb], writes=[keb])
                        bg_ = proj_fm(l, 20 + hp, first)
                        gt, gb = evac_fm(bg_, AF.Silu, dst=(LL[hp][1], LLB_[hp][1]))
                        QE.append((qe, qeb)); KE.append((ke, keb)); PCx.append((ebt, ebb)); GT.append((gt, gb))
                    proj_tm(l, 2)
                    chunk_engine('gla', QE, KE, PCx)
                    Ot = evac_O()
                    rs = out_rstd(Ot, bones, 64, NORM_EPS)
                    for hp in range(2):
                        t1, b1 = fa()
                        OP('dve', lambda e, hp=hp, t1=t1, Ot=Ot, rs=rs: e.scalar_tensor_tensor(out=t1[:], in0=Ot[hp][0][:], scalar=pv[:, PV['gnorm'] + hp:PV['gnorm'] + hp + 1],
                                                                           in1=rs[hp][0][:], op0=ALU.mult, op1=ALU.mult),
                           reads=[Ot[hp][1], rs[hp][1], pvB[l]], writes=[b1])
                        OP('dve', lambda e, hp=hp, t1=t1, GT=GT: e.tensor_tensor(out=YT[:, 6 + hp, :], in0=t1[:], in1=GT[hp][0][:], op=ALU.mult),
                           reads=[b1, GT[hp][1]], writes=[YB[6 + hp]])
                else:
                    for ck in (6, 7):
                        OP('pool', lambda e, ck=ck: e.memset(YT[:, ck, :], 0.0), writes=[YB[ck]])

                if 'rwkv' in mixers:
                    bwa = proj_fm(l, 14, first)
                    twa, twab = LLX[0], LLXB[0]
                    OP('act', lambda e: e.activation(out=twa[0:64, :], in_=PS[bwa][0:64, 0:TBM], func=AF.Tanh), reads=[PSB[bwa]], writes=[twab])
                    OP('act', lambda e: e.activation(out=twa[64:128, :], in_=PS[bwa][64:128, 0:TBM], func=AF.Copy), reads=[PSB[bwa]], writes=[twab])
                    bxg = proj_fm(l, 15, first)
                    sxg, sxgb = evac_fm(bxg, AF.Sigmoid, dst=(LLX[1], LLXB[1]))
                    QE, KE, AE, BE, PCx, GR, RKR, VF = [], [], [], [], [], [], [], []
                    for hp in range(2):
                        cs_ = slice(hp * 128, (hp + 1) * 128)
                        bk = pbank()
                        OP('pe', lambda e, bk=bk, hp=hp: e.matmul(PS[bk][:, 0:TBM], lhsT=smat[:, 256 + hp * 128:256 + (hp + 1) * 128], rhs=twa[:], start=True, stop=True),
                           reads=[twab, smatB], writes=[PSB[bk]])
                        lw, lwb = evac_fm(bk, AF.Sigmoid, bias=pv[:, PV['w0'] + hp:PV['w0'] + hp + 1])
                        bk = pbank()
                        OP('pe', lambda e, bk=bk, hp=hp: e.matmul(PS[bk][:, 0:TBM], lhsT=smat[:, 768 + hp * 128:768 + (hp + 1) * 128], rhs=twa[:], start=True, stop=True),
                           reads=[twab, smatB], writes=[PSB[bk]])
                        at, ab_ = evac_fm(bk, AF.Sigmoid, bias=pv[:, PV['a0'] + hp:PV['a0'] + hp + 1])
                        bk = pbank()
                        OP('pe', lambda e, bk=bk, hp=hp: e.matmul(PS[bk][:, 0:TBM], lhsT=smat[:, 512 + hp * 128:512 + (hp + 1) * 128], rhs=sxg[:], start=True, stop=True),
                           reads=[sxgb, smatB], writes=[PSB[bk]])
                        grt, grb = evac_fm(bk, AF.Copy, dst=(LL[hp][1], LLB_[hp][1]))
                        bt, bb = scan_decay(lw, lwb)
                        CW = -float(np.exp(-0.5))
                        ebt, ebb = LL[hp][0], LLB_[hp][0]
                        OP('act', lambda e, bt=bt, ebt=ebt: e.activation(out=ebt[:], in_=bt[:], func=AF.Exp, scale=CW), reads=[bb], writes=[ebb])
                        enb, enbb = fa()
                        OP('act', lambda e, bt=bt, enb=enb: e.activation(out=enb[:], in_=bt[:], func=AF.Exp, scale=-CW), reads=[bb], writes=[enbb])
                        OP('dve', lambda e, bt=bt, lw=lw: e.tensor_tensor(out=bt[:], in0=bt[:], in1=lw[:], op=ALU.subtract), reads=[bb, lwb], writes=[bb])
                        OP('act', lambda e, bt=bt: e.activation(out=bt[:], in_=bt[:], func=AF.Exp, scale=CW), reads=[bb], writes=[bb])
                        br = proj_fm(l, 8 + hp, first)
                        rt, rb = evac_fm(br, AF.Copy)
                        bkr = proj_fm(l, 10 + hp, first)
                        kt, kb = evac_fm(bkr, AF.Copy)
                        bv = proj_fm(l, 12 + hp, first)
                        vt, vb = evac_fm(bv, AF.Copy, dst=(LL[hp][2], LLB_[hp][2]))
                        kkt, kkb = fa()
                        OP('dve', lambda e, kt=kt, kkt=kkt, hp=hp: e.tensor_scalar(out=kkt[:], in0=kt[:], scalar1=pv[:, PV['kk'] + hp:PV['kk'] + hp + 1], scalar2=None, op0=ALU.mult),
                           reads=[kb, pvB[l]], writes=[kkb])
                        sq_, sqb_ = ba()
                        OP('act', lambda e, kkt=kkt, sq_=sq_: e.activation(out=sq_[:], in_=kkt[:], func=AF.Square), reads=[kkb], writes=[sqb_])
                        bk = pbank()
                        OP('pe', lambda e, bk=bk, sq_=sq_: e.matmul(PS[bk][:, 0:TBM], lhsT=bones[:], rhs=sq_[:], start=True, stop=True), reads=[sqb_, bonesB], writes=[PSB[bk]])
                        rn, rnb = fa()
                        rstd_from(PS[bk], PSB[bk], TBM, 1.0, 1e-24, rn, rnb)
                        OP('dve', lambda e, kkt=kkt, rn=rn: e.tensor_tensor(out=kkt[:], in0=kkt[:], in1=rn[:], op=ALU.mult), reads=[kkb, rnb], writes=[kkb])
                        fac, facb = fa()
                        OP('dve', lambda e, at=at, fac=fac, hp=hp: e.tensor_scalar(out=fac[:], in0=at[:], scalar1=-1.0, scalar2=pv[:, PV['ka'] + hp:PV['ka'] + hp + 1], op0=ALU.add, op1=ALU.mult),
                           reads=[ab_, pvB[l]], writes=[facb])
                        OP('dve', lambda e, fac=fac, kt=kt: e.scalar_tensor_tensor(out=kt[:], in0=fac[:], scalar=1.0, in1=kt[:], op0=ALU.add, op1=ALU.mult),
                           reads=[facb, kb], writes=[kb])
                        rk_, rkb_ = LB[hp][4], LBB[hp][4]
                        OP('dve', lambda e, rt=rt, kt=kt, rk_=rk_, hp=hp: e.scalar_tensor_tensor(out=rk_[:], in0=rt[:], scalar=pv[:, PV['rk'] + hp:PV['rk'] + hp + 1], in1=kt[:], op0=ALU.mult, op1=ALU.mult),
                           reads=[rb, kb, pvB[l]], writes=[rkb_])
                        qe, qeb = LB[hp][0], LBB[hp][0]
                        OP('dve', lambda e, rt=rt, ebt=ebt, qe=qe: e.tensor_tensor(out=qe[:], in0=rt[:], in1=ebt[:], op=ALU.mult), reads=[rb, ebb], writes=[qeb])
                        ke, keb = LB[hp][1], LBB[hp][1]
                        OP('dve', lambda e, kt=kt, enb=enb, ke=ke: e.tensor_tensor(out=ke[:], in0=kt[:], in1=enb[:], op=ALU.mult), reads=[kb, enbb], writes=[keb])
                        be, beb = LB[hp][3], LBB[hp][3]
                        OP('dve', lambda e, kkt=kkt, bt=bt, be=be: e.tensor_tensor(out=be[:], in0=kkt[:], in1=bt[:], op=ALU.mult), reads=[kkb, bb], writes=[beb])
                        OP('dve', lambda e, kkt=kkt, at=at: e.tensor_tensor(out=kkt[:], in0=kkt[:], in1=at[:], op=ALU.mult), reads=[kkb, ab_], writes=[kkb])
                        ae, aeb = LB[hp][2], LBB[hp][2]
                        OP('dve', lambda e, kkt=kkt, enb=enb, ae=ae: e.tensor_tensor(out=ae[:], in0=kkt[:], in1=enb[:], op=ALU.mult), reads=[kkb, enbb], writes=[aeb])
                        QE.append((qe, qeb)); KE.append((ke, keb)); AE.append((ae, aeb)); BE.append((be, beb)); PCx.append((ebt, ebb))
                        GR.append((grt, grb)); RKR.append((rk_, rkb_)); VF.append((vt, vb))
                    proj_tm(l, 1)
                    chunk_engine('rwkv', QE, KE, PCx, rw={'AE': AE, 'BE': BE})
                    Ot = evac_O()
                    for hp in range(2):
                        ob16, ob16b = ba()
                        OP('dve', lambda e, hp=hp, ob16=ob16, Ot=Ot: e.tensor_copy(out=ob16[:], in_=Ot[hp][0][:]), reads=[Ot[hp][1]], writes=[ob16b])
                        bk = pbank()
                        OP('pe', lambda e, bk=bk, ob16=ob16: e.matmul(PS[bk][:, 0:TBM], lhsT=bones[:], rhs=ob16[:], start=True, stop=True), reads=[ob16b, bonesB], writes=[PSB[bk]])
                        ct, cb = fa()
                        OP('dve', lambda e, hp=hp, bk=bk, ct=ct, Ot=Ot: e.scalar_tensor_tensor(out=ct[:], in0=PS[bk][:, 0:TBM], scalar=-1.0 / 64, in1=Ot[hp][0][:], op0=ALU.mult, op1=ALU.add),
                           reads=[PSB[bk], Ot[hp][1]], writes=[cb])
                        s2, s2b = ba()
                        OP('act', lambda e, ct=ct, s2=s2: e.activation(out=s2[:], in_=ct[:], func=AF.Square), reads=[cb], writes=[s2b])
                        bk = pbank()
                        OP('pe', lambda e, bk=bk, s2=s2: e.matmul(PS[bk][:, 0:TBM], lhsT=bones[:], rhs=s2[:], start=True, stop=True), reads=[s2b, bonesB], writes=[PSB[bk]])
                        rn, rnb = fa()
                        rstd_from(PS[bk], PSB[bk], TBM, 1.0 / 64, GN_EPS, rn, rnb)
                        OP('dve', lambda e, hp=hp, ct=ct, rn=rn: e.scalar_tensor_tensor(out=ct[:], in0=ct[:], scalar=pv[:, PV['lnw'] + hp:PV['lnw'] + hp + 1], in1=rn[:], op0=ALU.mult, op1=ALU.mult),
                           reads=[cb, rnb, pvB[l]], writes=[cb])
                        bk = pbank()
                        OP('pe', lambda e, bk=bk, hp=hp, RKR=RKR: e.matmul(PS[bk][:, 0:TBM], lhsT=bones[:], rhs=RKR[hp][0][:], start=True, stop=True), reads=[RKR[hp][1], bonesB], writes=[PSB[bk]])
                        bo, bob = fa()
                        OP('dve', lambda e, bk=bk, hp=hp, bo=bo, VF=VF: e.tensor_tensor(out=bo[:], in0=PS[bk][:, 0:TBM], in1=VF[hp][0][:], op=ALU.mult), reads=[PSB[bk], VF[hp][1]], writes=[bob])
                        OP('dve', lambda e, hp=hp, ct=ct, bo=bo: e.scalar_tensor_tensor(out=ct[:], in0=ct[:], scalar=pv[:, PV['lnb'] + hp:PV['lnb'] + hp + 1], in1=bo[:], op0=ALU.add, op1=ALU.add),
                           reads=[cb, bob, pvB[l]], writes=[cb])
                        OP('dve', lambda e, hp=hp, ct=ct, GR=GR: e.tensor_tensor(out=YT[:, 4 + hp, :], in0=ct[:], in1=GR[hp][0][:], op=ALU.mult),
                           reads=[cb, GR[hp][1]], writes=[YB[4 + hp]])
                else:
                    for ck in (4, 5):
                        OP('pool', lambda e, ck=ck: e.memset(YT[:, ck, :], 0.0), writes=[YB[ck]])

                nxt_o = load_o(m_wout[l][0], 'act')
                for j in range(KD):
                    i = nxt_o
                    if j + 1 < KD:
                        nxt_o = load_o(m_wout[l][j + 1], 'act')
                    for m in range(KD):
                        OP('pe', lambda e, m=m, j=j, i=i: e.matmul(PS[m][:, 0:TBM], lhsT=obf[i][:, m * 128:(m + 1) * 128], rhs=YT[:, j, :],
                                                               start=(j == 0), stop=(j == KD - 1)),
                           reads=[obfB[i], YB[j]], writes=[PSB[m]], sig=(m == KD - 1 or j == KD - 1))
                for m in range(KD):
                    OP('dve', lambda e, m=m: e.tensor_tensor(out=X[:, m, c0:c0 + TBM], in0=PS[m][:, 0:TBM], in1=X[:, m, c0:c0 + TBM], op=ALU.add),
                       reads=[PSB[m], XB[m][ti]], writes=[XB[m][ti]])

            def mixer_setup(l, s):
                OP('sp', lambda e: e.dma_start(out=mub[:], in_=mub_d[l]), writes=[mubB], dsem=muS)
                OP('sp', lambda e: e.dma_start(out=smat[:], in_=smat_d[l]), writes=[smatB], dsem=smS)
                OP('pool', lambda e: e.tensor_copy(out=smatb[:], in_=smat[:, 0:256]), reads=[smatB], writes=[smatB])
                for m in ('hgrn', 'gla', 'rwkv'):
                    for hp in range(2):
                        OP('pool', lambda e, m=m, hp=hp: e.memset(S32[m][hp][:], 0.0), writes=[S32B[m][hp]])
                        OP('pool', lambda e, m=m, hp=hp: e.memset(Sbf[m][hp][:], 0.0), writes=[SbfB[m][hp]])

        if do_mix:
            OP('act', lambda e: e.activation(out=lbt[:, 0:2], in_=pvec[0][:, PV['lb0']:PV['lb0'] + 2], func=AF.Exp), reads=[pvB[0]], writes=[lbtB])
            OP('act', lambda e: e.activation(out=lbt[:, 2:4], in_=pvec[L - 1][:, PV['lbl']:PV['lbl'] + 2], func=AF.Exp), reads=[pvB[L - 1], lbtB], writes=[lbtB])
            OP('dve', lambda e: e.tensor_tensor(out=lbt[:, 4:6], in0=lbt[:, 0:2], in1=lbt[:, 2:4], op=ALU.add), reads=[lbtB], writes=[lbtB])
            OP('dve', lambda e: e.reciprocal(out=lbt[:, 4:6], in_=lbt[:, 4:6]), reads=[lbtB], writes=[lbtB])
            OP('dve', lambda e: e.tensor_tensor(out=lbt[:, 6:8], in0=lbt[:, 2:4], in1=lbt[:, 4:6], op=ALU.mult), reads=[lbtB], writes=[lbtB])
            OP('dve', lambda e: e.memset(lbt[:, 4:6], 0.0), reads=[lbtB], writes=[lbtB])
            OP('dve', lambda e: e.tensor_scalar(out=lbt[:, 0:4], in0=lbt[:, 4:8], scalar1=-1.0, scalar2=1.0, op0=ALU.mult, op1=ALU.add), reads=[lbtB], writes=[lbtB])

        for s in range(NS):
            for k in range(KD):
                OP('sp', lambda e, k=k, s=s: e.dma_start(out=X[:, k, :], in_=xT[s, :, k, :]), writes=XB[k], dsem=xS[k])
            for l in range(L):
                if do_ffn:
                    for ti in range(NT):
                        ffn(l, 0, ti)
                if do_mix:
                    mixer_setup(l, s)
                    for bi in range(T // TBM):
                        mixer_block(l, s, bi)
                if do_ffn:
                    for ti in range(NT):
                        ffn(l, 1, ti)
            for ti in range(NT):
                c0 = ti * TB
                for k in range(KD):
                    i = counters['sq'] % 2
                    counters['sq'] += 1
                    OP('act', lambda e, k=k, i=i, c0=c0: e.activation(out=sq[i][:], in_=X[:, k, c0:c0 + TB], func=AF.Square),
                       reads=[XB[k][ti]], writes=[sqB[i]])
                    OP('pe', lambda e, k=k, i=i: e.matmul(PS[0][:, :], lhsT=ones[:], rhs=sq[i][:], start=(k == 0), stop=(k == KD - 1)),
                       reads=[sqB[i], onesB], writes=[PSB[0]])
                rstd_from(PS[0], PSB[0], TB, 1.0 / D, NORM_EPS, rstd, rstdB)
                for k in range(KD):
                    OP('dve', lambda e, k=k, c0=c0: e.scalar_tensor_tensor(out=X[:, k, c0:c0 + TB], in0=X[:, k, c0:c0 + TB],
                                                                        scalar=pvec[0][:, PV['nfin'] + k:PV['nfin'] + k + 1], in1=rstd[:],
                                                                        op0=ALU.mult, op1=ALU.mult),
                       reads=[XB[k][ti], rstdB, pvB[0]], writes=[XB[k][ti]])
            for k in range(KD):
                OP('sp', lambda e, k=k, s=s: e.dma_start(out=outT[s, :, k, :], in_=X[:, k, :]), reads=XB[k], writes=XB[k], dsem=oS[k])
        pg.ops['sp'].append((lambda e: e.nop(), {o_: o_.count for o_ in oS}, False, None))
        pg.emit(block, esems)
    return nc, pg


def _col(v, n):
    return np.ascontiguousarray(np.asarray(v, np.float32).reshape(n, 128).T)


def make_consts():
    cst = np.zeros((128, 1600), np.float32)
    p = np.arange(64)[:, None]
    f = np.arange(64)[None, :]
    for h in range(4):
        cst[0:64, 0 + h * 64:0 + (h + 1) * 64] = (p <= f)
        cst[0:64, 256 + h * 64:256 + (h + 1) * 64] = (p < f)
        cst[0:64, 512 + h * 64:512 + (h + 1) * 64] = (f < p)
        cst[0:64, 768 + h * 64:768 + (h + 1) * 64] = (p == f)
    scm = np.ones(512, np.float32)
    scm[::64] = 0.0
    cst[:, 1024:1536] = scm[None, :]
    wins = {(0, 0): 2, (0, 1): 4, (1, 0): 8, (1, 1): 16}
    for ck in range(2):
        for half in range(2):
            w = wins[(ck, half)]
            t = np.arange(16)
            cst[half * 64:(half + 1) * 64, 1536 + ck * 16:1536 + ck * 16 + 16] = (1.0 / np.minimum(t + 1, w))[None, :]
    return cst


def prep_weights(inp, L):
    out = {}
    f32 = np.float32
    for l in range(L):
        for w, (wi, wo) in enumerate((('ffn1_w_in', 'ffn1_w_out'), ('ffn2_w_in', 'ffn2_w_out'))):
            W = np.asarray(inp[wi][l], f32)
            Wk = W.reshape(KD, 128, 2 * FF)
            g = Wk[:, :, :FF].reshape(KD, 128, NJ, 128)
            u = Wk[:, :, FF:].reshape(KD, 128, NJ, 128)
            blk = np.concatenate([g, u], axis=3)
            out[f"f{w + 1}_win{l}"] = np.ascontiguousarray(blk.transpose(2, 1, 0, 3)).reshape(NJ, 128, KD * 256)
            out[f"f{w + 1}_wout{l}"] = np.ascontiguousarray(np.asarray(inp[wo][l], f32).reshape(NJ, 128, D))
        W = np.asarray(inp['w_in'][l], f32).reshape(KD, 128, DIN)
        fm = np.zeros((len(FMG), 128, KD, 128), f32)
        for gi, (c0, nc_, _) in enumerate(FMG):
            nc_ = min(nc_, DIN - c0)
            fm[gi, :, :, :nc_] = W[:, :, c0:c0 + nc_].transpose(1, 0, 2)
        out[f"m_fm{l}"] = fm.reshape(len(FMG), 128, KD * 128)
        tm = np.zeros((len(TMG), 128, KD, 256), f32)
        for gi, (c0, nc_, _) in enumerate(TMG):
            tm[gi] = W[:, :, c0:c0 + nc_].transpose(1, 0, 2)
        out[f"m_tm{l}"] = tm.reshape(len(TMG), 128, KD * 256)
        out[f"m_wout{l}"] = np.ascontiguousarray(np.asarray(inp['w_out'][l], f32).reshape(KD, 128, D))
        pv = np.zeros((128, NPV), f32)
        pv[:, PV['nf1']:PV['nf1'] + 8] = _col(inp['norm_ffn1'][l], 8)
        pv[:, PV['nmx']:PV['nmx'] + 8] = _col(inp['norm_mix'][l], 8)
        pv[:, PV['nf2']:PV['nf2'] + 8] = _col(inp['norm_ffn2'][l], 8)
        pv[:, PV['nfin']:PV['nfin'] + 8] = _col(inp['norm_final'], 8)
        pv[:, PV['lb0']:PV['lb0'] + 2] = _col(inp['hgrn_lb_logits'][0], 2)
        pv[:, PV['lbl']:PV['lbl'] + 2] = _col(inp['hgrn_lb_logits'][l], 2)
        for nm, key in (('pool_b', 'pool_b'), ('pool_s', 'pool_scale'), ('hnorm', 'hgrn_norm'), ('w0', 'rwkv_w0'), ('a0', 'rwkv_a0'),
                        ('kk', 'rwkv_k_k'), ('ka', 'rwkv_k_a'), ('rk', 'rwkv_r_k'), ('lnw', 'rwkv_ln_w'), ('lnb', 'rwkv_ln_b'),
                        ('glab', 'gla_b'), ('gnorm', 'gla_norm')):
            pv[:, PV[nm]:PV[nm] + 2] = _col(inp[key][l], 2)
        out[f"pvec{l}"] = pv
        out[f"mub{l}"] = np.ascontiguousarray(np.broadcast_to(np.asarray(inp['rwkv_mu'][l], f32)[None, :], (128, 1024)))
        sm = np.zeros((128, 5 * 256), f32)
        pw_ = np.asarray(inp['pool_w'][l], f32)
        for ck in range(2):
            sm[0:64, ck * 128:ck * 128 + 64] = pw_[2 * ck]
            sm[64:128, ck * 128 + 64:ck * 128 + 128] = pw_[2 * ck + 1]
        sm[0:64, 256:512] = np.asarray(inp['rwkv_w2'][l], f32)
        sm[64:128, 768:1024] = np.asarray(inp['rwkv_a2'][l], f32)
        sm[:, 512:768] = np.asarray(inp['rwkv_g2'][l], f32)
        sm[0:16, 1024:1280] = np.asarray(inp['gla_w2'][l], f32)
        out[f"smat{l}"] = sm
    out["cst"] = make_consts()
    return out


def prep_x(xc):
    NS, T, _ = xc.shape
    return np.ascontiguousarray(xc.reshape(NS, T, KD, 128).transpose(0, 3, 2, 1))


def unprep_out(o):
    NS, _, _, T = o.shape
    return np.ascontiguousarray(o.transpose(0, 3, 2, 1)).reshape(NS, T, D)


def kernel(**inputs):
    x = np.asarray(inputs['x'], np.float32)
    B, T, _ = x.shape
    NCORES = 8
    NS = B // NCORES
    L = 2
    nc, pg = build_program(T, NS, L)
    wts = prep_weights(inputs, L)
    in_maps = []
    for c in range(NCORES):
        m = dict(wts)
        m["xT"] = prep_x(x[c * NS:(c + 1) * NS])
        in_maps.append(m)
    res = run_bass_kernel_spmd(nc, in_maps, core_ids=list(range(NCORES)))
    outs = [unprep_out(np.asarray(r["outT"])) for r in res.results]
    return np.concatenate(outs, axis=0).astype(np.float32)
```

```python
import numpy as np
from contextlib import ExitStack
import concourse.bass as bass
import concourse.mybir as mybir
from concourse.bass_utils import run_bass_kernel_spmd

F32 = mybir.dt.float32
BF16 = mybir.dt.bfloat16
AF = mybir.ActivationFunctionType
ALU = mybir.AluOpType
ENGS = ['pe', 'act', 'dve', 'pool', 'sp']

D = 1024
KD = 8
FF = 2816
NJ = 22
G = 256
DIN = 3344
NORM_EPS = 1e-6
GN_EPS = 64e-5
QK = 0.125
TB = 512
TBM = 256
CH = 64
NCH = TBM // CH
DEBUG_STAGE = 99

FMG = [(0, 128, 0), (128, 128, 0), (256, 128, 0), (384, 128, 0), (512, 128, 0), (640, 128, 0),
       (1024, 128, 0), (1152, 128, 0),
       (1280, 128, 1), (1408, 128, 1), (1536, 128, 1), (1664, 128, 1), (1792, 128, 1), (1920, 128, 1),
       (2048, 128, 1), (2176, 128, 1),
       (2304, 128, 0), (2432, 128, 0), (2560, 128, 0), (2688, 128, 0), (3072, 128, 0), (3200, 128, 0),
       (3328, 128, 0)]
TMG = [(768, 256, 0), (1792, 256, 1), (2816, 256, 0)]
RW0 = 1280

PV = {}
_o = 0
for _n, _w in [('nf1', 8), ('nmx', 8), ('nf2', 8), ('pool_b', 2), ('pool_s', 2), ('lb0', 2), ('lbl', 2),
               ('hnorm', 2), ('w0', 2), ('a0', 2), ('kk', 2), ('ka', 2), ('rk', 2), ('lnw', 2), ('lnb', 2),
               ('glab', 2), ('gnorm', 2), ('nfin', 8)]:
    PV[_n] = _o
    _o += _w
NPV = _o


class Buf:
    __slots__ = ('name', 'w', 'r', 'const')

    def __init__(self, name, const=False):
        self.name = name
        self.w = None
        self.r = []
        self.const = const


class DSem:
    def __init__(self, h):
        self.h = h
        self.count = 0


class Prog:
    def __init__(self, nc, same_engine_sync=True):
        self.nc = nc
        self.ops = {e: [] for e in ENGS}
        self.cnt = {e: 0 for e in ENGS}
        self.same = same_engine_sync
        self.nops = 0
        self.last_rg = None
        self.last_pe_sig = True

    def op(self, eng, fn, reads=(), writes=(), sig=True, dsem=None, rg=None):
        waits = {}
        if eng == 'pe':
            if rg is not None and self.last_rg is not None and rg != self.last_rg:
                assert self.last_pe_sig
                waits['pe'] = self.cnt['pe']
            self.last_rg = rg
            self.last_pe_sig = sig

        def addw(tok):
            if tok is None:
                return
            k, v = tok
            if k == eng and (eng == 'pe' or not self.same):
                return
            if waits.get(k, 0) < v:
                waits[k] = v
        for b in reads:
            addw(b.w)
        for b in writes:
            addw(b.w)
            for t in b.r:
                addw(t)
        if dsem is not None:
            dsem.count += 16
            tok = (dsem, dsem.count)
            sig = False
        elif sig:
            self.cnt[eng] += 1
            tok = (eng, self.cnt[eng])
        else:
            tok = (eng, self.cnt[eng] + 1)
        for b in reads:
            if not b.const:
                b.r.append(tok)
                if len(b.r) > 48:
                    mx = {}
                    for k, v in b.r:
                        if mx.get(k, 0) < v:
                            mx[k] = v
                    b.r = list(mx.items())
        for b in writes:
            b.w = tok
            b.r = []
        self.ops[eng].append((fn, waits, sig, dsem))
        self.nops += 1
        return tok

    def emit(self, block, esems):
        nc = self.nc
        deco = {'pe': block.tensor, 'act': block.scalar, 'dve': block.vector,
                'pool': block.gpsimd, 'sp': block.sync}
        for e in ENGS:
            ops = self.ops[e]

            def body(eng, ops=ops, e=e):
                known = {}
                for fn, waits, sig, dsem in ops:
                    for k, v in waits.items():
                        if known.get(k, 0) >= v:
                            continue
                        known[k] = v
                        h = k.h if isinstance(k, DSem) else esems[k]
                        eng.wait_ge(h, v)
                    inst = fn(eng)
                    if dsem is not None:
                        inst.then_inc(dsem.h, 16)
                    elif sig:
                        inst.then_inc(esems[e], 1)
            deco[e](body)


def build_program(T, NS, L, mixers=('pool', 'hgrn', 'rwkv', 'gla'), do_ffn=True, do_mix=True):
    NT = T // TB
    nc = bass.Bass("TRN2", target_bir_lowering=False)
    dr = {}

    def din(name, shape):
        dr[name] = nc.dram_tensor(name, list(shape), F32, kind="ExternalInput").ap()
        return dr[name]
    xT = din("xT", [NS, 128, KD, T])
    outT = nc.dram_tensor("outT", [NS, 128, KD, T], F32, kind="ExternalOutput").ap()
    f_win = [[din(f"f{w}_win{l}", [NJ, 128, KD * 256]) for w in (1, 2)] for l in range(L)]
    f_wout = [[din(f"f{w}_wout{l}", [NJ, 128, D]) for w in (1, 2)] for l in range(L)]
    m_fm = [din(f"m_fm{l}", [len(FMG), 128, KD * 128]) for l in range(L)]
    m_tm = [din(f"m_tm{l}", [len(TMG), 128, KD * 256]) for l in range(L)]
    m_wout = [din(f"m_wout{l}", [KD, 128, D]) for l in range(L)]
    pvec_d = [din(f"pvec{l}", [128, NPV]) for l in range(L)]
    mub_d = [din(f"mub{l}", [128, 1024]) for l in range(L)]
    smat_d = [din(f"smat{l}", [128, 5 * 256]) for l in range(L)]
    cst_d = din("cst", [128, 1600])
    es = ExitStack()
    with es:
        def sb(name, shape, dt=F32):
            return es.enter_context(nc.sbuf_tensor("sb_" + name, list(shape), dt))

        def psum(name, shape, dt=F32):
            return es.enter_context(nc.psum_tensor("pp_" + name, list(shape), dt))
        esems = {e: es.enter_context(nc.semaphore("s_" + e)) for e in ENGS}

        def dsem(name):
            return DSem(es.enter_context(nc.semaphore(name)))
        pg = Prog(nc)
        block = es.enter_context(nc.Block())

        X = sb("X", [128, KD, T])
        XB = [[Buf(f"X{k}_{t}") for t in range(NT)] for k in range(KD)]
        xn = sb("xn", [128, KD, TB], BF16)
        xns = sb("xns", [128, KD, TBM], BF16)
        xnsB = Buf("xns")
        xnB = [Buf(f"xn{k}") for k in range(KD)]
        sq = [sb(f"sq{i}", [128, TB], BF16) for i in range(2)]
        sqB = [Buf(f"sq{i}") for i in range(2)]
        rstd = sb("rstd", [128, TB]); rstdB = Buf("rstd")
        ones = sb("ones", [128, 128], BF16); onesB = Buf("ones", const=True)
        bones = sb("bones", [128, 128], BF16)
        ident = sb("ident", [128, 128], BF16)
        cst = sb("cst", [128, 1600])
        cstB = Buf("cst", const=True)
        MI, MSU, MSL, ID4 = 0, 256, 512, 768
        SCM = 1024
        ICN = 1536
        pvec = [sb(f"pvec{l}", [128, NPV]) for l in range(L)]
        pvB = [Buf(f"pvec{l}", const=True) for l in range(L)]
        lbt = sb("lbt", [128, 8])
        mub = sb("mub", [128, 1024]); mubB = Buf("mub")
        smat = sb("smat", [128, 5 * 256]); smatB = Buf("smat")
        smatb = sb("smatb", [128, 2 * 128], BF16)
        wst = [sb(f"wst{i}", [128, KD * 256]) for i in range(2)]
        wstB = [Buf(f"wst{i}") for i in range(2)]
        wstS = [dsem(f"dwst{i}") for i in range(2)]
        wbf = [sb(f"wbf{i}", [128, KD * 256], BF16) for i in range(2)]
        wbfB = [Buf(f"wbf{i}") for i in range(2)]
        wbfB2 = [[Buf(f"wbf{i}a"), Buf(f"wbf{i}b")] for i in range(2)]
        wtmp = sb("wtmp", [128, KD * 256]); wtmpB = Buf("wtmp")
        ost = [sb("ost0", [128, D])] * 2
        ostB = [Buf("ost0")] * 2
        ostS = [dsem("dost0")] * 2
        obf = [sb(f"obf{i}", [128, D], BF16) for i in range(2)]
        obfB = [Buf(f"obf{i}") for i in range(2)]
        hT = sb("hT", [128, NJ, TB], BF16)
        hB = [Buf(f"h{j}") for j in range(NJ)]
        sg = [sb("sg0", [128, TB])] * 2
        sgB = [Buf("sg0")] * 2
        PS = [psum(f"ps{i}", [128, 512]) for i in range(8)]
        PSB = [Buf(f"ps{i}") for i in range(8)]
        xS = [dsem(f"dx{k}") for k in range(KD)]
        oS = [dsem(f"dout{k}") for k in range(KD)]
        cS = dsem("dcst")
        pS = [dsem(f"dpv{l}") for l in range(L)]
        muS = dsem("dmu")
        smS = dsem("dsm")
        counters = {'w': 0, 'o': 0, 'sq': 0, 'sg': 0, 'pp': 0}

        def OP(eng, fn, reads=(), writes=(), sig=True, dsem=None, rg=None):
            return pg.op(eng, fn, reads, writes, sig, dsem, rg)

        def load_w(src_ap, ncols):
            i = counters['w'] % 2
            counters['w'] += 1
            OP('sp', lambda e: e.dma_start(out=wst[i][:, 0:ncols], in_=src_ap), writes=[wstB[i]], dsem=wstS[i])
            return i

        def load_w2(src_ap2):
            i = counters['w'] % 2
            counters['w'] += 1
            OP('sp', lambda e: e.dma_start(out=wst[i][:, 0:2 * D].rearrange("p (j c) -> p j c", j=2), in_=src_ap2.rearrange("j p c -> p j c")),
               writes=[wstB[i]], dsem=wstS[i])
            return i

        def cast_w(i, ncols, eng='pool'):
            if eng == 'act':
                OP('act', lambda e: e.activation(out=wbf[i][:, 0:ncols], in_=wst[i][:, 0:ncols], func=AF.Copy),
                   reads=[wstB[i]], writes=[wbfB[i], wbfB2[i][0], wbfB2[i][1]])
            else:
                OP(eng, lambda e: e.tensor_copy(out=wbf[i][:, 0:ncols], in_=wst[i][:, 0:ncols]),
                   reads=[wstB[i]], writes=[wbfB[i], wbfB2[i][0], wbfB2[i][1]])

        def cast_w_split(i, ncols):
            c1 = (ncols * 3 // 4) // 128 * 128
            OP('dve', lambda e: e.tensor_copy(out=wbf[i][:, 0:c1], in_=wst[i][:, 0:c1]), reads=[wstB[i]], writes=[wbfB2[i][0], wbfB[i]])
            OP('pool', lambda e: e.tensor_copy(out=wbf[i][:, c1:ncols], in_=wst[i][:, c1:ncols]), reads=[wstB[i]], writes=[wbfB2[i][1]])

        def load_o(src_ap, eng='act'):
            i = counters['o'] % 2
            counters['o'] += 1
            OP('sp', lambda e: e.dma_start(out=ost[i][:], in_=src_ap), writes=[ostB[i]], dsem=ostS[i])
            if eng == 'act':
                OP('act', lambda e: e.activation(out=obf[i][:], in_=ost[i][:], func=AF.Copy),
                   reads=[ostB[i]], writes=[obfB[i]])
            else:
                OP(eng, lambda e: e.tensor_copy(out=obf[i][:], in_=ost[i][:]), reads=[ostB[i]], writes=[obfB[i]])
            return i

        def rstd_from(psb, psbuf, n, scale, eps, dst, dstB):
            OP('act', lambda e: e.activation(out=dst[:, 0:n], in_=psb[:, 0:n], func=AF.Ln, scale=scale, bias=eps),
               reads=[psbuf], writes=[dstB])
            OP('act', lambda e: e.activation(out=dst[:, 0:n], in_=dst[:, 0:n], func=AF.Exp, scale=-0.5),
               reads=[dstB], writes=[dstB])

        def rmsnorm_to_xn(l, gcol, c0, n, bank):
            ti = c0 // TB
            for k in range(KD):
                i = counters['sq'] % 2
                counters['sq'] += 1
                OP('act', lambda e, k=k, i=i: e.activation(out=sq[i][:, 0:n], in_=X[:, k, c0:c0 + n], func=AF.Square),
                   reads=[XB[k][ti]], writes=[sqB[i]])
                OP('pe', lambda e, k=k, i=i: e.matmul(PS[bank][:, 0:n], lhsT=ones[:], rhs=sq[i][:, 0:n], start=(k == 0), stop=(k == KD - 1)),
                   reads=[sqB[i], onesB], writes=[PSB[bank]])
            rstd_from(PS[bank], PSB[bank], n, 1.0 / D, NORM_EPS, rstd, rstdB)
            for k in range(KD):
                OP('dve', lambda e, k=k: e.scalar_tensor_tensor(out=xn[:, k, 0:n], in0=X[:, k, c0:c0 + n],
                                                                 scalar=pvec[l][:, gcol + k:gcol + k + 1], in1=rstd[:, 0:n],
                                                                 op0=ALU.mult, op1=ALU.mult),
                   reads=[XB[k][ti], rstdB, pvB[l]], writes=[xnB[k]])

        def ffn(l, w, ti):
            gcol = PV['nf1'] if w == 0 else PV['nf2']
            c0 = ti * TB
            blocks = [('in', j) for j in range(NJ)] + [('out', jp) for jp in range(NJ // 2)]

            slots = {}

            def do_load(t):
                if t < len(blocks) and t not in slots:
                    kind_, j_ = blocks[t]
                    if kind_ == 'in':
                        slots[t] = load_w(f_win[l][w][j_], KD * 256)
                    else:
                        slots[t] = load_w2(f_wout[l][w][2 * j_:2 * j_ + 2])

            def do_ready(t):
                if t >= len(blocks):
                    return
                do_load(t)
                cast_w_split(slots[t], KD * 256)
            do_load(0)
            do_load(1)
            do_ready(0)
            rmsnorm_to_xn(l, gcol, c0, TB, 0)
            for t, (kind, j) in enumerate(blocks):
                i = slots[t]
                do_ready(t + 1)
                do_load(t + 2)
                if kind == 'in':
                    wv = wbf[i][:].rearrange("p (k c) -> p k c", k=KD)
                    pp = counters['pp'] % 2
                    counters['pp'] += 1
                    bg, bu = 2 * pp, 2 * pp + 1
                    for half, bk in ((0, bg), (1, bu)):
                        for k in range(KD):
                            OP('pe', lambda e, k=k, half=half, bk=bk, wv=wv: e.matmul(
                                PS[bk][:, :], lhsT=wv[:, k, half * 128:(half + 1) * 128], rhs=xn[:, k, 0:TB],
                                start=(k == 0), stop=(k == KD - 1)),
                               reads=[wbfB[i], wbfB2[i][0], wbfB2[i][1], xnB[k]], writes=[PSB[bk]], sig=(k == KD - 1))
                    si = counters['sg'] % 2
                    counters['sg'] += 1
                    OP('act', lambda e, si=si, bg=bg: e.activation(out=sg[si][:], in_=PS[bg][:, :], func=AF.Silu),
                       reads=[PSB[bg]], writes=[sgB[si]])
                    OP('dve', lambda e, si=si, bu=bu, j=j: e.tensor_tensor(out=hT[:, j, :], in0=sg[si][:], in1=PS[bu][:, :], op=ALU.mult),
                       reads=[sgB[si], PSB[bu]], writes=[hB[j]])
                else:
                    for q in range(2):
                        jj = 2 * j + q
                        for m in range(KD):
                            OP('pe', lambda e, m=m, jj=jj, q=q, i=i: e.matmul(PS[m][:, :], lhsT=wbf[i][:, q * D + m * 128:q * D + (m + 1) * 128], rhs=hT[:, jj, :],
                                                                   start=(jj == 0), stop=(jj == NJ - 1)),
                               reads=[wbfB[i], wbfB2[i][0], wbfB2[i][1], hB[jj]], writes=[PSB[m]], sig=(m == KD - 1 or jj == NJ - 1))
            for m in range(KD):
                OP('dve', lambda e, m=m: e.scalar_tensor_tensor(out=X[:, m, c0:c0 + TB], in0=PS[m][:, :], scalar=0.5,
                                                                 in1=X[:, m, c0:c0 + TB], op0=ALU.mult, op1=ALU.add),
                   reads=[PSB[m], XB[m][ti]], writes=[XB[m][ti]])

        OP('sp', lambda e: e.dma_start(out=cst[:], in_=cst_d), writes=[cstB], dsem=cS)
        for l in range(L):
            OP('sp', lambda e, l=l: e.dma_start(out=pvec[l][:], in_=pvec_d[l]), writes=[pvB[l]], dsem=pS[l])
        OP('pool', lambda e: e.memset(ones[:], 1.0), writes=[onesB])
        bonesB = Buf("bones", const=True)
        OP('pool', lambda e: e.memset(bones[:], 0.0), writes=[bonesB])
        OP('pool', lambda e: e.memset(bones[0:64, 0:64], 1.0), writes=[bonesB])
        OP('pool', lambda e: e.memset(bones[64:128, 64:128], 1.0), writes=[bonesB])
        identB = Buf("ident", const=True)
        OP('pool', lambda e: e.memset(ident[:], 1.0), writes=[identB])
        OP('pool', lambda e: e.affine_select(out=ident[:], in_=ident[:], pattern=[[-1, 128]], compare_op=ALU.is_equal,
                                             fill=0.0, base=0, channel_multiplier=1), reads=[identB], writes=[identB])

        if do_mix:
            YT = sb("YT", [128, KD, TBM], BF16)
            YB = [Buf(f"Y{k}") for k in range(KD)]
            NFA = 10
            FA = [sb(f"fa{i}", [128, TBM]) for i in range(NFA)]
            FAB = [Buf(f"fa{i}") for i in range(NFA)]
            NBA = 4
            BA = [sb(f"ba{i}", [128, TBM], BF16) for i in range(NBA)]
            BAB = [Buf(f"ba{i}") for i in range(NBA)]
            LL = [[sb(f"ll{hp}{i}", [128, TBM]) for i in range(3)] for hp in range(2)]
            LLB_ = [[Buf(f"ll{hp}{i}") for i in range(3)] for hp in range(2)]
            LLX = [sb(f"llx{i}", [128, TBM]) for i in range(2)]
            LLXB = [Buf(f"llx{i}") for i in range(2)]
            LB = [[sb(f"lb{hp}{i}", [128, TBM], BF16) for i in range(5)] for hp in range(2)]
            LBB = [[Buf(f"lb{hp}{i}") for i in range(5)] for hp in range(2)]
            PB16 = [sb(f"pb16{i}", [128, TBM], BF16) for i in range(2)]
            PB16B = [Buf(f"pb16{i}") for i in range(2)]
            Vt = [sb(f"vt{c}", [64, 256], BF16) for c in range(NCH)]
            VtB = [Buf(f"vt{c}") for c in range(NCH)]
            KEt = [sb(f"ket{c}", [64, 256], BF16) for c in range(NCH)]
            KEtB = [Buf(f"ket{c}") for c in range(NCH)]
            AEt = [sb(f"aet{c}", [64, 256], BF16) for c in range(NCH)]
            AEtB = [Buf(f"aet{c}") for c in range(NCH)]
            alias_ctr = [0]

            def mk(name, n):
                ts, bs = [], []
                for c in range(n):
                    idx = alias_ctr[0]
                    alias_ctr[0] += 1
                    j, half = idx // 2, idx % 2
                    ts.append(hT[0:64, j, half * 256:(half + 1) * 256])
                    bs.append(hB[j])
                return ts, bs
            ATs, ATsB = mk("ats", NCH)
            LKs, LKsB = mk("lks", NCH)
            ARs, ARsB = mk("ars", NCH)
            Nn, NnB = mk("nn", NCH)
            NTn, NTnB = mk("ntn", NCH)
            Nn2, Nn2B = mk("nn2", NCH)
            NTn2, NTn2B = mk("ntn2", NCH)
            Pn, PnB = mk("pn", NCH)
            Pn2, Pn2B = mk("pn2", NCH)
            Ysb = sb("ysb", [64, 256], BF16); YsbB = Buf("ysb")
            Usb = sb("usb", [64, 256], BF16); UsbB = Buf("usb")
            S32 = {m: [sb(f"s32{m}{hp}", [128, 64]) for hp in range(2)] for m in ('hgrn', 'gla', 'rwkv')}
            Sbf = {m: [sb(f"sbf{m}{hp}", [128, 64], BF16) for hp in range(2)] for m in ('hgrn', 'gla', 'rwkv')}
            S32B = {m: [Buf(f"s32{m}{hp}") for hp in range(2)] for m in ('hgrn', 'gla', 'rwkv')}
            SbfB = {m: [Buf(f"sbf{m}{hp}") for hp in range(2)] for m in ('hgrn', 'gla', 'rwkv')}
            stmp = [sb(f"stmp{hp}", [128, 64]) for hp in range(2)]
            stmpB = [Buf(f"stmp{hp}") for hp in range(2)]
            pext = sb("pext", [128, 2, 16 + TBM]); pextB = Buf("pext")
            pw = [sb(f"pw{i}", [128, 2, 16 + TBM]) for i in range(2)]
            pwB = [Buf(f"pw{i}") for i in range(2)]
            PST = PS[7][:, :].bitcast(BF16)
            lbtB = Buf("lbt", const=True)
            fa_ctr = [0]
            ba_ctr = [0]

            def fa():
                i = fa_ctr[0] % NFA
                fa_ctr[0] += 1
                return FA[i], FAB[i]

            def ba():
                i = ba_ctr[0] % NBA
                ba_ctr[0] += 1
                return BA[i], BAB[i]

            def pbank():
                b = counters['pp'] % 4
                counters['pp'] += 1
                return b

            wplan = {'plan': [], 'pos': 0, 'issued': {}, 'l': 0}

            def issue_load(l, d):
                kind, g = d
                if kind == 'fm':
                    return load_w(m_fm[l][g], KD * 128)
                if kind == 'wo':
                    return load_w2(m_wout[l][2 * g:2 * g + 2])
                return load_w(m_tm[l][g], KD * 256)

            def issue_cast(l, d, i):
                kind, g = d
                if kind == 'wo':
                    cast_w_split(i, 2 * D)
                    return (i,)
                if kind == 'fm':
                    col0, ncols, shift = FMG[g]
                    if shift:
                        mc = col0 - RW0
                        mu_b = mub[:, mc:mc + 128].unsqueeze(1).broadcast_to([128, KD, 128])
                        wv32 = wst[i][:, 0:KD * 128].rearrange("p (k c) -> p k c", k=KD)
                        wt = wtmp[:, 0:KD * 128].rearrange("p (k c) -> p k c", k=KD)
                        wbv = wbf[i][:].rearrange("p (k c) -> p k c", k=KD)
                        OP('pool', lambda e: e.tensor_tensor(out=wt, in0=wv32, in1=mu_b, op=ALU.mult),
                           reads=[wstB[i], mubB], writes=[wtmpB])
                        OP('pool', lambda e: e.tensor_tensor(out=wbv[:, :, 0:128], in0=wv32, in1=wt, op=ALU.subtract),
                           reads=[wstB[i], wtmpB], writes=[wbfB[i], wbfB2[i][0], wbfB2[i][1]])
                        OP('pool', lambda e: e.tensor_copy(out=wbv[:, :, 128:256], in_=wt), reads=[wtmpB], writes=[wbfB[i], wbfB2[i][0], wbfB2[i][1]])
                    else:
                        cast_w(i, KD * 128, 'act')
                        wbv = wbf[i][:, 0:KD * 128].rearrange("p (k c) -> p k c", k=KD)
                    return (i, wbv)
                col0, ncols, shift = TMG[g]
                i2 = None
                wb2 = None
                if shift:
                    i2 = counters['w'] % 2
                    counters['w'] += 1
                    mc = col0 - RW0
                    mu_b = mub[:, mc:mc + 256].unsqueeze(1).broadcast_to([128, KD, 256])
                    wv32 = wst[i][:].rearrange("p (k c) -> p k c", k=KD)
                    wt = wtmp[:].rearrange("p (k c) -> p k c", k=KD)
                    wa = wbf[i][:].rearrange("p (k c) -> p k c", k=KD)
                    wb2 = wbf[i2][:].rearrange("p (k c) -> p k c", k=KD)
                    OP('pool', lambda e: e.tensor_tensor(out=wt, in0=wv32, in1=mu_b, op=ALU.mult),
                       reads=[wstB[i], mubB], writes=[wtmpB])
                    OP('pool', lambda e: e.tensor_tensor(out=wa, in0=wv32, in1=wt, op=ALU.subtract),
                       reads=[wstB[i], wtmpB], writes=[wbfB[i], wbfB2[i][0], wbfB2[i][1]])
                    OP('pool', lambda e: e.tensor_copy(out=wb2, in_=wt), reads=[wtmpB], writes=[wbfB[i2], wbfB2[i2][0], wbfB2[i2][1]])
                else:
                    cast_w_split(i, KD * 256)
                    wa = wbf[i][:].rearrange("p (k c) -> p k c", k=KD)
                return (i, i2, wa, wb2)

            def two_slot(d):
                return d[0] == 'tm' and bool(TMG[d[1]][2])

            def ensure_load(l, pos):
                plan = wplan['plan']
                if pos < len(plan) and pos not in wplan['loaded'] and not two_slot(plan[pos]):
                    wplan['loaded'][pos] = issue_load(l, plan[pos])

            def ensure_cast(l, pos):
                plan = wplan['plan']
                if pos < len(plan) and pos not in wplan['issued'] and not two_slot(plan[pos]):
                    ensure_load(l, pos)
                    wplan['issued'][pos] = issue_cast(l, plan[pos], wplan['loaded'].pop(pos))

            def acq(l, d):
                pos = wplan['pos']
                plan = wplan['plan']
                assert plan[pos] == d, (plan[pos], d)
                if pos not in wplan['issued']:
                    if pos not in wplan['loaded']:
                        wplan['loaded'][pos] = issue_load(l, d)
                    wplan['issued'][pos] = issue_cast(l, d, wplan['loaded'].pop(pos))
                info = wplan['issued'].pop(pos)
                wplan['pos'] = pos + 1
                if not two_slot(d) and not (pos + 1 < len(plan) and two_slot(plan[pos + 1])):
                    ensure_cast(l, pos + 1)
                    if not (pos + 2 < len(plan) and two_slot(plan[pos + 2])):
                        ensure_load(l, pos + 2)
                return info

            def proj_fm(l, g, first_block):
                col0, ncols, shift = FMG[g]
                i, wbv = acq(l, ('fm', g))
                bk = pbank()
                nmm = KD * (2 if shift else 1)
                n = 0
                for k in range(KD):
                    n += 1
                    OP('pe', lambda e, k=k, n=n: e.matmul(PS[bk][0:ncols, 0:TBM], lhsT=wbv[:, k, 0:ncols], rhs=xn[:, k, 0:TBM],
                                                          start=(n == 1), stop=(n == nmm)),
                       reads=[wbfB[i], wbfB2[i][0], wbfB2[i][1], xnB[k]], writes=[PSB[bk]], sig=(n == nmm))
                if shift:
                    for k in range(KD):
                        n += 1
                        OP('pe', lambda e, k=k, n=n: e.matmul(PS[bk][0:ncols, 0:TBM], lhsT=wbv[:, k, 128:128 + ncols], rhs=xns[:, k, 0:TBM],
                                                              start=False, stop=(n == nmm)),
                           reads=[wbfB[i], wbfB2[i][0], wbfB2[i][1], xnsB], writes=[PSB[bk]], sig=(n == nmm))
                return bk

            def proj_tm(l, g):
                col0, ncols, shift = TMG[g]
                i, i2, wa, wb2 = acq(l, ('tm', g))
                for c in range(NCH):
                    bk = pbank()
                    nmm = KD * (2 if shift else 1)
                    n = 0
                    for k in range(KD):
                        n += 1
                        OP('pe', lambda e, k=k, n=n, c=c, bk=bk: e.matmul(PS[bk][0:64, 0:256], lhsT=xn[:, k, c * CH:(c + 1) * CH],
                                                                     rhs=wa[:, k, :], start=(n == 1), stop=(n == nmm)),
                           reads=[wbfB[i], wbfB2[i][0], wbfB2[i][1], xnB[k]], writes=[PSB[bk]], sig=(n == nmm))
                    if shift:
                        for k in range(KD):
                            n += 1
                            OP('pe', lambda e, k=k, n=n, c=c, bk=bk: e.matmul(PS[bk][0:64, 0:256], lhsT=xns[:, k, c * CH:(c + 1) * CH],
                                                                         rhs=wb2[:, k, :], start=False, stop=(n == nmm)),
                               reads=[wbfB[i2], wbfB2[i2][0], wbfB2[i2][1], xnsB], writes=[PSB[bk]], sig=(n == nmm))
                    OP('act', lambda e, c=c, bk=bk: e.activation(out=Vt[c][:], in_=PS[bk][0:64, 0:256], func=AF.Copy),
                       reads=[PSB[bk]], writes=[VtB[c]])

            def scan_decay(g_t, g_b):
                b_t, b_b = fa()
                OP('dve', lambda e: e.tensor_tensor_scan(out=b_t[:], data0=cst[:, SCM:SCM + TBM], data1=g_t[:], initial=0.0,
                                                         op0=ALU.mult, op1=ALU.add), reads=[g_b, cstB], writes=[b_b])
                return b_t, b_b

            def chunk_engine(mname, QE, KE, PC, rw=None):
                isrw = rw is not None
                if DEBUG_STAGE < 1:
                    return
                for c in range(NCH):
                    for (src, dst, dstB_) in ([(KE, KEt, KEtB)] + ([(rw['AE'], AEt, AEtB)] if isrw else [])):
                        for hp in range(2):
                            OP('pe', lambda e, hp=hp, c=c, src=src: e.transpose(PST[0:64, hp * 128:(hp + 1) * 128],
                                                                             src[hp][0][:, c * CH:(c + 1) * CH], ident[:]),
                               reads=[src[hp][1], identB], writes=[PSB[7]])
                        OP('act', lambda e, c=c, dst=dst: e.activation(out=dst[c][:], in_=PST[0:64, 0:256], func=AF.Copy),
                           reads=[PSB[7]], writes=[dstB_[c]])
                if DEBUG_STAGE < 2:
                    return
                for c in range(NCH):
                    cs = slice(c * CH, (c + 1) * CH)
                    def sc(lh, rh, dst_ps, cs=cs):
                        for h in range(4):
                            hp, r = h // 2, (h % 2) * 64
                            OP('pe', lambda e, h=h, hp=hp, r=r: e.matmul(dst_ps[0:64, h * 64:(h + 1) * 64], lhsT=lh[hp][0][r:r + 64, cs],
                                                                         rhs=rh[hp][0][r:r + 64, cs], start=True, stop=True),
                               reads=[lh[hp][1], rh[hp][1]], writes=[PSB[6]], rg=r)
                    sc(KE, QE, PS[6][:, 0:256])
                    OP('dve', lambda e, c=c: e.tensor_tensor(out=ATs[c][:], in0=PS[6][0:64, 0:256], in1=cst[0:64, MI:MI + 256], op=ALU.mult),
                       reads=[PSB[6], cstB], writes=[ATsB[c]])
                    if isrw:
                        sc(KE, rw['BE'], PS[6][:, 256:512])
                        OP('dve', lambda e, c=c: e.tensor_tensor(out=LKs[c][:], in0=PS[6][0:64, 256:512], in1=cst[0:64, MSU:MSU + 256], op=ALU.mult),
                           reads=[PSB[6], cstB], writes=[LKsB[c]])
                        sc(rw['AE'], QE, PS[6][:, 0:256])
                        OP('dve', lambda e, c=c: e.tensor_tensor(out=ARs[c][:], in0=PS[6][0:64, 0:256], in1=cst[0:64, MI:MI + 256], op=ALU.mult),
                           reads=[PSB[6], cstB], writes=[ARsB[c]])
                        sc(rw['AE'], rw['BE'], PS[6][:, 256:512])
                        OP('dve', lambda e, c=c: e.scalar_tensor_tensor(out=NTn[c][:], in0=PS[6][0:64, 256:512], scalar=-1.0,
                                                                         in1=cst[0:64, MSU:MSU + 256], op0=ALU.mult, op1=ALU.mult),
                           reads=[PSB[6], cstB], writes=[NTnB[c]])
                        sc(rw['BE'], rw['AE'], PS[6][:, 0:256])
                        OP('dve', lambda e, c=c: e.scalar_tensor_tensor(out=Nn[c][:], in0=PS[6][0:64, 0:256], scalar=-1.0,
                                                                         in1=cst[0:64, MSL:MSL + 256], op0=ALU.mult, op1=ALU.mult),
                           reads=[PSB[6], cstB], writes=[NnB[c]])
                        OP('pool', lambda e, c=c: e.tensor_tensor(out=Pn[c][:], in0=NTn[c][:], in1=cst[0:64, ID4:ID4 + 256], op=ALU.add),
                           reads=[NTnB[c], cstB], writes=[PnB[c]])
                if isrw:
                    curN, curNB, curNT, curNTB = Nn, NnB, NTn, NTnB
                    nxtN, nxtNB, nxtNT, nxtNTB = Nn2, Nn2B, NTn2, NTn2B
                    curP, curPB, nxtP, nxtPB = Pn, PnB, Pn2, Pn2B
                    for lev in range(1, 6):
                        for c in range(NCH):
                            bk = pbank()
                            for h in range(4):
                                hs = slice(h * 64, (h + 1) * 64)
                                OP('pe', lambda e, c=c, hs=hs, bk=bk, a=curNT, b=curN: e.matmul(PS[bk][0:64, hs], lhsT=a[c][:, hs], rhs=b[c][:, hs],
                                                                                         start=True, stop=True),
                                   reads=[curNTB[c], curNB[c]], writes=[PSB[bk]], rg=0)
                            if lev < 5:
                                for h in range(4):
                                    hs = slice(h * 64, (h + 1) * 64)
                                    hs2 = slice(256 + h * 64, 256 + (h + 1) * 64)
                                    OP('pe', lambda e, c=c, hs=hs, hs2=hs2, bk=bk, a=curN, b=curNT: e.matmul(PS[bk][0:64, hs2], lhsT=a[c][:, hs], rhs=b[c][:, hs],
                                                                                                     start=True, stop=True),
                                       reads=[curNTB[c], curNB[c]], writes=[PSB[bk]], rg=0)
                                OP('act', lambda e, c=c, bk=bk, d=nxtNT: e.activation(out=d[c][:], in_=PS[bk][0:64, 256:512], func=AF.Copy),
                                   reads=[PSB[bk]], writes=[nxtNTB[c]])
                            OP('act', lambda e, c=c, bk=bk, d=nxtN: e.activation(out=d[c][:], in_=PS[bk][0:64, 0:256], func=AF.Copy),
                               reads=[PSB[bk]], writes=[nxtNB[c]])
                        curN, curNB, nxtN, nxtNB = nxtN, nxtNB, curN, curNB
                        curNT, curNTB, nxtNT, nxtNTB = nxtNT, nxtNTB, curNT, curNTB
                        for c in range(NCH):
                            bk = pbank()
                            for h in range(4):
                                hs = slice(h * 64, (h + 1) * 64)
                                OP('pe', lambda e, c=c, hs=hs, bk=bk, a=curN, b=curP: e.matmul(PS[bk][0:64, hs], lhsT=a[c][:, hs], rhs=b[c][:, hs],
                                                                                        start=True, stop=True),
                                   reads=[curNB[c], curPB[c]], writes=[PSB[bk]], rg=0)
                            OP('dve', lambda e, c=c, bk=bk, s=curP, d=nxtP: e.tensor_tensor(out=d[c][:], in0=PS[bk][0:64, 0:256], in1=s[c][:], op=ALU.add),
                               reads=[PSB[bk], curPB[c]], writes=[nxtPB[c]])
                        curP, curPB, nxtP, nxtPB = nxtP, nxtPB, curP, curPB
                    TT, TTB = curP, curPB
                if DEBUG_STAGE < 3:
                    return
                S3, Sb, S3B, SbB = S32[mname], Sbf[mname], S32B[mname], SbfB[mname]
                for c in range(NCH):
                    cs = slice(c * CH, (c + 1) * CH)
                    if isrw:
                        for h in range(4):
                            hp, r = h // 2, (h % 2) * 64
                            hs = slice(h * 64, (h + 1) * 64)
                            OP('pe', lambda e, hp=hp, r=r, hs=hs, cs=cs: e.matmul(PS[6][0:64, hs], lhsT=rw['BE'][hp][0][r:r + 64, cs], rhs=Sb[hp][r:r + 64, :],
                                                                         start=True, stop=False),
                               reads=[rw['BE'][hp][1], SbB[hp]], writes=[PSB[6]], rg=r)
                            OP('pe', lambda e, hs=hs, c=c: e.matmul(PS[6][0:64, hs], lhsT=LKs[c][:, hs], rhs=Vt[c][:, hs], start=False, stop=True),
                               reads=[LKsB[c], VtB[c]], writes=[PSB[6]], rg=0)
                        OP('act', lambda e: e.activation(out=Ysb[:], in_=PS[6][0:64, 0:256], func=AF.Copy), reads=[PSB[6]], writes=[YsbB])
                        for h in range(4):
                            hs = slice(h * 64, (h + 1) * 64)
                            hs2 = slice(256 + h * 64, 256 + (h + 1) * 64)
                            OP('pe', lambda e, hs=hs, hs2=hs2, c=c: e.matmul(PS[6][0:64, hs2], lhsT=TT[c][:, hs], rhs=Ysb[:, hs], start=True, stop=True),
                               reads=[TTB[c], YsbB], writes=[PSB[6]], rg=0)
                        OP('act', lambda e: e.activation(out=Usb[:], in_=PS[6][0:64, 256:512], func=AF.Copy, scale=-1.0),
                           reads=[PSB[6]], writes=[UsbB])
                    for h in range(4):
                        hp, r = h // 2, (h % 2) * 64
                        hs = slice(h * 64, (h + 1) * 64)
                        ob = PS[4 + hp][r:r + 64, cs]
                        OP('pe', lambda e, ob=ob, hs=hs, c=c: e.matmul(ob, lhsT=Vt[c][:, hs], rhs=ATs[c][:, hs], start=True, stop=False),
                           reads=[VtB[c], ATsB[c]], writes=[PSB[4 + hp]], rg=0)
                        if isrw:
                            OP('pe', lambda e, ob=ob, hs=hs, c=c: e.matmul(ob, lhsT=Usb[:, hs], rhs=ARs[c][:, hs], start=False, stop=False),
                               reads=[UsbB, ARsB[c]], writes=[PSB[4 + hp]], rg=0)
                        OP('pe', lambda e, ob=ob, hp=hp, r=r, cs=cs: e.matmul(ob, lhsT=Sb[hp][r:r + 64, :], rhs=QE[hp][0][r:r + 64, cs], start=False, stop=True),
                           reads=[SbB[hp], QE[hp][1]], writes=[PSB[4 + hp]], rg=r)
                    for h in range(4):
                        hp, r = h // 2, (h % 2) * 64
                        hs = slice(h * 64, (h + 1) * 64)
                        sp_ = PS[7][r:r + 64, 256 + hp * 64:256 + (hp + 1) * 64]
                        OP('pe', lambda e, sp_=sp_, hs=hs, c=c: e.matmul(sp_, lhsT=KEt[c][:, hs], rhs=Vt[c][:, hs], start=True, stop=(not isrw)),
                           reads=[KEtB[c], VtB[c]], writes=[PSB[7]], rg=0)
                        if isrw:
                            OP('pe', lambda e, sp_=sp_, hs=hs, c=c: e.matmul(sp_, lhsT=AEt[c][:, hs], rhs=Usb[:, hs], start=False, stop=True),
                               reads=[AEtB[c], UsbB], writes=[PSB[7]], rg=0)
                    for hp in range(2):
                        pc = PC[hp][0][:, (c + 1) * CH - 1:(c + 1) * CH]
                        OP('dve', lambda e, hp=hp: e.tensor_tensor(out=stmp[hp][:], in0=PS[7][:, 256 + hp * 64:256 + (hp + 1) * 64], in1=S3[hp][:], op=ALU.add),
                           reads=[PSB[7], S3B[hp]], writes=[stmpB[hp]])
                        OP('dve', lambda e, hp=hp, pc=pc: e.tensor_scalar(out=S3[hp][:], in0=stmp[hp][:], scalar1=pc, scalar2=None, op0=ALU.mult),
                           reads=[stmpB[hp], PC[hp][1]], writes=[S3B[hp]])
                        OP('dve', lambda e, hp=hp, pc=pc: e.tensor_scalar(out=Sb[hp][:], in0=stmp[hp][:], scalar1=pc, scalar2=None, op0=ALU.mult),
                           reads=[stmpB[hp], PC[hp][1]], writes=[SbB[hp]])

            def evac_fm(bk, func=AF.Copy, scale=1.0, bias=None, dt='f', rows=128, dst=None):
                t, b = dst if dst is not None else (fa() if dt == 'f' else ba())
                kw = {}
                if bias is not None:
                    kw['bias'] = bias
                OP('act', lambda e: e.activation(out=t[0:rows, :], in_=PS[bk][0:rows, 0:TBM], func=func, scale=scale, **kw),
                   reads=[PSB[bk]] + ([pvB[0]] if bias is not None else []), writes=[b])
                return t, b

            def mixer_block(l, s, bi):
                first = (bi == 0)
                c0 = bi * TBM
                ti = c0 // TB
                plan = []
                if 'pool' in mixers:
                    plan += [('fm', 0), ('fm', 1)]
                if 'hgrn' in mixers:
                    for hp_ in range(2):
                        plan += [('fm', 2 + hp_), ('fm', 4 + hp_), ('fm', 6 + hp_)]
                    plan += [('tm', 0)]
                if 'gla' in mixers:
                    plan += [('fm', 22)]
                    for hp_ in range(2):
                        plan += [('fm', 16 + hp_), ('fm', 18 + hp_), ('fm', 20 + hp_)]
                    plan += [('tm', 2)]
                if 'rwkv' in mixers:
                    plan += [('fm', 14), ('fm', 15)]
                    for hp_ in range(2):
                        plan += [('fm', 8 + hp_), ('fm', 10 + hp_), ('fm', 12 + hp_)]
                    plan += [('tm', 1)]
                plan += [('wo', jp) for jp in range(KD // 2)]
                wplan['plan'] = plan
                wplan['pos'] = 0
                wplan['issued'] = {}
                wplan['loaded'] = {}
                if plan:
                    ensure_load(l, 0)
                    ensure_load(l, 1)
                    ensure_cast(l, 0)
                pv = pvec[l]
                if 'rwkv' in mixers:
                    if first:
                        OP('pool', lambda e: e.memset(xns[:, :, 0:2], 0.0), writes=[xnsB])
                    else:
                        OP('pool', lambda e: e.tensor_copy(out=xns[:, :, 0:1], in_=xn[:, :, TBM - 1:TBM]), reads=xnB, writes=[xnsB])
                rmsnorm_to_xn(l, PV['nmx'], c0, TBM, 0)
                if 'rwkv' in mixers:
                    OP('pool', lambda e: e.tensor_copy(out=xns[:, :, 1:TBM], in_=xn[:, :, 0:TBM - 1]), reads=xnB, writes=[xnsB])
                if 'pool' in mixers:
                    if first:
                        OP('pool', lambda e: e.memset(pext[:, :, 0:16], 0.0), writes=[pextB])
                    else:
                        OP('pool', lambda e: e.tensor_copy(out=pext[:, :, 0:16], in_=pext[:, :, TBM:TBM + 16]), reads=[pextB], writes=[pextB])
                    for ck in range(2):
                        bk = proj_fm(l, ck, first)
                        OP('act', lambda e, ck=ck, bk=bk: e.activation(out=pext[:, ck, 16:16 + TBM], in_=PS[bk][:, 0:TBM], func=AF.Copy),
                           reads=[PSB[bk]], writes=[pextB])
                    W_ = 16 + TBM
                    OP('dve', lambda e: e.tensor_tensor(out=pw[0][:, :, 1:W_], in0=pext[:, :, 1:W_], in1=pext[:, :, 0:W_ - 1], op=ALU.add),
                       reads=[pextB], writes=[pwB[0]])
                    def poolfin(src, ck, r0, wdw, first=first):
                        yt, yb = PB16[ck], PB16B[ck]
                        OP('dve', lambda e: e.scalar_tensor_tensor(out=yt[r0:r0 + 64, :], in0=src[r0:r0 + 64, ck, 16:16 + TBM], scalar=1.0 / wdw,
                                                                   in1=pext[r0:r0 + 64, ck, 16:16 + TBM], op0=ALU.mult, op1=ALU.subtract),
                           reads=[pwB[0], pwB[1], pextB], writes=[yb])
                        if first:
                            t2, b2 = FA[0], FAB[0]
                            OP('dve', lambda e: e.tensor_tensor(out=t2[r0:r0 + 64, 0:16], in0=src[r0:r0 + 64, ck, 16:32],
                                                                in1=cst[r0:r0 + 64, ICN + ck * 16:ICN + ck * 16 + 16], op=ALU.mult),
                               reads=[pwB[0], pwB[1], cstB], writes=[b2])
                            OP('dve', lambda e: e.tensor_tensor(out=yt[r0:r0 + 64, 0:16], in0=t2[r0:r0 + 64, 0:16],
                                                                in1=pext[r0:r0 + 64, ck, 16:32], op=ALU.subtract),
                               reads=[b2, pextB], writes=[yb])
                    poolfin(pw[0], 0, 0, 2)
                    OP('dve', lambda e: e.tensor_tensor(out=pw[1][:, :, 3:W_], in0=pw[0][:, :, 3:W_], in1=pw[0][:, :, 1:W_ - 2], op=ALU.add),
                       reads=[pwB[0]], writes=[pwB[1]])
                    poolfin(pw[1], 0, 64, 4)
                    OP('dve', lambda e: e.tensor_tensor(out=pw[0][:, :, 7:W_], in0=pw[1][:, :, 7:W_], in1=pw[1][:, :, 3:W_ - 4], op=ALU.add),
                       reads=[pwB[1]], writes=[pwB[0]])
                    poolfin(pw[0], 1, 0, 8)
                    OP('dve', lambda e: e.tensor_tensor(out=pw[1][:, :, 15:W_], in0=pw[0][:, :, 15:W_], in1=pw[0][:, :, 7:W_ - 8], op=ALU.add),
                       reads=[pwB[0]], writes=[pwB[1]])
                    poolfin(pw[1], 1, 64, 16)
                    for ck in range(2):
                        bk = pbank()
                        OP('pe', lambda e, ck=ck, bk=bk: e.matmul(PS[bk][:, 0:TBM], lhsT=smatb[:, ck * 128:(ck + 1) * 128], rhs=PB16[ck][:], start=True, stop=True),
                           reads=[PB16B[ck], smatB], writes=[PSB[bk]])
                        OP('dve', lambda e, ck=ck, bk=bk: e.tensor_scalar(out=YT[:, ck, :], in0=PS[bk][:, 0:TBM], scalar1=pv[:, PV['pool_b'] + ck:PV['pool_b'] + ck + 1],
                                                                      scalar2=pv[:, PV['pool_s'] + ck:PV['pool_s'] + ck + 1], op0=ALU.add, op1=ALU.mult),
                           reads=[PSB[bk], pvB[l]], writes=[YB[ck]])
                else:
                    for ck in range(2):
                        OP('pool', lambda e, ck=ck: e.memset(YT[:, ck, :], 0.0), writes=[YB[ck]])

                def out_rstd(Ot, lhs_ones, n_ch, eps):
                    bk = pbank()
                    for hp in range(2):
                        st, sbb = ba()
                        OP('act', lambda e, hp=hp, st=st, Ot=Ot: e.activation(out=st[:], in_=Ot[hp][0][:], func=AF.Square), reads=[Ot[hp][1]], writes=[sbb])
                        if lhs_ones is ones:
                            OP('pe', lambda e, hp=hp, st=st: e.matmul(PS[bk][:, 0:TBM], lhsT=ones[:], rhs=st[:], start=(hp == 0), stop=(hp == 1)),
                               reads=[sbb, onesB], writes=[PSB[bk]])
                        else:
                            bk2 = bk if hp == 0 else pbank()
                            OP('pe', lambda e, hp=hp, st=st, bk2=bk2: e.matmul(PS[bk2][:, 0:TBM], lhsT=bones[:], rhs=st[:], start=True, stop=True),
                               reads=[sbb, bonesB], writes=[PSB[bk2]])
                            if hp == 0:
                                bk0 = bk2
                            else:
                                bk1 = bk2
                    if lhs_ones is ones:
                        rt, rb = fa()
                        rstd_from(PS[bk], PSB[bk], TBM, 1.0 / n_ch, eps, rt, rb)
                        return [(rt, rb), (rt, rb)]
                    res = []
                    for bkx in (bk0, bk1):
                        rt, rb = fa()
                        rstd_from(PS[bkx], PSB[bkx], TBM, 1.0 / n_ch, eps, rt, rb)
                        res.append((rt, rb))
                    return res

                def evac_O():
                    Ot = []
                    for hp in range(2):
                        t, b = fa()
                        OP('act', lambda e, hp=hp, t=t: e.activation(out=t[:], in_=PS[4 + hp][:, 0:TBM], func=AF.Copy), reads=[PSB[4 + hp]], writes=[b])
                        Ot.append((t, b))
                    return Ot

                if 'hgrn' in mixers:
                    QE, KE, PCx, GT = [], [], [], []
                    for hp in range(2):
                        bq = proj_fm(l, 2 + hp, first)
                        qt, qb = evac_fm(bq, AF.Silu)
                        bf_ = proj_fm(l, 4 + hp, first)
                        st_, sb_ = evac_fm(bf_, AF.Sigmoid)
                        ft, fb = fa()
                        OP('dve', lambda e, hp=hp, st_=st_, ft=ft: e.tensor_scalar(out=ft[:], in0=st_[:], scalar1=lbt[:, 2 * l + hp:2 * l + hp + 1],
                                                                             scalar2=lbt[:, 4 + 2 * l + hp:4 + 2 * l + hp + 1], op0=ALU.mult, op1=ALU.add),
                           reads=[sb_, lbtB], writes=[fb])
                        lt, lb_ = fa()
                        OP('dve', lambda e, ft=ft, lt=lt: e.tensor_scalar_max(out=lt[:], in0=ft[:], scalar1=1e-30), reads=[fb], writes=[lb_])
                        OP('act', lambda e, lt=lt: e.activation(out=lt[:], in_=lt[:], func=AF.Ln), reads=[lb_], writes=[lb_])
                        bt, bb = scan_decay(lt, lb_)
                        ebt, ebb = LL[hp][0], LLB_[hp][0]
                        OP('act', lambda e, bt=bt, ebt=ebt: e.activation(out=ebt[:], in_=bt[:], func=AF.Exp), reads=[bb], writes=[ebb])
                        OP('act', lambda e, bt=bt: e.activation(out=bt[:], in_=bt[:], func=AF.Exp, scale=-1.0), reads=[bb], writes=[bb])
                        qe, qeb = LB[hp][0], LBB[hp][0]
                        OP('dve', lambda e, qt=qt, ebt=ebt, qe=qe: e.scalar_tensor_tensor(out=qe[:], in0=qt[:], scalar=QK, in1=ebt[:], op0=ALU.mult, op1=ALU.mult),
                           reads=[qb, ebb], writes=[qeb])
                        OP('dve', lambda e, ft=ft: e.tensor_scalar(out=ft[:], in0=ft[:], scalar1=-1.0, scalar2=1.0, op0=ALU.mult, op1=ALU.add),
                           reads=[fb], writes=[fb])
                        ke, keb = LB[hp][1], LBB[hp][1]
                        OP('dve', lambda e, ft=ft, bt=bt, ke=ke: e.tensor_tensor(out=ke[:], in0=ft[:], in1=bt[:], op=ALU.mult), reads=[fb, bb], writes=[keb])
                        bg_ = proj_fm(l, 6 + hp, first)
                        gt, gb = evac_fm(bg_, AF.Sigmoid, dst=(LL[hp][1], LLB_[hp][1]))
                        QE.append((qe, qeb)); KE.append((ke, keb)); PCx.append((ebt, ebb)); GT.append((gt, gb))
                    proj_tm(l, 0)
                    chunk_engine('hgrn', QE, KE, PCx)
                    Ot = evac_O()
                    rs = out_rstd(Ot, ones, 256, NORM_EPS)
                    for hp in range(2):
                        t1, b1 = fa()
                        OP('dve', lambda e, hp=hp, t1=t1, Ot=Ot, rs=rs: e.scalar_tensor_tensor(out=t1[:], in0=Ot[hp][0][:], scalar=pv[:, PV['hnorm'] + hp:PV['hnorm'] + hp + 1],
                                                                           in1=rs[hp][0][:], op0=ALU.mult, op1=ALU.mult),
                           reads=[Ot[hp][1], rs[hp][1], pvB[l]], writes=[b1])
                        OP('dve', lambda e, hp=hp, t1=t1, GT=GT: e.tensor_tensor(out=YT[:, 2 + hp, :], in0=t1[:], in1=GT[hp][0][:], op=ALU.mult),
                           reads=[b1, GT[hp][1]], writes=[YB[2 + hp]])
                else:
                    for ck in (2, 3):
                        OP('pool', lambda e, ck=ck: e.memset(YT[:, ck, :], 0.0), writes=[YB[ck]])

                if 'gla' in mixers:
                    bga = proj_fm(l, 22, first)
                    gat, gab = LLX[0], LLXB[0]
                    OP('act', lambda e: e.activation(out=gat[:], in_=PS[bga][:, 0:TBM], func=AF.Copy), reads=[PSB[bga]], writes=[gab])
                    QE, KE, PCx, GT = [], [], [], []
                    for hp in range(2):
                        bk = pbank()
                        OP('pe', lambda e, hp=hp, bk=bk: e.matmul(PS[bk][:, 0:TBM], lhsT=smat[:, 1024 + hp * 128:1024 + (hp + 1) * 128], rhs=gat[:], start=True, stop=True),
                           reads=[gab, smatB], writes=[PSB[bk]])
                        lt, lb_ = evac_fm(bk, AF.Sigmoid, bias=pv[:, PV['glab'] + hp:PV['glab'] + hp + 1])
                        OP('act', lambda e, lt=lt: e.activation(out=lt[:], in_=lt[:], func=AF.Ln), reads=[lb_], writes=[lb_])
                        bt, bb = scan_decay(lt, lb_)
                        ebt, ebb = LL[hp][0], LLB_[hp][0]
                        OP('act', lambda e, bt=bt, ebt=ebt: e.activation(out=ebt[:], in_=bt[:], func=AF.Exp, scale=1.0 / 16), reads=[bb], writes=[ebb])
                        OP('act', lambda e, bt=bt: e.activation(out=bt[:], in_=bt[:], func=AF.Exp, scale=-1.0 / 16), reads=[bb], writes=[bb])
                        bq = proj_fm(l, 16 + hp, first)
                        qe, qeb = LB[hp][0], LBB[hp][0]
                        OP('dve', lambda e, bq=bq, ebt=ebt, qe=qe: e.scalar_tensor_tensor(out=qe[:], in0=PS[bq][:, 0:TBM], scalar=QK, in1=ebt[:], op0=ALU.mult, op1=ALU.mult),
                           reads=[PSB[bq], ebb], writes=[qeb])
                        bkk = proj_fm(l, 18 + hp, first)
                        ke, keb = LB[hp][1], LBB[hp][1]
                        OP('dve', lambda e, bkk=bkk, bt=bt, ke=ke: e.tensor_tensor(out=ke[:], in0=PS[bkk][:, 0:TBM], in1=bt[:], op=ALU.mult), reads=[PSB[bkk], bb], writes=[keb])
                        bg_ = proj_fm(l, 20 + hp, first)
                        gt, gb = evac_fm(bg_, AF.Silu, dst=(LL[hp][1], LLB_[hp][1]))
                        QE.append((qe, qeb)); KE.append((ke, keb)); PCx.append((ebt, ebb)); GT.append((gt, gb))
                    proj_tm(l, 2)
                    chunk_engine('gla', QE, KE, PCx)
                    Ot = evac_O()
                    rs = out_rstd(Ot, bones, 64, NORM_EPS)
                    for hp in range(2):
                        t1, b1 = fa()
                        OP('dve', lambda e, hp=hp, t1=t1, Ot=Ot, rs=rs: e.scalar_tensor_tensor(out=t1[:], in0=Ot[hp][0][:], scalar=pv[:, PV['gnorm'] + hp:PV['gnorm'] + hp + 1],
                                                                           in1=rs[hp][0][:], op0=ALU.mult, op1=ALU.mult),
                           reads=[Ot[hp][1], rs[hp][1], pvB[l]], writes=[b1])
                        OP('dve', lambda e, hp=hp, t1=t1, GT=GT: e.tensor_tensor(out=YT[:, 6 + hp, :], in0=t1[:], in1=GT[hp][0][:], op=ALU.mult),
                           reads=[b1, GT[hp][1]], writes=[YB[6 + hp]])
                else:
                    for ck in (6, 7):
                        OP('pool', lambda e, ck=ck: e.memset(YT[:, ck, :], 0.0), writes=[YB[ck]])

                if 'rwkv' in mixers:
                    bwa = proj_fm(l, 14, first)
                    twa, twab = LLX[0], LLXB[0]
                    OP('act', lambda e: e.activation(out=twa[0:64, :], in_=PS[bwa][0:64, 0:TBM], func=AF.Tanh), reads=[PSB[bwa]], writes=[twab])
                    OP('act', lambda e: e.activation(out=twa[64:128, :], in_=PS[bwa][64:128, 0:TBM], func=AF.Copy), reads=[PSB[bwa]], writes=[twab])
                    bxg = proj_fm(l, 15, first)
                    sxg, sxgb = evac_fm(bxg, AF.Sigmoid, dst=(LLX[1], LLXB[1]))
                    QE, KE, AE, BE, PCx, GR, RKR, VF = [], [], [], [], [], [], [], []
                    for hp in range(2):
                        cs_ = slice(hp * 128, (hp + 1) * 128)
                        bk = pbank()
                        OP('pe', lambda e, bk=bk, hp=hp: e.matmul(PS[bk][:, 0:TBM], lhsT=smat[:, 256 + hp * 128:256 + (hp + 1) * 128], rhs=twa[:], start=True, stop=True),
                           reads=[twab, smatB], writes=[PSB[bk]])
                        lw, lwb = evac_fm(bk, AF.Sigmoid, bias=pv[:, PV['w0'] + hp:PV['w0'] + hp + 1])
                        bk = pbank()
                        OP('pe', lambda e, bk=bk, hp=hp: e.matmul(PS[bk][:, 0:TBM], lhsT=smat[:, 768 + hp * 128:768 + (hp + 1) * 128], rhs=twa[:], start=True, stop=True),
                           reads=[twab, smatB], writes=[PSB[bk]])
                        at, ab_ = evac_fm(bk, AF.Sigmoid, bias=pv[:, PV['a0'] + hp:PV['a0'] + hp + 1])
                        bk = pbank()
                        OP('pe', lambda e, bk=bk, hp=hp: e.matmul(PS[bk][:, 0:TBM], lhsT=smat[:, 512 + hp * 128:512 + (hp + 1) * 128], rhs=sxg[:], start=True, stop=True),
                           reads=[sxgb, smatB], writes=[PSB[bk]])
                        grt, grb = evac_fm(bk, AF.Copy, dst=(LL[hp][1], LLB_[hp][1]))
                        bt, bb = scan_decay(lw, lwb)
                        CW = -float(np.exp(-0.5))
                        ebt, ebb = LL[hp][0], LLB_[hp][0]
                        OP('act', lambda e, bt=bt, ebt=ebt: e.activation(out=ebt[:], in_=bt[:], func=AF.Exp, scale=CW), reads=[bb], writes=[ebb])
                        enb, enbb = fa()
                        OP('act', lambda e, bt=bt, enb=enb: e.activation(out=enb[:], in_=bt[:], func=AF.Exp, scale=-CW), reads=[bb], writes=[enbb])
                        OP('dve', lambda e, bt=bt, lw=lw: e.tensor_tensor(out=bt[:], in0=bt[:], in1=lw[:], op=ALU.subtract), reads=[bb, lwb], writes=[bb])
                        OP('act', lambda e, bt=bt: e.activation(out=bt[:], in_=bt[:], func=AF.Exp, scale=CW), reads=[bb], writes=[bb])
                        br = proj_fm(l, 8 + hp, first)
                        rt, rb = evac_fm(br, AF.Copy)
                        bkr = proj_fm(l, 10 + hp, first)
                        kt, kb = evac_fm(bkr, AF.Copy)
                        bv = proj_fm(l, 12 + hp, first)
                        vt, vb = evac_fm(bv, AF.Copy, dst=(LL[hp][2], LLB_[hp][2]))
                        kkt, kkb = fa()
                        OP('dve', lambda e, kt=kt, kkt=kkt, hp=hp: e.tensor_scalar(out=kkt[:], in0=kt[:], scalar1=pv[:, PV['kk'] + hp:PV['kk'] + hp + 1], scalar2=None, op0=ALU.mult),
                           reads=[kb, pvB[l]], writes=[kkb])
                        sq_, sqb_ = ba()
                        OP('act', lambda e, kkt=kkt, sq_=sq_: e.activation(out=sq_[:], in_=kkt[:], func=AF.Square), reads=[kkb], writes=[sqb_])
                        bk = pbank()
                        OP('pe', lambda e, bk=bk, sq_=sq_: e.matmul(PS[bk][:, 0:TBM], lhsT=bones[:], rhs=sq_[:], start=True, stop=True), reads=[sqb_, bonesB], writes=[PSB[bk]])
                        rn, rnb = fa()
                        rstd_from(PS[bk], PSB[bk], TBM, 1.0, 1e-24, rn, rnb)
                        OP('dve', lambda e, kkt=kkt, rn=rn: e.tensor_tensor(out=kkt[:], in0=kkt[:], in1=rn[:], op=ALU.mult), reads=[kkb, rnb], writes=[kkb])
                        fac, facb = fa()
                        OP('dve', lambda e, at=at, fac=fac, hp=hp: e.tensor_scalar(out=fac[:], in0=at[:], scalar1=-1.0, scalar2=pv[:, PV['ka'] + hp:PV['ka'] + hp + 1], op0=ALU.add, op1=ALU.mult),
                           reads=[ab_, pvB[l]], writes=[facb])
                        OP('dve', lambda e, fac=fac, kt=kt: e.scalar_tensor_tensor(out=kt[:], in0=fac[:], scalar=1.0, in1=kt[:], op0=ALU.add, op1=ALU.mult),
                           reads=[facb, kb], writes=[kb])
                        rk_, rkb_ = LB[hp][4], LBB[hp][4]
                        OP('dve', lambda e, rt=rt, kt=kt, rk_=rk_, hp=hp: e.scalar_tensor_tensor(out=rk_[:], in0=rt[:], scalar=pv[:, PV['rk'] + hp:PV['rk'] + hp + 1], in1=kt[:], op0=ALU.mult, op1=ALU.mult),
                           reads=[rb, kb, pvB[l]], writes=[rkb_])
                        qe, qeb = LB[hp][0], LBB[hp][0]
                        OP('dve', lambda e, rt=rt, ebt=ebt, qe=qe: e.tensor_tensor(out=qe[:], in0=rt[:], in1=ebt[:], op=ALU.mult), reads=[rb, ebb], writes=[qeb])
                        ke, keb = LB[hp][1], LBB[hp][1]
                        OP('dve', lambda e, kt=kt, enb=enb, ke=ke: e.tensor_tensor(out=ke[:], in0=kt[:], in1=enb[:], op=ALU.mult), reads=[kb, enbb], writes=[keb])
                        be, beb = LB[hp][3], LBB[hp][3]
                        OP('dve', lambda e, kkt=kkt, bt=bt, be=be: e.tensor_tensor(out=be[:], in0=kkt[:], in1=bt[:], op=ALU.mult), reads=[kkb, bb], writes=[beb])
                        OP('dve', lambda e, kkt=kkt, at=at: e.tensor_tensor(out=kkt[:], in0=kkt[:], in1=at[:], op=ALU.mult), reads=[kkb, ab_], writes=[kkb])
                        ae, aeb = LB[hp][2], LBB[hp][2]
                        OP('dve', lambda e, kkt=kkt, enb=enb, ae=ae: e.tensor_tensor(out=ae[:], in0=kkt[:], in1=enb[:], op=ALU.mult), reads=[kkb, enbb], writes=[aeb])
                        QE.append((qe, qeb)); KE.append((ke, keb)); AE.append((ae, aeb)); BE.append((be, beb)); PCx.append((ebt, ebb))
                        GR.append((grt, grb)); RKR.append((rk_, rkb_)); VF.append((vt, vb))
                    proj_tm(l, 1)
                    chunk_engine('rwkv', QE, KE, PCx, rw={'AE': AE, 'BE': BE})
                    Ot = evac_O()
                    for hp in range(2):
                        ob16, ob16b = ba()
                        OP('dve', lambda e, hp=hp, ob16=ob16, Ot=Ot: e.tensor_copy(out=ob16[:], in_=Ot[hp][0][:]), reads=[Ot[hp][1]], writes=[ob16b])
                        bk = pbank()
                        OP('pe', lambda e, bk=bk, ob16=ob16: e.matmul(PS[bk][:, 0:TBM], lhsT=bones[:], rhs=ob16[:], start=True, stop=True), reads=[ob16b, bonesB], writes=[PSB[bk]])
                        ct, cb = fa()
                        OP('dve', lambda e, hp=hp, bk=bk, ct=ct, Ot=Ot: e.scalar_tensor_tensor(out=ct[:], in0=PS[bk][:, 0:TBM], scalar=-1.0 / 64, in1=Ot[hp][0][:], op0=ALU.mult, op1=ALU.add),
                           reads=[PSB[bk], Ot[hp][1]], writes=[cb])
                        s2, s2b = ba()
                        OP('act', lambda e, ct=ct, s2=s2: e.activation(out=s2[:], in_=ct[:], func=AF.Square), reads=[cb], writes=[s2b])
                        bk = pbank()
                        OP('pe', lambda e, bk=bk, s2=s2: e.matmul(PS[bk][:, 0:TBM], lhsT=bones[:], rhs=s2[:], start=True, stop=True), reads=[s2b, bonesB], writes=[PSB[bk]])
                        rn, rnb = fa()
                        rstd_from(PS[bk], PSB[bk], TBM, 1.0 / 64, GN_EPS, rn, rnb)
                        OP('dve', lambda e, hp=hp, ct=ct, rn=rn: e.scalar_tensor_tensor(out=ct[:], in0=ct[:], scalar=pv[:, PV['lnw'] + hp:PV['lnw'] + hp + 1], in1=rn[:], op0=ALU.mult, op1=ALU.mult),
                           reads=[cb, rnb, pvB[l]], writes=[cb])
                        bk = pbank()
                        OP('pe', lambda e, bk=bk, hp=hp, RKR=RKR: e.matmul(PS[bk][:, 0:TBM], lhsT=bones[:], rhs=RKR[hp][0][:], start=True, stop=True), reads=[RKR[hp][1], bonesB], writes=[PSB[bk]])
                        bo, bob = fa()
                        OP('dve', lambda e, bk=bk, hp=hp, bo=bo, VF=VF: e.tensor_tensor(out=bo[:], in0=PS[bk][:, 0:TBM], in1=VF[hp][0][:], op=ALU.mult), reads=[PSB[bk], VF[hp][1]], writes=[bob])
                        OP('dve', lambda e, hp=hp, ct=ct, bo=bo: e.scalar_tensor_tensor(out=ct[:], in0=ct[:], scalar=pv[:, PV['lnb'] + hp:PV['lnb'] + hp + 1], in1=bo[:], op0=ALU.add, op1=ALU.add),
                           reads=[cb, bob, pvB[l]], writes=[cb])
                        OP('dve', lambda e, hp=hp, ct=ct, GR=GR: e.tensor_tensor(out=YT[:, 4 + hp, :], in0=ct[:], in1=GR[hp][0][:], op=ALU.mult),
                           reads=[cb, GR[hp][1]], writes=[YB[4 + hp]])
                else:
                    for ck in (4, 5):
                        OP('pool', lambda e, ck=ck: e.memset(YT[:, ck, :], 0.0), writes=[YB[ck]])

                for jp in range(KD // 2):
                    (i,) = acq(l, ('wo', jp))
                    for q in range(2):
                        j = 2 * jp + q
                        for m in range(KD):
                            OP('pe', lambda e, m=m, j=j, q=q, i=i: e.matmul(PS[m][:, 0:TBM], lhsT=wbf[i][:, q * D + m * 128:q * D + (m + 1) * 128], rhs=YT[:, j, :],
                                                                   start=(j == 0), stop=(j == KD - 1)),
                               reads=[wbfB[i], wbfB2[i][0], wbfB2[i][1], YB[j]], writes=[PSB[m]], sig=(m == KD - 1 or j == KD - 1))
                for m in range(KD):
                    OP('dve', lambda e, m=m: e.tensor_tensor(out=X[:, m, c0:c0 + TBM], in0=PS[m][:, 0:TBM], in1=X[:, m, c0:c0 + TBM], op=ALU.add),
                       reads=[PSB[m], XB[m][ti]], writes=[XB[m][ti]])

            def mixer_setup(l, s):
                OP('sp', lambda e: e.dma_start(out=mub[:], in_=mub_d[l]), writes=[mubB], dsem=muS)
                OP('sp', lambda e: e.dma_start(out=smat[:], in_=smat_d[l]), writes=[smatB], dsem=smS)
                OP('pool', lambda e: e.tensor_copy(out=smatb[:], in_=smat[:, 0:256]), reads=[smatB], writes=[smatB])
                for m in ('hgrn', 'gla', 'rwkv'):
                    for hp in range(2):
                        OP('pool', lambda e, m=m, hp=hp: e.memset(S32[m][hp][:], 0.0), writes=[S32B[m][hp]])
                        OP('pool', lambda e, m=m, hp=hp: e.memset(Sbf[m][hp][:], 0.0), writes=[SbfB[m][hp]])

        if do_mix:
            OP('act', lambda e: e.activation(out=lbt[:, 0:2], in_=pvec[0][:, PV['lb0']:PV['lb0'] + 2], func=AF.Exp), reads=[pvB[0]], writes=[lbtB])
            OP('act', lambda e: e.activation(out=lbt[:, 2:4], in_=pvec[L - 1][:, PV['lbl']:PV['lbl'] + 2], func=AF.Exp), reads=[pvB[L - 1], lbtB], writes=[lbtB])
            OP('dve', lambda e: e.tensor_tensor(out=lbt[:, 4:6], in0=lbt[:, 0:2], in1=lbt[:, 2:4], op=ALU.add), reads=[lbtB], writes=[lbtB])
            OP('dve', lambda e: e.reciprocal(out=lbt[:, 4:6], in_=lbt[:, 4:6]), reads=[lbtB], writes=[lbtB])
            OP('dve', lambda e: e.tensor_tensor(out=lbt[:, 6:8], in0=lbt[:, 2:4], in1=lbt[:, 4:6], op=ALU.mult), reads=[lbtB], writes=[lbtB])
            OP('dve', lambda e: e.memset(lbt[:, 4:6], 0.0), reads=[lbtB], writes=[lbtB])
            OP('dve', lambda e: e.tensor_scalar(out=lbt[:, 0:4], in0=lbt[:, 4:8], scalar1=-1.0, scalar2=1.0, op0=ALU.mult, op1=ALU.add), reads=[lbtB], writes=[lbtB])

        for s in range(NS):
            for k in range(KD):
                OP('sp', lambda e, k=k, s=s: e.dma_start(out=X[:, k, :], in_=xT[s, :, k, :]), writes=XB[k], dsem=xS[k])
            for l in range(L):
                if do_ffn:
                    for ti in range(NT):
                        ffn(l, 0, ti)
                if do_mix:
                    mixer_setup(l, s)
                    for bi in range(T // TBM):
                        mixer_block(l, s, bi)
                if do_ffn:
                    for ti in range(NT):
                        ffn(l, 1, ti)
            for ti in range(NT):
                c0 = ti * TB
                for k in range(KD):
                    i = counters['sq'] % 2
                    counters['sq'] += 1
                    OP('act', lambda e, k=k, i=i, c0=c0: e.activation(out=sq[i][:], in_=X[:, k, c0:c0 + TB], func=AF.Square),
                       reads=[XB[k][ti]], writes=[sqB[i]])
                    OP('pe', lambda e, k=k, i=i: e.matmul(PS[0][:, :], lhsT=ones[:], rhs=sq[i][:], start=(k == 0), stop=(k == KD - 1)),
                       reads=[sqB[i], onesB], writes=[PSB[0]])
                rstd_from(PS[0], PSB[0], TB, 1.0 / D, NORM_EPS, rstd, rstdB)
                for k in range(KD):
                    OP('dve', lambda e, k=k, c0=c0: e.scalar_tensor_tensor(out=X[:, k, c0:c0 + TB], in0=X[:, k, c0:c0 + TB],
                                                                        scalar=pvec[0][:, PV['nfin'] + k:PV['nfin'] + k + 1], in1=rstd[:],
                                                                        op0=ALU.mult, op1=ALU.mult),
                       reads=[XB[k][ti], rstdB, pvB[0]], writes=[XB[k][ti]])
            for k in range(KD):
                OP('sp', lambda e, k=k, s=s: e.dma_start(out=outT[s, :, k, :], in_=X[:, k, :]), reads=XB[k], writes=XB[k], dsem=oS[k])
        pg.ops['sp'].append((lambda e: e.nop(), {o_: o_.count for o_ in oS}, False, None))
        pg.emit(block, esems)
    return nc, pg


def _col(v, n):
    return np.ascontiguousarray(np.asarray(v, np.float32).reshape(n, 128).T)


def make_consts():
    cst = np.zeros((128, 1600), np.float32)
    p = np.arange(64)[:, None]
    f = np.arange(64)[None, :]
    for h in range(4):
        cst[0:64, 0 + h * 64:0 + (h + 1) * 64] = (p <= f)
        cst[0:64, 256 + h * 64:256 + (h + 1) * 64] = (p < f)
        cst[0:64, 512 + h * 64:512 + (h + 1) * 64] = (f < p)
        cst[0:64, 768 + h * 64:768 + (h + 1) * 64] = (p == f)
    scm = np.ones(512, np.float32)
    scm[::64] = 0.0
    cst[:, 1024:1536] = scm[None, :]
    wins = {(0, 0): 2, (0, 1): 4, (1, 0): 8, (1, 1): 16}
    for ck in range(2):
        for half in range(2):
            w = wins[(ck, half)]
            t = np.arange(16)
            cst[half * 64:(half + 1) * 64, 1536 + ck * 16:1536 + ck * 16 + 16] = (1.0 / np.minimum(t + 1, w))[None, :]
    return cst


def prep_weights(inp, L):
    out = {}
    f32 = np.float32
    for l in range(L):
        for w, (wi, wo) in enumerate((('ffn1_w_in', 'ffn1_w_out'), ('ffn2_w_in', 'ffn2_w_out'))):
            W = np.asarray(inp[wi][l], f32)
            Wk = W.reshape(KD, 128, 2 * FF)
            g = Wk[:, :, :FF].reshape(KD, 128, NJ, 128)
            u = Wk[:, :, FF:].reshape(KD, 128, NJ, 128)
            blk = np.concatenate([g, u], axis=3)
            out[f"f{w + 1}_win{l}"] = np.ascontiguousarray(blk.transpose(2, 1, 0, 3)).reshape(NJ, 128, KD * 256)
            out[f"f{w + 1}_wout{l}"] = np.ascontiguousarray(np.asarray(inp[wo][l], f32).reshape(NJ, 128, D))
        W = np.asarray(inp['w_in'][l], f32).reshape(KD, 128, DIN)
        fm = np.zeros((len(FMG), 128, KD, 128), f32)
        for gi, (c0, nc_, _) in enumerate(FMG):
            nc_ = min(nc_, DIN - c0)
            fm[gi, :, :, :nc_] = W[:, :, c0:c0 + nc_].transpose(1, 0, 2)
        out[f"m_fm{l}"] = fm.reshape(len(FMG), 128, KD * 128)
        tm = np.zeros((len(TMG), 128, KD, 256), f32)
        for gi, (c0, nc_, _) in enumerate(TMG):
            tm[gi] = W[:, :, c0:c0 + nc_].transpose(1, 0, 2)
        out[f"m_tm{l}"] = tm.reshape(len(TMG), 128, KD * 256)
        out[f"m_wout{l}"] = np.ascontiguousarray(np.asarray(inp['w_out'][l], f32).reshape(KD, 128, D))
        pv = np.zeros((128, NPV), f32)
        pv[:, PV['nf1']:PV['nf1'] + 8] = _col(inp['norm_ffn1'][l], 8)
        pv[:, PV['nmx']:PV['nmx'] + 8] = _col(inp['norm_mix'][l], 8)
        pv[:, PV['nf2']:PV['nf2'] + 8] = _col(inp['norm_ffn2'][l], 8)
        pv[:, PV['nfin']:PV['nfin'] + 8] = _col(inp['norm_final'], 8)
        pv[:, PV['lb0']:PV['lb0'] + 2] = _col(inp['hgrn_lb_logits'][0], 2)
        pv[:, PV['lbl']:PV['lbl'] + 2] = _col(inp['hgrn_lb_logits'][l], 2)
        for nm, key in (('pool_b', 'pool_b'), ('pool_s', 'pool_scale'), ('hnorm', 'hgrn_norm'), ('w0', 'rwkv_w0'), ('a0', 'rwkv_a0'),
                        ('kk', 'rwkv_k_k'), ('ka', 'rwkv_k_a'), ('rk', 'rwkv_r_k'), ('lnw', 'rwkv_ln_w'), ('lnb', 'rwkv_ln_b'),
                        ('glab', 'gla_b'), ('gnorm', 'gla_norm')):
            pv[:, PV[nm]:PV[nm] + 2] = _col(inp[key][l], 2)
        out[f"pvec{l}"] = pv
        out[f"mub{l}"] = np.ascontiguousarray(np.broadcast_to(np.asarray(inp['rwkv_mu'][l], f32)[None, :], (128, 1024)))
        sm = np.zeros((128, 5 * 256), f32)
        pw_ = np.asarray(inp['pool_w'][l], f32)
        for ck in range(2):
            sm[0:64, ck * 128:ck * 128 + 64] = pw_[2 * ck]
            sm[64:128, ck * 128 + 64:ck * 128 + 128] = pw_[2 * ck + 1]
        sm[0:64, 256:512] = np.asarray(inp['rwkv_w2'][l], f32)
        sm[64:128, 768:1024] = np.asarray(inp['rwkv_a2'][l], f32)
        sm[:, 512:768] = np.asarray(inp['rwkv_g2'][l], f32)
        sm[0:16, 1024:1280] = np.asarray(inp['gla_w2'][l], f32)
        out[f"smat{l}"] = sm
    out["cst"] = make_consts()
    return out


def prep_x(xc):
    NS, T, _ = xc.shape
    return np.ascontiguousarray(xc.reshape(NS, T, KD, 128).transpose(0, 3, 2, 1))


def unprep_out(o):
    NS, _, _, T = o.shape
    return np.ascontiguousarray(o.transpose(0, 3, 2, 1)).reshape(NS, T, D)


def kernel(**inputs):
    x = np.asarray(inputs['x'], np.float32)
    B, T, _ = x.shape
    NCORES = 8
    NS = B // NCORES
    L = 2
    nc, pg = build_program(T, NS, L)
    wts = prep_weights(inputs, L)
    in_maps = []
    for c in range(NCORES):
        m = dict(wts)
        m["xT"] = prep_x(x[c * NS:(c + 1) * NS])
        in_maps.append(m)
    res = run_bass_kernel_spmd(nc, in_maps, core_ids=list(range(NCORES)))
    outs = [unprep_out(np.asarray(r["outT"])) for r in res.results]
    return np.concatenate(outs, axis=0).astype(np.float32)
```

```python
import numpy as np
from contextlib import ExitStack
import concourse.bass as bass
import concourse.mybir as mybir
from concourse.bass_utils import run_bass_kernel_spmd

F32 = mybir.dt.float32
BF16 = mybir.dt.bfloat16
AF = mybir.ActivationFunctionType
ALU = mybir.AluOpType
ENGS = ['pe', 'act', 'dve', 'pool', 'sp']

D = 1024
KD = 8
FF = 2816
NJ = 22
G = 256
DIN = 3344
NORM_EPS = 1e-6
GN_EPS = 64e-5
QK = 0.125
TB = 512
TBM = 256
CH = 64
NCH = TBM // CH
DEBUG_STAGE = 99

FMG = [(0, 128, 0), (128, 128, 0), (256, 128, 0), (384, 128, 0), (512, 128, 0), (640, 128, 0),
       (1024, 128, 0), (1152, 128, 0),
       (1280, 128, 1), (1408, 128, 1), (1536, 128, 1), (1664, 128, 1), (1792, 128, 1), (1920, 128, 1),
       (2048, 128, 1), (2176, 128, 1),
       (2304, 128, 0), (2432, 128, 0), (2560, 128, 0), (2688, 128, 0), (3072, 128, 0), (3200, 128, 0),
       (3328, 128, 0)]
TMG = [(768, 256, 0), (1792, 256, 1), (2816, 256, 0)]
RW0 = 1280

PV = {}
_o = 0
for _n, _w in [('nf1', 8), ('nmx', 8), ('nf2', 8), ('pool_b', 2), ('pool_s', 2), ('lb0', 2), ('lbl', 2),
               ('hnorm', 2), ('w0', 2), ('a0', 2), ('kk', 2), ('ka', 2), ('rk', 2), ('lnw', 2), ('lnb', 2),
               ('glab', 2), ('gnorm', 2), ('nfin', 8)]:
    PV[_n] = _o
    _o += _w
NPV = _o


class Buf:
    __slots__ = ('name', 'w', 'r', 'const')

    def __init__(self, name, const=False):
        self.name = name
        self.w = None
        self.r = []
        self.const = const


class DSem:
    def __init__(self, h):
        self.h = h
        self.count = 0


class Prog:
    def __init__(self, nc, same_engine_sync=True):
        self.nc = nc
        self.ops = {e: [] for e in ENGS}
        self.cnt = {e: 0 for e in ENGS}
        self.same = same_engine_sync
        self.nops = 0
        self.last_rg = None
        self.last_pe_sig = True

    def op(self, eng, fn, reads=(), writes=(), sig=True, dsem=None, rg=None):
        waits = {}
        if eng == 'pe':
            if rg is not None and self.last_rg is not None and rg != self.last_rg:
                assert self.last_pe_sig
                waits['pe'] = self.cnt['pe']
            self.last_rg = rg
            self.last_pe_sig = sig

        def addw(tok):
            if tok is None:
                return
            k, v = tok
            if k == eng and (eng == 'pe' or not self.same):
                return
            if waits.get(k, 0) < v:
                waits[k] = v
        for b in reads:
            addw(b.w)
        for b in writes:
            addw(b.w)
            for t in b.r:
                addw(t)
        if dsem is not None:
            dsem.count += 16
            tok = (dsem, dsem.count)
            sig = False
        elif sig:
            self.cnt[eng] += 1
            tok = (eng, self.cnt[eng])
        else:
            tok = (eng, self.cnt[eng] + 1)
        for b in reads:
            if not b.const:
                b.r.append(tok)
                if len(b.r) > 48:
                    mx = {}
                    for k, v in b.r:
                        if mx.get(k, 0) < v:
                            mx[k] = v
                    b.r = list(mx.items())
        for b in writes:
            b.w = tok
            b.r = []
        self.ops[eng].append((fn, waits, sig, dsem))
        self.nops += 1
        return tok

    def emit(self, block, esems):
        nc = self.nc
        deco = {'pe': block.tensor, 'act': block.scalar, 'dve': block.vector,
                'pool': block.gpsimd, 'sp': block.sync}
        for e in ENGS:
            ops = self.ops[e]

            def body(eng, ops=ops, e=e):
                known = {}
                for fn, waits, sig, dsem in ops:
                    for k, v in waits.items():
                        if known.get(k, 0) >= v:
                            continue
                        known[k] = v
                        h = k.h if isinstance(k, DSem) else esems[k]
                        eng.wait_ge(h, v)
                    inst = fn(eng)
                    if dsem is not None:
                        inst.then_inc(dsem.h, 16)
                    elif sig:
                        inst.then_inc(esems[e], 1)
            deco[e](body)


def build_program(T, NS, L, mixers=('pool', 'hgrn', 'rwkv', 'gla'), do_ffn=True, do_mix=True):
    NT = T // TB
    nc = bass.Bass("TRN2", target_bir_lowering=False)
    dr = {}

    def din(name, shape):
        dr[name] = nc.dram_tensor(name, list(shape), F32, kind="ExternalInput").ap()
        return dr[name]
    xT = din("xT", [NS, 128, KD, T])
    outT = nc.dram_tensor("outT", [NS, 128, KD, T], F32, kind="ExternalOutput").ap()
    f_win = [[din(f"f{w}_win{l}", [NJ, 128, KD * 256]) for w in (1, 2)] for l in range(L)]
    f_wout = [[din(f"f{w}_wout{l}", [NJ, 128, D]) for w in (1, 2)] for l in range(L)]
    m_fm = [din(f"m_fm{l}", [len(FMG), 128, KD * 128]) for l in range(L)]
    m_tm = [din(f"m_tm{l}", [len(TMG), 128, KD * 256]) for l in range(L)]
    m_wout = [din(f"m_wout{l}", [KD, 128, D]) for l in range(L)]
    pvec_d = [din(f"pvec{l}", [128, NPV]) for l in range(L)]
    mub_d = [din(f"mub{l}", [128, 1024]) for l in range(L)]
    smat_d = [din(f"smat{l}", [128, 5 * 256]) for l in range(L)]
    cst_d = din("cst", [128, 1600])
    es = ExitStack()
    with es:
        def sb(name, shape, dt=F32):
            return es.enter_context(nc.sbuf_tensor("sb_" + name, list(shape), dt))

        def psum(name, shape, dt=F32):
            return es.enter_context(nc.psum_tensor("pp_" + name, list(shape), dt))
        esems = {e: es.enter_context(nc.semaphore("s_" + e)) for e in ENGS}

        def dsem(name):
            return DSem(es.enter_context(nc.semaphore(name)))
        pg = Prog(nc)
        block = es.enter_context(nc.Block())

        X = sb("X", [128, KD, T])
        XB = [[Buf(f"X{k}_{t}") for t in range(NT)] for k in range(KD)]
        xn = sb("xn", [128, KD, TB], BF16)
        xns = sb("xns", [128, KD, TBM], BF16)
        xnsB = Buf("xns")
        xnB = [Buf(f"xn{k}") for k in range(KD)]
        sq = [sb(f"sq{i}", [128, TB], BF16) for i in range(2)]
        sqB = [Buf(f"sq{i}") for i in range(2)]
        rstd = sb("rstd", [128, TB]); rstdB = Buf("rstd")
        ones = sb("ones", [128, 128], BF16); onesB = Buf("ones", const=True)
        bones = sb("bones", [128, 128], BF16)
        ident = sb("ident", [128, 128], BF16)
        cst = sb("cst", [128, 1600])
        cstB = Buf("cst", const=True)
        MI, MSU, MSL, ID4 = 0, 256, 512, 768
        SCM = 1024
        ICN = 1536
        pvec = [sb(f"pvec{l}", [128, NPV]) for l in range(L)]
        pvB = [Buf(f"pvec{l}", const=True) for l in range(L)]
        lbt = sb("lbt", [128, 8])
        mub = sb("mub", [128, 1024]); mubB = Buf("mub")
        omub = sb("omub", [128, 1024]); omubB = Buf("omub")
        smat = sb("smat", [128, 5 * 256]); smatB = Buf("smat")
        smatb = sb("smatb", [128, 2 * 128], BF16)
        wst = [sb(f"wst{i}", [128, KD * 256]) for i in range(2)]
        wstB = [Buf(f"wst{i}") for i in range(2)]
        wstS = [dsem(f"dwst{i}") for i in range(2)]
        wbf = [sb(f"wbf{i}", [128, KD * 256], BF16) for i in range(2)]
        wbfB = [Buf(f"wbf{i}") for i in range(2)]
        wbfB2 = [[Buf(f"wbf{i}a"), Buf(f"wbf{i}b")] for i in range(2)]
        hT = sb("hT", [128, NJ, TB], BF16)
        hB = [Buf(f"h{j}") for j in range(NJ)]
        sg = [sb("sg0", [128, TB])] * 2
        sgB = [Buf("sg0")] * 2
        PS = [psum(f"ps{i}", [128, 512]) for i in range(8)]
        PSB = [Buf(f"ps{i}") for i in range(8)]
        xS = [dsem(f"dx{k}") for k in range(KD)]
        oS = [dsem(f"dout{k}") for k in range(KD)]
        cS = dsem("dcst")
        pS = [dsem(f"dpv{l}") for l in range(L)]
        muS = dsem("dmu")
        smS = dsem("dsm")
        counters = {'w': 0, 'o': 0, 'sq': 0, 'sg': 0, 'pp': 0}

        def OP(eng, fn, reads=(), writes=(), sig=True, dsem=None, rg=None):
            return pg.op(eng, fn, reads, writes, sig, dsem, rg)

        def load_w(src_ap, ncols):
            i = counters['w'] % 2
            counters['w'] += 1
            OP('sp', lambda e: e.dma_start(out=wst[i][:, 0:ncols], in_=src_ap), writes=[wstB[i]], dsem=wstS[i])
            return i

        def load_w2(src_ap2):
            i = counters['w'] % 2
            counters['w'] += 1
            OP('sp', lambda e: e.dma_start(out=wst[i][:, 0:2 * D].rearrange("p (j c) -> p j c", j=2), in_=src_ap2.rearrange("j p c -> p j c")),
               writes=[wstB[i]], dsem=wstS[i])
            return i

        def cast_w(i, ncols, eng='pool'):
            if eng == 'act':
                OP('act', lambda e: e.activation(out=wbf[i][:, 0:ncols], in_=wst[i][:, 0:ncols], func=AF.Copy),
                   reads=[wstB[i]], writes=[wbfB[i], wbfB2[i][0], wbfB2[i][1]])
            else:
                OP(eng, lambda e: e.tensor_copy(out=wbf[i][:, 0:ncols], in_=wst[i][:, 0:ncols]),
                   reads=[wstB[i]], writes=[wbfB[i], wbfB2[i][0], wbfB2[i][1]])

        def cast_w_split(i, ncols):
            c1 = (ncols * 3 // 4) // 128 * 128
            OP('dve', lambda e: e.tensor_copy(out=wbf[i][:, 0:c1], in_=wst[i][:, 0:c1]), reads=[wstB[i]], writes=[wbfB2[i][0], wbfB[i]])
            OP('pool', lambda e: e.tensor_copy(out=wbf[i][:, c1:ncols], in_=wst[i][:, c1:ncols]), reads=[wstB[i]], writes=[wbfB2[i][1]])

        def rstd_from(psb, psbuf, n, scale, eps, dst, dstB):
            OP('act', lambda e: e.activation(out=dst[:, 0:n], in_=psb[:, 0:n], func=AF.Ln, scale=scale, bias=eps),
               reads=[psbuf], writes=[dstB])
            OP('act', lambda e: e.activation(out=dst[:, 0:n], in_=dst[:, 0:n], func=AF.Exp, scale=-0.5),
               reads=[dstB], writes=[dstB])

        def rmsnorm_to_xn(l, gcol, c0, n, bank):
            ti = c0 // TB
            for k in range(KD):
                i = counters['sq'] % 2
                counters['sq'] += 1
                OP('act', lambda e, k=k, i=i: e.activation(out=sq[i][:, 0:n], in_=X[:, k, c0:c0 + n], func=AF.Square),
                   reads=[XB[k][ti]], writes=[sqB[i]])
                OP('pe', lambda e, k=k, i=i: e.matmul(PS[bank][:, 0:n], lhsT=ones[:], rhs=sq[i][:, 0:n], start=(k == 0), stop=(k == KD - 1)),
                   reads=[sqB[i], onesB], writes=[PSB[bank]])
            rstd_from(PS[bank], PSB[bank], n, 1.0 / D, NORM_EPS, rstd, rstdB)
            for k in range(KD):
                OP('dve', lambda e, k=k: e.scalar_tensor_tensor(out=xn[:, k, 0:n], in0=X[:, k, c0:c0 + n],
                                                                 scalar=pvec[l][:, gcol + k:gcol + k + 1], in1=rstd[:, 0:n],
                                                                 op0=ALU.mult, op1=ALU.mult),
                   reads=[XB[k][ti], rstdB, pvB[l]], writes=[xnB[k]])

        def ffn(l, w, ti):
            gcol = PV['nf1'] if w == 0 else PV['nf2']
            c0 = ti * TB
            blocks = [('in', j) for j in range(NJ)] + [('out', jp) for jp in range(NJ // 2)]

            slots = {}

            def do_load(t):
                if t < len(blocks) and t not in slots:
                    kind_, j_ = blocks[t]
                    if kind_ == 'in':
                        slots[t] = load_w(f_win[l][w][j_], KD * 256)
                    else:
                        slots[t] = load_w2(f_wout[l][w][2 * j_:2 * j_ + 2])

            def do_ready(t):
                if t >= len(blocks):
                    return
                do_load(t)
                cast_w_split(slots[t], KD * 256)
            do_load(0)
            do_load(1)
            do_ready(0)
            rmsnorm_to_xn(l, gcol, c0, TB, 0)
            for t, (kind, j) in enumerate(blocks):
                i = slots[t]
                do_ready(t + 1)
                do_load(t + 2)
                if kind == 'in':
                    wv = wbf[i][:].rearrange("p (k c) -> p k c", k=KD)
                    pp = counters['pp'] % 2
                    counters['pp'] += 1
                    bg, bu = 2 * pp, 2 * pp + 1
                    for half, bk in ((0, bg), (1, bu)):
                        for k in range(KD):
                            OP('pe', lambda e, k=k, half=half, bk=bk, wv=wv: e.matmul(
                                PS[bk][:, :], lhsT=wv[:, k, half * 128:(half + 1) * 128], rhs=xn[:, k, 0:TB],
                                start=(k == 0), stop=(k == KD - 1)),
                               reads=[wbfB[i], wbfB2[i][0], wbfB2[i][1], xnB[k]], writes=[PSB[bk]], sig=(k == KD - 1))
                    si = counters['sg'] % 2
                    counters['sg'] += 1
                    OP('act', lambda e, si=si, bg=bg: e.activation(out=sg[si][:], in_=PS[bg][:, :], func=AF.Silu),
                       reads=[PSB[bg]], writes=[sgB[si]])
                    OP('dve', lambda e, si=si, bu=bu, j=j: e.tensor_tensor(out=hT[:, j, :], in0=sg[si][:], in1=PS[bu][:, :], op=ALU.mult),
                       reads=[sgB[si], PSB[bu]], writes=[hB[j]])
                else:
                    for q in range(2):
                        jj = 2 * j + q
                        for m in range(KD):
                            OP('pe', lambda e, m=m, jj=jj, q=q, i=i: e.matmul(PS[m][:, :], lhsT=wbf[i][:, q * D + m * 128:q * D + (m + 1) * 128], rhs=hT[:, jj, :],
                                                                   start=(jj == 0), stop=(jj == NJ - 1)),
                               reads=[wbfB[i], wbfB2[i][0], wbfB2[i][1], hB[jj]], writes=[PSB[m]], sig=(m == KD - 1 or jj == NJ - 1))
            for m in range(KD):
                OP('dve', lambda e, m=m: e.scalar_tensor_tensor(out=X[:, m, c0:c0 + TB], in0=PS[m][:, :], scalar=0.5,
                                                                 in1=X[:, m, c0:c0 + TB], op0=ALU.mult, op1=ALU.add),
                   reads=[PSB[m], XB[m][ti]], writes=[XB[m][ti]])

        OP('sp', lambda e: e.dma_start(out=cst[:], in_=cst_d), writes=[cstB], dsem=cS)
        for l in range(L):
            OP('sp', lambda e, l=l: e.dma_start(out=pvec[l][:], in_=pvec_d[l]), writes=[pvB[l]], dsem=pS[l])
        OP('pool', lambda e: e.memset(ones[:], 1.0), writes=[onesB])
        bonesB = Buf("bones", const=True)
        OP('pool', lambda e: e.memset(bones[:], 0.0), writes=[bonesB])
        OP('pool', lambda e: e.memset(bones[0:64, 0:64], 1.0), writes=[bonesB])
        OP('pool', lambda e: e.memset(bones[64:128, 64:128], 1.0), writes=[bonesB])
        identB = Buf("ident", const=True)
        OP('pool', lambda e: e.memset(ident[:], 1.0), writes=[identB])
        OP('pool', lambda e: e.affine_select(out=ident[:], in_=ident[:], pattern=[[-1, 128]], compare_op=ALU.is_equal,
                                             fill=0.0, base=0, channel_multiplier=1), reads=[identB], writes=[identB])

        if do_mix:
            YT = sb("YT", [128, KD, TBM], BF16)
            YB = [Buf(f"Y{k}") for k in range(KD)]
            NFA = 10
            FA = [sb(f"fa{i}", [128, TBM]) for i in range(NFA)]
            FAB = [Buf(f"fa{i}") for i in range(NFA)]
            NBA = 4
            BA = [sb(f"ba{i}", [128, TBM], BF16) for i in range(NBA)]
            BAB = [Buf(f"ba{i}") for i in range(NBA)]
            LL = [[sb(f"ll{hp}{i}", [128, TBM]) for i in range(3)] for hp in range(2)]
            LLB_ = [[Buf(f"ll{hp}{i}") for i in range(3)] for hp in range(2)]
            LLX = [sb(f"llx{i}", [128, TBM]) for i in range(2)]
            LLXB = [Buf(f"llx{i}") for i in range(2)]
            LB = [[sb(f"lb{hp}{i}", [128, TBM], BF16) for i in range(5)] for hp in range(2)]
            LBB = [[Buf(f"lb{hp}{i}") for i in range(5)] for hp in range(2)]
            PB16 = [sb(f"pb16{i}", [128, TBM], BF16) for i in range(2)]
            PB16B = [Buf(f"pb16{i}") for i in range(2)]
            Vt = [sb(f"vt{c}", [64, 256], BF16) for c in range(NCH)]
            VtB = [Buf(f"vt{c}") for c in range(NCH)]
            KEt = [sb(f"ket{c}", [64, 256], BF16) for c in range(NCH)]
            KEtB = [Buf(f"ket{c}") for c in range(NCH)]
            AEt = [sb(f"aet{c}", [64, 256], BF16) for c in range(NCH)]
            AEtB = [Buf(f"aet{c}") for c in range(NCH)]
            alias_ctr = [0]

            def mk(name, n):
                ts, bs = [], []
                for c in range(n):
                    idx = alias_ctr[0]
                    alias_ctr[0] += 1
                    j, half = idx // 2, idx % 2
                    ts.append(hT[0:64, j, half * 256:(half + 1) * 256])
                    bs.append(hB[j])
                return ts, bs
            ATs, ATsB = mk("ats", NCH)
            LKs, LKsB = mk("lks", NCH)
            ARs, ARsB = mk("ars", NCH)
            Nn, NnB = mk("nn", NCH)
            NTn, NTnB = mk("ntn", NCH)
            Nn2, Nn2B = mk("nn2", NCH)
            NTn2, NTn2B = mk("ntn2", NCH)
            Pn, PnB = mk("pn", NCH)
            Pn2, Pn2B = mk("pn2", NCH)
            Ysb = sb("ysb", [64, 256], BF16); YsbB = Buf("ysb")
            Usb = sb("usb", [64, 256], BF16); UsbB = Buf("usb")
            S32 = {m: [sb(f"s32{m}{hp}", [128, 64]) for hp in range(2)] for m in ('hgrn', 'gla', 'rwkv')}
            Sbf = {m: [sb(f"sbf{m}{hp}", [128, 64], BF16) for hp in range(2)] for m in ('hgrn', 'gla', 'rwkv')}
            S32B = {m: [Buf(f"s32{m}{hp}") for hp in range(2)] for m in ('hgrn', 'gla', 'rwkv')}
            SbfB = {m: [Buf(f"sbf{m}{hp}") for hp in range(2)] for m in ('hgrn', 'gla', 'rwkv')}
            stmp = [sb(f"stmp{hp}", [128, 64]) for hp in range(2)]
            stmpB = [Buf(f"stmp{hp}") for hp in range(2)]
            pext = sb("pext", [128, 2, 16 + TBM]); pextB = Buf("pext")
            pw = [sb(f"pw{i}", [128, 2, 16 + TBM]) for i in range(2)]
            pwB = [Buf(f"pw{i}") for i in range(2)]
            PST = PS[7][:, :].bitcast(BF16)
            lbtB = Buf("lbt", const=True)
            fa_ctr = [0]
            ba_ctr = [0]

            def fa():
                i = fa_ctr[0] % NFA
                fa_ctr[0] += 1
                return FA[i], FAB[i]

            def ba():
                i = ba_ctr[0] % NBA
                ba_ctr[0] += 1
                return BA[i], BAB[i]

            def pbank():
                b = counters['pp'] % 4
                counters['pp'] += 1
                return b

            wplan = {'plan': [], 'pos': 0, 'issued': {}, 'l': 0}

            def issue_load(l, d):
                kind, g = d
                if kind == 'fm':
                    return load_w(m_fm[l][g], KD * 128)
                if kind == 'wo':
                    return load_w2(m_wout[l][2 * g:2 * g + 2])
                return load_w(m_tm[l][g], KD * 256)

            def issue_cast(l, d, i):
                kind, g = d
                if kind == 'wo':
                    cast_w_split(i, 2 * D)
                    return (i,)
                if kind == 'fm':
                    col0, ncols, shift = FMG[g]
                    if shift:
                        mc = col0 - RW0
                        mu_b = mub[:, mc:mc + 128].unsqueeze(1).broadcast_to([128, KD, 128])
                        omu_b = omub[:, mc:mc + 128].unsqueeze(1).broadcast_to([128, KD, 128])
                        wv32 = wst[i][:, 0:KD * 128].rearrange("p (k c) -> p k c", k=KD)
                        wbv = wbf[i][:].rearrange("p (k c) -> p k c", k=KD)
                        OP('dve', lambda e: e.tensor_tensor(out=wbv[:, :, 0:128], in0=wv32, in1=omu_b, op=ALU.mult),
                           reads=[wstB[i], omubB], writes=[wbfB2[i][0], wbfB[i]])
                        OP('pool', lambda e: e.tensor_tensor(out=wbv[:, :, 128:256], in0=wv32, in1=mu_b, op=ALU.mult),
                           reads=[wstB[i], mubB], writes=[wbfB2[i][1]])
                    else:
                        cast_w(i, KD * 128, 'act')
                        wbv = wbf[i][:, 0:KD * 128].rearrange("p (k c) -> p k c", k=KD)
                    return (i, wbv)
                col0, ncols, shift = TMG[g]
                i2 = None
                wb2 = None
                if shift:
                    i2 = counters['w'] % 2
                    counters['w'] += 1
                    mc = col0 - RW0
                    mu_b = mub[:, mc:mc + 256].unsqueeze(1).broadcast_to([128, KD, 256])
                    omu_b = omub[:, mc:mc + 256].unsqueeze(1).broadcast_to([128, KD, 256])
                    wv32 = wst[i][:].rearrange("p (k c) -> p k c", k=KD)
                    wa = wbf[i][:].rearrange("p (k c) -> p k c", k=KD)
                    wb2 = wbf[i2][:].rearrange("p (k c) -> p k c", k=KD)
                    OP('dve', lambda e: e.tensor_tensor(out=wa, in0=wv32, in1=omu_b, op=ALU.mult),
                       reads=[wstB[i], omubB], writes=[wbfB[i], wbfB2[i][0], wbfB2[i][1]])
                    OP('pool', lambda e: e.tensor_tensor(out=wb2, in0=wv32, in1=mu_b, op=ALU.mult),
                       reads=[wstB[i], mubB], writes=[wbfB[i2], wbfB2[i2][0], wbfB2[i2][1]])
                else:
                    cast_w_split(i, KD * 256)
                    wa = wbf[i][:].rearrange("p (k c) -> p k c", k=KD)
                return (i, i2, wa, wb2)

            def two_slot(d):
                return d[0] == 'tm' and bool(TMG[d[1]][2])

            def ensure_load(l, pos):
                plan = wplan['plan']
                if pos < len(plan) and pos not in wplan['loaded'] and not two_slot(plan[pos]):
                    wplan['loaded'][pos] = issue_load(l, plan[pos])

            def ensure_cast(l, pos):
                plan = wplan['plan']
                if pos < len(plan) and pos not in wplan['issued'] and not two_slot(plan[pos]):
                    ensure_load(l, pos)
                    wplan['issued'][pos] = issue_cast(l, plan[pos], wplan['loaded'].pop(pos))

            def acq(l, d):
                pos = wplan['pos']
                plan = wplan['plan']
                assert plan[pos] == d, (plan[pos], d)
                if pos not in wplan['issued']:
                    if pos not in wplan['loaded']:
                        wplan['loaded'][pos] = issue_load(l, d)
                    wplan['issued'][pos] = issue_cast(l, d, wplan['loaded'].pop(pos))
                info = wplan['issued'].pop(pos)
                wplan['pos'] = pos + 1
                if not two_slot(d) and not (pos + 1 < len(plan) and two_slot(plan[pos + 1])):
                    ensure_cast(l, pos + 1)
                    if not (pos + 2 < len(plan) and two_slot(plan[pos + 2])):
                        ensure_load(l, pos + 2)
                return info

            def proj_fm(l, g, first_block):
                col0, ncols, shift = FMG[g]
                i, wbv = acq(l, ('fm', g))
                bk = pbank()
                nmm = KD * (2 if shift else 1)
                n = 0
                for k in range(KD):
                    n += 1
                    OP('pe', lambda e, k=k, n=n: e.matmul(PS[bk][0:ncols, 0:TBM], lhsT=wbv[:, k, 0:ncols], rhs=xn[:, k, 0:TBM],
                                                          start=(n == 1), stop=(n == nmm)),
                       reads=[wbfB[i], wbfB2[i][0], wbfB2[i][1], xnB[k]], writes=[PSB[bk]], sig=(n == nmm))
                if shift:
                    for k in range(KD):
                        n += 1
                        OP('pe', lambda e, k=k, n=n: e.matmul(PS[bk][0:ncols, 0:TBM], lhsT=wbv[:, k, 128:128 + ncols], rhs=xns[:, k, 0:TBM],
                                                              start=False, stop=(n == nmm)),
                           reads=[wbfB[i], wbfB2[i][0], wbfB2[i][1], xnsB], writes=[PSB[bk]], sig=(n == nmm))
                return bk

            def proj_tm(l, g):
                col0, ncols, shift = TMG[g]
                i, i2, wa, wb2 = acq(l, ('tm', g))
                for c in range(NCH):
                    bk = pbank()
                    nmm = KD * (2 if shift else 1)
                    n = 0
                    for k in range(KD):
                        n += 1
                        OP('pe', lambda e, k=k, n=n, c=c, bk=bk: e.matmul(PS[bk][0:64, 0:256], lhsT=xn[:, k, c * CH:(c + 1) * CH],
                                                                     rhs=wa[:, k, :], start=(n == 1), stop=(n == nmm)),
                           reads=[wbfB[i], wbfB2[i][0], wbfB2[i][1], xnB[k]], writes=[PSB[bk]], sig=(n == nmm))
                    if shift:
                        for k in range(KD):
                            n += 1
                            OP('pe', lambda e, k=k, n=n, c=c, bk=bk: e.matmul(PS[bk][0:64, 0:256], lhsT=xns[:, k, c * CH:(c + 1) * CH],
                                                                         rhs=wb2[:, k, :], start=False, stop=(n == nmm)),
                               reads=[wbfB[i2], wbfB2[i2][0], wbfB2[i2][1], xnsB], writes=[PSB[bk]], sig=(n == nmm))
                    OP('act', lambda e, c=c, bk=bk: e.activation(out=Vt[c][:], in_=PS[bk][0:64, 0:256], func=AF.Copy),
                       reads=[PSB[bk]], writes=[VtB[c]])

            def scan_decay(g_t, g_b):
                b_t, b_b = fa()
                OP('dve', lambda e: e.tensor_tensor_scan(out=b_t[:], data0=cst[:, SCM:SCM + TBM], data1=g_t[:], initial=0.0,
                                                         op0=ALU.mult, op1=ALU.add), reads=[g_b, cstB], writes=[b_b])
                return b_t, b_b

            def chunk_engine(mname, QE, KE, PC, rw=None):
                isrw = rw is not None
                if DEBUG_STAGE < 1:
                    return
                for c in range(NCH):
                    for (src, dst, dstB_) in ([(KE, KEt, KEtB)] + ([(rw['AE'], AEt, AEtB)] if isrw else [])):
                        for hp in range(2):
                            OP('pe', lambda e, hp=hp, c=c, src=src: e.transpose(PST[0:64, hp * 128:(hp + 1) * 128],
                                                                             src[hp][0][:, c * CH:(c + 1) * CH], ident[:]),
                               reads=[src[hp][1], identB], writes=[PSB[7]])
                        OP('act', lambda e, c=c, dst=dst: e.activation(out=dst[c][:], in_=PST[0:64, 0:256], func=AF.Copy),
                           reads=[PSB[7]], writes=[dstB_[c]])
                if DEBUG_STAGE < 2:
                    return
                for c in range(NCH):
                    cs = slice(c * CH, (c + 1) * CH)
                    def sc(lh, rh, dst_ps, cs=cs):
                        for h in range(4):
                            hp, r = h // 2, (h % 2) * 64
                            OP('pe', lambda e, h=h, hp=hp, r=r: e.matmul(dst_ps[0:64, h * 64:(h + 1) * 64], lhsT=lh[hp][0][r:r + 64, cs],
                                                                         rhs=rh[hp][0][r:r + 64, cs], start=True, stop=True),
                               reads=[lh[hp][1], rh[hp][1]], writes=[PSB[6]], rg=r)
                    sc(KE, QE, PS[6][:, 0:256])
                    OP('dve', lambda e, c=c: e.tensor_tensor(out=ATs[c][:], in0=PS[6][0:64, 0:256], in1=cst[0:64, MI:MI + 256], op=ALU.mult),
                       reads=[PSB[6], cstB], writes=[ATsB[c]])
                    if isrw:
                        sc(KE, rw['BE'], PS[6][:, 256:512])
                        OP('dve', lambda e, c=c: e.tensor_tensor(out=LKs[c][:], in0=PS[6][0:64, 256:512], in1=cst[0:64, MSU:MSU + 256], op=ALU.mult),
                           reads=[PSB[6], cstB], writes=[LKsB[c]])
                        sc(rw['AE'], QE, PS[6][:, 0:256])
                        OP('dve', lambda e, c=c: e.tensor_tensor(out=ARs[c][:], in0=PS[6][0:64, 0:256], in1=cst[0:64, MI:MI + 256], op=ALU.mult),
                           reads=[PSB[6], cstB], writes=[ARsB[c]])
                        sc(rw['AE'], rw['BE'], PS[6][:, 256:512])
                        OP('dve', lambda e, c=c: e.scalar_tensor_tensor(out=NTn[c][:], in0=PS[6][0:64, 256:512], scalar=-1.0,
                                                                         in1=cst[0:64, MSU:MSU + 256], op0=ALU.mult, op1=ALU.mult),
                           reads=[PSB[6], cstB], writes=[NTnB[c]])
                        sc(rw['BE'], rw['AE'], PS[6][:, 0:256])
                        OP('dve', lambda e, c=c: e.scalar_tensor_tensor(out=Nn[c][:], in0=PS[6][0:64, 0:256], scalar=-1.0,
                                                                         in1=cst[0:64, MSL:MSL + 256], op0=ALU.mult, op1=ALU.mult),
                           reads=[PSB[6], cstB], writes=[NnB[c]])
                        OP('pool', lambda e, c=c: e.tensor_tensor(out=Pn[c][:], in0=NTn[c][:], in1=cst[0:64, ID4:ID4 + 256], op=ALU.add),
                           reads=[NTnB[c], cstB], writes=[PnB[c]])
                if isrw:
                    curN, curNB, curNT, curNTB = Nn, NnB, NTn, NTnB
                    nxtN, nxtNB, nxtNT, nxtNTB = Nn2, Nn2B, NTn2, NTn2B
                    curP, curPB, nxtP, nxtPB = Pn, PnB, Pn2, Pn2B
                    for lev in range(1, 6):
                        for c in range(NCH):
                            bk = pbank()
                            for h in range(4):
                                hs = slice(h * 64, (h + 1) * 64)
                                OP('pe', lambda e, c=c, hs=hs, bk=bk, a=curNT, b=curN: e.matmul(PS[bk][0:64, hs], lhsT=a[c][:, hs], rhs=b[c][:, hs],
                                                                                         start=True, stop=True),
                                   reads=[curNTB[c], curNB[c]], writes=[PSB[bk]], rg=0)
                            if lev < 5:
                                for h in range(4):
                                    hs = slice(h * 64, (h + 1) * 64)
                                    hs2 = slice(256 + h * 64, 256 + (h + 1) * 64)
                                    OP('pe', lambda e, c=c, hs=hs, hs2=hs2, bk=bk, a=curN, b=curNT: e.matmul(PS[bk][0:64, hs2], lhsT=a[c][:, hs], rhs=b[c][:, hs],
                                                                                                     start=True, stop=True),
                                       reads=[curNTB[c], curNB[c]], writes=[PSB[bk]], rg=0)
                                OP('act', lambda e, c=c, bk=bk, d=nxtNT: e.activation(out=d[c][:], in_=PS[bk][0:64, 256:512], func=AF.Copy),
                                   reads=[PSB[bk]], writes=[nxtNTB[c]])
                            OP('act', lambda e, c=c, bk=bk, d=nxtN: e.activation(out=d[c][:], in_=PS[bk][0:64, 0:256], func=AF.Copy),
                               reads=[PSB[bk]], writes=[nxtNB[c]])
                        curN, curNB, nxtN, nxtNB = nxtN, nxtNB, curN, curNB
                        curNT, curNTB, nxtNT, nxtNTB = nxtNT, nxtNTB, curNT, curNTB
                        for c in range(NCH):
                            bk = pbank()
                            for h in range(4):
                                hs = slice(h * 64, (h + 1) * 64)
                                OP('pe', lambda e, c=c, hs=hs, bk=bk, a=curN, b=curP: e.matmul(PS[bk][0:64, hs], lhsT=a[c][:, hs], rhs=b[c][:, hs],
                                                                                        start=True, stop=True),
                                   reads=[curNB[c], curPB[c]], writes=[PSB[bk]], rg=0)
                            OP('dve', lambda e, c=c, bk=bk, s=curP, d=nxtP: e.tensor_tensor(out=d[c][:], in0=PS[bk][0:64, 0:256], in1=s[c][:], op=ALU.add),
                               reads=[PSB[bk], curPB[c]], writes=[nxtPB[c]])
                        curP, curPB, nxtP, nxtPB = nxtP, nxtPB, curP, curPB
                    TT, TTB = curP, curPB
                if DEBUG_STAGE < 3:
                    return
                S3, Sb, S3B, SbB = S32[mname], Sbf[mname], S32B[mname], SbfB[mname]
                for c in range(NCH):
                    cs = slice(c * CH, (c + 1) * CH)
                    if isrw:
                        for h in range(4):
                            hp, r = h // 2, (h % 2) * 64
                            hs = slice(h * 64, (h + 1) * 64)
                            OP('pe', lambda e, hp=hp, r=r, hs=hs, cs=cs: e.matmul(PS[6][0:64, hs], lhsT=rw['BE'][hp][0][r:r + 64, cs], rhs=Sb[hp][r:r + 64, :],
                                                                         start=True, stop=False),
                               reads=[rw['BE'][hp][1], SbB[hp]], writes=[PSB[6]], rg=r)
                            OP('pe', lambda e, hs=hs, c=c: e.matmul(PS[6][0:64, hs], lhsT=LKs[c][:, hs], rhs=Vt[c][:, hs], start=False, stop=True),
                               reads=[LKsB[c], VtB[c]], writes=[PSB[6]], rg=0)
                        OP('act', lambda e: e.activation(out=Ysb[:], in_=PS[6][0:64, 0:256], func=AF.Copy), reads=[PSB[6]], writes=[YsbB])
                        for h in range(4):
                            hs = slice(h * 64, (h + 1) * 64)
                            hs2 = slice(256 + h * 64, 256 + (h + 1) * 64)
                            OP('pe', lambda e, hs=hs, hs2=hs2, c=c: e.matmul(PS[6][0:64, hs2], lhsT=TT[c][:, hs], rhs=Ysb[:, hs], start=True, stop=True),
                               reads=[TTB[c], YsbB], writes=[PSB[6]], rg=0)
                        OP('act', lambda e: e.activation(out=Usb[:], in_=PS[6][0:64, 256:512], func=AF.Copy, scale=-1.0),
                           reads=[PSB[6]], writes=[UsbB])
                    for h in range(4):
                        hp, r = h // 2, (h % 2) * 64
                        hs = slice(h * 64, (h + 1) * 64)
                        ob = PS[4 + hp][r:r + 64, cs]
                        OP('pe', lambda e, ob=ob, hs=hs, c=c: e.matmul(ob, lhsT=Vt[c][:, hs], rhs=ATs[c][:, hs], start=True, stop=False),
                           reads=[VtB[c], ATsB[c]], writes=[PSB[4 + hp]], rg=0)
                        if isrw:
                            OP('pe', lambda e, ob=ob, hs=hs, c=c: e.matmul(ob, lhsT=Usb[:, hs], rhs=ARs[c][:, hs], start=False, stop=False),
                               reads=[UsbB, ARsB[c]], writes=[PSB[4 + hp]], rg=0)
                        OP('pe', lambda e, ob=ob, hp=hp, r=r, cs=cs: e.matmul(ob, lhsT=Sb[hp][r:r + 64, :], rhs=QE[hp][0][r:r + 64, cs], start=False, stop=True),
                           reads=[SbB[hp], QE[hp][1]], writes=[PSB[4 + hp]], rg=r)
                    for h in range(4):
                        hp, r = h // 2, (h % 2) * 64
                        hs = slice(h * 64, (h + 1) * 64)
                        sp_ = PS[7][r:r + 64, 256 + hp * 64:256 + (hp + 1) * 64]
                        OP('pe', lambda e, sp_=sp_, hs=hs, c=c: e.matmul(sp_, lhsT=KEt[c][:, hs], rhs=Vt[c][:, hs], start=True, stop=(not isrw)),
                           reads=[KEtB[c], VtB[c]], writes=[PSB[7]], rg=0)
                        if isrw:
                            OP('pe', lambda e, sp_=sp_, hs=hs, c=c: e.matmul(sp_, lhsT=AEt[c][:, hs], rhs=Usb[:, hs], start=False, stop=True),
                               reads=[AEtB[c], UsbB], writes=[PSB[7]], rg=0)
                    for hp in range(2):
                        pc = PC[hp][0][:, (c + 1) * CH - 1:(c + 1) * CH]
                        OP('dve', lambda e, hp=hp: e.tensor_tensor(out=stmp[hp][:], in0=PS[7][:, 256 + hp * 64:256 + (hp + 1) * 64], in1=S3[hp][:], op=ALU.add),
                           reads=[PSB[7], S3B[hp]], writes=[stmpB[hp]])
                        OP('dve', lambda e, hp=hp, pc=pc: e.tensor_scalar(out=S3[hp][:], in0=stmp[hp][:], scalar1=pc, scalar2=None, op0=ALU.mult),
                           reads=[stmpB[hp], PC[hp][1]], writes=[S3B[hp]])
                        OP('dve', lambda e, hp=hp, pc=pc: e.tensor_scalar(out=Sb[hp][:], in0=stmp[hp][:], scalar1=pc, scalar2=None, op0=ALU.mult),
                           reads=[stmpB[hp], PC[hp][1]], writes=[SbB[hp]])

            def evac_fm(bk, func=AF.Copy, scale=1.0, bias=None, dt='f', rows=128, dst=None):
                t, b = dst if dst is not None else (fa() if dt == 'f' else ba())
                kw = {}
                if bias is not None:
                    kw['bias'] = bias
                OP('act', lambda e: e.activation(out=t[0:rows, :], in_=PS[bk][0:rows, 0:TBM], func=func, scale=scale, **kw),
                   reads=[PSB[bk]] + ([pvB[0]] if bias is not None else []), writes=[b])
                return t, b

            def mixer_block(l, s, bi):
                first = (bi == 0)
                c0 = bi * TBM
                ti = c0 // TB
                plan = []
                if 'pool' in mixers:
                    plan += [('fm', 0), ('fm', 1)]
                if 'hgrn' in mixers:
                    for hp_ in range(2):
                        plan += [('fm', 2 + hp_), ('fm', 4 + hp_), ('fm', 6 + hp_)]
                    plan += [('tm', 0)]
                if 'gla' in mixers:
                    plan += [('fm', 22)]
                    for hp_ in range(2):
                        plan += [('fm', 16 + hp_), ('fm', 18 + hp_), ('fm', 20 + hp_)]
                    plan += [('tm', 2)]
                if 'rwkv' in mixers:
                    plan += [('fm', 14), ('fm', 15)]
                    for hp_ in range(2):
                        plan += [('fm', 8 + hp_), ('fm', 10 + hp_), ('fm', 12 + hp_)]
                    plan += [('tm', 1)]
                plan += [('wo', jp) for jp in range(KD // 2)]
                wplan['plan'] = plan
                wplan['pos'] = 0
                wplan['issued'] = {}
                wplan['loaded'] = {}
                if plan:
                    ensure_load(l, 0)
                    ensure_load(l, 1)
                    ensure_cast(l, 0)
                pv = pvec[l]
                if 'rwkv' in mixers:
                    if first:
                        OP('pool', lambda e: e.memset(xns[:, :, 0:2], 0.0), writes=[xnsB])
                    else:
                        OP('pool', lambda e: e.tensor_copy(out=xns[:, :, 0:1], in_=xn[:, :, TBM - 1:TBM]), reads=xnB, writes=[xnsB])
                rmsnorm_to_xn(l, PV['nmx'], c0, TBM, 0)
                if 'rwkv' in mixers:
                    OP('pool', lambda e: e.tensor_copy(out=xns[:, :, 1:TBM], in_=xn[:, :, 0:TBM - 1]), reads=xnB, writes=[xnsB])
                if 'pool' in mixers:
                    if first:
                        OP('pool', lambda e: e.memset(pext[:, :, 0:16], 0.0), writes=[pextB])
                    else:
                        OP('pool', lambda e: e.tensor_copy(out=pext[:, :, 0:16], in_=pext[:, :, TBM:TBM + 16]), reads=[pextB], writes=[pextB])
                    for ck in range(2):
                        bk = proj_fm(l, ck, first)
                        OP('act', lambda e, ck=ck, bk=bk: e.activation(out=pext[:, ck, 16:16 + TBM], in_=PS[bk][:, 0:TBM], func=AF.Copy),
                           reads=[PSB[bk]], writes=[pextB])
                    W_ = 16 + TBM
                    OP('dve', lambda e: e.tensor_tensor(out=pw[0][:, :, 1:W_], in0=pext[:, :, 1:W_], in1=pext[:, :, 0:W_ - 1], op=ALU.add),
                       reads=[pextB], writes=[pwB[0]])
                    def poolfin(src, ck, r0, wdw, first=first):
                        yt, yb = PB16[ck], PB16B[ck]
                        OP('dve', lambda e: e.scalar_tensor_tensor(out=yt[r0:r0 + 64, :], in0=src[r0:r0 + 64, ck, 16:16 + TBM], scalar=1.0 / wdw,
                                                                   in1=pext[r0:r0 + 64, ck, 16:16 + TBM], op0=ALU.mult, op1=ALU.subtract),
                           reads=[pwB[0], pwB[1], pextB], writes=[yb])
                        if first:
                            t2, b2 = FA[0], FAB[0]
                            OP('dve', lambda e: e.tensor_tensor(out=t2[r0:r0 + 64, 0:16], in0=src[r0:r0 + 64, ck, 16:32],
                                                                in1=cst[r0:r0 + 64, ICN + ck * 16:ICN + ck * 16 + 16], op=ALU.mult),
                               reads=[pwB[0], pwB[1], cstB], writes=[b2])
                            OP('dve', lambda e: e.tensor_tensor(out=yt[r0:r0 + 64, 0:16], in0=t2[r0:r0 + 64, 0:16],
                                                                in1=pext[r0:r0 + 64, ck, 16:32], op=ALU.subtract),
                               reads=[b2, pextB], writes=[yb])
                    poolfin(pw[0], 0, 0, 2)
                    OP('dve', lambda e: e.tensor_tensor(out=pw[1][:, :, 3:W_], in0=pw[0][:, :, 3:W_], in1=pw[0][:, :, 1:W_ - 2], op=ALU.add),
                       reads=[pwB[0]], writes=[pwB[1]])
                    poolfin(pw[1], 0, 64, 4)
                    OP('dve', lambda e: e.tensor_tensor(out=pw[0][:, :, 7:W_], in0=pw[1][:, :, 7:W_], in1=pw[1][:, :, 3:W_ - 4], op=ALU.add),
                       reads=[pwB[1]], writes=[pwB[0]])
                    poolfin(pw[0], 1, 0, 8)
                    OP('dve', lambda e: e.tensor_tensor(out=pw[1][:, :, 15:W_], in0=pw[0][:, :, 15:W_], in1=pw[0][:, :, 7:W_ - 8], op=ALU.add),
                       reads=[pwB[0]], writes=[pwB[1]])
                    poolfin(pw[1], 1, 64, 16)
                    for ck in range(2):
                        bk = pbank()
                        OP('pe', lambda e, ck=ck, bk=bk: e.matmul(PS[bk][:, 0:TBM], lhsT=smatb[:, ck * 128:(ck + 1) * 128], rhs=PB16[ck][:], start=True, stop=True),
                           reads=[PB16B[ck], smatB], writes=[PSB[bk]])
                        OP('dve', lambda e, ck=ck, bk=bk: e.tensor_scalar(out=YT[:, ck, :], in0=PS[bk][:, 0:TBM], scalar1=pv[:, PV['pool_b'] + ck:PV['pool_b'] + ck + 1],
                                                                      scalar2=pv[:, PV['pool_s'] + ck:PV['pool_s'] + ck + 1], op0=ALU.add, op1=ALU.mult),
                           reads=[PSB[bk], pvB[l]], writes=[YB[ck]])
                else:
                    for ck in range(2):
                        OP('pool', lambda e, ck=ck: e.memset(YT[:, ck, :], 0.0), writes=[YB[ck]])

                def out_rstd(Ot, lhs_ones, n_ch, eps):
                    bk = pbank()
                    for hp in range(2):
                        st, sbb = ba()
                        OP('act', lambda e, hp=hp, st=st, Ot=Ot: e.activation(out=st[:], in_=Ot[hp][0][:], func=AF.Square), reads=[Ot[hp][1]], writes=[sbb])
                        if lhs_ones is ones:
                            OP('pe', lambda e, hp=hp, st=st: e.matmul(PS[bk][:, 0:TBM], lhsT=ones[:], rhs=st[:], start=(hp == 0), stop=(hp == 1)),
                               reads=[sbb, onesB], writes=[PSB[bk]])
                        else:
                            bk2 = bk if hp == 0 else pbank()
                            OP('pe', lambda e, hp=hp, st=st, bk2=bk2: e.matmul(PS[bk2][:, 0:TBM], lhsT=bones[:], rhs=st[:], start=True, stop=True),
                               reads=[sbb, bonesB], writes=[PSB[bk2]])
                            if hp == 0:
                                bk0 = bk2
                            else:
                                bk1 = bk2
                    if lhs_ones is ones:
                        rt, rb = fa()
                        rstd_from(PS[bk], PSB[bk], TBM, 1.0 / n_ch, eps, rt, rb)
                        return [(rt, rb), (rt, rb)]
                    res = []
                    for bkx in (bk0, bk1):
                        rt, rb = fa()
                        rstd_from(PS[bkx], PSB[bkx], TBM, 1.0 / n_ch, eps, rt, rb)
                        res.append((rt, rb))
                    return res

                def evac_O():
                    Ot = []
                    for hp in range(2):
                        t, b = fa()
                        OP('act', lambda e, hp=hp, t=t: e.activation(out=t[:], in_=PS[4 + hp][:, 0:TBM], func=AF.Copy), reads=[PSB[4 + hp]], writes=[b])
                        Ot.append((t, b))
                    return Ot

                if 'hgrn' in mixers:
                    QE, KE, PCx, GT = [], [], [], []
                    for hp in range(2):
                        bq = proj_fm(l, 2 + hp, first)
                        qt, qb = evac_fm(bq, AF.Silu)
                        bf_ = proj_fm(l, 4 + hp, first)
                        st_, sb_ = evac_fm(bf_, AF.Sigmoid)
                        ft, fb = fa()
                        OP('dve', lambda e, hp=hp, st_=st_, ft=ft: e.tensor_scalar(out=ft[:], in0=st_[:], scalar1=lbt[:, 2 * l + hp:2 * l + hp + 1],
                                                                             scalar2=lbt[:, 4 + 2 * l + hp:4 + 2 * l + hp + 1], op0=ALU.mult, op1=ALU.add),
                           reads=[sb_, lbtB], writes=[fb])
                        lt, lb_ = fa()
                        OP('dve', lambda e, ft=ft, lt=lt: e.tensor_scalar_max(out=lt[:], in0=ft[:], scalar1=1e-30), reads=[fb], writes=[lb_])
                        OP('act', lambda e, lt=lt: e.activation(out=lt[:], in_=lt[:], func=AF.Ln), reads=[lb_], writes=[lb_])
                        bt, bb = scan_decay(lt, lb_)
                        ebt, ebb = LL[hp][0], LLB_[hp][0]
                        OP('act', lambda e, bt=bt, ebt=ebt: e.activation(out=ebt[:], in_=bt[:], func=AF.Exp), reads=[bb], writes=[ebb])
                        OP('act', lambda e, bt=bt: e.activation(out=bt[:], in_=bt[:], func=AF.Exp, scale=-1.0), reads=[bb], writes=[bb])
                        qe, qeb = LB[hp][0], LBB[hp][0]
                        OP('dve', lambda e, qt=qt, ebt=ebt, qe=qe: e.scalar_tensor_tensor(out=qe[:], in0=qt[:], scalar=QK, in1=ebt[:], op0=ALU.mult, op1=ALU.mult),
                           reads=[qb, ebb], writes=[qeb])
                        OP('dve', lambda e, ft=ft: e.tensor_scalar(out=ft[:], in0=ft[:], scalar1=-1.0, scalar2=1.0, op0=ALU.mult, op1=ALU.add),
                           reads=[fb], writes=[fb])
                        ke, keb = LB[hp][1], LBB[hp][1]
                        OP('dve', lambda e, ft=ft, bt=bt, ke=ke: e.tensor_tensor(out=ke[:], in0=ft[:], in1=bt[:], op=ALU.mult), reads=[fb, bb], writes=[keb])
                        bg_ = proj_fm(l, 6 + hp, first)
                        gt, gb = evac_fm(bg_, AF.Sigmoid, dst=(LL[hp][1], LLB_[hp][1]))
                        QE.append((qe, qeb)); KE.append((ke, keb)); PCx.append((ebt, ebb)); GT.append((gt, gb))
                    proj_tm(l, 0)
                    chunk_engine('hgrn', QE, KE, PCx)
                    Ot = evac_O()
                    rs = out_rstd(Ot, ones, 256, NORM_EPS)
                    for hp in range(2):
                        t1, b1 = fa()
                        OP('dve', lambda e, hp=hp, t1=t1, Ot=Ot, rs=rs: e.scalar_tensor_tensor(out=t1[:], in0=Ot[hp][0][:], scalar=pv[:, PV['hnorm'] + hp:PV['hnorm'] + hp + 1],
                                                                           in1=rs[hp][0][:], op0=ALU.mult, op1=ALU.mult),
                           reads=[Ot[hp][1], rs[hp][1], pvB[l]], writes=[b1])
                        OP('dve', lambda e, hp=hp, t1=t1, GT=GT: e.tensor_tensor(out=YT[:, 2 + hp, :], in0=t1[:], in1=GT[hp][0][:], op=ALU.mult),
                           reads=[b1, GT[hp][1]], writes=[YB[2 + hp]])
                else:
                    for ck in (2, 3):
                        OP('pool', lambda e, ck=ck: e.memset(YT[:, ck, :], 0.0), writes=[YB[ck]])

                if 'gla' in mixers:
                    bga = proj_fm(l, 22, first)
                    gat, gab = LLX[0], LLXB[0]
                    OP('act', lambda e: e.activation(out=gat[:], in_=PS[bga][:, 0:TBM], func=AF.Copy), reads=[PSB[bga]], writes=[gab])
                    QE, KE, PCx, GT = [], [], [], []
                    for hp in range(2):
                        bk = pbank()
                        OP('pe', lambda e, hp=hp, bk=bk: e.matmul(PS[bk][:, 0:TBM], lhsT=smat[:, 1024 + hp * 128:1024 + (hp + 1) * 128], rhs=gat[:], start=True, stop=True),
                           reads=[gab, smatB], writes=[PSB[bk]])
                        lt, lb_ = evac_fm(bk, AF.Sigmoid, bias=pv[:, PV['glab'] + hp:PV['glab'] + hp + 1])
                        OP('act', lambda e, lt=lt: e.activation(out=lt[:], in_=lt[:], func=AF.Ln), reads=[lb_], writes=[lb_])
                        bt, bb = scan_decay(lt, lb_)
                        ebt, ebb = LL[hp][0], LLB_[hp][0]
                        OP('act', lambda e, bt=bt, ebt=ebt: e.activation(out=ebt[:], in_=bt[:], func=AF.Exp, scale=1.0 / 16), reads=[bb], writes=[ebb])
                        OP('act', lambda e, bt=bt: e.activation(out=bt[:], in_=bt[:], func=AF.Exp, scale=-1.0 / 16), reads=[bb], writes=[bb])
                        bq = proj_fm(l, 16 + hp, first)
                        qe, qeb = LB[hp][0], LBB[hp][0]
                        OP('dve', lambda e, bq=bq, ebt=ebt, qe=qe: e.scalar_tensor_tensor(out=qe[:], in0=PS[bq][:, 0:TBM], scalar=QK, in1=ebt[:], op0=ALU.mult, op1=ALU.mult),
                           reads=[PSB[bq], ebb], writes=[qeb])
                        bkk = proj_fm(l, 18 + hp, first)
                        ke, keb = LB[hp][1], LBB[hp][1]
                        OP('dve', lambda e, bkk=bkk, bt=bt, ke=ke: e.tensor_tensor(out=ke[:], in0=PS[bkk][:, 0:TBM], in1=bt[:], op=ALU.mult), reads=[PSB[bkk], bb], writes=[keb])
                        bg_ = proj_fm(l, 20 + hp, first)
                        gt, gb = evac_fm(bg_, AF.Silu, dst=(LL[hp][1], LLB_[hp][1]))
                        QE.append((qe, qeb)); KE.append((ke, keb)); PCx.append((ebt, ebb)); GT.append((gt, gb))
                    proj_tm(l, 2)
                    chunk_engine('gla', QE, KE, PCx)
                    Ot = evac_O()
                    rs = out_rstd(Ot, bones, 64, NORM_EPS)
                    for hp in range(2):
                        t1, b1 = fa()
                        OP('dve', lambda e, hp=hp, t1=t1, Ot=Ot, rs=rs: e.scalar_tensor_tensor(out=t1[:], in0=Ot[hp][0][:], scalar=pv[:, PV['gnorm'] + hp:PV['gnorm'] + hp + 1],
                                                                           in1=rs[hp][0][:], op0=ALU.mult, op1=ALU.mult),
                           reads=[Ot[hp][1], rs[hp][1], pvB[l]], writes=[b1])
                        OP('dve', lambda e, hp=hp, t1=t1, GT=GT: e.tensor_tensor(out=YT[:, 6 + hp, :], in0=t1[:], in1=GT[hp][0][:], op=ALU.mult),
                           reads=[b1, GT[hp][1]], writes=[YB[6 + hp]])
                else:
                    for ck in (6, 7):
                        OP('pool', lambda e, ck=ck: e.memset(YT[:, ck, :], 0.0), writes=[YB[ck]])

                if 'rwkv' in mixers:
                    bwa = proj_fm(l, 14, first)
                    twa, twab = LLX[0], LLXB[0]
                    OP('act', lambda e: e.activation(out=twa[0:64, :], in_=PS[bwa][0:64, 0:TBM], func=AF.Tanh), reads=[PSB[bwa]], writes=[twab])
                    OP('act', lambda e: e.activation(out=twa[64:128, :], in_=PS[bwa][64:128, 0:TBM], func=AF.Copy), reads=[PSB[bwa]], writes=[twab])
                    bxg = proj_fm(l, 15, first)
                    sxg, sxgb = evac_fm(bxg, AF.Sigmoid, dst=(LLX[1], LLXB[1]))
                    QE, KE, AE, BE, PCx, GR, RKR, VF = [], [], [], [], [], [], [], []
                    for hp in range(2):
                        cs_ = slice(hp * 128, (hp + 1) * 128)
                        bk = pbank()
                        OP('pe', lambda e, bk=bk, hp=hp: e.matmul(PS[bk][:, 0:TBM], lhsT=smat[:, 256 + hp * 128:256 + (hp + 1) * 128], rhs=twa[:], start=True, stop=True),
                           reads=[twab, smatB], writes=[PSB[bk]])
                        lw, lwb = evac_fm(bk, AF.Sigmoid, bias=pv[:, PV['w0'] + hp:PV['w0'] + hp + 1])
                        bk = pbank()
                        OP('pe', lambda e, bk=bk, hp=hp: e.matmul(PS[bk][:, 0:TBM], lhsT=smat[:, 768 + hp * 128:768 + (hp + 1) * 128], rhs=twa[:], start=True, stop=True),
                           reads=[twab, smatB], writes=[PSB[bk]])
                        at, ab_ = evac_fm(bk, AF.Sigmoid, bias=pv[:, PV['a0'] + hp:PV['a0'] + hp + 1])
                        bk = pbank()
                        OP('pe', lambda e, bk=bk, hp=hp: e.matmul(PS[bk][:, 0:TBM], lhsT=smat[:, 512 + hp * 128:512 + (hp + 1) * 128], rhs=sxg[:], start=True, stop=True),
                           reads=[sxgb, smatB], writes=[PSB[bk]])
                        grt, grb = evac_fm(bk, AF.Copy, dst=(LL[hp][1], LLB_[hp][1]))
                        bt, bb = scan_decay(lw, lwb)
                        CW = -float(np.exp(-0.5))
                        ebt, ebb = LL[hp][0], LLB_[hp][0]
                        OP('act', lambda e, bt=bt, ebt=ebt: e.activation(out=ebt[:], in_=bt[:], func=AF.Exp, scale=CW), reads=[bb], writes=[ebb])
                        enb, enbb = fa()
                        OP('act', lambda e, bt=bt, enb=enb: e.activation(out=enb[:], in_=bt[:], func=AF.Exp, scale=-CW), reads=[bb], writes=[enbb])
                        OP('dve', lambda e, bt=bt, lw=lw: e.tensor_tensor(out=bt[:], in0=bt[:], in1=lw[:], op=ALU.subtract), reads=[bb, lwb], writes=[bb])
                        OP('act', lambda e, bt=bt: e.activation(out=bt[:], in_=bt[:], func=AF.Exp, scale=CW), reads=[bb], writes=[bb])
                        br = proj_fm(l, 8 + hp, first)
                        rt, rb = evac_fm(br, AF.Copy)
                        bkr = proj_fm(l, 10 + hp, first)
                        kt, kb = evac_fm(bkr, AF.Copy)
                        bv = proj_fm(l, 12 + hp, first)
                        vt, vb = evac_fm(bv, AF.Copy, dst=(LL[hp][2], LLB_[hp][2]))
                        kkt, kkb = fa()
                        OP('dve', lambda e, kt=kt, kkt=kkt, hp=hp: e.tensor_scalar(out=kkt[:], in0=kt[:], scalar1=pv[:, PV['kk'] + hp:PV['kk'] + hp + 1], scalar2=None, op0=ALU.mult),
                           reads=[kb, pvB[l]], writes=[kkb])
                        sq_, sqb_ = ba()
                        OP('act', lambda e, kkt=kkt, sq_=sq_: e.activation(out=sq_[:], in_=kkt[:], func=AF.Square), reads=[kkb], writes=[sqb_])
                        bk = pbank()
                        OP('pe', lambda e, bk=bk, sq_=sq_: e.matmul(PS[bk][:, 0:TBM], lhsT=bones[:], rhs=sq_[:], start=True, stop=True), reads=[sqb_, bonesB], writes=[PSB[bk]])
                        rn, rnb = fa()
                        rstd_from(PS[bk], PSB[bk], TBM, 1.0, 1e-24, rn, rnb)
                        OP('dve', lambda e, kkt=kkt, rn=rn: e.tensor_tensor(out=kkt[:], in0=kkt[:], in1=rn[:], op=ALU.mult), reads=[kkb, rnb], writes=[kkb])
                        fac, facb = fa()
                        OP('dve', lambda e, at=at, fac=fac, hp=hp: e.tensor_scalar(out=fac[:], in0=at[:], scalar1=-1.0, scalar2=pv[:, PV['ka'] + hp:PV['ka'] + hp + 1], op0=ALU.add, op1=ALU.mult),
                           reads=[ab_, pvB[l]], writes=[facb])
                        OP('dve', lambda e, fac=fac, kt=kt: e.scalar_tensor_tensor(out=kt[:], in0=fac[:], scalar=1.0, in1=kt[:], op0=ALU.add, op1=ALU.mult),
                           reads=[facb, kb], writes=[kb])
                        rk_, rkb_ = LB[hp][4], LBB[hp][4]
                        OP('dve', lambda e, rt=rt, kt=kt, rk_=rk_, hp=hp: e.scalar_tensor_tensor(out=rk_[:], in0=rt[:], scalar=pv[:, PV['rk'] + hp:PV['rk'] + hp + 1], in1=kt[:], op0=ALU.mult, op1=ALU.mult),
                           reads=[rb, kb, pvB[l]], writes=[rkb_])
                        qe, qeb = LB[hp][0], LBB[hp][0]
                        OP('dve', lambda e, rt=rt, ebt=ebt, qe=qe: e.tensor_tensor(out=qe[:], in0=rt[:], in1=ebt[:], op=ALU.mult), reads=[rb, ebb], writes=[qeb])
                        ke, keb = LB[hp][1], LBB[hp][1]
                        OP('dve', lambda e, kt=kt, enb=enb, ke=ke: e.tensor_tensor(out=ke[:], in0=kt[:], in1=enb[:], op=ALU.mult), reads=[kb, enbb], writes=[keb])
                        be, beb = LB[hp][3], LBB[hp][3]
                        OP('dve', lambda e, kkt=kkt, bt=bt, be=be: e.tensor_tensor(out=be[:], in0=kkt[:], in1=bt[:], op=ALU.mult), reads=[kkb, bb], writes=[beb])
                        OP('dve', lambda e, kkt=kkt, at=at: e.tensor_tensor(out=kkt[:], in0=kkt[:], in1=at[:], op=ALU.mult), reads=[kkb, ab_], writes=[kkb])
                        ae, aeb = LB[hp][2], LBB[hp][2]
                        OP('dve', lambda e, kkt=kkt, enb=enb, ae=ae: e.tensor_tensor(out=ae[:], in0=kkt[:], in1=enb[:], op=ALU.mult), reads=[kkb, enbb], writes=[aeb])
                        QE.append((qe, qeb)); KE.append((ke, keb)); AE.append((ae, aeb)); BE.append((be, beb)); PCx.append((ebt, ebb))
                        GR.append((grt, grb)); RKR.append((rk_, rkb_)); VF.append((vt, vb))
                    proj_tm(l, 1)
                    chunk_engine('rwkv', QE, KE, PCx, rw={'AE': AE, 'BE': BE})
                    Ot = evac_O()
                    for hp in range(2):
                        ob16, ob16b = ba()
                        OP('dve', lambda e, hp=hp, ob16=ob16, Ot=Ot: e.tensor_copy(out=ob16[:], in_=Ot[hp][0][:]), reads=[Ot[hp][1]], writes=[ob16b])
                        bk = pbank()
                        OP('pe', lambda e, bk=bk, ob16=ob16: e.matmul(PS[bk][:, 0:TBM], lhsT=bones[:], rhs=ob16[:], start=True, stop=True), reads=[ob16b, bonesB], writes=[PSB[bk]])
                        ct, cb = fa()
                        OP('dve', lambda e, hp=hp, bk=bk, ct=ct, Ot=Ot: e.scalar_tensor_tensor(out=ct[:], in0=PS[bk][:, 0:TBM], scalar=-1.0 / 64, in1=Ot[hp][0][:], op0=ALU.mult, op1=ALU.add),
                           reads=[PSB[bk], Ot[hp][1]], writes=[cb])
                        s2, s2b = ba()
                        OP('act', lambda e, ct=ct, s2=s2: e.activation(out=s2[:], in_=ct[:], func=AF.Square), reads=[cb], writes=[s2b])
                        bk = pbank()
                        OP('pe', lambda e, bk=bk, s2=s2: e.matmul(PS[bk][:, 0:TBM], lhsT=bones[:], rhs=s2[:], start=True, stop=True), reads=[s2b, bonesB], writes=[PSB[bk]])
                        rn, rnb = fa()
                        rstd_from(PS[bk], PSB[bk], TBM, 1.0 / 64, GN_EPS, rn, rnb)
                        OP('dve', lambda e, hp=hp, ct=ct, rn=rn: e.scalar_tensor_tensor(out=ct[:], in0=ct[:], scalar=pv[:, PV['lnw'] + hp:PV['lnw'] + hp + 1], in1=rn[:], op0=ALU.mult, op1=ALU.mult),
                           reads=[cb, rnb, pvB[l]], writes=[cb])
                        bk = pbank()
                        OP('pe', lambda e, bk=bk, hp=hp, RKR=RKR: e.matmul(PS[bk][:, 0:TBM], lhsT=bones[:], rhs=RKR[hp][0][:], start=True, stop=True), reads=[RKR[hp][1], bonesB], writes=[PSB[bk]])
                        bo, bob = fa()
                        OP('dve', lambda e, bk=bk, hp=hp, bo=bo, VF=VF: e.tensor_tensor(out=bo[:], in0=PS[bk][:, 0:TBM], in1=VF[hp][0][:], op=ALU.mult), reads=[PSB[bk], VF[hp][1]], writes=[bob])
                        OP('dve', lambda e, hp=hp, ct=ct, bo=bo: e.scalar_tensor_tensor(out=ct[:], in0=ct[:], scalar=pv[:, PV['lnb'] + hp:PV['lnb'] + hp + 1], in1=bo[:], op0=ALU.add, op1=ALU.add),
                           reads=[cb, bob, pvB[l]], writes=[cb])
                        OP('dve', lambda e, hp=hp, ct=ct, GR=GR: e.tensor_tensor(out=YT[:, 4 + hp, :], in0=ct[:], in1=GR[hp][0][:], op=ALU.mult),
                           reads=[cb, GR[hp][1]], writes=[YB[4 + hp]])
                else:
                    for ck in (4, 5):
                        OP('pool', lambda e, ck=ck: e.memset(YT[:, ck, :], 0.0), writes=[YB[ck]])

                for jp in range(KD // 2):
                    (i,) = acq(l, ('wo', jp))
                    for q in range(2):
                        j = 2 * jp + q
                        for m in range(KD):
                            OP('pe', lambda e, m=m, j=j, q=q, i=i: e.matmul(PS[m][:, 0:TBM], lhsT=wbf[i][:, q * D + m * 128:q * D + (m + 1) * 128], rhs=YT[:, j, :],
                                                                   start=(j == 0), stop=(j == KD - 1)),
                               reads=[wbfB[i], wbfB2[i][0], wbfB2[i][1], YB[j]], writes=[PSB[m]], sig=(m == KD - 1 or j == KD - 1))
                for m in range(KD):
                    OP('dve', lambda e, m=m: e.tensor_tensor(out=X[:, m, c0:c0 + TBM], in0=PS[m][:, 0:TBM], in1=X[:, m, c0:c0 + TBM], op=ALU.add),
                       reads=[PSB[m], XB[m][ti]], writes=[XB[m][ti]])

            def mixer_setup(l, s):
                OP('sp', lambda e: e.dma_start(out=mub[:], in_=mub_d[l]), writes=[mubB], dsem=muS)
                OP('dve', lambda e: e.tensor_scalar(out=omub[:], in0=mub[:], scalar1=-1.0, scalar2=1.0, op0=ALU.mult, op1=ALU.add), reads=[mubB], writes=[omubB])
                OP('sp', lambda e: e.dma_start(out=smat[:], in_=smat_d[l]), writes=[smatB], dsem=smS)
                OP('pool', lambda e: e.tensor_copy(out=smatb[:], in_=smat[:, 0:256]), reads=[smatB], writes=[smatB])
                for m in ('hgrn', 'gla', 'rwkv'):
                    for hp in range(2):
                        OP('pool', lambda e, m=m, hp=hp: e.memset(S32[m][hp][:], 0.0), writes=[S32B[m][hp]])
                        OP('pool', lambda e, m=m, hp=hp: e.memset(Sbf[m][hp][:], 0.0), writes=[SbfB[m][hp]])

        if do_mix:
            OP('act', lambda e: e.activation(out=lbt[:, 0:2], in_=pvec[0][:, PV['lb0']:PV['lb0'] + 2], func=AF.Exp), reads=[pvB[0]], writes=[lbtB])
            OP('act', lambda e: e.activation(out=lbt[:, 2:4], in_=pvec[L - 1][:, PV['lbl']:PV['lbl'] + 2], func=AF.Exp), reads=[pvB[L - 1], lbtB], writes=[lbtB])
            OP('dve', lambda e: e.tensor_tensor(out=lbt[:, 4:6], in0=lbt[:, 0:2], in1=lbt[:, 2:4], op=ALU.add), reads=[lbtB], writes=[lbtB])
            OP('dve', lambda e: e.reciprocal(out=lbt[:, 4:6], in_=lbt[:, 4:6]), reads=[lbtB], writes=[lbtB])
            OP('dve', lambda e: e.tensor_tensor(out=lbt[:, 6:8], in0=lbt[:, 2:4], in1=lbt[:, 4:6], op=ALU.mult), reads=[lbtB], writes=[lbtB])
            OP('dve', lambda e: e.memset(lbt[:, 4:6], 0.0), reads=[lbtB], writes=[lbtB])
            OP('dve', lambda e: e.tensor_scalar(out=lbt[:, 0:4], in0=lbt[:, 4:8], scalar1=-1.0, scalar2=1.0, op0=ALU.mult, op1=ALU.add), reads=[lbtB], writes=[lbtB])

        for s in range(NS):
            for k in range(KD):
                OP('sp', lambda e, k=k, s=s: e.dma_start(out=X[:, k, :], in_=xT[s, :, k, :]), writes=XB[k], dsem=xS[k])
            for l in range(L):
                if do_ffn:
                    for ti in range(NT):
                        ffn(l, 0, ti)
                if do_mix:
                    mixer_setup(l, s)
                    for bi in range(T // TBM):
                        mixer_block(l, s, bi)
                if do_ffn:
                    for ti in range(NT):
                        ffn(l, 1, ti)
            for ti in range(NT):
                c0 = ti * TB
                for k in range(KD):
                    i = counters['sq'] % 2
                    counters['sq'] += 1
                    OP('act', lambda e, k=k, i=i, c0=c0: e.activation(out=sq[i][:], in_=X[:, k, c0:c0 + TB], func=AF.Square),
                       reads=[XB[k][ti]], writes=[sqB[i]])
                    OP('pe', lambda e, k=k, i=i: e.matmul(PS[0][:, :], lhsT=ones[:], rhs=sq[i][:], start=(k == 0), stop=(k == KD - 1)),
                       reads=[sqB[i], onesB], writes=[PSB[0]])
                rstd_from(PS[0], PSB[0], TB, 1.0 / D, NORM_EPS, rstd, rstdB)
                for k in range(KD):
                    OP('dve', lambda e, k=k, c0=c0: e.scalar_tensor_tensor(out=X[:, k, c0:c0 + TB], in0=X[:, k, c0:c0 + TB],
                                                                        scalar=pvec[0][:, PV['nfin'] + k:PV['nfin'] + k + 1], in1=rstd[:],
                                                                        op0=ALU.mult, op1=ALU.mult),
                       reads=[XB[k][ti], rstdB, pvB[0]], writes=[XB[k][ti]])
            for k in range(KD):
                OP('sp', lambda e, k=k, s=s: e.dma_start(out=outT[s, :, k, :], in_=X[:, k, :]), reads=XB[k], writes=XB[k], dsem=oS[k])
        pg.ops['sp'].append((lambda e: e.nop(), {o_: o_.count for o_ in oS}, False, None))
        pg.emit(block, esems)
    return nc, pg


def _col(v, n):
    return np.ascontiguousarray(np.asarray(v, np.float32).reshape(n, 128).T)


def make_consts():
    cst = np.zeros((128, 1600), np.float32)
    p = np.arange(64)[:, None]
    f = np.arange(64)[None, :]
    for h in range(4):
        cst[0:64, 0 + h * 64:0 + (h + 1) * 64] = (p <= f)
        cst[0:64, 256 + h * 64:256 + (h + 1) * 64] = (p < f)
        cst[0:64, 512 + h * 64:512 + (h + 1) * 64] = (f < p)
        cst[0:64, 768 + h * 64:768 + (h + 1) * 64] = (p == f)
    scm = np.ones(512, np.float32)
    scm[::64] = 0.0
    cst[:, 1024:1536] = scm[None, :]
    wins = {(0, 0): 2, (0, 1): 4, (1, 0): 8, (1, 1): 16}
    for ck in range(2):
        for half in range(2):
            w = wins[(ck, half)]
            t = np.arange(16)
            cst[half * 64:(half + 1) * 64, 1536 + ck * 16:1536 + ck * 16 + 16] = (1.0 / np.minimum(t + 1, w))[None, :]
    return cst


def prep_weights(inp, L):
    out = {}
    f32 = np.float32
    for l in range(L):
        for w, (wi, wo) in enumerate((('ffn1_w_in', 'ffn1_w_out'), ('ffn2_w_in', 'ffn2_w_out'))):
            W = np.asarray(inp[wi][l], f32)
            Wk = W.reshape(KD, 128, 2 * FF)
            g = Wk[:, :, :FF].reshape(KD, 128, NJ, 128)
            u = Wk[:, :, FF:].reshape(KD, 128, NJ, 128)
            blk = np.concatenate([g, u], axis=3)
            out[f"f{w + 1}_win{l}"] = np.ascontiguousarray(blk.transpose(2, 1, 0, 3)).reshape(NJ, 128, KD * 256)
            out[f"f{w + 1}_wout{l}"] = np.ascontiguousarray(np.asarray(inp[wo][l], f32).reshape(NJ, 128, D))
        W = np.asarray(inp['w_in'][l], f32).reshape(KD, 128, DIN)
        fm = np.zeros((len(FMG), 128, KD, 128), f32)
        for gi, (c0, nc_, _) in enumerate(FMG):
            nc_ = min(nc_, DIN - c0)
            fm[gi, :, :, :nc_] = W[:, :, c0:c0 + nc_].transpose(1, 0, 2)
        out[f"m_fm{l}"] = fm.reshape(len(FMG), 128, KD * 128)
        tm = np.zeros((len(TMG), 128, KD, 256), f32)
        for gi, (c0, nc_, _) in enumerate(TMG):
            tm[gi] = W[:, :, c0:c0 + nc_].transpose(1, 0, 2)
        out[f"m_tm{l}"] = tm.reshape(len(TMG), 128, KD * 256)
        out[f"m_wout{l}"] = np.ascontiguousarray(np.asarray(inp['w_out'][l], f32).reshape(KD, 128, D))
        pv = np.zeros((128, NPV), f32)
        pv[:, PV['nf1']:PV['nf1'] + 8] = _col(inp['norm_ffn1'][l], 8)
        pv[:, PV['nmx']:PV['nmx'] + 8] = _col(inp['norm_mix'][l], 8)
        pv[:, PV['nf2']:PV['nf2'] + 8] = _col(inp['norm_ffn2'][l], 8)
        pv[:, PV['nfin']:PV['nfin'] + 8] = _col(inp['norm_final'], 8)
        pv[:, PV['lb0']:PV['lb0'] + 2] = _col(inp['hgrn_lb_logits'][0], 2)
        pv[:, PV['lbl']:PV['lbl'] + 2] = _col(inp['hgrn_lb_logits'][l], 2)
        for nm, key in (('pool_b', 'pool_b'), ('pool_s', 'pool_scale'), ('hnorm', 'hgrn_norm'), ('w0', 'rwkv_w0'), ('a0', 'rwkv_a0'),
                        ('kk', 'rwkv_k_k'), ('ka', 'rwkv_k_a'), ('rk', 'rwkv_r_k'), ('lnw', 'rwkv_ln_w'), ('lnb', 'rwkv_ln_b'),
                        ('glab', 'gla_b'), ('gnorm', 'gla_norm')):
            pv[:, PV[nm]:PV[nm] + 2] = _col(inp[key][l], 2)
        out[f"pvec{l}"] = pv
        out[f"mub{l}"] = np.ascontiguousarray(np.broadcast_to(np.asarray(inp['rwkv_mu'][l], f32)[None, :], (128, 1024)))
        sm = np.zeros((128, 5 * 256), f32)
        pw_ = np.asarray(inp['pool_w'][l], f32)
        for ck in range(2):
            sm[0:64, ck * 128:ck * 128 + 64] = pw_[2 * ck]
            sm[64:128, ck * 128 + 64:ck * 128 + 128] = pw_[2 * ck + 1]
        sm[0:64, 256:512] = np.asarray(inp['rwkv_w2'][l], f32)
        sm[64:128, 768:1024] = np.asarray(inp['rwkv_a2'][l], f32)
        sm[:, 512:768] = np.asarray(inp['rwkv_g2'][l], f32)
        sm[0:16, 1024:1280] = np.asarray(inp['gla_w2'][l], f32)
        out[f"smat{l}"] = sm
    out["cst"] = make_consts()
    return out


def prep_x(xc):
    NS, T, _ = xc.shape
    return np.ascontiguousarray(xc.reshape(NS, T, KD, 128).transpose(0, 3, 2, 1))


def unprep_out(o):
    NS, _, _, T = o.shape
    return np.ascontiguousarray(o.transpose(0, 3, 2, 1)).reshape(NS, T, D)


def kernel(**inputs):
    x = np.asarray(inputs['x'], np.float32)
    B, T, _ = x.shape
    NCORES = 8
    NS = B // NCORES
    L = 2
    nc, pg = build_program(T, NS, L)
    wts = prep_weights(inputs, L)
    in_maps = []
    for c in range(NCORES):
        m = dict(wts)
        m["xT"] = prep_x(x[c * NS:(c + 1) * NS])
        in_maps.append(m)
    res = run_bass_kernel_spmd(nc, in_maps, core_ids=list(range(NCORES)))
    outs = [unprep_out(np.asarray(r["outT"])) for r in res.results]
    return np.concatenate(outs, axis=0).astype(np.float32)
```

```python
import numpy as np
from contextlib import ExitStack
import concourse.bass as bass
import concourse.mybir as mybir
from concourse.bass_utils import run_bass_kernel_spmd

F32 = mybir.dt.float32
BF16 = mybir.dt.bfloat16
AF = mybir.ActivationFunctionType
ALU = mybir.AluOpType
ENGS = ['pe', 'act', 'dve', 'pool', 'sp']

D = 1024
KD = 8
FF = 2816
NJ = 22
G = 256
DIN = 3344
NORM_EPS = 1e-6
GN_EPS = 64e-5
QK = 0.125
TB = 512
TBM = 256
CH = 64
NCH = TBM // CH
DEBUG_STAGE = 99

FMG = [(0, 128, 0), (128, 128, 0), (256, 128, 0), (384, 128, 0), (512, 128, 0), (640, 128, 0),
       (1024, 128, 0), (1152, 128, 0),
       (1280, 128, 1), (1408, 128, 1), (1536, 128, 1), (1664, 128, 1), (1792, 128, 1), (1920, 128, 1),
       (2048, 128, 1), (2176, 128, 1),
       (2304, 128, 0), (2432, 128, 0), (2560, 128, 0), (2688, 128, 0), (3072, 128, 0), (3200, 128, 0),
       (3328, 128, 0)]
TMG = [(768, 256, 0), (1792, 256, 1), (2816, 256, 0)]
RW0 = 1280

PV = {}
_o = 0
for _n, _w in [('nf1', 8), ('nmx', 8), ('nf2', 8), ('pool_b', 2), ('pool_s', 2), ('lb0', 2), ('lbl', 2),
               ('hnorm', 2), ('w0', 2), ('a0', 2), ('kk', 2), ('ka', 2), ('rk', 2), ('lnw', 2), ('lnb', 2),
               ('glab', 2), ('gnorm', 2), ('nfin', 8)]:
    PV[_n] = _o
    _o += _w
NPV = _o


class Buf:
    __slots__ = ('name', 'w', 'r', 'const')

    def __init__(self, name, const=False):
        self.name = name
        self.w = None
        self.r = []
        self.const = const


class DSem:
    def __init__(self, h):
        self.h = h
        self.count = 0


class Prog:
    def __init__(self, nc, same_engine_sync=True):
        self.nc = nc
        self.ops = {e: [] for e in ENGS}
        self.cnt = {e: 0 for e in ENGS}
        self.same = same_engine_sync
        self.nops = 0
        self.last_rg = None
        self.last_pe_sig = True

    def op(self, eng, fn, reads=(), writes=(), sig=True, dsem=None, rg=None):
        waits = {}
        if eng == 'pe':
            if rg is not None and self.last_rg is not None and rg != self.last_rg:
                assert self.last_pe_sig
                waits['pe'] = self.cnt['pe']
            self.last_rg = rg
            self.last_pe_sig = sig

        def addw(tok):
            if tok is None:
                return
            k, v = tok
            if k == eng and (eng == 'pe' or not self.same):
                return
            if waits.get(k, 0) < v:
                waits[k] = v
        for b in reads:
            addw(b.w)
        for b in writes:
            addw(b.w)
            for t in b.r:
                addw(t)
        if dsem is not None:
            dsem.count += 16
            tok = (dsem, dsem.count)
            sig = False
        elif sig:
            self.cnt[eng] += 1
            tok = (eng, self.cnt[eng])
        else:
            tok = (eng, self.cnt[eng] + 1)
        for b in reads:
            if not b.const:
                b.r.append(tok)
                if len(b.r) > 48:
                    mx = {}
                    for k, v in b.r:
                        if mx.get(k, 0) < v:
                            mx[k] = v
                    b.r = list(mx.items())
        for b in writes:
            b.w = tok
            b.r = []
        self.ops[eng].append((fn, waits, sig, dsem))
        self.nops += 1
        return tok

    def emit(self, block, esems):
        nc = self.nc
        deco = {'pe': block.tensor, 'act': block.scalar, 'dve': block.vector,
                'pool': block.gpsimd, 'sp': block.sync}
        for e in ENGS:
            ops = self.ops[e]

            def body(eng, ops=ops, e=e):
                known = {}
                for fn, waits, sig, dsem in ops:
                    for k, v in waits.items():
                        if known.get(k, 0) >= v:
                            continue
                        known[k] = v
                        h = k.h if isinstance(k, DSem) else esems[k]
                        eng.wait_ge(h, v)
                    inst = fn(eng)
                    if dsem is not None:
                        inst.then_inc(dsem.h, 16)
                    elif sig:
                        inst.then_inc(esems[e], 1)
            deco[e](body)


def build_program(T, NS, L, mixers=('pool', 'hgrn', 'rwkv', 'gla'), do_ffn=True, do_mix=True):
    NT = T // TB
    nc = bass.Bass("TRN2", target_bir_lowering=False)
    dr = {}

    def din(name, shape):
        dr[name] = nc.dram_tensor(name, list(shape), F32, kind="ExternalInput").ap()
        return dr[name]
    xT = din("xT", [NS, 128, KD, T])
    outT = nc.dram_tensor("outT", [NS, 128, KD, T], F32, kind="ExternalOutput").ap()
    f_win = [[din(f"f{w}_win{l}", [NJ, 128, KD * 256]) for w in (1, 2)] for l in range(L)]
    f_wout = [[din(f"f{w}_wout{l}", [NJ, 128, D]) for w in (1, 2)] for l in range(L)]
    m_fm = [din(f"m_fm{l}", [len(FMG), 128, KD * 128]) for l in range(L)]
    m_tm = [din(f"m_tm{l}", [len(TMG), 128, KD * 256]) for l in range(L)]
    m_wout = [din(f"m_wout{l}", [KD, 128, D]) for l in range(L)]
    pvec_d = [din(f"pvec{l}", [128, NPV]) for l in range(L)]
    mub_d = [din(f"mub{l}", [128, 1024]) for l in range(L)]
    smat_d = [din(f"smat{l}", [128, 5 * 256]) for l in range(L)]
    cst_d = din("cst", [128, 1600])
    es = ExitStack()
    with es:
        def sb(name, shape, dt=F32):
            return es.enter_context(nc.sbuf_tensor("sb_" + name, list(shape), dt))

        def psum(name, shape, dt=F32):
            return es.enter_context(nc.psum_tensor("pp_" + name, list(shape), dt))
        esems = {e: es.enter_context(nc.semaphore("s_" + e)) for e in ENGS}

        def dsem(name):
            return DSem(es.enter_context(nc.semaphore(name)))
        pg = Prog(nc)
        block = es.enter_context(nc.Block())

        X = sb("X", [128, KD, T])
        XB = [[Buf(f"X{k}_{t}") for t in range(NT)] for k in range(KD)]
        xn = sb("xn", [128, KD, TB], BF16)
        xns = sb("xns", [128, KD, TBM], BF16)
        xnsB = Buf("xns")
        xnB = [Buf(f"xn{k}") for k in range(KD)]
        sq = [sb(f"sq{i}", [128, TB], BF16) for i in range(2)]
        sqB = [Buf(f"sq{i}") for i in range(2)]
        rstd = sb("rstd", [128, TB]); rstdB = Buf("rstd")
        ones = sb("ones", [128, 128], BF16); onesB = Buf("ones", const=True)
        bones = sb("bones", [128, 128], BF16)
        ident = sb("ident", [128, 128], BF16)
        cst = sb("cst", [128, 1600])
        cstB = Buf("cst", const=True)
        MI, MSU, MSL, ID4 = 0, 256, 512, 768
        SCM = 1024
        ICN = 1536
        pvec = [sb(f"pvec{l}", [128, NPV]) for l in range(L)]
        pvB = [Buf(f"pvec{l}", const=True) for l in range(L)]
        lbt = sb("lbt", [128, 8])
        mub = sb("mub", [128, 1024]); mubB = Buf("mub")
        omub = sb("omub", [128, 1024]); omubB = Buf("omub")
        smat = sb("smat", [128, 5 * 256]); smatB = Buf("smat")
        smatb = sb("smatb", [128, 2 * 128], BF16)
        wst = [sb(f"wst{i}", [128, KD * 256]) for i in range(2)]
        wstB = [Buf(f"wst{i}") for i in range(2)]
        wstS = [dsem(f"dwst{i}") for i in range(2)]
        wbf = [sb(f"wbf{i}", [128, KD * 256], BF16) for i in range(2)]
        wbfB = [Buf(f"wbf{i}") for i in range(2)]
        wbfB2 = [[Buf(f"wbf{i}a"), Buf(f"wbf{i}b")] for i in range(2)]
        hT = sb("hT", [128, NJ, TB], BF16)
        hB = [Buf(f"h{j}") for j in range(NJ)]
        sg = [sb("sg0", [128, TB])] * 2
        sgB = [Buf("sg0")] * 2
        PS = [psum(f"ps{i}", [128, 512]) for i in range(8)]
        PSB = [Buf(f"ps{i}") for i in range(8)]
        xS = [dsem(f"dx{k}") for k in range(KD)]
        oS = [dsem(f"dout{k}") for k in range(KD)]
        cS = dsem("dcst")
        pS = [dsem(f"dpv{l}") for l in range(L)]
        muS = dsem("dmu")
        smS = dsem("dsm")
        counters = {'w': 0, 'o': 0, 'sq': 0, 'sg': 0, 'pp': 0}

        def OP(eng, fn, reads=(), writes=(), sig=True, dsem=None, rg=None):
            return pg.op(eng, fn, reads, writes, sig, dsem, rg)

        def load_w(src_ap, ncols):
            i = counters['w'] % 2
            counters['w'] += 1
            OP('sp', lambda e: e.dma_start(out=wst[i][:, 0:ncols], in_=src_ap), writes=[wstB[i]], dsem=wstS[i])
            return i

        def load_w2(src_ap2):
            i = counters['w'] % 2
            counters['w'] += 1
            OP('sp', lambda e: e.dma_start(out=wst[i][:, 0:2 * D].rearrange("p (j c) -> p j c", j=2), in_=src_ap2.rearrange("j p c -> p j c")),
               writes=[wstB[i]], dsem=wstS[i])
            return i

        def cast_w(i, ncols, eng='pool'):
            if eng == 'act':
                OP('act', lambda e: e.activation(out=wbf[i][:, 0:ncols], in_=wst[i][:, 0:ncols], func=AF.Copy),
                   reads=[wstB[i]], writes=[wbfB[i], wbfB2[i][0], wbfB2[i][1]])
            else:
                OP(eng, lambda e: e.tensor_copy(out=wbf[i][:, 0:ncols], in_=wst[i][:, 0:ncols]),
                   reads=[wstB[i]], writes=[wbfB[i], wbfB2[i][0], wbfB2[i][1]])

        def cast_w_split(i, ncols):
            c1 = (ncols * 3 // 4) // 128 * 128
            OP('dve', lambda e: e.tensor_copy(out=wbf[i][:, 0:c1], in_=wst[i][:, 0:c1]), reads=[wstB[i]], writes=[wbfB2[i][0], wbfB[i]])
            OP('pool', lambda e: e.tensor_copy(out=wbf[i][:, c1:ncols], in_=wst[i][:, c1:ncols]), reads=[wstB[i]], writes=[wbfB2[i][1]])

        def rstd_from(psb, psbuf, n, scale, eps, dst, dstB):
            OP('act', lambda e: e.activation(out=dst[:, 0:n], in_=psb[:, 0:n], func=AF.Ln, scale=scale, bias=eps),
               reads=[psbuf], writes=[dstB])
            OP('act', lambda e: e.activation(out=dst[:, 0:n], in_=dst[:, 0:n], func=AF.Exp, scale=-0.5),
               reads=[dstB], writes=[dstB])

        def rmsnorm_to_xn(l, gcol, c0, n, bank):
            ti = c0 // TB
            for k in range(KD):
                i = counters['sq'] % 2
                counters['sq'] += 1
                OP('act', lambda e, k=k, i=i: e.activation(out=sq[i][:, 0:n], in_=X[:, k, c0:c0 + n], func=AF.Square),
                   reads=[XB[k][ti]], writes=[sqB[i]])
                OP('pe', lambda e, k=k, i=i: e.matmul(PS[bank][:, 0:n], lhsT=ones[:], rhs=sq[i][:, 0:n], start=(k == 0), stop=(k == KD - 1)),
                   reads=[sqB[i], onesB], writes=[PSB[bank]])
            rstd_from(PS[bank], PSB[bank], n, 1.0 / D, NORM_EPS, rstd, rstdB)
            for k in range(KD):
                OP('dve', lambda e, k=k: e.scalar_tensor_tensor(out=xn[:, k, 0:n], in0=X[:, k, c0:c0 + n],
                                                                 scalar=pvec[l][:, gcol + k:gcol + k + 1], in1=rstd[:, 0:n],
                                                                 op0=ALU.mult, op1=ALU.mult),
                   reads=[XB[k][ti], rstdB, pvB[l]], writes=[xnB[k]])

        def ffn(l, w, ti):
            gcol = PV['nf1'] if w == 0 else PV['nf2']
            c0 = ti * TB
            blocks = [('in', j) for j in range(NJ)] + [('out', jp) for jp in range(NJ // 2)]

            slots = {}

            def do_load(t):
                if t < len(blocks) and t not in slots:
                    kind_, j_ = blocks[t]
                    if kind_ == 'in':
                        slots[t] = load_w(f_win[l][w][j_], KD * 256)
                    else:
                        slots[t] = load_w2(f_wout[l][w][2 * j_:2 * j_ + 2])

            def do_ready(t):
                if t >= len(blocks):
                    return
                do_load(t)
                cast_w_split(slots[t], KD * 256)
            do_load(0)
            do_load(1)
            do_ready(0)
            rmsnorm_to_xn(l, gcol, c0, TB, 0)
            for t, (kind, j) in enumerate(blocks):
                i = slots[t]
                do_ready(t + 1)
                do_load(t + 2)
                if kind == 'in':
                    wv = wbf[i][:].rearrange("p (k c) -> p k c", k=KD)
                    pp = counters['pp'] % 2
                    counters['pp'] += 1
                    bg, bu = 2 * pp, 2 * pp + 1
                    for half, bk in ((0, bg), (1, bu)):
                        for k in range(KD):
                            OP('pe', lambda e, k=k, half=half, bk=bk, wv=wv: e.matmul(
                                PS[bk][:, :], lhsT=wv[:, k, half * 128:(half + 1) * 128], rhs=xn[:, k, 0:TB],
                                start=(k == 0), stop=(k == KD - 1)),
                               reads=[wbfB[i], wbfB2[i][0], wbfB2[i][1], xnB[k]], writes=[PSB[bk]], sig=(k == KD - 1))
                    si = counters['sg'] % 2
                    counters['sg'] += 1
                    OP('act', lambda e, si=si, bg=bg: e.activation(out=sg[si][:], in_=PS[bg][:, :], func=AF.Silu),
                       reads=[PSB[bg]], writes=[sgB[si]])
                    OP('dve', lambda e, si=si, bu=bu, j=j: e.tensor_tensor(out=hT[:, j, :], in0=sg[si][:], in1=PS[bu][:, :], op=ALU.mult),
                       reads=[sgB[si], PSB[bu]], writes=[hB[j]])
                else:
                    for q in range(2):
                        jj = 2 * j + q
                        for m in range(KD):
                            OP('pe', lambda e, m=m, jj=jj, q=q, i=i: e.matmul(PS[m][:, :], lhsT=wbf[i][:, q * D + m * 128:q * D + (m + 1) * 128], rhs=hT[:, jj, :],
                                                                   start=(jj == 0), stop=(jj == NJ - 1)),
                               reads=[wbfB[i], wbfB2[i][0], wbfB2[i][1], hB[jj]], writes=[PSB[m]], sig=(m == KD - 1 or jj == NJ - 1))
            for m in range(KD):
                OP('dve', lambda e, m=m: e.scalar_tensor_tensor(out=X[:, m, c0:c0 + TB], in0=PS[m][:, :], scalar=0.5,
                                                                 in1=X[:, m, c0:c0 + TB], op0=ALU.mult, op1=ALU.add),
                   reads=[PSB[m], XB[m][ti]], writes=[XB[m][ti]])

        OP('sp', lambda e: e.dma_start(out=cst[:], in_=cst_d), writes=[cstB], dsem=cS)
        for l in range(L):
            OP('sp', lambda e, l=l: e.dma_start(out=pvec[l][:], in_=pvec_d[l]), writes=[pvB[l]], dsem=pS[l])
        OP('pool', lambda e: e.memset(ones[:], 1.0), writes=[onesB])
        bonesB = Buf("bones", const=True)
        OP('pool', lambda e: e.memset(bones[:], 0.0), writes=[bonesB])
        OP('pool', lambda e: e.memset(bones[0:64, 0:64], 1.0), writes=[bonesB])
        OP('pool', lambda e: e.memset(bones[64:128, 64:128], 1.0), writes=[bonesB])
        identB = Buf("ident", const=True)
        OP('pool', lambda e: e.memset(ident[:], 1.0), writes=[identB])
        OP('pool', lambda e: e.affine_select(out=ident[:], in_=ident[:], pattern=[[-1, 128]], compare_op=ALU.is_equal,
                                             fill=0.0, base=0, channel_multiplier=1), reads=[identB], writes=[identB])

        if do_mix:
            YT = sb("YT", [128, KD, TBM], BF16)
            YB = [Buf(f"Y{k}") for k in range(KD)]
            NFA = 10
            FA = [sb(f"fa{i}", [128, TBM]) for i in range(NFA)]
            FAB = [Buf(f"fa{i}") for i in range(NFA)]
            NBA = 4
            BA = [sb(f"ba{i}", [128, TBM], BF16) for i in range(NBA)]
            BAB = [Buf(f"ba{i}") for i in range(NBA)]
            LL = [[sb(f"ll{hp}{i}", [128, TBM]) for i in range(3)] for hp in range(2)]
            LLB_ = [[Buf(f"ll{hp}{i}") for i in range(3)] for hp in range(2)]
            LLX = [sb(f"llx{i}", [128, TBM]) for i in range(2)]
            LLXB = [Buf(f"llx{i}") for i in range(2)]
            LB = [[sb(f"lb{hp}{i}", [128, TBM], BF16) for i in range(5)] for hp in range(2)]
            LBB = [[Buf(f"lb{hp}{i}") for i in range(5)] for hp in range(2)]
            PB16 = [sb(f"pb16{i}", [128, TBM], BF16) for i in range(2)]
            PB16B = [Buf(f"pb16{i}") for i in range(2)]
            Vt = [sb(f"vt{c}", [64, 256], BF16) for c in range(NCH)]
            VtB = [Buf(f"vt{c}") for c in range(NCH)]
            KEt = [sb(f"ket{c}", [64, 256], BF16) for c in range(NCH)]
            KEtB = [Buf(f"ket{c}") for c in range(NCH)]
            AEt = [sb(f"aet{c}", [64, 256], BF16) for c in range(NCH)]
            AEtB = [Buf(f"aet{c}") for c in range(NCH)]
            alias_ctr = [0]

            def mk(name, n):
                ts, bs = [], []
                for c in range(n):
                    idx = alias_ctr[0]
                    alias_ctr[0] += 1
                    j, half = idx // 2, idx % 2
                    ts.append(hT[0:64, j, half * 256:(half + 1) * 256])
                    bs.append(hB[j])
                return ts, bs
            ATs, ATsB = mk("ats", NCH)
            LKs, LKsB = mk("lks", NCH)
            ARs, ARsB = mk("ars", NCH)
            Nn, NnB = mk("nn", NCH)
            NTn, NTnB = mk("ntn", NCH)
            Nn2, Nn2B = mk("nn2", NCH)
            NTn2, NTn2B = mk("ntn2", NCH)
            Pn, PnB = mk("pn", NCH)
            Pn2, Pn2B = mk("pn2", NCH)
            Ysb = sb("ysb", [64, 256], BF16); YsbB = Buf("ysb")
            Usb = sb("usb", [64, 256], BF16); UsbB = Buf("usb")
            S32 = {m: [sb(f"s32{m}{hp}", [128, 64]) for hp in range(2)] for m in ('hgrn', 'gla', 'rwkv')}
            Sbf = {m: [sb(f"sbf{m}{hp}", [128, 64], BF16) for hp in range(2)] for m in ('hgrn', 'gla', 'rwkv')}
            S32B = {m: [Buf(f"s32{m}{hp}") for hp in range(2)] for m in ('hgrn', 'gla', 'rwkv')}
            SbfB = {m: [Buf(f"sbf{m}{hp}") for hp in range(2)] for m in ('hgrn', 'gla', 'rwkv')}
            stmp = [sb(f"stmp{hp}", [128, 64]) for hp in range(2)]
            stmpB = [Buf(f"stmp{hp}") for hp in range(2)]
            pext = sb("pext", [128, 2, 16 + TBM]); pextB = Buf("pext")
            pw = [sb(f"pw{i}", [128, 2, 16 + TBM]) for i in range(2)]
            pwB = [Buf(f"pw{i}") for i in range(2)]
            PST = PS[7][:, :].bitcast(BF16)
            lbtB = Buf("lbt", const=True)
            fa_ctr = [0]
            ba_ctr = [0]

            def fa():
                i = fa_ctr[0] % NFA
                fa_ctr[0] += 1
                return FA[i], FAB[i]

            def ba():
                i = ba_ctr[0] % NBA
                ba_ctr[0] += 1
                return BA[i], BAB[i]

            def pbank():
                b = counters['pp'] % 4
                counters['pp'] += 1
                return b

            wplan = {'plan': [], 'pos': 0, 'issued': {}, 'l': 0}

            def issue_load(l, d):
                kind, g = d
                if kind == 'fm':
                    return load_w(m_fm[l][g], KD * 128)
                if kind == 'wo':
                    return load_w2(m_wout[l][2 * g:2 * g + 2])
                return load_w(m_tm[l][g], KD * 256)

            def issue_cast(l, d, i):
                kind, g = d
                if kind == 'wo':
                    cast_w_split(i, 2 * D)
                    return (i,)
                if kind == 'fm':
                    col0, ncols, shift = FMG[g]
                    if shift:
                        mc = col0 - RW0
                        mu_b = mub[:, mc:mc + 128].unsqueeze(1).broadcast_to([128, KD, 128])
                        omu_b = omub[:, mc:mc + 128].unsqueeze(1).broadcast_to([128, KD, 128])
                        wv32 = wst[i][:, 0:KD * 128].rearrange("p (k c) -> p k c", k=KD)
                        wbv = wbf[i][:].rearrange("p (k c) -> p k c", k=KD)
                        OP('dve', lambda e: e.tensor_tensor(out=wbv[:, :, 0:128], in0=wv32, in1=omu_b, op=ALU.mult),
                           reads=[wstB[i], omubB], writes=[wbfB2[i][0], wbfB[i]])
                        OP('pool', lambda e: e.tensor_tensor(out=wbv[:, :, 128:256], in0=wv32, in1=mu_b, op=ALU.mult),
                           reads=[wstB[i], mubB], writes=[wbfB2[i][1]])
                    else:
                        cast_w_split(i, KD * 128)
                        wbv = wbf[i][:, 0:KD * 128].rearrange("p (k c) -> p k c", k=KD)
                    return (i, wbv)
                col0, ncols, shift = TMG[g]
                i2 = None
                wb2 = None
                if shift:
                    i2 = counters['w'] % 2
                    counters['w'] += 1
                    mc = col0 - RW0
                    mu_b = mub[:, mc:mc + 256].unsqueeze(1).broadcast_to([128, KD, 256])
                    omu_b = omub[:, mc:mc + 256].unsqueeze(1).broadcast_to([128, KD, 256])
                    wv32 = wst[i][:].rearrange("p (k c) -> p k c", k=KD)
                    wa = wbf[i][:].rearrange("p (k c) -> p k c", k=KD)
                    wb2 = wbf[i2][:].rearrange("p (k c) -> p k c", k=KD)
                    OP('dve', lambda e: e.tensor_tensor(out=wa, in0=wv32, in1=omu_b, op=ALU.mult),
                       reads=[wstB[i], omubB], writes=[wbfB[i], wbfB2[i][0], wbfB2[i][1]])
                    OP('pool', lambda e: e.tensor_tensor(out=wb2, in0=wv32, in1=mu_b, op=ALU.mult),
                       reads=[wstB[i], mubB], writes=[wbfB[i2], wbfB2[i2][0], wbfB2[i2][1]])
                else:
                    cast_w_split(i, KD * 256)
                    wa = wbf[i][:].rearrange("p (k c) -> p k c", k=KD)
                return (i, i2, wa, wb2)

            def two_slot(d):
                return d[0] == 'tm' and bool(TMG[d[1]][2])

            def ensure_load(l, pos):
                plan = wplan['plan']
                if pos < len(plan) and pos not in wplan['loaded'] and not two_slot(plan[pos]):
                    wplan['loaded'][pos] = issue_load(l, plan[pos])

            def ensure_cast(l, pos):
                plan = wplan['plan']
                if pos < len(plan) and pos not in wplan['issued'] and not two_slot(plan[pos]):
                    ensure_load(l, pos)
                    wplan['issued'][pos] = issue_cast(l, plan[pos], wplan['loaded'].pop(pos))

            def acq(l, d):
                pos = wplan['pos']
                plan = wplan['plan']
                assert plan[pos] == d, (plan[pos], d)
                if pos not in wplan['issued']:
                    if pos not in wplan['loaded']:
                        wplan['loaded'][pos] = issue_load(l, d)
                    wplan['issued'][pos] = issue_cast(l, d, wplan['loaded'].pop(pos))
                info = wplan['issued'].pop(pos)
                wplan['pos'] = pos + 1
                if not two_slot(d) and not (pos + 1 < len(plan) and two_slot(plan[pos + 1])):
                    ensure_cast(l, pos + 1)
                    if not (pos + 2 < len(plan) and two_slot(plan[pos + 2])):
                        ensure_load(l, pos + 2)
                return info

            def proj_fm(l, g, first_block):
                col0, ncols, shift = FMG[g]
                i, wbv = acq(l, ('fm', g))
                bk = pbank()
                nmm = KD * (2 if shift else 1)
                n = 0
                for k in range(KD):
                    n += 1
                    OP('pe', lambda e, k=k, n=n: e.matmul(PS[bk][0:ncols, 0:TBM], lhsT=wbv[:, k, 0:ncols], rhs=xn[:, k, 0:TBM],
                                                          start=(n == 1), stop=(n == nmm)),
                       reads=[wbfB[i], wbfB2[i][0], wbfB2[i][1], xnB[k]], writes=[PSB[bk]], sig=(n == nmm))
                if shift:
                    for k in range(KD):
                        n += 1
                        OP('pe', lambda e, k=k, n=n: e.matmul(PS[bk][0:ncols, 0:TBM], lhsT=wbv[:, k, 128:128 + ncols], rhs=xns[:, k, 0:TBM],
                                                              start=False, stop=(n == nmm)),
                           reads=[wbfB[i], wbfB2[i][0], wbfB2[i][1], xnsB], writes=[PSB[bk]], sig=(n == nmm))
                return bk

            def proj_tm(l, g):
                col0, ncols, shift = TMG[g]
                i, i2, wa, wb2 = acq(l, ('tm', g))
                for c in range(NCH):
                    bk = pbank()
                    nmm = KD * (2 if shift else 1)
                    n = 0
                    for k in range(KD):
                        n += 1
                        OP('pe', lambda e, k=k, n=n, c=c, bk=bk: e.matmul(PS[bk][0:64, 0:256], lhsT=xn[:, k, c * CH:(c + 1) * CH],
                                                                     rhs=wa[:, k, :], start=(n == 1), stop=(n == nmm)),
                           reads=[wbfB[i], wbfB2[i][0], wbfB2[i][1], xnB[k]], writes=[PSB[bk]], sig=(n == nmm))
                    if shift:
                        for k in range(KD):
                            n += 1
                            OP('pe', lambda e, k=k, n=n, c=c, bk=bk: e.matmul(PS[bk][0:64, 0:256], lhsT=xns[:, k, c * CH:(c + 1) * CH],
                                                                         rhs=wb2[:, k, :], start=False, stop=(n == nmm)),
                               reads=[wbfB[i2], wbfB2[i2][0], wbfB2[i2][1], xnsB], writes=[PSB[bk]], sig=(n == nmm))
                    OP('act', lambda e, c=c, bk=bk: e.activation(out=Vt[c][:], in_=PS[bk][0:64, 0:256], func=AF.Copy),
                       reads=[PSB[bk]], writes=[VtB[c]])

            def scan_decay(g_t, g_b):
                b_t, b_b = fa()
                OP('dve', lambda e: e.tensor_tensor_scan(out=b_t[:], data0=cst[:, SCM:SCM + TBM], data1=g_t[:], initial=0.0,
                                                         op0=ALU.mult, op1=ALU.add), reads=[g_b, cstB], writes=[b_b])
                return b_t, b_b

            def chunk_engine(mname, QE, KE, PC, rw=None):
                isrw = rw is not None
                if DEBUG_STAGE < 1:
                    return
                for c in range(NCH):
                    for (src, dst, dstB_) in ([(KE, KEt, KEtB)] + ([(rw['AE'], AEt, AEtB)] if isrw else [])):
                        for hp in range(2):
                            OP('pe', lambda e, hp=hp, c=c, src=src: e.transpose(PST[0:64, hp * 128:(hp + 1) * 128],
                                                                             src[hp][0][:, c * CH:(c + 1) * CH], ident[:]),
                               reads=[src[hp][1], identB], writes=[PSB[7]])
                        OP('act', lambda e, c=c, dst=dst: e.activation(out=dst[c][:], in_=PST[0:64, 0:256], func=AF.Copy),
                           reads=[PSB[7]], writes=[dstB_[c]])
                if DEBUG_STAGE < 2:
                    return
                for c in range(NCH):
                    cs = slice(c * CH, (c + 1) * CH)
                    def sc(lh, rh, dst_ps, cs=cs):
                        for h in range(4):
                            hp, r = h // 2, (h % 2) * 64
                            OP('pe', lambda e, h=h, hp=hp, r=r: e.matmul(dst_ps[0:64, h * 64:(h + 1) * 64], lhsT=lh[hp][0][r:r + 64, cs],
                                                                         rhs=rh[hp][0][r:r + 64, cs], start=True, stop=True),
                               reads=[lh[hp][1], rh[hp][1]], writes=[PSB[6]], rg=r)
                    sc(KE, QE, PS[6][:, 0:256])
                    OP('dve', lambda e, c=c: e.tensor_tensor(out=ATs[c][:], in0=PS[6][0:64, 0:256], in1=cst[0:64, MI:MI + 256], op=ALU.mult),
                       reads=[PSB[6], cstB], writes=[ATsB[c]])
                    if isrw:
                        sc(KE, rw['BE'], PS[6][:, 256:512])
                        OP('dve', lambda e, c=c: e.tensor_tensor(out=LKs[c][:], in0=PS[6][0:64, 256:512], in1=cst[0:64, MSU:MSU + 256], op=ALU.mult),
                           reads=[PSB[6], cstB], writes=[LKsB[c]])
                        sc(rw['AE'], QE, PS[6][:, 0:256])
                        OP('dve', lambda e, c=c: e.tensor_tensor(out=ARs[c][:], in0=PS[6][0:64, 0:256], in1=cst[0:64, MI:MI + 256], op=ALU.mult),
                           reads=[PSB[6], cstB], writes=[ARsB[c]])
                        sc(rw['AE'], rw['BE'], PS[6][:, 256:512])
                        OP('dve', lambda e, c=c: e.scalar_tensor_tensor(out=NTn[c][:], in0=PS[6][0:64, 256:512], scalar=-1.0,
                                                                         in1=cst[0:64, MSU:MSU + 256], op0=ALU.mult, op1=ALU.mult),
                           reads=[PSB[6], cstB], writes=[NTnB[c]])
                        sc(rw['BE'], rw['AE'], PS[6][:, 0:256])
                        OP('dve', lambda e, c=c: e.scalar_tensor_tensor(out=Nn[c][:], in0=PS[6][0:64, 0:256], scalar=-1.0,
                                                                         in1=cst[0:64, MSL:MSL + 256], op0=ALU.mult, op1=ALU.mult),
                           reads=[PSB[6], cstB], writes=[NnB[c]])
                        OP('pool', lambda e, c=c: e.tensor_tensor(out=Pn[c][:], in0=NTn[c][:], in1=cst[0:64, ID4:ID4 + 256], op=ALU.add),
                           reads=[NTnB[c], cstB], writes=[PnB[c]])
                if isrw:
                    curN, curNB, curNT, curNTB = Nn, NnB, NTn, NTnB
                    nxtN, nxtNB, nxtNT, nxtNTB = Nn2, Nn2B, NTn2, NTn2B
                    curP, curPB, nxtP, nxtPB = Pn, PnB, Pn2, Pn2B
                    for lev in range(1, 6):
                        for c in range(NCH):
                            bk = pbank()
                            for h in range(4):
                                hs = slice(h * 64, (h + 1) * 64)
                                OP('pe', lambda e, c=c, hs=hs, bk=bk, a=curNT, b=curN: e.matmul(PS[bk][0:64, hs], lhsT=a[c][:, hs], rhs=b[c][:, hs],
                                                                                         start=True, stop=True),
                                   reads=[curNTB[c], curNB[c]], writes=[PSB[bk]], rg=0)
                            if lev < 5:
                                for h in range(4):
                                    hs = slice(h * 64, (h + 1) * 64)
                                    hs2 = slice(256 + h * 64, 256 + (h + 1) * 64)
                                    OP('pe', lambda e, c=c, hs=hs, hs2=hs2, bk=bk, a=curN, b=curNT: e.matmul(PS[bk][0:64, hs2], lhsT=a[c][:, hs], rhs=b[c][:, hs],
                                                                                                     start=True, stop=True),
                                       reads=[curNTB[c], curNB[c]], writes=[PSB[bk]], rg=0)
                                OP('act', lambda e, c=c, bk=bk, d=nxtNT: e.activation(out=d[c][:], in_=PS[bk][0:64, 256:512], func=AF.Copy),
                                   reads=[PSB[bk]], writes=[nxtNTB[c]])
                            OP('act', lambda e, c=c, bk=bk, d=nxtN: e.activation(out=d[c][:], in_=PS[bk][0:64, 0:256], func=AF.Copy),
                               reads=[PSB[bk]], writes=[nxtNB[c]])
                        curN, curNB, nxtN, nxtNB = nxtN, nxtNB, curN, curNB
                        curNT, curNTB, nxtNT, nxtNTB = nxtNT, nxtNTB, curNT, curNTB
                        for c in range(NCH):
                            bk = pbank()
                            for h in range(4):
                                hs = slice(h * 64, (h + 1) * 64)
                                OP('pe', lambda e, c=c, hs=hs, bk=bk, a=curN, b=curP: e.matmul(PS[bk][0:64, hs], lhsT=a[c][:, hs], rhs=b[c][:, hs],
                                                                                        start=True, stop=True),
                                   reads=[curNB[c], curPB[c]], writes=[PSB[bk]], rg=0)
                            OP('dve', lambda e, c=c, bk=bk, s=curP, d=nxtP: e.tensor_tensor(out=d[c][:], in0=PS[bk][0:64, 0:256], in1=s[c][:], op=ALU.add),
                               reads=[PSB[bk], curPB[c]], writes=[nxtPB[c]])
                        curP, curPB, nxtP, nxtPB = nxtP, nxtPB, curP, curPB
                    TT, TTB = curP, curPB
                if DEBUG_STAGE < 3:
                    return
                S3, Sb, S3B, SbB = S32[mname], Sbf[mname], S32B[mname], SbfB[mname]
                for c in range(NCH):
                    cs = slice(c * CH, (c + 1) * CH)
                    if isrw:
                        for h in range(4):
                            hp, r = h // 2, (h % 2) * 64
                            hs = slice(h * 64, (h + 1) * 64)
                            OP('pe', lambda e, hp=hp, r=r, hs=hs, cs=cs: e.matmul(PS[6][0:64, hs], lhsT=rw['BE'][hp][0][r:r + 64, cs], rhs=Sb[hp][r:r + 64, :],
                                                                         start=True, stop=False),
                               reads=[rw['BE'][hp][1], SbB[hp]], writes=[PSB[6]], rg=r)
                            OP('pe', lambda e, hs=hs, c=c: e.matmul(PS[6][0:64, hs], lhsT=LKs[c][:, hs], rhs=Vt[c][:, hs], start=False, stop=True),
                               reads=[LKsB[c], VtB[c]], writes=[PSB[6]], rg=0)
                        OP('act', lambda e: e.activation(out=Ysb[:], in_=PS[6][0:64, 0:256], func=AF.Copy), reads=[PSB[6]], writes=[YsbB])
                        for h in range(4):
                            hs = slice(h * 64, (h + 1) * 64)
                            hs2 = slice(256 + h * 64, 256 + (h + 1) * 64)
                            OP('pe', lambda e, hs=hs, hs2=hs2, c=c: e.matmul(PS[6][0:64, hs2], lhsT=TT[c][:, hs], rhs=Ysb[:, hs], start=True, stop=True),
                               reads=[TTB[c], YsbB], writes=[PSB[6]], rg=0)
                        OP('act', lambda e: e.activation(out=Usb[:], in_=PS[6][0:64, 256:512], func=AF.Copy, scale=-1.0),
                           reads=[PSB[6]], writes=[UsbB])
                    for h in range(4):
                        hp, r = h // 2, (h % 2) * 64
                        hs = slice(h * 64, (h + 1) * 64)
                        ob = PS[4 + hp][r:r + 64, cs]
                        OP('pe', lambda e, ob=ob, hs=hs, c=c: e.matmul(ob, lhsT=Vt[c][:, hs], rhs=ATs[c][:, hs], start=True, stop=False),
                           reads=[VtB[c], ATsB[c]], writes=[PSB[4 + hp]], rg=0)
                        if isrw:
                            OP('pe', lambda e, ob=ob, hs=hs, c=c: e.matmul(ob, lhsT=Usb[:, hs], rhs=ARs[c][:, hs], start=False, stop=False),
                               reads=[UsbB, ARsB[c]], writes=[PSB[4 + hp]], rg=0)
                        OP('pe', lambda e, ob=ob, hp=hp, r=r, cs=cs: e.matmul(ob, lhsT=Sb[hp][r:r + 64, :], rhs=QE[hp][0][r:r + 64, cs], start=False, stop=True),
                           reads=[SbB[hp], QE[hp][1]], writes=[PSB[4 + hp]], rg=r)
                    for h in range(4):
                        hp, r = h // 2, (h % 2) * 64
                        hs = slice(h * 64, (h + 1) * 64)
                        sp_ = PS[7][r:r + 64, 256 + hp * 64:256 + (hp + 1) * 64]
                        OP('pe', lambda e, sp_=sp_, hs=hs, c=c: e.matmul(sp_, lhsT=KEt[c][:, hs], rhs=Vt[c][:, hs], start=True, stop=(not isrw)),
                           reads=[KEtB[c], VtB[c]], writes=[PSB[7]], rg=0)
                        if isrw:
                            OP('pe', lambda e, sp_=sp_, hs=hs, c=c: e.matmul(sp_, lhsT=AEt[c][:, hs], rhs=Usb[:, hs], start=False, stop=True),
                               reads=[AEtB[c], UsbB], writes=[PSB[7]], rg=0)
                    for hp in range(2):
                        pc = PC[hp][0][:, (c + 1) * CH - 1:(c + 1) * CH]
                        OP('dve', lambda e, hp=hp: e.tensor_tensor(out=stmp[hp][:], in0=PS[7][:, 256 + hp * 64:256 + (hp + 1) * 64], in1=S3[hp][:], op=ALU.add),
                           reads=[PSB[7], S3B[hp]], writes=[stmpB[hp]])
                        OP('dve', lambda e, hp=hp, pc=pc: e.tensor_scalar(out=S3[hp][:], in0=stmp[hp][:], scalar1=pc, scalar2=None, op0=ALU.mult),
                           reads=[stmpB[hp], PC[hp][1]], writes=[S3B[hp]])
                        OP('dve', lambda e, hp=hp, pc=pc: e.tensor_scalar(out=Sb[hp][:], in0=stmp[hp][:], scalar1=pc, scalar2=None, op0=ALU.mult),
                           reads=[stmpB[hp], PC[hp][1]], writes=[SbB[hp]])

            def evac_fm(bk, func=AF.Copy, scale=1.0, bias=None, dt='f', rows=128, dst=None):
                t, b = dst if dst is not None else (fa() if dt == 'f' else ba())
                kw = {}
                if bias is not None:
                    kw['bias'] = bias
                OP('act', lambda e: e.activation(out=t[0:rows, :], in_=PS[bk][0:rows, 0:TBM], func=func, scale=scale, **kw),
                   reads=[PSB[bk]] + ([pvB[0]] if bias is not None else []), writes=[b])
                return t, b

            def mixer_block(l, s, bi):
                first = (bi == 0)
                c0 = bi * TBM
                ti = c0 // TB
                plan = []
                if 'pool' in mixers:
                    plan += [('fm', 0), ('fm', 1)]
                if 'hgrn' in mixers:
                    for hp_ in range(2):
                        plan += [('fm', 2 + hp_), ('fm', 4 + hp_), ('fm', 6 + hp_)]
                    plan += [('tm', 0)]
                if 'gla' in mixers:
                    plan += [('fm', 22)]
                    for hp_ in range(2):
                        plan += [('fm', 16 + hp_), ('fm', 18 + hp_), ('fm', 20 + hp_)]
                    plan += [('tm', 2)]
                if 'rwkv' in mixers:
                    plan += [('fm', 14), ('fm', 15)]
                    for hp_ in range(2):
                        plan += [('fm', 8 + hp_), ('fm', 10 + hp_), ('fm', 12 + hp_)]
                    plan += [('tm', 1)]
                plan += [('wo', jp) for jp in range(KD // 2)]
                wplan['plan'] = plan
                wplan['pos'] = 0
                wplan['issued'] = {}
                wplan['loaded'] = {}
                if plan:
                    ensure_load(l, 0)
                    ensure_load(l, 1)
                    ensure_cast(l, 0)
                pv = pvec[l]
                if 'rwkv' in mixers:
                    if first:
                        OP('pool', lambda e: e.memset(xns[:, :, 0:2], 0.0), writes=[xnsB])
                    else:
                        OP('pool', lambda e: e.tensor_copy(out=xns[:, :, 0:1], in_=xn[:, :, TBM - 1:TBM]), reads=xnB, writes=[xnsB])
                rmsnorm_to_xn(l, PV['nmx'], c0, TBM, 0)
                if 'rwkv' in mixers:
                    OP('pool', lambda e: e.tensor_copy(out=xns[:, :, 1:TBM], in_=xn[:, :, 0:TBM - 1]), reads=xnB, writes=[xnsB])
                if 'pool' in mixers:
                    if first:
                        OP('pool', lambda e: e.memset(pext[:, :, 0:16], 0.0), writes=[pextB])
                    else:
                        OP('pool', lambda e: e.tensor_copy(out=pext[:, :, 0:16], in_=pext[:, :, TBM:TBM + 16]), reads=[pextB], writes=[pextB])
                    for ck in range(2):
                        bk = proj_fm(l, ck, first)
                        OP('act', lambda e, ck=ck, bk=bk: e.activation(out=pext[:, ck, 16:16 + TBM], in_=PS[bk][:, 0:TBM], func=AF.Copy),
                           reads=[PSB[bk]], writes=[pextB])
                    W_ = 16 + TBM
                    OP('dve', lambda e: e.tensor_tensor(out=pw[0][:, :, 1:W_], in0=pext[:, :, 1:W_], in1=pext[:, :, 0:W_ - 1], op=ALU.add),
                       reads=[pextB], writes=[pwB[0]])
                    def poolfin(src, ck, r0, wdw, first=first):
                        yt, yb = PB16[ck], PB16B[ck]
                        OP('dve', lambda e: e.scalar_tensor_tensor(out=yt[r0:r0 + 64, :], in0=src[r0:r0 + 64, ck, 16:16 + TBM], scalar=1.0 / wdw,
                                                                   in1=pext[r0:r0 + 64, ck, 16:16 + TBM], op0=ALU.mult, op1=ALU.subtract),
                           reads=[pwB[0], pwB[1], pextB], writes=[yb])
                        if first:
                            t2, b2 = FA[0], FAB[0]
                            OP('dve', lambda e: e.tensor_tensor(out=t2[r0:r0 + 64, 0:16], in0=src[r0:r0 + 64, ck, 16:32],
                                                                in1=cst[r0:r0 + 64, ICN + ck * 16:ICN + ck * 16 + 16], op=ALU.mult),
                               reads=[pwB[0], pwB[1], cstB], writes=[b2])
                            OP('dve', lambda e: e.tensor_tensor(out=yt[r0:r0 + 64, 0:16], in0=t2[r0:r0 + 64, 0:16],
                                                                in1=pext[r0:r0 + 64, ck, 16:32], op=ALU.subtract),
                               reads=[b2, pextB], writes=[yb])
                    poolfin(pw[0], 0, 0, 2)
                    OP('dve', lambda e: e.tensor_tensor(out=pw[1][:, :, 3:W_], in0=pw[0][:, :, 3:W_], in1=pw[0][:, :, 1:W_ - 2], op=ALU.add),
                       reads=[pwB[0]], writes=[pwB[1]])
                    poolfin(pw[1], 0, 64, 4)
                    OP('dve', lambda e: e.tensor_tensor(out=pw[0][:, :, 7:W_], in0=pw[1][:, :, 7:W_], in1=pw[1][:, :, 3:W_ - 4], op=ALU.add),
                       reads=[pwB[1]], writes=[pwB[0]])
                    poolfin(pw[0], 1, 0, 8)
                    OP('dve', lambda e: e.tensor_tensor(out=pw[1][:, :, 15:W_], in0=pw[0][:, :, 15:W_], in1=pw[0][:, :, 7:W_ - 8], op=ALU.add),
                       reads=[pwB[0]], writes=[pwB[1]])
                    poolfin(pw[1], 1, 64, 16)
                    for ck in range(2):
                        bk = pbank()
                        OP('pe', lambda e, ck=ck, bk=bk: e.matmul(PS[bk][:, 0:TBM], lhsT=smatb[:, ck * 128:(ck + 1) * 128], rhs=PB16[ck][:], start=True, stop=True),
                           reads=[PB16B[ck], smatB], writes=[PSB[bk]])
                        OP('dve', lambda e, ck=ck, bk=bk: e.tensor_scalar(out=YT[:, ck, :], in0=PS[bk][:, 0:TBM], scalar1=pv[:, PV['pool_b'] + ck:PV['pool_b'] + ck + 1],
                                                                      scalar2=pv[:, PV['pool_s'] + ck:PV['pool_s'] + ck + 1], op0=ALU.add, op1=ALU.mult),
                           reads=[PSB[bk], pvB[l]], writes=[YB[ck]])
                else:
                    for ck in range(2):
                        OP('pool', lambda e, ck=ck: e.memset(YT[:, ck, :], 0.0), writes=[YB[ck]])

                def out_rstd(Ot, lhs_ones, n_ch, eps):
                    bk = pbank()
                    for hp in range(2):
                        st, sbb = ba()
                        OP('act', lambda e, hp=hp, st=st, Ot=Ot: e.activation(out=st[:], in_=Ot[hp][0][:], func=AF.Square), reads=[Ot[hp][1]], writes=[sbb])
                        if lhs_ones is ones:
                            OP('pe', lambda e, hp=hp, st=st: e.matmul(PS[bk][:, 0:TBM], lhsT=ones[:], rhs=st[:], start=(hp == 0), stop=(hp == 1)),
                               reads=[sbb, onesB], writes=[PSB[bk]])
                        else:
                            bk2 = bk if hp == 0 else pbank()
                            OP('pe', lambda e, hp=hp, st=st, bk2=bk2: e.matmul(PS[bk2][:, 0:TBM], lhsT=bones[:], rhs=st[:], start=True, stop=True),
                               reads=[sbb, bonesB], writes=[PSB[bk2]])
                            if hp == 0:
                                bk0 = bk2
                            else:
                                bk1 = bk2
                    if lhs_ones is ones:
                        rt, rb = fa()
                        rstd_from(PS[bk], PSB[bk], TBM, 1.0 / n_ch, eps, rt, rb)
                        return [(rt, rb), (rt, rb)]
                    res = []
                    for bkx in (bk0, bk1):
                        rt, rb = fa()
                        rstd_from(PS[bkx], PSB[bkx], TBM, 1.0 / n_ch, eps, rt, rb)
                        res.append((rt, rb))
                    return res

                def evac_O():
                    Ot = []
                    for hp in range(2):
                        t, b = fa()
                        OP('act', lambda e, hp=hp, t=t: e.activation(out=t[:], in_=PS[4 + hp][:, 0:TBM], func=AF.Copy), reads=[PSB[4 + hp]], writes=[b])
                        Ot.append((t, b))
                    return Ot

                if 'hgrn' in mixers:
                    QE, KE, PCx, GT = [], [], [], []
                    for hp in range(2):
                        bq = proj_fm(l, 2 + hp, first)
                        qt, qb = evac_fm(bq, AF.Silu)
                        bf_ = proj_fm(l, 4 + hp, first)
                        st_, sb_ = evac_fm(bf_, AF.Sigmoid)
                        ft, fb = fa()
                        OP('dve', lambda e, hp=hp, st_=st_, ft=ft: e.tensor_scalar(out=ft[:], in0=st_[:], scalar1=lbt[:, 2 * l + hp:2 * l + hp + 1],
                                                                             scalar2=lbt[:, 4 + 2 * l + hp:4 + 2 * l + hp + 1], op0=ALU.mult, op1=ALU.add),
                           reads=[sb_, lbtB], writes=[fb])
                        lt, lb_ = fa()
                        OP('dve', lambda e, ft=ft, lt=lt: e.tensor_scalar_max(out=lt[:], in0=ft[:], scalar1=1e-30), reads=[fb], writes=[lb_])
                        OP('act', lambda e, lt=lt: e.activation(out=lt[:], in_=lt[:], func=AF.Ln), reads=[lb_], writes=[lb_])
                        bt, bb = scan_decay(lt, lb_)
                        ebt, ebb = LL[hp][0], LLB_[hp][0]
                        OP('act', lambda e, bt=bt, ebt=ebt: e.activation(out=ebt[:], in_=bt[:], func=AF.Exp), reads=[bb], writes=[ebb])
                        OP('act', lambda e, bt=bt: e.activation(out=bt[:], in_=bt[:], func=AF.Exp, scale=-1.0), reads=[bb], writes=[bb])
                        qe, qeb = LB[hp][0], LBB[hp][0]
                        OP('dve', lambda e, qt=qt, ebt=ebt, qe=qe: e.scalar_tensor_tensor(out=qe[:], in0=qt[:], scalar=QK, in1=ebt[:], op0=ALU.mult, op1=ALU.mult),
                           reads=[qb, ebb], writes=[qeb])
                        OP('dve', lambda e, ft=ft: e.tensor_scalar(out=ft[:], in0=ft[:], scalar1=-1.0, scalar2=1.0, op0=ALU.mult, op1=ALU.add),
                           reads=[fb], writes=[fb])
                        ke, keb = LB[hp][1], LBB[hp][1]
                        OP('dve', lambda e, ft=ft, bt=bt, ke=ke: e.tensor_tensor(out=ke[:], in0=ft[:], in1=bt[:], op=ALU.mult), reads=[fb, bb], writes=[keb])
                        bg_ = proj_fm(l, 6 + hp, first)
                        gt, gb = evac_fm(bg_, AF.Sigmoid, dst=(LL[hp][1], LLB_[hp][1]))
                        QE.append((qe, qeb)); KE.append((ke, keb)); PCx.append((ebt, ebb)); GT.append((gt, gb))
                    proj_tm(l, 0)
                    chunk_engine('hgrn', QE, KE, PCx)
                    Ot = evac_O()
                    rs = out_rstd(Ot, ones, 256, NORM_EPS)
                    for hp in range(2):
                        t1, b1 = fa()
                        OP('dve', lambda e, hp=hp, t1=t1, Ot=Ot, rs=rs: e.scalar_tensor_tensor(out=t1[:], in0=Ot[hp][0][:], scalar=pv[:, PV['hnorm'] + hp:PV['hnorm'] + hp + 1],
                                                                           in1=rs[hp][0][:], op0=ALU.mult, op1=ALU.mult),
                           reads=[Ot[hp][1], rs[hp][1], pvB[l]], writes=[b1])
                        OP('dve', lambda e, hp=hp, t1=t1, GT=GT: e.tensor_tensor(out=YT[:, 2 + hp, :], in0=t1[:], in1=GT[hp][0][:], op=ALU.mult),
                           reads=[b1, GT[hp][1]], writes=[YB[2 + hp]])
                else:
                    for ck in (2, 3):
                        OP('pool', lambda e, ck=ck: e.memset(YT[:, ck, :], 0.0), writes=[YB[ck]])

                if 'gla' in mixers:
                    bga = proj_fm(l, 22, first)
                    gat, gab = LLX[0], LLXB[0]
                    OP('act', lambda e: e.activation(out=gat[:], in_=PS[bga][:, 0:TBM], func=AF.Copy), reads=[PSB[bga]], writes=[gab])
                    QE, KE, PCx, GT = [], [], [], []
                    for hp in range(2):
                        bk = pbank()
                        OP('pe', lambda e, hp=hp, bk=bk: e.matmul(PS[bk][:, 0:TBM], lhsT=smat[:, 1024 + hp * 128:1024 + (hp + 1) * 128], rhs=gat[:], start=True, stop=True),
                           reads=[gab, smatB], writes=[PSB[bk]])
                        lt, lb_ = evac_fm(bk, AF.Sigmoid, bias=pv[:, PV['glab'] + hp:PV['glab'] + hp + 1])
                        OP('act', lambda e, lt=lt: e.activation(out=lt[:], in_=lt[:], func=AF.Ln), reads=[lb_], writes=[lb_])
                        bt, bb = scan_decay(lt, lb_)
                        ebt, ebb = LL[hp][0], LLB_[hp][0]
                        OP('act', lambda e, bt=bt, ebt=ebt: e.activation(out=ebt[:], in_=bt[:], func=AF.Exp, scale=1.0 / 16), reads=[bb], writes=[ebb])
                        OP('act', lambda e, bt=bt: e.activation(out=bt[:], in_=bt[:], func=AF.Exp, scale=-1.0 / 16), reads=[bb], writes=[bb])
                        bq = proj_fm(l, 16 + hp, first)
                        qe, qeb = LB[hp][0], LBB[hp][0]
                        OP('dve', lambda e, bq=bq, ebt=ebt, qe=qe: e.scalar_tensor_tensor(out=qe[:], in0=PS[bq][:, 0:TBM], scalar=QK, in1=ebt[:], op0=ALU.mult, op1=ALU.mult),
                           reads=[PSB[bq], ebb], writes=[qeb])
                        bkk = proj_fm(l, 18 + hp, first)
                        ke, keb = LB[hp][1], LBB[hp][1]
                        OP('dve', lambda e, bkk=bkk, bt=bt, ke=ke: e.tensor_tensor(out=ke[:], in0=PS[bkk][:, 0:TBM], in1=bt[:], op=ALU.mult), reads=[PSB[bkk], bb], writes=[keb])
                        bg_ = proj_fm(l, 20 + hp, first)
                        gt, gb = evac_fm(bg_, AF.Silu, dst=(LL[hp][1], LLB_[hp][1]))
                        QE.append((qe, qeb)); KE.append((ke, keb)); PCx.append((ebt, ebb)); GT.append((gt, gb))
                    proj_tm(l, 2)
                    chunk_engine('gla', QE, KE, PCx)
                    Ot = evac_O()
                    rs = out_rstd(Ot, bones, 64, NORM_EPS)
                    for hp in range(2):
                        t1, b1 = fa()
                        OP('dve', lambda e, hp=hp, t1=t1, Ot=Ot, rs=rs: e.scalar_tensor_tensor(out=t1[:], in0=Ot[hp][0][:], scalar=pv[:, PV['gnorm'] + hp:PV['gnorm'] + hp + 1],
                                                                           in1=rs[hp][0][:], op0=ALU.mult, op1=ALU.mult),
                           reads=[Ot[hp][1], rs[hp][1], pvB[l]], writes=[b1])
                        OP('dve', lambda e, hp=hp, t1=t1, GT=GT: e.tensor_tensor(out=YT[:, 6 + hp, :], in0=t1[:], in1=GT[hp][0][:], op=ALU.mult),
                           reads=[b1, GT[hp][1]], writes=[YB[6 + hp]])
                else:
                    for ck in (6, 7):
                        OP('pool', lambda e, ck=ck: e.memset(YT[:, ck, :], 0.0), writes=[YB[ck]])

                if 'rwkv' in mixers:
                    bwa = proj_fm(l, 14, first)
                    twa, twab = LLX[0], LLXB[0]
                    OP('act', lambda e: e.activation(out=twa[0:64, :], in_=PS[bwa][0:64, 0:TBM], func=AF.Tanh), reads=[PSB[bwa]], writes=[twab])
                    OP('act', lambda e: e.activation(out=twa[64:128, :], in_=PS[bwa][64:128, 0:TBM], func=AF.Copy), reads=[PSB[bwa]], writes=[twab])
                    bxg = proj_fm(l, 15, first)
                    sxg, sxgb = evac_fm(bxg, AF.Sigmoid, dst=(LLX[1], LLXB[1]))
                    QE, KE, AE, BE, PCx, GR, RKR, VF = [], [], [], [], [], [], [], []
                    for hp in range(2):
                        cs_ = slice(hp * 128, (hp + 1) * 128)
                        bk = pbank()
                        OP('pe', lambda e, bk=bk, hp=hp: e.matmul(PS[bk][:, 0:TBM], lhsT=smat[:, 256 + hp * 128:256 + (hp + 1) * 128], rhs=twa[:], start=True, stop=True),
                           reads=[twab, smatB], writes=[PSB[bk]])
                        lw, lwb = evac_fm(bk, AF.Sigmoid, bias=pv[:, PV['w0'] + hp:PV['w0'] + hp + 1])
                        bk = pbank()
                        OP('pe', lambda e, bk=bk, hp=hp: e.matmul(PS[bk][:, 0:TBM], lhsT=smat[:, 768 + hp * 128:768 + (hp + 1) * 128], rhs=twa[:], start=True, stop=True),
                           reads=[twab, smatB], writes=[PSB[bk]])
                        at, ab_ = evac_fm(bk, AF.Sigmoid, bias=pv[:, PV['a0'] + hp:PV['a0'] + hp + 1])
                        bk = pbank()
                        OP('pe', lambda e, bk=bk, hp=hp: e.matmul(PS[bk][:, 0:TBM], lhsT=smat[:, 512 + hp * 128:512 + (hp + 1) * 128], rhs=sxg[:], start=True, stop=True),
                           reads=[sxgb, smatB], writes=[PSB[bk]])
                        grt, grb = evac_fm(bk, AF.Copy, dst=(LL[hp][1], LLB_[hp][1]))
                        bt, bb = scan_decay(lw, lwb)
                        CW = -float(np.exp(-0.5))
                        ebt, ebb = LL[hp][0], LLB_[hp][0]
                        OP('act', lambda e, bt=bt, ebt=ebt: e.activation(out=ebt[:], in_=bt[:], func=AF.Exp, scale=CW), reads=[bb], writes=[ebb])
                        enb, enbb = fa()
                        OP('act', lambda e, bt=bt, enb=enb: e.activation(out=enb[:], in_=bt[:], func=AF.Exp, scale=-CW), reads=[bb], writes=[enbb])
                        OP('dve', lambda e, bt=bt, lw=lw: e.tensor_tensor(out=bt[:], in0=bt[:], in1=lw[:], op=ALU.subtract), reads=[bb, lwb], writes=[bb])
                        OP('act', lambda e, bt=bt: e.activation(out=bt[:], in_=bt[:], func=AF.Exp, scale=CW), reads=[bb], writes=[bb])
                        br = proj_fm(l, 8 + hp, first)
                        rt, rb = evac_fm(br, AF.Copy)
                        bkr = proj_fm(l, 10 + hp, first)
                        kt, kb = evac_fm(bkr, AF.Copy)
                        bv = proj_fm(l, 12 + hp, first)
                        vt, vb = evac_fm(bv, AF.Copy, dst=(LL[hp][2], LLB_[hp][2]))
                        kkt, kkb = fa()
                        OP('dve', lambda e, kt=kt, kkt=kkt, hp=hp: e.tensor_scalar(out=kkt[:], in0=kt[:], scalar1=pv[:, PV['kk'] + hp:PV['kk'] + hp + 1], scalar2=None, op0=ALU.mult),
                           reads=[kb, pvB[l]], writes=[kkb])
                        sq_, sqb_ = ba()
                        OP('act', lambda e, kkt=kkt, sq_=sq_: e.activation(out=sq_[:], in_=kkt[:], func=AF.Square), reads=[kkb], writes=[sqb_])
                        bk = pbank()
                        OP('pe', lambda e, bk=bk, sq_=sq_: e.matmul(PS[bk][:, 0:TBM], lhsT=bones[:], rhs=sq_[:], start=True, stop=True), reads=[sqb_, bonesB], writes=[PSB[bk]])
                        rn, rnb = fa()
                        rstd_from(PS[bk], PSB[bk], TBM, 1.0, 1e-24, rn, rnb)
                        OP('dve', lambda e, kkt=kkt, rn=rn: e.tensor_tensor(out=kkt[:], in0=kkt[:], in1=rn[:], op=ALU.mult), reads=[kkb, rnb], writes=[kkb])
                        fac, facb = fa()
                        OP('dve', lambda e, at=at, fac=fac, hp=hp: e.tensor_scalar(out=fac[:], in0=at[:], scalar1=-1.0, scalar2=pv[:, PV['ka'] + hp:PV['ka'] + hp + 1], op0=ALU.add, op1=ALU.mult),
                           reads=[ab_, pvB[l]], writes=[facb])
                        OP('dve', lambda e, fac=fac, kt=kt: e.scalar_tensor_tensor(out=kt[:], in0=fac[:], scalar=1.0, in1=kt[:], op0=ALU.add, op1=ALU.mult),
                           reads=[facb, kb], writes=[kb])
                        rk_, rkb_ = LB[hp][4], LBB[hp][4]
                        OP('dve', lambda e, rt=rt, kt=kt, rk_=rk_, hp=hp: e.scalar_tensor_tensor(out=rk_[:], in0=rt[:], scalar=pv[:, PV['rk'] + hp:PV['rk'] + hp + 1], in1=kt[:], op0=ALU.mult, op1=ALU.mult),
                           reads=[rb, kb, pvB[l]], writes=[rkb_])
                        qe, qeb = LB[hp][0], LBB[hp][0]
                        OP('dve', lambda e, rt=rt, ebt=ebt, qe=qe: e.tensor_tensor(out=qe[:], in0=rt[:], in1=ebt[:], op=ALU.mult), reads=[rb, ebb], writes=[qeb])
                        ke, keb = LB[hp][1], LBB[hp][1]
                        OP('dve', lambda e, kt=kt, enb=enb, ke=ke: e.tensor_tensor(out=ke[:], in0=kt[:], in1=enb[:], op=ALU.mult), reads=[kb, enbb], writes=[keb])
                        be, beb = LB[hp][3], LBB[hp][3]
                        OP('dve', lambda e, kkt=kkt, bt=bt, be=be: e.tensor_tensor(out=be[:], in0=kkt[:], in1=bt[:], op=ALU.mult), reads=[kkb, bb], writes=[beb])
                        OP('dve', lambda e, kkt=kkt, at=at: e.tensor_tensor(out=kkt[:], in0=kkt[:], in1=at[:], op=ALU.mult), reads=[kkb, ab_], writes=[kkb])
                        ae, aeb = LB[hp][2], LBB[hp][2]
                        OP('dve', lambda e, kkt=kkt, enb=enb, ae=ae: e.tensor_tensor(out=ae[:], in0=kkt[:], in1=enb[:], op=ALU.mult), reads=[kkb, enbb], writes=[aeb])
                        QE.append((qe, qeb)); KE.append((ke, keb)); AE.append((ae, aeb)); BE.append((be, beb)); PCx.append((ebt, ebb))
                        GR.append((grt, grb)); RKR.append((rk_, rkb_)); VF.append((vt, vb))
                    proj_tm(l, 1)
                    chunk_engine('rwkv', QE, KE, PCx, rw={'AE': AE, 'BE': BE})
                    Ot = evac_O()
                    for hp in range(2):
                        ob16, ob16b = ba()
                        OP('dve', lambda e, hp=hp, ob16=ob16, Ot=Ot: e.tensor_copy(out=ob16[:], in_=Ot[hp][0][:]), reads=[Ot[hp][1]], writes=[ob16b])
                        bk = pbank()
                        OP('pe', lambda e, bk=bk, ob16=ob16: e.matmul(PS[bk][:, 0:TBM], lhsT=bones[:], rhs=ob16[:], start=True, stop=True), reads=[ob16b, bonesB], writes=[PSB[bk]])
                        ct, cb = fa()
                        OP('dve', lambda e, hp=hp, bk=bk, ct=ct, Ot=Ot: e.scalar_tensor_tensor(out=ct[:], in0=PS[bk][:, 0:TBM], scalar=-1.0 / 64, in1=Ot[hp][0][:], op0=ALU.mult, op1=ALU.add),
                           reads=[PSB[bk], Ot[hp][1]], writes=[cb])
                        s2, s2b = ba()
                        OP('act', lambda e, ct=ct, s2=s2: e.activation(out=s2[:], in_=ct[:], func=AF.Square), reads=[cb], writes=[s2b])
                        bk = pbank()
                        OP('pe', lambda e, bk=bk, s2=s2: e.matmul(PS[bk][:, 0:TBM], lhsT=bones[:], rhs=s2[:], start=True, stop=True), reads=[s2b, bonesB], writes=[PSB[bk]])
                        rn, rnb = fa()
                        rstd_from(PS[bk], PSB[bk], TBM, 1.0 / 64, GN_EPS, rn, rnb)
                        OP('dve', lambda e, hp=hp, ct=ct, rn=rn: e.scalar_tensor_tensor(out=ct[:], in0=ct[:], scalar=pv[:, PV['lnw'] + hp:PV['lnw'] + hp + 1], in1=rn[:], op0=ALU.mult, op1=ALU.mult),
                           reads=[cb, rnb, pvB[l]], writes=[cb])
                        bk = pbank()
                        OP('pe', lambda e, bk=bk, hp=hp, RKR=RKR: e.matmul(PS[bk][:, 0:TBM], lhsT=bones[:], rhs=RKR[hp][0][:], start=True, stop=True), reads=[RKR[hp][1], bonesB], writes=[PSB[bk]])
                        bo, bob = fa()
                        OP('dve', lambda e, bk=bk, hp=hp, bo=bo, VF=VF: e.tensor_tensor(out=bo[:], in0=PS[bk][:, 0:TBM], in1=VF[hp][0][:], op=ALU.mult), reads=[PSB[bk], VF[hp][1]], writes=[bob])
                        OP('dve', lambda e, hp=hp, ct=ct, bo=bo: e.scalar_tensor_tensor(out=ct[:], in0=ct[:], scalar=pv[:, PV['lnb'] + hp:PV['lnb'] + hp + 1], in1=bo[:], op0=ALU.add, op1=ALU.add),
                           reads=[cb, bob, pvB[l]], writes=[cb])
                        OP('dve', lambda e, hp=hp, ct=ct, GR=GR: e.tensor_tensor(out=YT[:, 4 + hp, :], in0=ct[:], in1=GR[hp][0][:], op=ALU.mult),
                           reads=[cb, GR[hp][1]], writes=[YB[4 + hp]])
                else:
                    for ck in (4, 5):
                        OP('pool', lambda e, ck=ck: e.memset(YT[:, ck, :], 0.0), writes=[YB[ck]])

                for jp in range(KD // 2):
                    (i,) = acq(l, ('wo', jp))
                    for q in range(2):
                        j = 2 * jp + q
                        for m in range(KD):
                            OP('pe', lambda e, m=m, j=j, q=q, i=i: e.matmul(PS[m][:, 0:TBM], lhsT=wbf[i][:, q * D + m * 128:q * D + (m + 1) * 128], rhs=YT[:, j, :],
                                                                   start=(j == 0), stop=(j == KD - 1)),
                               reads=[wbfB[i], wbfB2[i][0], wbfB2[i][1], YB[j]], writes=[PSB[m]], sig=(m == KD - 1 or j == KD - 1))
                for m in range(KD):
                    OP('dve', lambda e, m=m: e.tensor_tensor(out=X[:, m, c0:c0 + TBM], in0=PS[m][:, 0:TBM], in1=X[:, m, c0:c0 + TBM], op=ALU.add),
                       reads=[PSB[m], XB[m][ti]], writes=[XB[m][ti]])

            def mixer_setup(l, s):
                OP('sp', lambda e: e.dma_start(out=mub[:], in_=mub_d[l]), writes=[mubB], dsem=muS)
                OP('dve', lambda e: e.tensor_scalar(out=omub[:], in0=mub[:], scalar1=-1.0, scalar2=1.0, op0=ALU.mult, op1=ALU.add), reads=[mubB], writes=[omubB])
                OP('sp', lambda e: e.dma_start(out=smat[:], in_=smat_d[l]), writes=[smatB], dsem=smS)
                OP('pool', lambda e: e.tensor_copy(out=smatb[:], in_=smat[:, 0:256]), reads=[smatB], writes=[smatB])
                for m in ('hgrn', 'gla', 'rwkv'):
                    for hp in range(2):
                        OP('pool', lambda e, m=m, hp=hp: e.memset(S32[m][hp][:], 0.0), writes=[S32B[m][hp]])
                        OP('pool', lambda e, m=m, hp=hp: e.memset(Sbf[m][hp][:], 0.0), writes=[SbfB[m][hp]])

        if do_mix:
            OP('act', lambda e: e.activation(out=lbt[:, 0:2], in_=pvec[0][:, PV['lb0']:PV['lb0'] + 2], func=AF.Exp), reads=[pvB[0]], writes=[lbtB])
            OP('act', lambda e: e.activation(out=lbt[:, 2:4], in_=pvec[L - 1][:, PV['lbl']:PV['lbl'] + 2], func=AF.Exp), reads=[pvB[L - 1], lbtB], writes=[lbtB])
            OP('dve', lambda e: e.tensor_tensor(out=lbt[:, 4:6], in0=lbt[:, 0:2], in1=lbt[:, 2:4], op=ALU.add), reads=[lbtB], writes=[lbtB])
            OP('dve', lambda e: e.reciprocal(out=lbt[:, 4:6], in_=lbt[:, 4:6]), reads=[lbtB], writes=[lbtB])
            OP('dve', lambda e: e.tensor_tensor(out=lbt[:, 6:8], in0=lbt[:, 2:4], in1=lbt[:, 4:6], op=ALU.mult), reads=[lbtB], writes=[lbtB])
            OP('dve', lambda e: e.memset(lbt[:, 4:6], 0.0), reads=[lbtB], writes=[lbtB])
            OP('dve', lambda e: e.tensor_scalar(out=lbt[:, 0:4], in0=lbt[:, 4:8], scalar1=-1.0, scalar2=1.0, op0=ALU.mult, op1=ALU.add), reads=[lbtB], writes=[lbtB])

        for s in range(NS):
            for k in range(KD):
                OP('sp', lambda e, k=k, s=s: e.dma_start(out=X[:, k, :], in_=xT[s, :, k, :]), writes=XB[k], dsem=xS[k])
            for l in range(L):
                if do_ffn:
                    for ti in range(NT):
                        ffn(l, 0, ti)
                if do_mix:
                    mixer_setup(l, s)
                    for bi in range(T // TBM):
                        mixer_block(l, s, bi)
                if do_ffn:
                    for ti in range(NT):
                        ffn(l, 1, ti)
            for ti in range(NT):
                c0 = ti * TB
                for k in range(KD):
                    i = counters['sq'] % 2
                    counters['sq'] += 1
                    OP('act', lambda e, k=k, i=i, c0=c0: e.activation(out=sq[i][:], in_=X[:, k, c0:c0 + TB], func=AF.Square),
                       reads=[XB[k][ti]], writes=[sqB[i]])
                    OP('pe', lambda e, k=k, i=i: e.matmul(PS[0][:, :], lhsT=ones[:], rhs=sq[i][:], start=(k == 0), stop=(k == KD - 1)),
                       reads=[sqB[i], onesB], writes=[PSB[0]])
                rstd_from(PS[0], PSB[0], TB, 1.0 / D, NORM_EPS, rstd, rstdB)
                for k in range(KD):
                    OP('dve', lambda e, k=k, c0=c0: e.scalar_tensor_tensor(out=X[:, k, c0:c0 + TB], in0=X[:, k, c0:c0 + TB],
                                                                        scalar=pvec[0][:, PV['nfin'] + k:PV['nfin'] + k + 1], in1=rstd[:],
                                                                        op0=ALU.mult, op1=ALU.mult),
                       reads=[XB[k][ti], rstdB, pvB[0]], writes=[XB[k][ti]])
            for k in range(KD):
                OP('sp', lambda e, k=k, s=s: e.dma_start(out=outT[s, :, k, :], in_=X[:, k, :]), reads=XB[k], writes=XB[k], dsem=oS[k])
        pg.ops['sp'].append((lambda e: e.nop(), {o_: o_.count for o_ in oS}, False, None))
        pg.emit(block, esems)
    return nc, pg


def _col(v, n):
    return np.ascontiguousarray(np.asarray(v, np.float32).reshape(n, 128).T)


def make_consts():
    cst = np.zeros((128, 1600), np.float32)
    p = np.arange(64)[:, None]
    f = np.arange(64)[None, :]
    for h in range(4):
        cst[0:64, 0 + h * 64:0 + (h + 1) * 64] = (p <= f)
        cst[0:64, 256 + h * 64:256 + (h + 1) * 64] = (p < f)
        cst[0:64, 512 + h * 64:512 + (h + 1) * 64] = (f < p)
        cst[0:64, 768 + h * 64:768 + (h + 1) * 64] = (p == f)
    scm = np.ones(512, np.float32)
    scm[::64] = 0.0
    cst[:, 1024:1536] = scm[None, :]
    wins = {(0, 0): 2, (0, 1): 4, (1, 0): 8, (1, 1): 16}
    for ck in range(2):
        for half in range(2):
            w = wins[(ck, half)]
            t = np.arange(16)
            cst[half * 64:(half + 1) * 64, 1536 + ck * 16:1536 + ck * 16 + 16] = (1.0 / np.minimum(t + 1, w))[None, :]
    return cst


def prep_weights(inp, L):
    out = {}
    f32 = np.float32
    for l in range(L):
        for w, (wi, wo) in enumerate((('ffn1_w_in', 'ffn1_w_out'), ('ffn2_w_in', 'ffn2_w_out'))):
            W = np.asarray(inp[wi][l], f32)
            Wk = W.reshape(KD, 128, 2 * FF)
            g = Wk[:, :, :FF].reshape(KD, 128, NJ, 128)
            u = Wk[:, :, FF:].reshape(KD, 128, NJ, 128)
            blk = np.concatenate([g, u], axis=3)
            out[f"f{w + 1}_win{l}"] = np.ascontiguousarray(blk.transpose(2, 1, 0, 3)).reshape(NJ, 128, KD * 256)
            out[f"f{w + 1}_wout{l}"] = np.ascontiguousarray(np.asarray(inp[wo][l], f32).reshape(NJ, 128, D))
        W = np.asarray(inp['w_in'][l], f32).reshape(KD, 128, DIN)
        fm = np.zeros((len(FMG), 128, KD, 128), f32)
        for gi, (c0, nc_, _) in enumerate(FMG):
            nc_ = min(nc_, DIN - c0)
            fm[gi, :, :, :nc_] = W[:, :, c0:c0 + nc_].transpose(1, 0, 2)
        out[f"m_fm{l}"] = fm.reshape(len(FMG), 128, KD * 128)
        tm = np.zeros((len(TMG), 128, KD, 256), f32)
        for gi, (c0, nc_, _) in enumerate(TMG):
            tm[gi] = W[:, :, c0:c0 + nc_].transpose(1, 0, 2)
        out[f"m_tm{l}"] = tm.reshape(len(TMG), 128, KD * 256)
        out[f"m_wout{l}"] = np.ascontiguousarray(np.asarray(inp['w_out'][l], f32).reshape(KD, 128, D))
        pv = np.zeros((128, NPV), f32)
        pv[:, PV['nf1']:PV['nf1'] + 8] = _col(inp['norm_ffn1'][l], 8)
        pv[:, PV['nmx']:PV['nmx'] + 8] = _col(inp['norm_mix'][l], 8)
        pv[:, PV['nf2']:PV['nf2'] + 8] = _col(inp['norm_ffn2'][l], 8)
        pv[:, PV['nfin']:PV['nfin'] + 8] = _col(inp['norm_final'], 8)
        pv[:, PV['lb0']:PV['lb0'] + 2] = _col(inp['hgrn_lb_logits'][0], 2)
        pv[:, PV['lbl']:PV['lbl'] + 2] = _col(inp['hgrn_lb_logits'][l], 2)
        for nm, key in (('pool_b', 'pool_b'), ('pool_s', 'pool_scale'), ('hnorm', 'hgrn_norm'), ('w0', 'rwkv_w0'), ('a0', 'rwkv_a0'),
                        ('kk', 'rwkv_k_k'), ('ka', 'rwkv_k_a'), ('rk', 'rwkv_r_k'), ('lnw', 'rwkv_ln_w'), ('lnb', 'rwkv_ln_b'),
                        ('glab', 'gla_b'), ('gnorm', 'gla_norm')):
            pv[:, PV[nm]:PV[nm] + 2] = _col(inp[key][l], 2)
        out[f"pvec{l}"] = pv
        out[f"mub{l}"] = np.ascontiguousarray(np.broadcast_to(np.asarray(inp['rwkv_mu'][l], f32)[None, :], (128, 1024)))
        sm = np.zeros((128, 5 * 256), f32)
        pw_ = np.asarray(inp['pool_w'][l], f32)
        for ck in range(2):
            sm[0:64, ck * 128:ck * 128 + 64] = pw_[2 * ck]
            sm[64:128, ck * 128 + 64:ck * 128 + 128] = pw_[2 * ck + 1]
        sm[0:64, 256:512] = np.asarray(inp['rwkv_w2'][l], f32)
        sm[64:128, 768:1024] = np.asarray(inp['rwkv_a2'][l], f32)
        sm[:, 512:768] = np.asarray(inp['rwkv_g2'][l], f32)
        sm[0:16, 1024:1280] = np.asarray(inp['gla_w2'][l], f32)
        out[f"smat{l}"] = sm
    out["cst"] = make_consts()
    return out


def prep_x(xc):
    NS, T, _ = xc.shape
    return np.ascontiguousarray(xc.reshape(NS, T, KD, 128).transpose(0, 3, 2, 1))


def unprep_out(o):
    NS, _, _, T = o.shape
    return np.ascontiguousarray(o.transpose(0, 3, 2, 1)).reshape(NS, T, D)


def kernel(**inputs):
    x = np.asarray(inputs['x'], np.float32)
    B, T, _ = x.shape
    NCORES = 8
    NS = B // NCORES
    L = 2
    nc, pg = build_program(T, NS, L)
    wts = prep_weights(inputs, L)
    in_maps = []
    for c in range(NCORES):
        m = dict(wts)
        m["xT"] = prep_x(x[c * NS:(c + 1) * NS])
        in_maps.append(m)
    res = run_bass_kernel_spmd(nc, in_maps, core_ids=list(range(NCORES)))
    outs = [unprep_out(np.asarray(r["outT"])) for r in res.results]
    return np.concatenate(outs, axis=0).astype(np.float32)
```

```python
import numpy as np
from contextlib import ExitStack
import concourse.bass as bass
import concourse.mybir as mybir
from concourse.bass_utils import run_bass_kernel_spmd

F32 = mybir.dt.float32
BF16 = mybir.dt.bfloat16
AF = mybir.ActivationFunctionType
ALU = mybir.AluOpType
ENGS = ['pe', 'act', 'dve', 'pool', 'sp']

D = 1024
KD = 8
FF = 2816
NJ = 22
G = 256
DIN = 3344
NORM_EPS = 1e-6
GN_EPS = 64e-5
QK = 0.125
TB = 512
TBM = 256
CH = 64
NCH = TBM // CH
DEBUG_STAGE = 99

FMG = [(0, 128, 0), (128, 128, 0), (256, 128, 0), (384, 128, 0), (512, 128, 0), (640, 128, 0),
       (1024, 128, 0), (1152, 128, 0),
       (1280, 128, 1), (1408, 128, 1), (1536, 128, 1), (1664, 128, 1), (1792, 128, 1), (1920, 128, 1),
       (2048, 128, 1), (2176, 128, 1),
       (2304, 128, 0), (2432, 128, 0), (2560, 128, 0), (2688, 128, 0), (3072, 128, 0), (3200, 128, 0),
       (3328, 128, 0)]
TMG = [(768, 256, 0), (1792, 256, 1), (2816, 256, 0)]
RW0 = 1280

PV = {}
_o = 0
for _n, _w in [('nf1', 8), ('nmx', 8), ('nf2', 8), ('pool_b', 2), ('pool_s', 2), ('lb0', 2), ('lbl', 2),
               ('hnorm', 2), ('w0', 2), ('a0', 2), ('kk', 2), ('ka', 2), ('rk', 2), ('lnw', 2), ('lnb', 2),
               ('glab', 2), ('gnorm', 2), ('nfin', 8)]:
    PV[_n] = _o
    _o += _w
NPV = _o


class Buf:
    __slots__ = ('name', 'w', 'r', 'const')

    def __init__(self, name, const=False):
        self.name = name
        self.w = None
        self.r = []
        self.const = const


class DSem:
    def __init__(self, h):
        self.h = h
        self.count = 0


class Prog:
    def __init__(self, nc, same_engine_sync=True):
        self.nc = nc
        self.ops = {e: [] for e in ENGS}
        self.cnt = {e: 0 for e in ENGS}
        self.same = same_engine_sync
        self.nops = 0
        self.last_rg = None
        self.last_pe_sig = True

    def op(self, eng, fn, reads=(), writes=(), sig=True, dsem=None, rg=None):
        waits = {}
        if eng == 'pe':
            if rg is not None and self.last_rg is not None and rg != self.last_rg:
                assert self.last_pe_sig
                waits['pe'] = self.cnt['pe']
            self.last_rg = rg
            self.last_pe_sig = sig

        def addw(tok):
            if tok is None:
                return
            k, v = tok
            if k == eng and (eng == 'pe' or not self.same):
                return
            if waits.get(k, 0) < v:
                waits[k] = v
        for b in reads:
            addw(b.w)
        for b in writes:
            addw(b.w)
            for t in b.r:
                addw(t)
        if dsem is not None:
            dsem.count += 16
            tok = (dsem, dsem.count)
            sig = False
        elif sig:
            self.cnt[eng] += 1
            tok = (eng, self.cnt[eng])
        else:
            tok = (eng, self.cnt[eng] + 1)
        for b in reads:
            if not b.const:
                b.r.append(tok)
                if len(b.r) > 48:
                    mx = {}
                    for k, v in b.r:
                        if mx.get(k, 0) < v:
                            mx[k] = v
                    b.r = list(mx.items())
        for b in writes:
            b.w = tok
            b.r = []
        self.ops[eng].append((fn, waits, sig, dsem))
        self.nops += 1
        return tok

    def emit(self, block, esems):
        nc = self.nc
        deco = {'pe': block.tensor, 'act': block.scalar, 'dve': block.vector,
                'pool': block.gpsimd, 'sp': block.sync}
        for e in ENGS:
            ops = self.ops[e]

            def body(eng, ops=ops, e=e):
                known = {}
                for fn, waits, sig, dsem in ops:
                    for k, v in waits.items():
                        if known.get(k, 0) >= v:
                            continue
                        known[k] = v
                        h = k.h if isinstance(k, DSem) else esems[k]
                        eng.wait_ge(h, v)
                    inst = fn(eng)
                    if dsem is not None:
                        inst.then_inc(dsem.h, 16)
                    elif sig:
                        inst.then_inc(esems[e], 1)
            deco[e](body)


def build_program(T, NS, L, mixers=('pool', 'hgrn', 'rwkv', 'gla'), do_ffn=True, do_mix=True):
    NT = T // TB
    nc = bass.Bass("TRN2", target_bir_lowering=False)
    dr = {}

    def din(name, shape):
        dr[name] = nc.dram_tensor(name, list(shape), F32, kind="ExternalInput").ap()
        return dr[name]
    xT = din("xT", [NS, 128, KD, T])
    outT = nc.dram_tensor("outT", [NS, 128, KD, T], F32, kind="ExternalOutput").ap()
    f_win = [[din(f"f{w}_win{l}", [NJ, 128, KD * 256]) for w in (1, 2)] for l in range(L)]
    f_wout = [[din(f"f{w}_wout{l}", [NJ, 128, D]) for w in (1, 2)] for l in range(L)]
    m_fm = [din(f"m_fm{l}", [len(FMG), 128, KD * 128]) for l in range(L)]
    m_tm = [din(f"m_tm{l}", [len(TMG), 128, KD * 256]) for l in range(L)]
    m_wout = [din(f"m_wout{l}", [KD, 128, D]) for l in range(L)]
    pvec_d = [din(f"pvec{l}", [128, NPV]) for l in range(L)]
    mub_d = [din(f"mub{l}", [128, 1024]) for l in range(L)]
    smat_d = [din(f"smat{l}", [128, 5 * 256]) for l in range(L)]
    cst_d = din("cst", [128, 1600])
    es = ExitStack()
    with es:
        def sb(name, shape, dt=F32):
            return es.enter_context(nc.sbuf_tensor("sb_" + name, list(shape), dt))

        def psum(name, shape, dt=F32):
            return es.enter_context(nc.psum_tensor("pp_" + name, list(shape), dt))
        esems = {e: es.enter_context(nc.semaphore("s_" + e)) for e in ENGS}

        def dsem(name):
            return DSem(es.enter_context(nc.semaphore(name)))
        pg = Prog(nc)
        block = es.enter_context(nc.Block())

        X = sb("X", [128, KD, T])
        XB = [[Buf(f"X{k}_{t}") for t in range(NT)] for k in range(KD)]
        xn = sb("xn", [128, KD, TB], BF16)
        xns = sb("xns", [128, KD, TBM], BF16)
        xnsB = Buf("xns")
        xnB = [Buf(f"xn{k}") for k in range(KD)]
        sq = [sb(f"sq{i}", [128, TB], BF16) for i in range(2)]
        sqB = [Buf(f"sq{i}") for i in range(2)]
        rstd = sb("rstd", [128, TB]); rstdB = Buf("rstd")
        ones = sb("ones", [128, 128], BF16); onesB = Buf("ones", const=True)
        bones = sb("bones", [128, 128], BF16)
        ident = sb("ident", [128, 128], BF16)
        cst = sb("cst", [128, 1600])
        cstB = Buf("cst", const=True)
        MI, MSU, MSL, ID4 = 0, 256, 512, 768
        SCM = 1024
        ICN = 1536
        pvec = [sb(f"pvec{l}", [128, NPV]) for l in range(L)]
        pvB = [Buf(f"pvec{l}", const=True) for l in range(L)]
        lbt = sb("lbt", [128, 8])
        mub = sb("mub", [128, 1024]); mubB = Buf("mub")
        omub = sb("omub", [128, 1024]); omubB = Buf("omub")
        smat = sb("smat", [128, 5 * 256]); smatB = Buf("smat")
        smatb = sb("smatb", [128, 2 * 128], BF16)
        wst = [sb(f"wst{i}", [128, KD * 256]) for i in range(2)]
        wstB = [Buf(f"wst{i}") for i in range(2)]
        wstS = [dsem(f"dwst{i}") for i in range(2)]
        wbf = [sb(f"wbf{i}", [128, KD * 256], BF16) for i in range(2)]
        wbfB = [Buf(f"wbf{i}") for i in range(2)]
        wbfB2 = [[Buf(f"wbf{i}a"), Buf(f"wbf{i}b")] for i in range(2)]
        hT = sb("hT", [128, NJ, TB], BF16)
        hB = [Buf(f"h{j}") for j in range(NJ)]
        sg = [sb("sg0", [128, TB])] * 2
        sgB = [Buf("sg0")] * 2
        PS = [psum(f"ps{i}", [128, 512]) for i in range(8)]
        PSB = [Buf(f"ps{i}") for i in range(8)]
        xS = [dsem(f"dx{k}") for k in range(KD)]
        oS = [dsem(f"dout{k}") for k in range(KD)]
        cS = dsem("dcst")
        pS = [dsem(f"dpv{l}") for l in range(L)]
        muS = dsem("dmu")
        smS = dsem("dsm")
        counters = {'w': 0, 'o': 0, 'sq': 0, 'sg': 0, 'pp': 0}

        def OP(eng, fn, reads=(), writes=(), sig=True, dsem=None, rg=None):
            return pg.op(eng, fn, reads, writes, sig, dsem, rg)

        def load_w(src_ap, ncols):
            i = counters['w'] % 2
            counters['w'] += 1
            OP('sp', lambda e: e.dma_start(out=wst[i][:, 0:ncols], in_=src_ap), writes=[wstB[i]], dsem=wstS[i])
            return i

        def load_w2(src_ap2):
            i = counters['w'] % 2
            counters['w'] += 1
            OP('sp', lambda e: e.dma_start(out=wst[i][:, 0:2 * D].rearrange("p (j c) -> p j c", j=2), in_=src_ap2.rearrange("j p c -> p j c")),
               writes=[wstB[i]], dsem=wstS[i])
            return i

        def cast_w(i, ncols, eng='pool'):
            if eng == 'act':
                OP('act', lambda e: e.activation(out=wbf[i][:, 0:ncols], in_=wst[i][:, 0:ncols], func=AF.Copy),
                   reads=[wstB[i]], writes=[wbfB[i], wbfB2[i][0], wbfB2[i][1]])
            else:
                OP(eng, lambda e: e.tensor_copy(out=wbf[i][:, 0:ncols], in_=wst[i][:, 0:ncols]),
                   reads=[wstB[i]], writes=[wbfB[i], wbfB2[i][0], wbfB2[i][1]])

        def cast_w_split(i, ncols):
            c1 = (ncols * 3 // 4) // 128 * 128
            OP('dve', lambda e: e.tensor_copy(out=wbf[i][:, 0:c1], in_=wst[i][:, 0:c1]), reads=[wstB[i]], writes=[wbfB2[i][0], wbfB[i]])
            OP('pool', lambda e: e.tensor_copy(out=wbf[i][:, c1:ncols], in_=wst[i][:, c1:ncols]), reads=[wstB[i]], writes=[wbfB2[i][1]])

        def rstd_from(psb, psbuf, n, scale, eps, dst, dstB):
            OP('act', lambda e: e.activation(out=dst[:, 0:n], in_=psb[:, 0:n], func=AF.Ln, scale=scale, bias=eps),
               reads=[psbuf], writes=[dstB])
            OP('act', lambda e: e.activation(out=dst[:, 0:n], in_=dst[:, 0:n], func=AF.Exp, scale=-0.5),
               reads=[dstB], writes=[dstB])

        def rmsnorm_to_xn(l, gcol, c0, n, bank):
            ti = c0 // TB
            for k in range(KD):
                i = counters['sq'] % 2
                counters['sq'] += 1
                OP('act', lambda e, k=k, i=i: e.activation(out=sq[i][:, 0:n], in_=X[:, k, c0:c0 + n], func=AF.Square),
                   reads=[XB[k][ti]], writes=[sqB[i]])
                OP('pe', lambda e, k=k, i=i: e.matmul(PS[bank][:, 0:n], lhsT=ones[:], rhs=sq[i][:, 0:n], start=(k == 0), stop=(k == KD - 1)),
                   reads=[sqB[i], onesB], writes=[PSB[bank]])
            rstd_from(PS[bank], PSB[bank], n, 1.0 / D, NORM_EPS, rstd, rstdB)
            for k in range(KD):
                OP('dve', lambda e, k=k: e.scalar_tensor_tensor(out=xn[:, k, 0:n], in0=X[:, k, c0:c0 + n],
                                                                 scalar=pvec[l][:, gcol + k:gcol + k + 1], in1=rstd[:, 0:n],
                                                                 op0=ALU.mult, op1=ALU.mult),
                   reads=[XB[k][ti], rstdB, pvB[l]], writes=[xnB[k]])

        def ffn(l, w, ti):
            gcol = PV['nf1'] if w == 0 else PV['nf2']
            c0 = ti * TB
            blocks = [('in', j) for j in range(NJ)] + [('out', jp) for jp in range(NJ // 2)]

            slots = {}

            def do_load(t):
                if t < len(blocks) and t not in slots:
                    kind_, j_ = blocks[t]
                    if kind_ == 'in':
                        slots[t] = load_w(f_win[l][w][j_], KD * 256)
                    else:
                        slots[t] = load_w2(f_wout[l][w][2 * j_:2 * j_ + 2])

            def do_ready(t):
                if t >= len(blocks):
                    return
                do_load(t)
                cast_w_split(slots[t], KD * 256)
            do_load(0)
            do_load(1)
            do_ready(0)
            rmsnorm_to_xn(l, gcol, c0, TB, 0)
            for t, (kind, j) in enumerate(blocks):
                i = slots[t]
                do_ready(t + 1)
                do_load(t + 2)
                if kind == 'in':
                    wv = wbf[i][:].rearrange("p (k c) -> p k c", k=KD)
                    pp = counters['pp'] % 2
                    counters['pp'] += 1
                    bg, bu = 2 * pp, 2 * pp + 1
                    for half, bk in ((0, bg), (1, bu)):
                        for k in range(KD):
                            OP('pe', lambda e, k=k, half=half, bk=bk, wv=wv: e.matmul(
                                PS[bk][:, :], lhsT=wv[:, k, half * 128:(half + 1) * 128], rhs=xn[:, k, 0:TB],
                                start=(k == 0), stop=(k == KD - 1)),
                               reads=[wbfB[i], wbfB2[i][0], wbfB2[i][1], xnB[k]], writes=[PSB[bk]], sig=(k == KD - 1))
                    si = counters['sg'] % 2
                    counters['sg'] += 1
                    OP('act', lambda e, si=si, bg=bg: e.activation(out=sg[si][:], in_=PS[bg][:, :], func=AF.Silu),
                       reads=[PSB[bg]], writes=[sgB[si]])
                    OP('dve', lambda e, si=si, bu=bu, j=j: e.tensor_tensor(out=hT[:, j, :], in0=sg[si][:], in1=PS[bu][:, :], op=ALU.mult),
                       reads=[sgB[si], PSB[bu]], writes=[hB[j]])
                else:
                    for q in range(2):
                        jj = 2 * j + q
                        for m in range(KD):
                            OP('pe', lambda e, m=m, jj=jj, q=q, i=i: e.matmul(PS[m][:, :], lhsT=wbf[i][:, q * D + m * 128:q * D + (m + 1) * 128], rhs=hT[:, jj, :],
                                                                   start=(jj == 0), stop=(jj == NJ - 1)),
                               reads=[wbfB[i], wbfB2[i][0], wbfB2[i][1], hB[jj]], writes=[PSB[m]], sig=(m == KD - 1 or jj == NJ - 1))
            for m in range(KD):
                OP('dve', lambda e, m=m: e.scalar_tensor_tensor(out=X[:, m, c0:c0 + TB], in0=PS[m][:, :], scalar=0.5,
                                                                 in1=X[:, m, c0:c0 + TB], op0=ALU.mult, op1=ALU.add),
                   reads=[PSB[m], XB[m][ti]], writes=[XB[m][ti]])

        OP('sp', lambda e: e.dma_start(out=cst[:], in_=cst_d), writes=[cstB], dsem=cS)
        for l in range(L):
            OP('sp', lambda e, l=l: e.dma_start(out=pvec[l][:], in_=pvec_d[l]), writes=[pvB[l]], dsem=pS[l])
        OP('pool', lambda e: e.memset(ones[:], 1.0), writes=[onesB])
        bonesB = Buf("bones", const=True)
        OP('pool', lambda e: e.memset(bones[:], 0.0), writes=[bonesB])
        OP('pool', lambda e: e.memset(bones[0:64, 0:64], 1.0), writes=[bonesB])
        OP('pool', lambda e: e.memset(bones[64:128, 64:128], 1.0), writes=[bonesB])
        identB = Buf("ident", const=True)
        OP('pool', lambda e: e.memset(ident[:], 1.0), writes=[identB])
        OP('pool', lambda e: e.affine_select(out=ident[:], in_=ident[:], pattern=[[-1, 128]], compare_op=ALU.is_equal,
                                             fill=0.0, base=0, channel_multiplier=1), reads=[identB], writes=[identB])

        if do_mix:
            YT = sb("YT", [128, KD, TBM], BF16)
            YB = [Buf(f"Y{k}") for k in range(KD)]
            NFA = 10
            FA = [sb(f"fa{i}", [128, TBM]) for i in range(NFA)]
            FAB = [Buf(f"fa{i}") for i in range(NFA)]
            NBA = 4
            BA = [sb(f"ba{i}", [128, TBM], BF16) for i in range(NBA)]
            BAB = [Buf(f"ba{i}") for i in range(NBA)]
            LL = [[sb(f"ll{hp}{i}", [128, TBM]) for i in range(3)] for hp in range(2)]
            LLB_ = [[Buf(f"ll{hp}{i}") for i in range(3)] for hp in range(2)]
            LLX = [sb(f"llx{i}", [128, TBM]) for i in range(2)]
            LLXB = [Buf(f"llx{i}") for i in range(2)]
            LB = [[sb(f"lb{hp}{i}", [128, TBM], BF16) for i in range(5)] for hp in range(2)]
            LBB = [[Buf(f"lb{hp}{i}") for i in range(5)] for hp in range(2)]
            PB16 = [sb(f"pb16{i}", [128, TBM], BF16) for i in range(2)]
            PB16B = [Buf(f"pb16{i}") for i in range(2)]
            Vt = [sb(f"vt{c}", [64, 256], BF16) for c in range(NCH)]
            VtB = [Buf(f"vt{c}") for c in range(NCH)]
            KEt = [sb(f"ket{c}", [64, 256], BF16) for c in range(NCH)]
            KEtB = [Buf(f"ket{c}") for c in range(NCH)]
            AEt = [sb(f"aet{c}", [64, 256], BF16) for c in range(NCH)]
            AEtB = [Buf(f"aet{c}") for c in range(NCH)]
            alias_ctr = [0]

            def mk(name, n):
                ts, bs = [], []
                for c in range(n):
                    idx = alias_ctr[0]
                    alias_ctr[0] += 1
                    j, half = idx // 2, idx % 2
                    ts.append(hT[0:64, j, half * 256:(half + 1) * 256])
                    bs.append(hB[j])
                return ts, bs
            ATs, ATsB = mk("ats", NCH)
            LKs, LKsB = mk("lks", NCH)
            ARs, ARsB = mk("ars", NCH)
            Nn, NnB = mk("nn", NCH)
            NTn, NTnB = mk("ntn", NCH)
            Nn2, Nn2B = mk("nn2", NCH)
            NTn2, NTn2B = mk("ntn2", NCH)
            Pn, PnB = mk("pn", NCH)
            Pn2, Pn2B = mk("pn2", NCH)
            Ysb = sb("ysb", [64, 256], BF16); YsbB = Buf("ysb")
            Usb = sb("usb", [64, 256], BF16); UsbB = Buf("usb")
            S32 = {m: [sb(f"s32{m}{hp}", [128, 64]) for hp in range(2)] for m in ('hgrn', 'gla', 'rwkv')}
            Sbf = {m: [sb(f"sbf{m}{hp}", [128, 64], BF16) for hp in range(2)] for m in ('hgrn', 'gla', 'rwkv')}
            S32B = {m: [Buf(f"s32{m}{hp}") for hp in range(2)] for m in ('hgrn', 'gla', 'rwkv')}
            SbfB = {m: [Buf(f"sbf{m}{hp}") for hp in range(2)] for m in ('hgrn', 'gla', 'rwkv')}
            stmp = [sb(f"stmp{hp}", [128, 64]) for hp in range(2)]
            stmpB = [Buf(f"stmp{hp}") for hp in range(2)]
            pext = sb("pext", [128, 2, 16 + TBM]); pextB = Buf("pext")
            pw = [sb(f"pw{i}", [128, 2, 16 + TBM]) for i in range(2)]
            pwB = [Buf(f"pw{i}") for i in range(2)]
            PST = PS[7][:, :].bitcast(BF16)
            lbtB = Buf("lbt", const=True)
            fa_ctr = [0]
            ba_ctr = [0]

            def fa():
                i = fa_ctr[0] % NFA
                fa_ctr[0] += 1
                return FA[i], FAB[i]

            def ba():
                i = ba_ctr[0] % NBA
                ba_ctr[0] += 1
                return BA[i], BAB[i]

            def pbank():
                b = counters['pp'] % 4
                counters['pp'] += 1
                return b

            wplan = {'plan': [], 'pos': 0, 'issued': {}, 'l': 0}

            def issue_load(l, d):
                kind, g = d
                if kind == 'fm':
                    return load_w(m_fm[l][g], KD * 128)
                if kind == 'wo':
                    return load_w2(m_wout[l][2 * g:2 * g + 2])
                return load_w(m_tm[l][g], KD * 256)

            def issue_cast(l, d, i):
                kind, g = d
                if kind == 'wo':
                    cast_w_split(i, 2 * D)
                    return (i,)
                if kind == 'fm':
                    col0, ncols, shift = FMG[g]
                    if shift:
                        mc = col0 - RW0
                        mu_b = mub[:, mc:mc + 128].unsqueeze(1).broadcast_to([128, KD, 128])
                        omu_b = omub[:, mc:mc + 128].unsqueeze(1).broadcast_to([128, KD, 128])
                        wv32 = wst[i][:, 0:KD * 128].rearrange("p (k c) -> p k c", k=KD)
                        wbv = wbf[i][:].rearrange("p (k c) -> p k c", k=KD)
                        OP('dve', lambda e: e.tensor_tensor(out=wbv[:, :, 0:128], in0=wv32, in1=omu_b, op=ALU.mult),
                           reads=[wstB[i], omubB], writes=[wbfB2[i][0], wbfB[i]])
                        OP('pool', lambda e: e.tensor_tensor(out=wbv[:, :, 128:256], in0=wv32, in1=mu_b, op=ALU.mult),
                           reads=[wstB[i], mubB], writes=[wbfB2[i][1]])
                    else:
                        cast_w(i, KD * 128, 'dve')
                        wbv = wbf[i][:, 0:KD * 128].rearrange("p (k c) -> p k c", k=KD)
                    return (i, wbv)
                col0, ncols, shift = TMG[g]
                i2 = None
                wb2 = None
                if shift:
                    i2 = counters['w'] % 2
                    counters['w'] += 1
                    mc = col0 - RW0
                    mu_b = mub[:, mc:mc + 256].unsqueeze(1).broadcast_to([128, KD, 256])
                    omu_b = omub[:, mc:mc + 256].unsqueeze(1).broadcast_to([128, KD, 256])
                    wv32 = wst[i][:].rearrange("p (k c) -> p k c", k=KD)
                    wa = wbf[i][:].rearrange("p (k c) -> p k c", k=KD)
                    wb2 = wbf[i2][:].rearrange("p (k c) -> p k c", k=KD)
                    OP('dve', lambda e: e.tensor_tensor(out=wa, in0=wv32, in1=omu_b, op=ALU.mult),
                       reads=[wstB[i], omubB], writes=[wbfB[i], wbfB2[i][0], wbfB2[i][1]])
                    OP('pool', lambda e: e.tensor_tensor(out=wb2, in0=wv32, in1=mu_b, op=ALU.mult),
                       reads=[wstB[i], mubB], writes=[wbfB[i2], wbfB2[i2][0], wbfB2[i2][1]])
                else:
                    cast_w_split(i, KD * 256)
                    wa = wbf[i][:].rearrange("p (k c) -> p k c", k=KD)
                return (i, i2, wa, wb2)

            def two_slot(d):
                return d[0] == 'tm' and bool(TMG[d[1]][2])

            def ensure_load(l, pos):
                plan = wplan['plan']
                if pos < len(plan) and pos not in wplan['loaded'] and not two_slot(plan[pos]):
                    wplan['loaded'][pos] = issue_load(l, plan[pos])

            def ensure_cast(l, pos):
                plan = wplan['plan']
                if pos < len(plan) and pos not in wplan['issued'] and not two_slot(plan[pos]):
                    ensure_load(l, pos)
                    wplan['issued'][pos] = issue_cast(l, plan[pos], wplan['loaded'].pop(pos))

            def acq(l, d):
                pos = wplan['pos']
                plan = wplan['plan']
                assert plan[pos] == d, (plan[pos], d)
                if pos not in wplan['issued']:
                    if pos not in wplan['loaded']:
                        wplan['loaded'][pos] = issue_load(l, d)
                    wplan['issued'][pos] = issue_cast(l, d, wplan['loaded'].pop(pos))
                info = wplan['issued'].pop(pos)
                wplan['pos'] = pos + 1
                if not two_slot(d) and not (pos + 1 < len(plan) and two_slot(plan[pos + 1])):
                    ensure_cast(l, pos + 1)
                    if not (pos + 2 < len(plan) and two_slot(plan[pos + 2])):
                        ensure_load(l, pos + 2)
                return info

            def proj_fm(l, g, first_block):
                col0, ncols, shift = FMG[g]
                i, wbv = acq(l, ('fm', g))
                bk = pbank()
                nmm = KD * (2 if shift else 1)
                n = 0
                for k in range(KD):
                    n += 1
                    OP('pe', lambda e, k=k, n=n: e.matmul(PS[bk][0:ncols, 0:TBM], lhsT=wbv[:, k, 0:ncols], rhs=xn[:, k, 0:TBM],
                                                          start=(n == 1), stop=(n == nmm)),
                       reads=[wbfB[i], wbfB2[i][0], wbfB2[i][1], xnB[k]], writes=[PSB[bk]], sig=(n == nmm))
                if shift:
                    for k in range(KD):
                        n += 1
                        OP('pe', lambda e, k=k, n=n: e.matmul(PS[bk][0:ncols, 0:TBM], lhsT=wbv[:, k, 128:128 + ncols], rhs=xns[:, k, 0:TBM],
                                                              start=False, stop=(n == nmm)),
                           reads=[wbfB[i], wbfB2[i][0], wbfB2[i][1], xnsB], writes=[PSB[bk]], sig=(n == nmm))
                return bk

            def proj_tm(l, g):
                col0, ncols, shift = TMG[g]
                i, i2, wa, wb2 = acq(l, ('tm', g))
                for c in range(NCH):
                    bk = pbank()
                    nmm = KD * (2 if shift else 1)
                    n = 0
                    for k in range(KD):
                        n += 1
                        OP('pe', lambda e, k=k, n=n, c=c, bk=bk: e.matmul(PS[bk][0:64, 0:256], lhsT=xn[:, k, c * CH:(c + 1) * CH],
                                                                     rhs=wa[:, k, :], start=(n == 1), stop=(n == nmm)),
                           reads=[wbfB[i], wbfB2[i][0], wbfB2[i][1], xnB[k]], writes=[PSB[bk]], sig=(n == nmm))
                    if shift:
                        for k in range(KD):
                            n += 1
                            OP('pe', lambda e, k=k, n=n, c=c, bk=bk: e.matmul(PS[bk][0:64, 0:256], lhsT=xns[:, k, c * CH:(c + 1) * CH],
                                                                         rhs=wb2[:, k, :], start=False, stop=(n == nmm)),
                               reads=[wbfB[i2], wbfB2[i2][0], wbfB2[i2][1], xnsB], writes=[PSB[bk]], sig=(n == nmm))
                    OP('act', lambda e, c=c, bk=bk: e.activation(out=Vt[c][:], in_=PS[bk][0:64, 0:256], func=AF.Copy),
                       reads=[PSB[bk]], writes=[VtB[c]])

            def scan_decay(g_t, g_b):
                b_t, b_b = fa()
                OP('dve', lambda e: e.tensor_tensor_scan(out=b_t[:], data0=cst[:, SCM:SCM + TBM], data1=g_t[:], initial=0.0,
                                                         op0=ALU.mult, op1=ALU.add), reads=[g_b, cstB], writes=[b_b])
                return b_t, b_b

            def chunk_engine(mname, QE, KE, PC, rw=None):
                isrw = rw is not None
                if DEBUG_STAGE < 1:
                    return
                for c in range(NCH):
                    for (src, dst, dstB_) in ([(KE, KEt, KEtB)] + ([(rw['AE'], AEt, AEtB)] if isrw else [])):
                        for hp in range(2):
                            OP('pe', lambda e, hp=hp, c=c, src=src: e.transpose(PST[0:64, hp * 128:(hp + 1) * 128],
                                                                             src[hp][0][:, c * CH:(c + 1) * CH], ident[:]),
                               reads=[src[hp][1], identB], writes=[PSB[7]])
                        OP('act', lambda e, c=c, dst=dst: e.activation(out=dst[c][:], in_=PST[0:64, 0:256], func=AF.Copy),
                           reads=[PSB[7]], writes=[dstB_[c]])
                if DEBUG_STAGE < 2:
                    return
                for c in range(NCH):
                    cs = slice(c * CH, (c + 1) * CH)
                    def sc(lh, rh, dst_ps, cs=cs):
                        for h in range(4):
                            hp, r = h // 2, (h % 2) * 64
                            OP('pe', lambda e, h=h, hp=hp, r=r: e.matmul(dst_ps[0:64, h * 64:(h + 1) * 64], lhsT=lh[hp][0][r:r + 64, cs],
                                                                         rhs=rh[hp][0][r:r + 64, cs], start=True, stop=True),
                               reads=[lh[hp][1], rh[hp][1]], writes=[PSB[6]], rg=r)
                    sc(KE, QE, PS[6][:, 0:256])
                    OP('dve', lambda e, c=c: e.tensor_tensor(out=ATs[c][:], in0=PS[6][0:64, 0:256], in1=cst[0:64, MI:MI + 256], op=ALU.mult),
                       reads=[PSB[6], cstB], writes=[ATsB[c]])
                    if isrw:
                        sc(KE, rw['BE'], PS[6][:, 256:512])
                        OP('dve', lambda e, c=c: e.tensor_tensor(out=LKs[c][:], in0=PS[6][0:64, 256:512], in1=cst[0:64, MSU:MSU + 256], op=ALU.mult),
                           reads=[PSB[6], cstB], writes=[LKsB[c]])
                        sc(rw['AE'], QE, PS[6][:, 0:256])
                        OP('dve', lambda e, c=c: e.tensor_tensor(out=ARs[c][:], in0=PS[6][0:64, 0:256], in1=cst[0:64, MI:MI + 256], op=ALU.mult),
                           reads=[PSB[6], cstB], writes=[ARsB[c]])
                        sc(rw['AE'], rw['BE'], PS[6][:, 256:512])
                        OP('dve', lambda e, c=c: e.scalar_tensor_tensor(out=NTn[c][:], in0=PS[6][0:64, 256:512], scalar=-1.0,
                                                                         in1=cst[0:64, MSU:MSU + 256], op0=ALU.mult, op1=ALU.mult),
                           reads=[PSB[6], cstB], writes=[NTnB[c]])
                        sc(rw['BE'], rw['AE'], PS[6][:, 0:256])
                        OP('dve', lambda e, c=c: e.scalar_tensor_tensor(out=Nn[c][:], in0=PS[6][0:64, 0:256], scalar=-1.0,
                                                                         in1=cst[0:64, MSL:MSL + 256], op0=ALU.mult, op1=ALU.mult),
                           reads=[PSB[6], cstB], writes=[NnB[c]])
                        OP('pool', lambda e, c=c: e.tensor_tensor(out=Pn[c][:], in0=NTn[c][:], in1=cst[0:64, ID4:ID4 + 256], op=ALU.add),
                           reads=[NTnB[c], cstB], writes=[PnB[c]])
                if isrw:
                    curN, curNB, curNT, curNTB = Nn, NnB, NTn, NTnB
                    nxtN, nxtNB, nxtNT, nxtNTB = Nn2, Nn2B, NTn2, NTn2B
                    curP, curPB, nxtP, nxtPB = Pn, PnB, Pn2, Pn2B
                    for lev in range(1, 6):
                        for c in range(NCH):
                            bk = pbank()
                            for h in range(4):
                                hs = slice(h * 64, (h + 1) * 64)
                                OP('pe', lambda e, c=c, hs=hs, bk=bk, a=curNT, b=curN: e.matmul(PS[bk][0:64, hs], lhsT=a[c][:, hs], rhs=b[c][:, hs],
                                                                                         start=True, stop=True),
                                   reads=[curNTB[c], curNB[c]], writes=[PSB[bk]], rg=0)
                            if lev < 5:
                                for h in range(4):
                                    hs = slice(h * 64, (h + 1) * 64)
                                    hs2 = slice(256 + h * 64, 256 + (h + 1) * 64)
                                    OP('pe', lambda e, c=c, hs=hs, hs2=hs2, bk=bk, a=curN, b=curNT: e.matmul(PS[bk][0:64, hs2], lhsT=a[c][:, hs], rhs=b[c][:, hs],
                                                                                                     start=True, stop=True),
                                       reads=[curNTB[c], curNB[c]], writes=[PSB[bk]], rg=0)
                                OP('act', lambda e, c=c, bk=bk, d=nxtNT: e.activation(out=d[c][:], in_=PS[bk][0:64, 256:512], func=AF.Copy),
                                   reads=[PSB[bk]], writes=[nxtNTB[c]])
                            OP('act', lambda e, c=c, bk=bk, d=nxtN: e.activation(out=d[c][:], in_=PS[bk][0:64, 0:256], func=AF.Copy),
                               reads=[PSB[bk]], writes=[nxtNB[c]])
                        curN, curNB, nxtN, nxtNB = nxtN, nxtNB, curN, curNB
                        curNT, curNTB, nxtNT, nxtNTB = nxtNT, nxtNTB, curNT, curNTB
                        for c in range(NCH):
                            bk = pbank()
                            for h in range(4):
                                hs = slice(h * 64, (h + 1) * 64)
                                OP('pe', lambda e, c=c, hs=hs, bk=bk, a=curN, b=curP: e.matmul(PS[bk][0:64, hs], lhsT=a[c][:, hs], rhs=b[c][:, hs],
                                                                                        start=True, stop=True),
                                   reads=[curNB[c], curPB[c]], writes=[PSB[bk]], rg=0)
                            OP('dve', lambda e, c=c, bk=bk, s=curP, d=nxtP: e.tensor_tensor(out=d[c][:], in0=PS[bk][0:64, 0:256], in1=s[c][:], op=ALU.add),
                               reads=[PSB[bk], curPB[c]], writes=[nxtPB[c]])
                        curP, curPB, nxtP, nxtPB = nxtP, nxtPB, curP, curPB
                    TT, TTB = curP, curPB
                if DEBUG_STAGE < 3:
                    return
                S3, Sb, S3B, SbB = S32[mname], Sbf[mname], S32B[mname], SbfB[mname]
                for c in range(NCH):
                    cs = slice(c * CH, (c + 1) * CH)
                    if isrw:
                        for h in range(4):
                            hp, r = h // 2, (h % 2) * 64
                            hs = slice(h * 64, (h + 1) * 64)
                            OP('pe', lambda e, hp=hp, r=r, hs=hs, cs=cs: e.matmul(PS[6][0:64, hs], lhsT=rw['BE'][hp][0][r:r + 64, cs], rhs=Sb[hp][r:r + 64, :],
                                                                         start=True, stop=False),
                               reads=[rw['BE'][hp][1], SbB[hp]], writes=[PSB[6]], rg=r)
                            OP('pe', lambda e, hs=hs, c=c: e.matmul(PS[6][0:64, hs], lhsT=LKs[c][:, hs], rhs=Vt[c][:, hs], start=False, stop=True),
                               reads=[LKsB[c], VtB[c]], writes=[PSB[6]], rg=0)
                        OP('act', lambda e: e.activation(out=Ysb[:], in_=PS[6][0:64, 0:256], func=AF.Copy), reads=[PSB[6]], writes=[YsbB])
                        for h in range(4):
                            hs = slice(h * 64, (h + 1) * 64)
                            hs2 = slice(256 + h * 64, 256 + (h + 1) * 64)
                            OP('pe', lambda e, hs=hs, hs2=hs2, c=c: e.matmul(PS[6][0:64, hs2], lhsT=TT[c][:, hs], rhs=Ysb[:, hs], start=True, stop=True),
                               reads=[TTB[c], YsbB], writes=[PSB[6]], rg=0)
                        OP('act', lambda e: e.activation(out=Usb[:], in_=PS[6][0:64, 256:512], func=AF.Copy, scale=-1.0),
                           reads=[PSB[6]], writes=[UsbB])
                    for h in range(4):
                        hp, r = h // 2, (h % 2) * 64
                        hs = slice(h * 64, (h + 1) * 64)
                        ob = PS[4 + hp][r:r + 64, cs]
                        OP('pe', lambda e, ob=ob, hs=hs, c=c: e.matmul(ob, lhsT=Vt[c][:, hs], rhs=ATs[c][:, hs], start=True, stop=False),
                           reads=[VtB[c], ATsB[c]], writes=[PSB[4 + hp]], rg=0)
                        if isrw:
                            OP('pe', lambda e, ob=ob, hs=hs, c=c: e.matmul(ob, lhsT=Usb[:, hs], rhs=ARs[c][:, hs], start=False, stop=False),
                               reads=[UsbB, ARsB[c]], writes=[PSB[4 + hp]], rg=0)
                        OP('pe', lambda e, ob=ob, hp=hp, r=r, cs=cs: e.matmul(ob, lhsT=Sb[hp][r:r + 64, :], rhs=QE[hp][0][r:r + 64, cs], start=False, stop=True),
                           reads=[SbB[hp], QE[hp][1]], writes=[PSB[4 + hp]], rg=r)
                    for h in range(4):
                        hp, r = h // 2, (h % 2) * 64
                        hs = slice(h * 64, (h + 1) * 64)
                        sp_ = PS[7][r:r + 64, 256 + hp * 64:256 + (hp + 1) * 64]
                        OP('pe', lambda e, sp_=sp_, hs=hs, c=c: e.matmul(sp_, lhsT=KEt[c][:, hs], rhs=Vt[c][:, hs], start=True, stop=(not isrw)),
                           reads=[KEtB[c], VtB[c]], writes=[PSB[7]], rg=0)
                        if isrw:
                            OP('pe', lambda e, sp_=sp_, hs=hs, c=c: e.matmul(sp_, lhsT=AEt[c][:, hs], rhs=Usb[:, hs], start=False, stop=True),
                               reads=[AEtB[c], UsbB], writes=[PSB[7]], rg=0)
                    for hp in range(2):
                        pc = PC[hp][0][:, (c + 1) * CH - 1:(c + 1) * CH]
                        OP('dve', lambda e, hp=hp: e.tensor_tensor(out=stmp[hp][:], in0=PS[7][:, 256 + hp * 64:256 + (hp + 1) * 64], in1=S3[hp][:], op=ALU.add),
                           reads=[PSB[7], S3B[hp]], writes=[stmpB[hp]])
                        OP('dve', lambda e, hp=hp, pc=pc: e.tensor_scalar(out=S3[hp][:], in0=stmp[hp][:], scalar1=pc, scalar2=None, op0=ALU.mult),
                           reads=[stmpB[hp], PC[hp][1]], writes=[S3B[hp]])
                        OP('dve', lambda e, hp=hp, pc=pc: e.tensor_scalar(out=Sb[hp][:], in0=stmp[hp][:], scalar1=pc, scalar2=None, op0=ALU.mult),
                           reads=[stmpB[hp], PC[hp][1]], writes=[SbB[hp]])

            def evac_fm(bk, func=AF.Copy, scale=1.0, bias=None, dt='f', rows=128, dst=None):
                t, b = dst if dst is not None else (fa() if dt == 'f' else ba())
                kw = {}
                if bias is not None:
                    kw['bias'] = bias
                OP('act', lambda e: e.activation(out=t[0:rows, :], in_=PS[bk][0:rows, 0:TBM], func=func, scale=scale, **kw),
                   reads=[PSB[bk]] + ([pvB[0]] if bias is not None else []), writes=[b])
                return t, b

            def mixer_block(l, s, bi):
                first = (bi == 0)
                c0 = bi * TBM
                ti = c0 // TB
                plan = []
                if 'pool' in mixers:
                    plan += [('fm', 0), ('fm', 1)]
                if 'hgrn' in mixers:
                    for hp_ in range(2):
                        plan += [('fm', 2 + hp_), ('fm', 4 + hp_), ('fm', 6 + hp_)]
                    plan += [('tm', 0)]
                if 'gla' in mixers:
                    plan += [('fm', 22)]
                    for hp_ in range(2):
                        plan += [('fm', 16 + hp_), ('fm', 18 + hp_), ('fm', 20 + hp_)]
                    plan += [('tm', 2)]
                if 'rwkv' in mixers:
                    plan += [('fm', 14), ('fm', 15)]
                    for hp_ in range(2):
                        plan += [('fm', 8 + hp_), ('fm', 10 + hp_), ('fm', 12 + hp_)]
                    plan += [('tm', 1)]
                plan += [('wo', jp) for jp in range(KD // 2)]
                wplan['plan'] = plan
                wplan['pos'] = 0
                wplan['issued'] = {}
                wplan['loaded'] = {}
                if plan:
                    ensure_load(l, 0)
                    ensure_load(l, 1)
                    ensure_cast(l, 0)
                pv = pvec[l]
                if 'rwkv' in mixers:
                    if first:
                        OP('pool', lambda e: e.memset(xns[:, :, 0:2], 0.0), writes=[xnsB])
                    else:
                        OP('pool', lambda e: e.tensor_copy(out=xns[:, :, 0:1], in_=xn[:, :, TBM - 1:TBM]), reads=xnB, writes=[xnsB])
                rmsnorm_to_xn(l, PV['nmx'], c0, TBM, 0)
                if 'rwkv' in mixers:
                    OP('pool', lambda e: e.tensor_copy(out=xns[:, :, 1:TBM], in_=xn[:, :, 0:TBM - 1]), reads=xnB, writes=[xnsB])
                if 'pool' in mixers:
                    if first:
                        OP('pool', lambda e: e.memset(pext[:, :, 0:16], 0.0), writes=[pextB])
                    else:
                        OP('pool', lambda e: e.tensor_copy(out=pext[:, :, 0:16], in_=pext[:, :, TBM:TBM + 16]), reads=[pextB], writes=[pextB])
                    for ck in range(2):
                        bk = proj_fm(l, ck, first)
                        OP('act', lambda e, ck=ck, bk=bk: e.activation(out=pext[:, ck, 16:16 + TBM], in_=PS[bk][:, 0:TBM], func=AF.Copy),
                           reads=[PSB[bk]], writes=[pextB])
                    W_ = 16 + TBM
                    OP('dve', lambda e: e.tensor_tensor(out=pw[0][:, :, 1:W_], in0=pext[:, :, 1:W_], in1=pext[:, :, 0:W_ - 1], op=ALU.add),
                       reads=[pextB], writes=[pwB[0]])
                    def poolfin(src, ck, r0, wdw, first=first):
                        yt, yb = PB16[ck], PB16B[ck]
                        OP('dve', lambda e: e.scalar_tensor_tensor(out=yt[r0:r0 + 64, :], in0=src[r0:r0 + 64, ck, 16:16 + TBM], scalar=1.0 / wdw,
                                                                   in1=pext[r0:r0 + 64, ck, 16:16 + TBM], op0=ALU.mult, op1=ALU.subtract),
                           reads=[pwB[0], pwB[1], pextB], writes=[yb])
                        if first:
                            t2, b2 = FA[0], FAB[0]
                            OP('dve', lambda e: e.tensor_tensor(out=t2[r0:r0 + 64, 0:16], in0=src[r0:r0 + 64, ck, 16:32],
                                                                in1=cst[r0:r0 + 64, ICN + ck * 16:ICN + ck * 16 + 16], op=ALU.mult),
                               reads=[pwB[0], pwB[1], cstB], writes=[b2])
                            OP('dve', lambda e: e.tensor_tensor(out=yt[r0:r0 + 64, 0:16], in0=t2[r0:r0 + 64, 0:16],
                                                                in1=pext[r0:r0 + 64, ck, 16:32], op=ALU.subtract),
                               reads=[b2, pextB], writes=[yb])
                    poolfin(pw[0], 0, 0, 2)
                    OP('dve', lambda e: e.tensor_tensor(out=pw[1][:, :, 3:W_], in0=pw[0][:, :, 3:W_], in1=pw[0][:, :, 1:W_ - 2], op=ALU.add),
                       reads=[pwB[0]], writes=[pwB[1]])
                    poolfin(pw[1], 0, 64, 4)
                    OP('dve', lambda e: e.tensor_tensor(out=pw[0][:, :, 7:W_], in0=pw[1][:, :, 7:W_], in1=pw[1][:, :, 3:W_ - 4], op=ALU.add),
                       reads=[pwB[1]], writes=[pwB[0]])
                    poolfin(pw[0], 1, 0, 8)
                    OP('dve', lambda e: e.tensor_tensor(out=pw[1][:, :, 15:W_], in0=pw[0][:, :, 15:W_], in1=pw[0][:, :, 7:W_ - 8], op=ALU.add),
                       reads=[pwB[0]], writes=[pwB[1]])
                    poolfin(pw[1], 1, 64, 16)
                    for ck in range(2):
                        bk = pbank()
                        OP('pe', lambda e, ck=ck, bk=bk: e.matmul(PS[bk][:, 0:TBM], lhsT=smatb[:, ck * 128:(ck + 1) * 128], rhs=PB16[ck][:], start=True, stop=True),
                           reads=[PB16B[ck], smatB], writes=[PSB[bk]])
                        OP('dve', lambda e, ck=ck, bk=bk: e.tensor_scalar(out=YT[:, ck, :], in0=PS[bk][:, 0:TBM], scalar1=pv[:, PV['pool_b'] + ck:PV['pool_b'] + ck + 1],
                                                                      scalar2=pv[:, PV['pool_s'] + ck:PV['pool_s'] + ck + 1], op0=ALU.add, op1=ALU.mult),
                           reads=[PSB[bk], pvB[l]], writes=[YB[ck]])
                else:
                    for ck in range(2):
                        OP('pool', lambda e, ck=ck: e.memset(YT[:, ck, :], 0.0), writes=[YB[ck]])

                def out_rstd(Ot, lhs_ones, n_ch, eps):
                    bk = pbank()
                    for hp in range(2):
                        st, sbb = ba()
                        OP('act', lambda e, hp=hp, st=st, Ot=Ot: e.activation(out=st[:], in_=Ot[hp][0][:], func=AF.Square), reads=[Ot[hp][1]], writes=[sbb])
                        if lhs_ones is ones:
                            OP('pe', lambda e, hp=hp, st=st: e.matmul(PS[bk][:, 0:TBM], lhsT=ones[:], rhs=st[:], start=(hp == 0), stop=(hp == 1)),
                               reads=[sbb, onesB], writes=[PSB[bk]])
                        else:
                            bk2 = bk if hp == 0 else pbank()
                            OP('pe', lambda e, hp=hp, st=st, bk2=bk2: e.matmul(PS[bk2][:, 0:TBM], lhsT=bones[:], rhs=st[:], start=True, stop=True),
                               reads=[sbb, bonesB], writes=[PSB[bk2]])
                            if hp == 0:
                                bk0 = bk2
                            else:
                                bk1 = bk2
                    if lhs_ones is ones:
                        rt, rb = fa()
                        rstd_from(PS[bk], PSB[bk], TBM, 1.0 / n_ch, eps, rt, rb)
                        return [(rt, rb), (rt, rb)]
                    res = []
                    for bkx in (bk0, bk1):
                        rt, rb = fa()
                        rstd_from(PS[bkx], PSB[bkx], TBM, 1.0 / n_ch, eps, rt, rb)
                        res.append((rt, rb))
                    return res

                def evac_O():
                    Ot = []
                    for hp in range(2):
                        t, b = fa()
                        OP('act', lambda e, hp=hp, t=t: e.activation(out=t[:], in_=PS[4 + hp][:, 0:TBM], func=AF.Copy), reads=[PSB[4 + hp]], writes=[b])
                        Ot.append((t, b))
                    return Ot

                if 'hgrn' in mixers:
                    QE, KE, PCx, GT = [], [], [], []
                    for hp in range(2):
                        bq = proj_fm(l, 2 + hp, first)
                        qt, qb = evac_fm(bq, AF.Silu)
                        bf_ = proj_fm(l, 4 + hp, first)
                        st_, sb_ = evac_fm(bf_, AF.Sigmoid)
                        ft, fb = fa()
                        OP('dve', lambda e, hp=hp, st_=st_, ft=ft: e.tensor_scalar(out=ft[:], in0=st_[:], scalar1=lbt[:, 2 * l + hp:2 * l + hp + 1],
                                                                             scalar2=lbt[:, 4 + 2 * l + hp:4 + 2 * l + hp + 1], op0=ALU.mult, op1=ALU.add),
                           reads=[sb_, lbtB], writes=[fb])
                        lt, lb_ = fa()
                        OP('dve', lambda e, ft=ft, lt=lt: e.tensor_scalar_max(out=lt[:], in0=ft[:], scalar1=1e-30), reads=[fb], writes=[lb_])
                        OP('act', lambda e, lt=lt: e.activation(out=lt[:], in_=lt[:], func=AF.Ln), reads=[lb_], writes=[lb_])
                        bt, bb = scan_decay(lt, lb_)
                        ebt, ebb = LL[hp][0], LLB_[hp][0]
                        OP('act', lambda e, bt=bt, ebt=ebt: e.activation(out=ebt[:], in_=bt[:], func=AF.Exp), reads=[bb], writes=[ebb])
                        OP('act', lambda e, bt=bt: e.activation(out=bt[:], in_=bt[:], func=AF.Exp, scale=-1.0), reads=[bb], writes=[bb])
                        qe, qeb = LB[hp][0], LBB[hp][0]
                        OP('dve', lambda e, qt=qt, ebt=ebt, qe=qe: e.scalar_tensor_tensor(out=qe[:], in0=qt[:], scalar=QK, in1=ebt[:], op0=ALU.mult, op1=ALU.mult),
                           reads=[qb, ebb], writes=[qeb])
                        OP('dve', lambda e, ft=ft: e.tensor_scalar(out=ft[:], in0=ft[:], scalar1=-1.0, scalar2=1.0, op0=ALU.mult, op1=ALU.add),
                           reads=[fb], writes=[fb])
                        ke, keb = LB[hp][1], LBB[hp][1]
                        OP('dve', lambda e, ft=ft, bt=bt, ke=ke: e.tensor_tensor(out=ke[:], in0=ft[:], in1=bt[:], op=ALU.mult), reads=[fb, bb], writes=[keb])
                        bg_ = proj_fm(l, 6 + hp, first)
                        gt, gb = evac_fm(bg_, AF.Sigmoid, dst=(LL[hp][1], LLB_[hp][1]))
                        QE.append((qe, qeb)); KE.append((ke, keb)); PCx.append((ebt, ebb)); GT.append((gt, gb))
                    proj_tm(l, 0)
                    chunk_engine('hgrn', QE, KE, PCx)
                    Ot = evac_O()
                    rs = out_rstd(Ot, ones, 256, NORM_EPS)
                    for hp in range(2):
                        t1, b1 = fa()
                        OP('dve', lambda e, hp=hp, t1=t1, Ot=Ot, rs=rs: e.scalar_tensor_tensor(out=t1[:], in0=Ot[hp][0][:], scalar=pv[:, PV['hnorm'] + hp:PV['hnorm'] + hp + 1],
                                                                           in1=rs[hp][0][:], op0=ALU.mult, op1=ALU.mult),
                           reads=[Ot[hp][1], rs[hp][1], pvB[l]], writes=[b1])
                        OP('dve', lambda e, hp=hp, t1=t1, GT=GT: e.tensor_tensor(out=YT[:, 2 + hp, :], in0=t1[:], in1=GT[hp][0][:], op=ALU.mult),
                           reads=[b1, GT[hp][1]], writes=[YB[2 + hp]])
                else:
                    for ck in (2, 3):
                        OP('pool', lambda e, ck=ck: e.memset(YT[:, ck, :], 0.0), writes=[YB[ck]])

                if 'gla' in mixers:
                    bga = proj_fm(l, 22, first)
                    gat, gab = LLX[0], LLXB[0]
                    OP('act', lambda e: e.activation(out=gat[:], in_=PS[bga][:, 0:TBM], func=AF.Copy), reads=[PSB[bga]], writes=[gab])
                    QE, KE, PCx, GT = [], [], [], []
                    for hp in range(2):
                        bk = pbank()
                        OP('pe', lambda e, hp=hp, bk=bk: e.matmul(PS[bk][:, 0:TBM], lhsT=smat[:, 1024 + hp * 128:1024 + (hp + 1) * 128], rhs=gat[:], start=True, stop=True),
                           reads=[gab, smatB], writes=[PSB[bk]])
                        lt, lb_ = evac_fm(bk, AF.Sigmoid, bias=pv[:, PV['glab'] + hp:PV['glab'] + hp + 1])
                        OP('act', lambda e, lt=lt: e.activation(out=lt[:], in_=lt[:], func=AF.Ln), reads=[lb_], writes=[lb_])
                        bt, bb = scan_decay(lt, lb_)
                        ebt, ebb = LL[hp][0], LLB_[hp][0]
                        OP('act', lambda e, bt=bt, ebt=ebt: e.activation(out=ebt[:], in_=bt[:], func=AF.Exp, scale=1.0 / 16), reads=[bb], writes=[ebb])
                        OP('act', lambda e, bt=bt: e.activation(out=bt[:], in_=bt[:], func=AF.Exp, scale=-1.0 / 16), reads=[bb], writes=[bb])
                        bq = proj_fm(l, 16 + hp, first)
                        qe, qeb = LB[hp][0], LBB[hp][0]
                        OP('dve', lambda e, bq=bq, ebt=ebt, qe=qe: e.scalar_tensor_tensor(out=qe[:], in0=PS[bq][:, 0:TBM], scalar=QK, in1=ebt[:], op0=ALU.mult, op1=ALU.mult),
                           reads=[PSB[bq], ebb], writes=[qeb])
                        bkk = proj_fm(l, 18 + hp, first)
                        ke, keb = LB[hp][1], LBB[hp][1]
                        OP('dve', lambda e, bkk=bkk, bt=bt, ke=ke: e.tensor_tensor(out=ke[:], in0=PS[bkk][:, 0:TBM], in1=bt[:], op=ALU.mult), reads=[PSB[bkk], bb], writes=[keb])
                        bg_ = proj_fm(l, 20 + hp, first)
                        gt, gb = evac_fm(bg_, AF.Silu, dst=(LL[hp][1], LLB_[hp][1]))
                        QE.append((qe, qeb)); KE.append((ke, keb)); PCx.append((ebt, ebb)); GT.append((gt, gb))
                    proj_tm(l, 2)
                    chunk_engine('gla', QE, KE, PCx)
                    Ot = evac_O()
                    rs = out_rstd(Ot, bones, 64, NORM_EPS)
                    for hp in range(2):
                        t1, b1 = fa()
                        OP('dve', lambda e, hp=hp, t1=t1, Ot=Ot, rs=rs: e.scalar_tensor_tensor(out=t1[:], in0=Ot[hp][0][:], scalar=pv[:, PV['gnorm'] + hp:PV['gnorm'] + hp + 1],
                                                                           in1=rs[hp][0][:], op0=ALU.mult, op1=ALU.mult),
                           reads=[Ot[hp][1], rs[hp][1], pvB[l]], writes=[b1])
                        OP('dve', lambda e, hp=hp, t1=t1, GT=GT: e.tensor_tensor(out=YT[:, 6 + hp, :], in0=t1[:], in1=GT[hp][0][:], op=ALU.mult),
                           reads=[b1, GT[hp][1]], writes=[YB[6 + hp]])
                else:
                    for ck in (6, 7):
                        OP('pool', lambda e, ck=ck: e.memset(YT[:, ck, :], 0.0), writes=[YB[ck]])

                if 'rwkv' in mixers:
                    bwa = proj_fm(l, 14, first)
                    twa, twab = LLX[0], LLXB[0]
                    OP('act', lambda e: e.activation(out=twa[0:64, :], in_=PS[bwa][0:64, 0:TBM], func=AF.Tanh), reads=[PSB[bwa]], writes=[twab])
                    OP('act', lambda e: e.activation(out=twa[64:128, :], in_=PS[bwa][64:128, 0:TBM], func=AF.Copy), reads=[PSB[bwa]], writes=[twab])
                    bxg = proj_fm(l, 15, first)
                    sxg, sxgb = evac_fm(bxg, AF.Sigmoid, dst=(LLX[1], LLXB[1]))
                    QE, KE, AE, BE, PCx, GR, RKR, VF = [], [], [], [], [], [], [], []
                    for hp in range(2):
                        cs_ = slice(hp * 128, (hp + 1) * 128)
                        bk = pbank()
                        OP('pe', lambda e, bk=bk, hp=hp: e.matmul(PS[bk][:, 0:TBM], lhsT=smat[:, 256 + hp * 128:256 + (hp + 1) * 128], rhs=twa[:], start=True, stop=True),
                           reads=[twab, smatB], writes=[PSB[bk]])
                        lw, lwb = evac_fm(bk, AF.Sigmoid, bias=pv[:, PV['w0'] + hp:PV['w0'] + hp + 1])
                        bk = pbank()
                        OP('pe', lambda e, bk=bk, hp=hp: e.matmul(PS[bk][:, 0:TBM], lhsT=smat[:, 768 + hp * 128:768 + (hp + 1) * 128], rhs=twa[:], start=True, stop=True),
                           reads=[twab, smatB], writes=[PSB[bk]])
                        at, ab_ = evac_fm(bk, AF.Sigmoid, bias=pv[:, PV['a0'] + hp:PV['a0'] + hp + 1])
                        bk = pbank()
                        OP('pe', lambda e, bk=bk, hp=hp: e.matmul(PS[bk][:, 0:TBM], lhsT=smat[:, 512 + hp * 128:512 + (hp + 1) * 128], rhs=sxg[:], start=True, stop=True),
                           reads=[sxgb, smatB], writes=[PSB[bk]])
                        grt, grb = evac_fm(bk, AF.Copy, dst=(LL[hp][1], LLB_[hp][1]))
                        bt, bb = scan_decay(lw, lwb)
                        CW = -float(np.exp(-0.5))
                        ebt, ebb = LL[hp][0], LLB_[hp][0]
                        OP('act', lambda e, bt=bt, ebt=ebt: e.activation(out=ebt[:], in_=bt[:], func=AF.Exp, scale=CW), reads=[bb], writes=[ebb])
                        enb, enbb = fa()
                        OP('act', lambda e, bt=bt, enb=enb: e.activation(out=enb[:], in_=bt[:], func=AF.Exp, scale=-CW), reads=[bb], writes=[enbb])
                        OP('dve', lambda e, bt=bt, lw=lw: e.tensor_tensor(out=bt[:], in0=bt[:], in1=lw[:], op=ALU.subtract), reads=[bb, lwb], writes=[bb])
                        OP('act', lambda e, bt=bt: e.activation(out=bt[:], in_=bt[:], func=AF.Exp, scale=CW), reads=[bb], writes=[bb])
                        br = proj_fm(l, 8 + hp, first)
                        rt, rb = evac_fm(br, AF.Copy)
                        bkr = proj_fm(l, 10 + hp, first)
                        kt, kb = evac_fm(bkr, AF.Copy)
                        bv = proj_fm(l, 12 + hp, first)
                        vt, vb = evac_fm(bv, AF.Copy, dst=(LL[hp][2], LLB_[hp][2]))
                        kkt, kkb = fa()
                        OP('dve', lambda e, kt=kt, kkt=kkt, hp=hp: e.tensor_scalar(out=kkt[:], in0=kt[:], scalar1=pv[:, PV['kk'] + hp:PV['kk'] + hp + 1], scalar2=None, op0=ALU.mult),
                           reads=[kb, pvB[l]], writes=[kkb])
                        sq_, sqb_ = ba()
                        OP('act', lambda e, kkt=kkt, sq_=sq_: e.activation(out=sq_[:], in_=kkt[:], func=AF.Square), reads=[kkb], writes=[sqb_])
                        bk = pbank()
                        OP('pe', lambda e, bk=bk, sq_=sq_: e.matmul(PS[bk][:, 0:TBM], lhsT=bones[:], rhs=sq_[:], start=True, stop=True), reads=[sqb_, bonesB], writes=[PSB[bk]])
                        rn, rnb = fa()
                        rstd_from(PS[bk], PSB[bk], TBM, 1.0, 1e-24, rn, rnb)
                        OP('dve', lambda e, kkt=kkt, rn=rn: e.tensor_tensor(out=kkt[:], in0=kkt[:], in1=rn[:], op=ALU.mult), reads=[kkb, rnb], writes=[kkb])
                        fac, facb = fa()
                        OP('dve', lambda e, at=at, fac=fac, hp=hp: e.tensor_scalar(out=fac[:], in0=at[:], scalar1=-1.0, scalar2=pv[:, PV['ka'] + hp:PV['ka'] + hp + 1], op0=ALU.add, op1=ALU.mult),
                           reads=[ab_, pvB[l]], writes=[facb])
                        OP('dve', lambda e, fac=fac, kt=kt: e.scalar_tensor_tensor(out=kt[:], in0=fac[:], scalar=1.0, in1=kt[:], op0=ALU.add, op1=ALU.mult),
                           reads=[facb, kb], writes=[kb])
                        rk_, rkb_ = LB[hp][4], LBB[hp][4]
                        OP('dve', lambda e, rt=rt, kt=kt, rk_=rk_, hp=hp: e.scalar_tensor_tensor(out=rk_[:], in0=rt[:], scalar=pv[:, PV['rk'] + hp:PV['rk'] + hp + 1], in1=kt[:], op0=ALU.mult, op1=ALU.mult),
                           reads=[rb, kb, pvB[l]], writes=[rkb_])
                        qe, qeb = LB[hp][0], LBB[hp][0]
                        OP('dve', lambda e, rt=rt, ebt=ebt, qe=qe: e.tensor_tensor(out=qe[:], in0=rt[:], in1=ebt[:], op=ALU.mult), reads=[rb, ebb], writes=[qeb])
                        ke, keb = LB[hp][1], LBB[hp][1]
                        OP('dve', lambda e, kt=kt, enb=enb, ke=ke: e.tensor_tensor(out=ke[:], in0=kt[:], in1=enb[:], op=ALU.mult), reads=[kb, enbb], writes=[keb])
                        be, beb = LB[hp][3], LBB[hp][3]
                        OP('dve', lambda e, kkt=kkt, bt=bt, be=be: e.tensor_tensor(out=be[:], in0=kkt[:], in1=bt[:], op=ALU.mult), reads=[kkb, bb], writes=[beb])
                        OP('dve', lambda e, kkt=kkt, at=at: e.tensor_tensor(out=kkt[:], in0=kkt[:], in1=at[:], op=ALU.mult), reads=[kkb, ab_], writes=[kkb])
                        ae, aeb = LB[hp][2], LBB[hp][2]
                        OP('dve', lambda e, kkt=kkt, enb=enb, ae=ae: e.tensor_tensor(out=ae[:], in0=kkt[:], in1=enb[:], op=ALU.mult), reads=[kkb, enbb], writes=[aeb])
                        QE.append((qe, qeb)); KE.append((ke, keb)); AE.append((ae, aeb)); BE.append((be, beb)); PCx.append((ebt, ebb))
                        GR.append((grt, grb)); RKR.append((rk_, rkb_)); VF.append((vt, vb))
                    proj_tm(l, 1)
                    chunk_engine('rwkv', QE, KE, PCx, rw={'AE': AE, 'BE': BE})
                    Ot = evac_O()
                    for hp in range(2):
                        ob16, ob16b = ba()
                        OP('dve', lambda e, hp=hp, ob16=ob16, Ot=Ot: e.tensor_copy(out=ob16[:], in_=Ot[hp][0][:]), reads=[Ot[hp][1]], writes=[ob16b])
                        bk = pbank()
                        OP('pe', lambda e, bk=bk, ob16=ob16: e.matmul(PS[bk][:, 0:TBM], lhsT=bones[:], rhs=ob16[:], start=True, stop=True), reads=[ob16b, bonesB], writes=[PSB[bk]])
                        ct, cb = fa()
                        OP('dve', lambda e, hp=hp, bk=bk, ct=ct, Ot=Ot: e.scalar_tensor_tensor(out=ct[:], in0=PS[bk][:, 0:TBM], scalar=-1.0 / 64, in1=Ot[hp][0][:], op0=ALU.mult, op1=ALU.add),
                           reads=[PSB[bk], Ot[hp][1]], writes=[cb])
                        s2, s2b = ba()
                        OP('act', lambda e, ct=ct, s2=s2: e.activation(out=s2[:], in_=ct[:], func=AF.Square), reads=[cb], writes=[s2b])
                        bk = pbank()
                        OP('pe', lambda e, bk=bk, s2=s2: e.matmul(PS[bk][:, 0:TBM], lhsT=bones[:], rhs=s2[:], start=True, stop=True), reads=[s2b, bonesB], writes=[PSB[bk]])
                        rn, rnb = fa()
                        rstd_from(PS[bk], PSB[bk], TBM, 1.0 / 64, GN_EPS, rn, rnb)
                        OP('dve', lambda e, hp=hp, ct=ct, rn=rn: e.scalar_tensor_tensor(out=ct[:], in0=ct[:], scalar=pv[:, PV['lnw'] + hp:PV['lnw'] + hp + 1], in1=rn[:], op0=ALU.mult, op1=ALU.mult),
                           reads=[cb, rnb, pvB[l]], writes=[cb])
                        bk = pbank()
                        OP('pe', lambda e, bk=bk, hp=hp, RKR=RKR: e.matmul(PS[bk][:, 0:TBM], lhsT=bones[:], rhs=RKR[hp][0][:], start=True, stop=True), reads=[RKR[hp][1], bonesB], writes=[PSB[bk]])
                        bo, bob = fa()
                        OP('dve', lambda e, bk=bk, hp=hp, bo=bo, VF=VF: e.tensor_tensor(out=bo[:], in0=PS[bk][:, 0:TBM], in1=VF[hp][0][:], op=ALU.mult), reads=[PSB[bk], VF[hp][1]], writes=[bob])
                        OP('dve', lambda e, hp=hp, ct=ct, bo=bo: e.scalar_tensor_tensor(out=ct[:], in0=ct[:], scalar=pv[:, PV['lnb'] + hp:PV['lnb'] + hp + 1], in1=bo[:], op0=ALU.add, op1=ALU.add),
                           reads=[cb, bob, pvB[l]], writes=[cb])
                        OP('dve', lambda e, hp=hp, ct=ct, GR=GR: e.tensor_tensor(out=YT[:, 4 + hp, :], in0=ct[:], in1=GR[hp][0][:], op=ALU.mult),
                           reads=[cb, GR[hp][1]], writes=[YB[4 + hp]])
                else:
                    for ck in (4, 5):
                        OP('pool', lambda e, ck=ck: e.memset(YT[:, ck, :], 0.0), writes=[YB[ck]])

                for jp in range(KD // 2):
                    (i,) = acq(l, ('wo', jp))
                    for q in range(2):
                        j = 2 * jp + q
                        for m in range(KD):
                            OP('pe', lambda e, m=m, j=j, q=q, i=i: e.matmul(PS[m][:, 0:TBM], lhsT=wbf[i][:, q * D + m * 128:q * D + (m + 1) * 128], rhs=YT[:, j, :],
                                                                   start=(j == 0), stop=(j == KD - 1)),
                               reads=[wbfB[i], wbfB2[i][0], wbfB2[i][1], YB[j]], writes=[PSB[m]], sig=(m == KD - 1 or j == KD - 1))
                for m in range(KD):
                    OP('dve', lambda e, m=m: e.tensor_tensor(out=X[:, m, c0:c0 + TBM], in0=PS[m][:, 0:TBM], in1=X[:, m, c0:c0 + TBM], op=ALU.add),
                       reads=[PSB[m], XB[m][ti]], writes=[XB[m][ti]])

            def mixer_setup(l, s):
                OP('sp', lambda e: e.dma_start(out=mub[:], in_=mub_d[l]), writes=[mubB], dsem=muS)
                OP('dve', lambda e: e.tensor_scalar(out=omub[:], in0=mub[:], scalar1=-1.0, scalar2=1.0, op0=ALU.mult, op1=ALU.add), reads=[mubB], writes=[omubB])
                OP('sp', lambda e: e.dma_start(out=smat[:], in_=smat_d[l]), writes=[smatB], dsem=smS)
                OP('pool', lambda e: e.tensor_copy(out=smatb[:], in_=smat[:, 0:256]), reads=[smatB], writes=[smatB])
                for m in ('hgrn', 'gla', 'rwkv'):
                    for hp in range(2):
                        OP('pool', lambda e, m=m, hp=hp: e.memset(S32[m][hp][:], 0.0), writes=[S32B[m][hp]])
                        OP('pool', lambda e, m=m, hp=hp: e.memset(Sbf[m][hp][:], 0.0), writes=[SbfB[m][hp]])

        if do_mix:
            OP('act', lambda e: e.activation(out=lbt[:, 0:2], in_=pvec[0][:, PV['lb0']:PV['lb0'] + 2], func=AF.Exp), reads=[pvB[0]], writes=[lbtB])
            OP('act', lambda e: e.activation(out=lbt[:, 2:4], in_=pvec[L - 1][:, PV['lbl']:PV['lbl'] + 2], func=AF.Exp), reads=[pvB[L - 1], lbtB], writes=[lbtB])
            OP('dve', lambda e: e.tensor_tensor(out=lbt[:, 4:6], in0=lbt[:, 0:2], in1=lbt[:, 2:4], op=ALU.add), reads=[lbtB], writes=[lbtB])
            OP('dve', lambda e: e.reciprocal(out=lbt[:, 4:6], in_=lbt[:, 4:6]), reads=[lbtB], writes=[lbtB])
            OP('dve', lambda e: e.tensor_tensor(out=lbt[:, 6:8], in0=lbt[:, 2:4], in1=lbt[:, 4:6], op=ALU.mult), reads=[lbtB], writes=[lbtB])
            OP('dve', lambda e: e.memset(lbt[:, 4:6], 0.0), reads=[lbtB], writes=[lbtB])
            OP('dve', lambda e: e.tensor_scalar(out=lbt[:, 0:4], in0=lbt[:, 4:8], scalar1=-1.0, scalar2=1.0, op0=ALU.mult, op1=ALU.add), reads=[lbtB], writes=[lbtB])

        for s in range(NS):
            for k in range(KD):
                OP('sp', lambda e, k=k, s=s: e.dma_start(out=X[:, k, :], in_=xT[s, :, k, :]), writes=XB[k], dsem=xS[k])
            for l in range(L):
                if do_ffn:
                    for ti in range(NT):
                        ffn(l, 0, ti)
                if do_mix:
                    mixer_setup(l, s)
                    for bi in range(T // TBM):
                        mixer_block(l, s, bi)
                if do_ffn:
                    for ti in range(NT):
                        ffn(l, 1, ti)
            for ti in range(NT):
                c0 = ti * TB
                for k in range(KD):
                    i = counters['sq'] % 2
                    counters['sq'] += 1
                    OP('act', lambda e, k=k, i=i, c0=c0: e.activation(out=sq[i][:], in_=X[:, k, c0:c0 + TB], func=AF.Square),
                       reads=[XB[k][ti]], writes=[sqB[i]])
                    OP('pe', lambda e, k=k, i=i: e.matmul(PS[0][:, :], lhsT=ones[:], rhs=sq[i][:], start=(k == 0), stop=(k == KD - 1)),
                       reads=[sqB[i], onesB], writes=[PSB[0]])
                rstd_from(PS[0], PSB[0], TB, 1.0 / D, NORM_EPS, rstd, rstdB)
                for k in range(KD):
                    OP('dve', lambda e, k=k, c0=c0: e.scalar_tensor_tensor(out=X[:, k, c0:c0 + TB], in0=X[:, k, c0:c0 + TB],
                                                                        scalar=pvec[0][:, PV['nfin'] + k:PV['nfin'] + k + 1], in1=rstd[:],
                                                                        op0=ALU.mult, op1=ALU.mult),
                       reads=[XB[k][ti], rstdB, pvB[0]], writes=[XB[k][ti]])
            for k in range(KD):
                OP('sp', lambda e, k=k, s=s: e.dma_start(out=outT[s, :, k, :], in_=X[:, k, :]), reads=XB[k], writes=XB[k], dsem=oS[k])
        pg.ops['sp'].append((lambda e: e.nop(), {o_: o_.count for o_ in oS}, False, None))
        pg.emit(block, esems)
    return nc, pg


def _col(v, n):
    return np.ascontiguousarray(np.asarray(v, np.float32).reshape(n, 128).T)


def make_consts():
    cst = np.zeros((128, 1600), np.float32)
    p = np.arange(64)[:, None]
    f = np.arange(64)[None, :]
    for h in range(4):
        cst[0:64, 0 + h * 64:0 + (h + 1) * 64] = (p <= f)
        cst[0:64, 256 + h * 64:256 + (h + 1) * 64] = (p < f)
        cst[0:64, 512 + h * 64:512 + (h + 1) * 64] = (f < p)
        cst[0:64, 768 + h * 64:768 + (h + 1) * 64] = (p == f)
    scm = np.ones(512, np.float32)
    scm[::64] = 0.0
    cst[:, 1024:1536] = scm[None, :]
    wins = {(0, 0): 2, (0, 1): 4, (1, 0): 8, (1, 1): 16}
    for ck in range(2):
        for half in range(2):
            w = wins[(ck, half)]
            t = np.arange(16)
            cst[half * 64:(half + 1) * 64, 1536 + ck * 16:1536 + ck * 16 + 16] = (1.0 / np.minimum(t + 1, w))[None, :]
    return cst


def prep_weights(inp, L):
    out = {}
    f32 = np.float32
    for l in range(L):
        for w, (wi, wo) in enumerate((('ffn1_w_in', 'ffn1_w_out'), ('ffn2_w_in', 'ffn2_w_out'))):
            W = np.asarray(inp[wi][l], f32)
            Wk = W.reshape(KD, 128, 2 * FF)
            g = Wk[:, :, :FF].reshape(KD, 128, NJ, 128)
            u = Wk[:, :, FF:].reshape(KD, 128, NJ, 128)
            blk = np.concatenate([g, u], axis=3)
            out[f"f{w + 1}_win{l}"] = np.ascontiguousarray(blk.transpose(2, 1, 0, 3)).reshape(NJ, 128, KD * 256)
            out[f"f{w + 1}_wout{l}"] = np.ascontiguousarray(np.asarray(inp[wo][l], f32).reshape(NJ, 128, D))
        W = np.asarray(inp['w_in'][l], f32).reshape(KD, 128, DIN)
        fm = np.zeros((len(FMG), 128, KD, 128), f32)
        for gi, (c0, nc_, _) in enumerate(FMG):
            nc_ = min(nc_, DIN - c0)
            fm[gi, :, :, :nc_] = W[:, :, c0:c0 + nc_].transpose(1, 0, 2)
        out[f"m_fm{l}"] = fm.reshape(len(FMG), 128, KD * 128)
        tm = np.zeros((len(TMG), 128, KD, 256), f32)
        for gi, (c0, nc_, _) in enumerate(TMG):
            tm[gi] = W[:, :, c0:c0 + nc_].transpose(1, 0, 2)
        out[f"m_tm{l}"] = tm.reshape(len(TMG), 128, KD * 256)
        out[f"m_wout{l}"] = np.ascontiguousarray(np.asarray(inp['w_out'][l], f32).reshape(KD, 128, D))
        pv = np.zeros((128, NPV), f32)
        pv[:, PV['nf1']:PV['nf1'] + 8] = _col(inp['norm_ffn1'][l], 8)
        pv[:, PV['nmx']:PV['nmx'] + 8] = _col(inp['norm_mix'][l], 8)
        pv[:, PV['nf2']:PV['nf2'] + 8] = _col(inp['norm_ffn2'][l], 8)
        pv[:, PV['nfin']:PV['nfin'] + 8] = _col(inp['norm_final'], 8)
        pv[:, PV['lb0']:PV['lb0'] + 2] = _col(inp['hgrn_lb_logits'][0], 2)
        pv[:, PV['lbl']:PV['lbl'] + 2] = _col(inp['hgrn_lb_logits'][l], 2)
        for nm, key in (('pool_b', 'pool_b'), ('pool_s', 'pool_scale'), ('hnorm', 'hgrn_norm'), ('w0', 'rwkv_w0'), ('a0', 'rwkv_a0'),
                        ('kk', 'rwkv_k_k'), ('ka', 'rwkv_k_a'), ('rk', 'rwkv_r_k'), ('lnw', 'rwkv_ln_w'), ('lnb', 'rwkv_ln_b'),
                        ('glab', 'gla_b'), ('gnorm', 'gla_norm')):
            pv[:, PV[nm]:PV[nm] + 2] = _col(inp[key][l], 2)
        out[f"pvec{l}"] = pv
        out[f"mub{l}"] = np.ascontiguousarray(np.broadcast_to(np.asarray(inp['rwkv_mu'][l], f32)[None, :], (128, 1024)))
        sm = np.zeros((128, 5 * 256), f32)
        pw_ = np.asarray(inp['pool_w'][l], f32)
        for ck in range(2):
            sm[0:64, ck * 128:ck * 128 + 64] = pw_[2 * ck]
            sm[64:128, ck * 128 + 64:ck * 128 + 128] = pw_[2 * ck + 1]
        sm[0:64, 256:512] = np.asarray(inp['rwkv_w2'][l], f32)
        sm[64:128, 768:1024] = np.asarray(inp['rwkv_a2'][l], f32)
        sm[:, 512:768] = np.asarray(inp['rwkv_g2'][l], f32)
        sm[0:16, 1024:1280] = np.asarray(inp['gla_w2'][l], f32)
        out[f"smat{l}"] = sm
    out["cst"] = make_consts()
    return out


def prep_x(xc):
    NS, T, _ = xc.shape
    return np.ascontiguousarray(xc.reshape(NS, T, KD, 128).transpose(0, 3, 2, 1))


def unprep_out(o):
    NS, _, _, T = o.shape
    return np.ascontiguousarray(o.transpose(0, 3, 2, 1)).reshape(NS, T, D)


def kernel(**inputs):
    x = np.asarray(inputs['x'], np.float32)
    B, T, _ = x.shape
    NCORES = 8
    NS = B // NCORES
    L = 2
    nc, pg = build_program(T, NS, L)
    wts = prep_weights(inputs, L)
    in_maps = []
    for c in range(NCORES):
        m = dict(wts)
        m["xT"] = prep_x(x[c * NS:(c + 1) * NS])
        in_maps.append(m)
    res = run_bass_kernel_spmd(nc, in_maps, core_ids=list(range(NCORES)))
    outs = [unprep_out(np.asarray(r["outT"])) for r in res.results]
    return np.concatenate(outs, axis=0).astype(np.float32)
```

```python
import numpy as np
from contextlib import ExitStack
import concourse.bass as bass
import concourse.mybir as mybir
from concourse.bass_utils import run_bass_kernel_spmd

F32 = mybir.dt.float32
BF16 = mybir.dt.bfloat16
AF = mybir.ActivationFunctionType
ALU = mybir.AluOpType
ENGS = ['pe', 'act', 'dve', 'pool', 'sp']

D = 1024
KD = 8
FF = 2816
NJ = 22
G = 256
DIN = 3344
NORM_EPS = 1e-6
GN_EPS = 64e-5
QK = 0.125
TB = 512
TBM = 256
CH = 64
NCH = TBM // CH
DEBUG_STAGE = 99

FMG = [(0, 128, 0), (128, 128, 0), (256, 128, 0), (384, 128, 0), (512, 128, 0), (640, 128, 0),
       (1024, 128, 0), (1152, 128, 0),
       (1280, 128, 1), (1408, 128, 1), (1536, 128, 1), (1664, 128, 1), (1792, 128, 1), (1920, 128, 1),
       (2048, 128, 1), (2176, 128, 1),
       (2304, 128, 0), (2432, 128, 0), (2560, 128, 0), (2688, 128, 0), (3072, 128, 0), (3200, 128, 0),
       (3328, 128, 0)]
TMG = [(768, 256, 0), (1792, 256, 1), (2816, 256, 0)]
RW0 = 1280

PV = {}
_o = 0
for _n, _w in [('nf1', 8), ('nmx', 8), ('nf2', 8), ('pool_b', 2), ('pool_s', 2), ('lb0', 2), ('lbl', 2),
               ('hnorm', 2), ('w0', 2), ('a0', 2), ('kk', 2), ('ka', 2), ('rk', 2), ('lnw', 2), ('lnb', 2),
               ('glab', 2), ('gnorm', 2), ('nfin', 8)]:
    PV[_n] = _o
    _o += _w
NPV = _o


class Buf:
    __slots__ = ('name', 'w', 'r', 'const')

    def __init__(self, name, const=False):
        self.name = name
        self.w = None
        self.r = []
        self.const = const


class DSem:
    def __init__(self, h):
        self.h = h
        self.count = 0


class Prog:
    def __init__(self, nc, same_engine_sync=True):
        self.nc = nc
        self.ops = {e: [] for e in ENGS}
        self.cnt = {e: 0 for e in ENGS}
        self.same = same_engine_sync
        self.nops = 0
        self.last_rg = None
        self.last_pe_sig = True

    def op(self, eng, fn, reads=(), writes=(), sig=True, dsem=None, rg=None):
        waits = {}
        if eng == 'pe':
            if rg is not None and self.last_rg is not None and rg != self.last_rg:
                assert self.last_pe_sig
                waits['pe'] = self.cnt['pe']
            self.last_rg = rg
            self.last_pe_sig = sig

        def addw(tok):
            if tok is None:
                return
            k, v = tok
            if k == eng and (eng == 'pe' or not self.same):
                return
            if waits.get(k, 0) < v:
                waits[k] = v
        for b in reads:
            addw(b.w)
        for b in writes:
            addw(b.w)
            for t in b.r:
                addw(t)
        if dsem is not None:
            dsem.count += 16
            tok = (dsem, dsem.count)
            sig = False
        elif sig:
            self.cnt[eng] += 1
            tok = (eng, self.cnt[eng])
        else:
            tok = (eng, self.cnt[eng] + 1)
        for b in reads:
            if not b.const:
                b.r.append(tok)
                if len(b.r) > 48:
                    mx = {}
                    for k, v in b.r:
                        if mx.get(k, 0) < v:
                            mx[k] = v
                    b.r = list(mx.items())
        for b in writes:
            b.w = tok
            b.r = []
        self.ops[eng].append((fn, waits, sig, dsem))
        self.nops += 1
        return tok

    def emit(self, block, esems):
        nc = self.nc
        deco = {'pe': block.tensor, 'act': block.scalar, 'dve': block.vector,
                'pool': block.gpsimd, 'sp': block.sync}
        for e in ENGS:
            ops = self.ops[e]

            def body(eng, ops=ops, e=e):
                known = {}
                for fn, waits, sig, dsem in ops:
                    for k, v in waits.items():
                        if known.get(k, 0) >= v:
                            continue
                        known[k] = v
                        h = k.h if isinstance(k, DSem) else esems[k]
                        eng.wait_ge(h, v)
                    inst = fn(eng)
                    if dsem is not None:
                        inst.then_inc(dsem.h, 16)
                    elif sig:
                        inst.then_inc(esems[e], 1)
            deco[e](body)


def build_program(T, NS, L, mixers=('pool', 'hgrn', 'rwkv', 'gla'), do_ffn=True, do_mix=True):
    NT = T // TB
    nc = bass.Bass("TRN2", target_bir_lowering=False)
    dr = {}

    def din(name, shape):
        dr[name] = nc.dram_tensor(name, list(shape), F32, kind="ExternalInput").ap()
        return dr[name]
    xT = din("xT", [NS, 128, KD, T])
    outT = nc.dram_tensor("outT", [NS, 128, KD, T], F32, kind="ExternalOutput").ap()
    f_win = [[din(f"f{w}_win{l}", [NJ, 128, KD * 256]) for w in (1, 2)] for l in range(L)]
    f_wout = [[din(f"f{w}_wout{l}", [NJ, 128, D]) for w in (1, 2)] for l in range(L)]
    m_fm = [din(f"m_fm{l}", [len(FMG), 128, KD * 128]) for l in range(L)]
    m_tm = [din(f"m_tm{l}", [len(TMG), 128, KD * 256]) for l in range(L)]
    m_wout = [din(f"m_wout{l}", [KD, 128, D]) for l in range(L)]
    pvec_d = [din(f"pvec{l}", [128, NPV]) for l in range(L)]
    mub_d = [din(f"mub{l}", [128, 1024]) for l in range(L)]
    smat_d = [din(f"smat{l}", [128, 5 * 256]) for l in range(L)]
    cst_d = din("cst", [128, 1600])
    es = ExitStack()
    with es:
        def sb(name, shape, dt=F32):
            return es.enter_context(nc.sbuf_tensor("sb_" + name, list(shape), dt))

        def psum(name, shape, dt=F32):
            return es.enter_context(nc.psum_tensor("pp_" + name, list(shape), dt))
        esems = {e: es.enter_context(nc.semaphore("s_" + e)) for e in ENGS}

        def dsem(name):
            return DSem(es.enter_context(nc.semaphore(name)))
        pg = Prog(nc)
        block = es.enter_context(nc.Block())

        X = sb("X", [128, KD, T])
        XB = [[Buf(f"X{k}_{t}") for t in range(NT)] for k in range(KD)]
        xn = sb("xn", [128, KD, TB], BF16)
        xns = sb("xns", [128, KD, TBM], BF16)
        xnsB = Buf("xns")
        xnB = [Buf(f"xn{k}") for k in range(KD)]
        sq = [sb(f"sq{i}", [128, TB], BF16) for i in range(2)]
        sqB = [Buf(f"sq{i}") for i in range(2)]
        rstd = sb("rstd", [128, TB]); rstdB = Buf("rstd")
        ones = sb("ones", [128, 128], BF16); onesB = Buf("ones", const=True)
        bones = sb("bones", [128, 128], BF16)
        ident = sb("ident", [128, 128], BF16)
        cst = sb("cst", [128, 1600])
        cstB = Buf("cst", const=True)
        MI, MSU, MSL, ID4 = 0, 256, 512, 768
        SCM = 1024
        ICN = 1536
        pvec = [sb(f"pvec{l}", [128, NPV]) for l in range(L)]
        pvB = [Buf(f"pvec{l}", const=True) for l in range(L)]
        lbt = sb("lbt", [128, 8])
        mub = sb("mub", [128, 1024]); mubB = Buf("mub")
        omub = sb("omub", [128, 1024]); omubB = Buf("omub")
        smat = sb("smat", [128, 5 * 256]); smatB = Buf("smat")
        smatb = sb("smatb", [128, 2 * 128], BF16)
        wst = [sb(f"wst{i}", [128, KD * 256]) for i in range(2)]
        wstB = [Buf(f"wst{i}") for i in range(2)]
        wstS = [dsem(f"dwst{i}") for i in range(2)]
        wbf = [sb(f"wbf{i}", [128, KD * 256], BF16) for i in range(2)]
        wbfB = [Buf(f"wbf{i}") for i in range(2)]
        wbfB2 = [[Buf(f"wbf{i}a"), Buf(f"wbf{i}b")] for i in range(2)]
        hT = sb("hT", [128, NJ, TB], BF16)
        hB = [Buf(f"h{j}") for j in range(NJ)]
        sg = [sb(f"sg{i}", [128, TB]) for i in range(2)]
        sgB = [Buf(f"sg{i}") for i in range(2)]
        PS = [psum(f"ps{i}", [128, 512]) for i in range(8)]
        PSB = [Buf(f"ps{i}") for i in range(8)]
        xS = [dsem(f"dx{k}") for k in range(KD)]
        oS = [dsem(f"dout{k}") for k in range(KD)]
        cS = dsem("dcst")
        pS = [dsem(f"dpv{l}") for l in range(L)]
        muS = dsem("dmu")
        smS = dsem("dsm")
        counters = {'w': 0, 'o': 0, 'sq': 0, 'sg': 0, 'pp': 0}

        def OP(eng, fn, reads=(), writes=(), sig=True, dsem=None, rg=None):
            return pg.op(eng, fn, reads, writes, sig, dsem, rg)

        def load_w(src_ap, ncols):
            i = counters['w'] % 2
            counters['w'] += 1
            OP('sp', lambda e: e.dma_start(out=wst[i][:, 0:ncols], in_=src_ap), writes=[wstB[i]], dsem=wstS[i])
            return i

        def load_w2(src_ap2):
            i = counters['w'] % 2
            counters['w'] += 1
            OP('sp', lambda e: e.dma_start(out=wst[i][:, 0:2 * D].rearrange("p (j c) -> p j c", j=2), in_=src_ap2.rearrange("j p c -> p j c")),
               writes=[wstB[i]], dsem=wstS[i])
            return i

        def cast_w(i, ncols, eng='pool'):
            if eng == 'act':
                OP('act', lambda e: e.activation(out=wbf[i][:, 0:ncols], in_=wst[i][:, 0:ncols], func=AF.Copy),
                   reads=[wstB[i]], writes=[wbfB[i], wbfB2[i][0], wbfB2[i][1]])
            else:
                OP(eng, lambda e: e.tensor_copy(out=wbf[i][:, 0:ncols], in_=wst[i][:, 0:ncols]),
                   reads=[wstB[i]], writes=[wbfB[i], wbfB2[i][0], wbfB2[i][1]])

        def cast_w_split(i, ncols):
            c1 = (ncols * 7 // 8) // 128 * 128
            OP('dve', lambda e: e.tensor_copy(out=wbf[i][:, 0:c1], in_=wst[i][:, 0:c1]), reads=[wstB[i]], writes=[wbfB2[i][0], wbfB[i]])
            OP('pool', lambda e: e.tensor_copy(out=wbf[i][:, c1:ncols], in_=wst[i][:, c1:ncols]), reads=[wstB[i]], writes=[wbfB2[i][1]])

        def rstd_from(psb, psbuf, n, scale, eps, dst, dstB):
            OP('act', lambda e: e.activation(out=dst[:, 0:n], in_=psb[:, 0:n], func=AF.Ln, scale=scale, bias=eps),
               reads=[psbuf], writes=[dstB])
            OP('act', lambda e: e.activation(out=dst[:, 0:n], in_=dst[:, 0:n], func=AF.Exp, scale=-0.5),
               reads=[dstB], writes=[dstB])

        def rmsnorm_to_xn(l, gcol, c0, n, bank):
            ti = c0 // TB
            for k in range(KD):
                i = counters['sq'] % 2
                counters['sq'] += 1
                OP('act', lambda e, k=k, i=i: e.activation(out=sq[i][:, 0:n], in_=X[:, k, c0:c0 + n], func=AF.Square),
                   reads=[XB[k][ti]], writes=[sqB[i]])
                OP('pe', lambda e, k=k, i=i: e.matmul(PS[bank][:, 0:n], lhsT=ones[:], rhs=sq[i][:, 0:n], start=(k == 0), stop=(k == KD - 1)),
                   reads=[sqB[i], onesB], writes=[PSB[bank]])
            rstd_from(PS[bank], PSB[bank], n, 1.0 / D, NORM_EPS, rstd, rstdB)
            for k in range(KD):
                OP('dve', lambda e, k=k: e.scalar_tensor_tensor(out=xn[:, k, 0:n], in0=X[:, k, c0:c0 + n],
                                                                 scalar=pvec[l][:, gcol + k:gcol + k + 1], in1=rstd[:, 0:n],
                                                                 op0=ALU.mult, op1=ALU.mult),
                   reads=[XB[k][ti], rstdB, pvB[l]], writes=[xnB[k]])

        def ffn(l, w, ti):
            gcol = PV['nf1'] if w == 0 else PV['nf2']
            c0 = ti * TB
            blocks = [('in', j) for j in range(NJ)] + [('out', jp) for jp in range(NJ // 2)]

            slots = {}

            def do_load(t):
                if t < len(blocks) and t not in slots:
                    kind_, j_ = blocks[t]
                    if kind_ == 'in':
                        slots[t] = load_w(f_win[l][w][j_], KD * 256)
                    else:
                        slots[t] = load_w2(f_wout[l][w][2 * j_:2 * j_ + 2])

            def do_ready(t):
                if t >= len(blocks):
                    return
                do_load(t)
                cast_w_split(slots[t], KD * 256)
            do_load(0)
            do_load(1)
            do_ready(0)
            rmsnorm_to_xn(l, gcol, c0, TB, 0)
            for t, (kind, j) in enumerate(blocks):
                i = slots[t]
                do_ready(t + 1)
                do_load(t + 2)
                if kind == 'in':
                    wv = wbf[i][:].rearrange("p (k c) -> p k c", k=KD)
                    pp = counters['pp'] % 2
                    counters['pp'] += 1
                    bg, bu = 2 * pp, 2 * pp + 1
                    for half, bk in ((0, bg), (1, bu)):
                        for k in range(KD):
                            OP('pe', lambda e, k=k, half=half, bk=bk, wv=wv: e.matmul(
                                PS[bk][:, :], lhsT=wv[:, k, half * 128:(half + 1) * 128], rhs=xn[:, k, 0:TB],
                                start=(k == 0), stop=(k == KD - 1)),
                               reads=[wbfB[i], wbfB2[i][0], wbfB2[i][1], xnB[k]], writes=[PSB[bk]], sig=(k == KD - 1))
                    si = counters['sg'] % 2
                    counters['sg'] += 1
                    OP('act', lambda e, si=si, bg=bg: e.activation(out=sg[si][:], in_=PS[bg][:, :], func=AF.Silu),
                       reads=[PSB[bg]], writes=[sgB[si]])
                    OP('dve', lambda e, si=si, bu=bu, j=j: e.tensor_tensor(out=hT[:, j, :], in0=sg[si][:], in1=PS[bu][:, :], op=ALU.mult),
                       reads=[sgB[si], PSB[bu]], writes=[hB[j]])
                else:
                    for q in range(2):
                        jj = 2 * j + q
                        for m in range(KD):
                            OP('pe', lambda e, m=m, jj=jj, q=q, i=i: e.matmul(PS[m][:, :], lhsT=wbf[i][:, q * D + m * 128:q * D + (m + 1) * 128], rhs=hT[:, jj, :],
                                                                   start=(jj == 0), stop=(jj == NJ - 1)),
                               reads=[wbfB[i], wbfB2[i][0], wbfB2[i][1], hB[jj]], writes=[PSB[m]], sig=(m == KD - 1 or jj == NJ - 1))
            for m in range(KD):
                OP('dve', lambda e, m=m: e.scalar_tensor_tensor(out=X[:, m, c0:c0 + TB], in0=PS[m][:, :], scalar=0.5,
                                                                 in1=X[:, m, c0:c0 + TB], op0=ALU.mult, op1=ALU.add),
                   reads=[PSB[m], XB[m][ti]], writes=[XB[m][ti]])

        OP('sp', lambda e: e.dma_start(out=cst[:], in_=cst_d), writes=[cstB], dsem=cS)
        for l in range(L):
            OP('sp', lambda e, l=l: e.dma_start(out=pvec[l][:], in_=pvec_d[l]), writes=[pvB[l]], dsem=pS[l])
        OP('pool', lambda e: e.memset(ones[:], 1.0), writes=[onesB])
        bonesB = Buf("bones", const=True)
        OP('pool', lambda e: e.memset(bones[:], 0.0), writes=[bonesB])
        OP('pool', lambda e: e.memset(bones[0:64, 0:64], 1.0), writes=[bonesB])
        OP('pool', lambda e: e.memset(bones[64:128, 64:128], 1.0), writes=[bonesB])
        identB = Buf("ident", const=True)
        OP('pool', lambda e: e.memset(ident[:], 1.0), writes=[identB])
        OP('pool', lambda e: e.affine_select(out=ident[:], in_=ident[:], pattern=[[-1, 128]], compare_op=ALU.is_equal,
                                             fill=0.0, base=0, channel_multiplier=1), reads=[identB], writes=[identB])

        if do_mix:
            YT = sb("YT", [128, KD, TBM], BF16)
            YB = [Buf(f"Y{k}") for k in range(KD)]
            NFA = 10
            FA = [sb(f"fa{i}", [128, TBM]) for i in range(NFA)]
            FAB = [Buf(f"fa{i}") for i in range(NFA)]
            NBA = 4
            BA = [sb(f"ba{i}", [128, TBM], BF16) for i in range(NBA)]
            BAB = [Buf(f"ba{i}") for i in range(NBA)]
            LL = [[sb(f"ll{hp}{i}", [128, TBM]) for i in range(3)] for hp in range(2)]
            LLB_ = [[Buf(f"ll{hp}{i}") for i in range(3)] for hp in range(2)]
            LLX = [sb(f"llx{i}", [128, TBM]) for i in range(2)]
            LLXB = [Buf(f"llx{i}") for i in range(2)]
            LB = [[sb(f"lb{hp}{i}", [128, TBM], BF16) for i in range(5)] for hp in range(2)]
            LBB = [[Buf(f"lb{hp}{i}") for i in range(5)] for hp in range(2)]
            PB16 = [sb(f"pb16{i}", [128, TBM], BF16) for i in range(2)]
            PB16B = [Buf(f"pb16{i}") for i in range(2)]
            Vt = [sb(f"vt{c}", [64, 256], BF16) for c in range(NCH)]
            VtB = [Buf(f"vt{c}") for c in range(NCH)]
            KEt = [sb(f"ket{c}", [64, 256], BF16) for c in range(NCH)]
            KEtB = [Buf(f"ket{c}") for c in range(NCH)]
            AEt = [sb(f"aet{c}", [64, 256], BF16) for c in range(NCH)]
            AEtB = [Buf(f"aet{c}") for c in range(NCH)]
            alias_ctr = [0]

            def mk(name, n):
                ts, bs = [], []
                for c in range(n):
                    idx = alias_ctr[0]
                    alias_ctr[0] += 1
                    j, half = idx // 2, idx % 2
                    ts.append(hT[0:64, j, half * 256:(half + 1) * 256])
                    bs.append(hB[j])
                return ts, bs
            ATs, ATsB = mk("ats", NCH)
            LKs, LKsB = mk("lks", NCH)
            ARs, ARsB = mk("ars", NCH)
            Nn, NnB = mk("nn", NCH)
            NTn, NTnB = mk("ntn", NCH)
            Nn2, Nn2B = mk("nn2", NCH)
            NTn2, NTn2B = mk("ntn2", NCH)
            Pn, PnB = mk("pn", NCH)
            Pn2, Pn2B = mk("pn2", NCH)
            Ysb = sb("ysb", [64, 256], BF16); YsbB = Buf("ysb")
            Usb = sb("usb", [64, 256], BF16); UsbB = Buf("usb")
            S32 = {m: [sb(f"s32{m}{hp}", [128, 64]) for hp in range(2)] for m in ('hgrn', 'gla', 'rwkv')}
            Sbf = {m: [sb(f"sbf{m}{hp}", [128, 64], BF16) for hp in range(2)] for m in ('hgrn', 'gla', 'rwkv')}
            S32B = {m: [Buf(f"s32{m}{hp}") for hp in range(2)] for m in ('hgrn', 'gla', 'rwkv')}
            SbfB = {m: [Buf(f"sbf{m}{hp}") for hp in range(2)] for m in ('hgrn', 'gla', 'rwkv')}
            stmp = [sb(f"stmp{hp}", [128, 64]) for hp in range(2)]
            stmpB = [Buf(f"stmp{hp}") for hp in range(2)]
            pext = sb("pext", [128, 2, 16 + TBM]); pextB = Buf("pext")
            pw = [sb(f"pw{i}", [128, 2, 16 + TBM]) for i in range(2)]
            pwB = [Buf(f"pw{i}") for i in range(2)]
            PST = PS[7][:, :].bitcast(BF16)
            lbtB = Buf("lbt", const=True)
            fa_ctr = [0]
            ba_ctr = [0]

            def fa():
                i = fa_ctr[0] % NFA
                fa_ctr[0] += 1
                return FA[i], FAB[i]

            def ba():
                i = ba_ctr[0] % NBA
                ba_ctr[0] += 1
                return BA[i], BAB[i]

            def pbank():
                b = counters['pp'] % 4
                counters['pp'] += 1
                return b

            wplan = {'plan': [], 'pos': 0, 'issued': {}, 'l': 0}

            def issue_load(l, d):
                kind, g = d
                if kind == 'fm':
                    return load_w(m_fm[l][g], KD * 128)
                if kind == 'wo':
                    return load_w2(m_wout[l][2 * g:2 * g + 2])
                return load_w(m_tm[l][g], KD * 256)

            def issue_cast(l, d, i):
                kind, g = d
                if kind == 'wo':
                    cast_w_split(i, 2 * D)
                    return (i,)
                if kind == 'fm':
                    col0, ncols, shift = FMG[g]
                    if shift:
                        mc = col0 - RW0
                        mu_b = mub[:, mc:mc + 128].unsqueeze(1).broadcast_to([128, KD, 128])
                        omu_b = omub[:, mc:mc + 128].unsqueeze(1).broadcast_to([128, KD, 128])
                        wv32 = wst[i][:, 0:KD * 128].rearrange("p (k c) -> p k c", k=KD)
                        wbv = wbf[i][:].rearrange("p (k c) -> p k c", k=KD)
                        OP('dve', lambda e: e.tensor_tensor(out=wbv[:, :, 0:128], in0=wv32, in1=omu_b, op=ALU.mult),
                           reads=[wstB[i], omubB], writes=[wbfB2[i][0], wbfB[i]])
                        OP('pool', lambda e: e.tensor_tensor(out=wbv[:, :, 128:256], in0=wv32, in1=mu_b, op=ALU.mult),
                           reads=[wstB[i], mubB], writes=[wbfB2[i][1]])
                    else:
                        cast_w(i, KD * 128, 'dve')
                        wbv = wbf[i][:, 0:KD * 128].rearrange("p (k c) -> p k c", k=KD)
                    return (i, wbv)
                col0, ncols, shift = TMG[g]
                i2 = None
                wb2 = None
                if shift:
                    i2 = counters['w'] % 2
                    counters['w'] += 1
                    mc = col0 - RW0
                    mu_b = mub[:, mc:mc + 256].unsqueeze(1).broadcast_to([128, KD, 256])
                    omu_b = omub[:, mc:mc + 256].unsqueeze(1).broadcast_to([128, KD, 256])
                    wv32 = wst[i][:].rearrange("p (k c) -> p k c", k=KD)
                    wa = wbf[i][:].rearrange("p (k c) -> p k c", k=KD)
                    wb2 = wbf[i2][:].rearrange("p (k c) -> p k c", k=KD)
                    OP('dve', lambda e: e.tensor_tensor(out=wa, in0=wv32, in1=omu_b, op=ALU.mult),
                       reads=[wstB[i], omubB], writes=[wbfB[i], wbfB2[i][0], wbfB2[i][1]])
                    OP('pool', lambda e: e.tensor_tensor(out=wb2, in0=wv32, in1=mu_b, op=ALU.mult),
                       reads=[wstB[i], mubB], writes=[wbfB[i2], wbfB2[i2][0], wbfB2[i2][1]])
                else:
                    cast_w_split(i, KD * 256)
                    wa = wbf[i][:].rearrange("p (k c) -> p k c", k=KD)
                return (i, i2, wa, wb2)

            def two_slot(d):
                return d[0] == 'tm' and bool(TMG[d[1]][2])

            def ensure_load(l, pos):
                plan = wplan['plan']
                if pos < len(plan) and pos not in wplan['loaded'] and not two_slot(plan[pos]):
                    wplan['loaded'][pos] = issue_load(l, plan[pos])

            def ensure_cast(l, pos):
                plan = wplan['plan']
                if pos < len(plan) and pos not in wplan['issued'] and not two_slot(plan[pos]):
                    ensure_load(l, pos)
                    wplan['issued'][pos] = issue_cast(l, plan[pos], wplan['loaded'].pop(pos))

            def acq(l, d):
                pos = wplan['pos']
                plan = wplan['plan']
                assert plan[pos] == d, (plan[pos], d)
                if pos not in wplan['issued']:
                    if pos not in wplan['loaded']:
                        wplan['loaded'][pos] = issue_load(l, d)
                    wplan['issued'][pos] = issue_cast(l, d, wplan['loaded'].pop(pos))
                info = wplan['issued'].pop(pos)
                wplan['pos'] = pos + 1
                if not two_slot(d) and not (pos + 1 < len(plan) and two_slot(plan[pos + 1])):
                    ensure_cast(l, pos + 1)
                    if not (pos + 2 < len(plan) and two_slot(plan[pos + 2])):
                        ensure_load(l, pos + 2)
                return info

            def proj_fm(l, g, first_block):
                col0, ncols, shift = FMG[g]
                i, wbv = acq(l, ('fm', g))
                bk = pbank()
                nmm = KD * (2 if shift else 1)
                n = 0
                for k in range(KD):
                    n += 1
                    OP('pe', lambda e, k=k, n=n: e.matmul(PS[bk][0:ncols, 0:TBM], lhsT=wbv[:, k, 0:ncols], rhs=xn[:, k, 0:TBM],
                                                          start=(n == 1), stop=(n == nmm)),
                       reads=[wbfB[i], wbfB2[i][0], wbfB2[i][1], xnB[k]], writes=[PSB[bk]], sig=(n == nmm))
                if shift:
                    for k in range(KD):
                        n += 1
                        OP('pe', lambda e, k=k, n=n: e.matmul(PS[bk][0:ncols, 0:TBM], lhsT=wbv[:, k, 128:128 + ncols], rhs=xns[:, k, 0:TBM],
                                                              start=False, stop=(n == nmm)),
                           reads=[wbfB[i], wbfB2[i][0], wbfB2[i][1], xnsB], writes=[PSB[bk]], sig=(n == nmm))
                return bk

            def proj_tm(l, g):
                col0, ncols, shift = TMG[g]
                i, i2, wa, wb2 = acq(l, ('tm', g))
                for c in range(NCH):
                    bk = pbank()
                    nmm = KD * (2 if shift else 1)
                    n = 0
                    for k in range(KD):
                        n += 1
                        OP('pe', lambda e, k=k, n=n, c=c, bk=bk: e.matmul(PS[bk][0:64, 0:256], lhsT=xn[:, k, c * CH:(c + 1) * CH],
                                                                     rhs=wa[:, k, :], start=(n == 1), stop=(n == nmm)),
                           reads=[wbfB[i], wbfB2[i][0], wbfB2[i][1], xnB[k]], writes=[PSB[bk]], sig=(n == nmm))
                    if shift:
                        for k in range(KD):
                            n += 1
                            OP('pe', lambda e, k=k, n=n, c=c, bk=bk: e.matmul(PS[bk][0:64, 0:256], lhsT=xns[:, k, c * CH:(c + 1) * CH],
                                                                         rhs=wb2[:, k, :], start=False, stop=(n == nmm)),
                               reads=[wbfB[i2], wbfB2[i2][0], wbfB2[i2][1], xnsB], writes=[PSB[bk]], sig=(n == nmm))
                    OP('act', lambda e, c=c, bk=bk: e.activation(out=Vt[c][:], in_=PS[bk][0:64, 0:256], func=AF.Copy),
                       reads=[PSB[bk]], writes=[VtB[c]])

            def scan_decay(g_t, g_b):
                b_t, b_b = fa()
                OP('dve', lambda e: e.tensor_tensor_scan(out=b_t[:], data0=cst[:, SCM:SCM + TBM], data1=g_t[:], initial=0.0,
                                                         op0=ALU.mult, op1=ALU.add), reads=[g_b, cstB], writes=[b_b])
                return b_t, b_b

            def chunk_engine(mname, QE, KE, PC, rw=None):
                isrw = rw is not None
                if DEBUG_STAGE < 1:
                    return
                for c in range(NCH):
                    for (src, dst, dstB_) in ([(KE, KEt, KEtB)] + ([(rw['AE'], AEt, AEtB)] if isrw else [])):
                        for hp in range(2):
                            OP('pe', lambda e, hp=hp, c=c, src=src: e.transpose(PST[0:64, hp * 128:(hp + 1) * 128],
                                                                             src[hp][0][:, c * CH:(c + 1) * CH], ident[:]),
                               reads=[src[hp][1], identB], writes=[PSB[7]])
                        OP('act', lambda e, c=c, dst=dst: e.activation(out=dst[c][:], in_=PST[0:64, 0:256], func=AF.Copy),
                           reads=[PSB[7]], writes=[dstB_[c]])
                if DEBUG_STAGE < 2:
                    return
                for c in range(NCH):
                    cs = slice(c * CH, (c + 1) * CH)
                    def sc(lh, rh, dst_ps, cs=cs):
                        for h in range(4):
                            hp, r = h // 2, (h % 2) * 64
                            OP('pe', lambda e, h=h, hp=hp, r=r: e.matmul(dst_ps[0:64, h * 64:(h + 1) * 64], lhsT=lh[hp][0][r:r + 64, cs],
                                                                         rhs=rh[hp][0][r:r + 64, cs], start=True, stop=True),
                               reads=[lh[hp][1], rh[hp][1]], writes=[PSB[6]], rg=r)
                    sc(KE, QE, PS[6][:, 0:256])
                    OP('dve', lambda e, c=c: e.tensor_tensor(out=ATs[c][:], in0=PS[6][0:64, 0:256], in1=cst[0:64, MI:MI + 256], op=ALU.mult),
                       reads=[PSB[6], cstB], writes=[ATsB[c]])
                    if isrw:
                        sc(KE, rw['BE'], PS[6][:, 256:512])
                        OP('dve', lambda e, c=c: e.tensor_tensor(out=LKs[c][:], in0=PS[6][0:64, 256:512], in1=cst[0:64, MSU:MSU + 256], op=ALU.mult),
                           reads=[PSB[6], cstB], writes=[LKsB[c]])
                        sc(rw['AE'], QE, PS[6][:, 0:256])
                        OP('dve', lambda e, c=c: e.tensor_tensor(out=ARs[c][:], in0=PS[6][0:64, 0:256], in1=cst[0:64, MI:MI + 256], op=ALU.mult),
                           reads=[PSB[6], cstB], writes=[ARsB[c]])
                        sc(rw['AE'], rw['BE'], PS[6][:, 256:512])
                        OP('dve', lambda e, c=c: e.scalar_tensor_tensor(out=NTn[c][:], in0=PS[6][0:64, 256:512], scalar=-1.0,
                                                                         in1=cst[0:64, MSU:MSU + 256], op0=ALU.mult, op1=ALU.mult),
                           reads=[PSB[6], cstB], writes=[NTnB[c]])
                        sc(rw['BE'], rw['AE'], PS[6][:, 0:256])
                        OP('dve', lambda e, c=c: e.scalar_tensor_tensor(out=Nn[c][:], in0=PS[6][0:64, 0:256], scalar=-1.0,
                                                                         in1=cst[0:64, MSL:MSL + 256], op0=ALU.mult, op1=ALU.mult),
                           reads=[PSB[6], cstB], writes=[NnB[c]])
                        OP('pool', lambda e, c=c: e.tensor_tensor(out=Pn[c][:], in0=NTn[c][:], in1=cst[0:64, ID4:ID4 + 256], op=ALU.add),
                           reads=[NTnB[c], cstB], writes=[PnB[c]])
                if isrw:
                    curN, curNB, curNT, curNTB = Nn, NnB, NTn, NTnB
                    nxtN, nxtNB, nxtNT, nxtNTB = Nn2, Nn2B, NTn2, NTn2B
                    curP, curPB, nxtP, nxtPB = Pn, PnB, Pn2, Pn2B
                    for lev in range(1, 6):
                        for c in range(NCH):
                            bk = pbank()
                            for h in range(4):
                                hs = slice(h * 64, (h + 1) * 64)
                                OP('pe', lambda e, c=c, hs=hs, bk=bk, a=curNT, b=curN: e.matmul(PS[bk][0:64, hs], lhsT=a[c][:, hs], rhs=b[c][:, hs],
                                                                                         start=True, stop=True),
                                   reads=[curNTB[c], curNB[c]], writes=[PSB[bk]], rg=0)
                            if lev < 5:
                                for h in range(4):
                                    hs = slice(h * 64, (h + 1) * 64)
                                    hs2 = slice(256 + h * 64, 256 + (h + 1) * 64)
                                    OP('pe', lambda e, c=c, hs=hs, hs2=hs2, bk=bk, a=curN, b=curNT: e.matmul(PS[bk][0:64, hs2], lhsT=a[c][:, hs], rhs=b[c][:, hs],
                                                                                                     start=True, stop=True),
                                       reads=[curNTB[c], curNB[c]], writes=[PSB[bk]], rg=0)
                                OP('act', lambda e, c=c, bk=bk, d=nxtNT: e.activation(out=d[c][:], in_=PS[bk][0:64, 256:512], func=AF.Copy),
                                   reads=[PSB[bk]], writes=[nxtNTB[c]])
                            OP('act', lambda e, c=c, bk=bk, d=nxtN: e.activation(out=d[c][:], in_=PS[bk][0:64, 0:256], func=AF.Copy),
                               reads=[PSB[bk]], writes=[nxtNB[c]])
                        curN, curNB, nxtN, nxtNB = nxtN, nxtNB, curN, curNB
                        curNT, curNTB, nxtNT, nxtNTB = nxtNT, nxtNTB, curNT, curNTB
                        for c in range(NCH):
                            bk = pbank()
                            for h in range(4):
                                hs = slice(h * 64, (h + 1) * 64)
                                OP('pe', lambda e, c=c, hs=hs, bk=bk, a=curN, b=curP: e.matmul(PS[bk][0:64, hs], lhsT=a[c][:, hs], rhs=b[c][:, hs],
                                                                                        start=True, stop=True),
                                   reads=[curNB[c], curPB[c]], writes=[PSB[bk]], rg=0)
                            OP('dve', lambda e, c=c, bk=bk, s=curP, d=nxtP: e.tensor_tensor(out=d[c][:], in0=PS[bk][0:64, 0:256], in1=s[c][:], op=ALU.add),
                               reads=[PSB[bk], curPB[c]], writes=[nxtPB[c]])
                        curP, curPB, nxtP, nxtPB = nxtP, nxtPB, curP, curPB
                    TT, TTB = curP, curPB
                if DEBUG_STAGE < 3:
                    return
                S3, Sb, S3B, SbB = S32[mname], Sbf[mname], S32B[mname], SbfB[mname]
                for c in range(NCH):
                    cs = slice(c * CH, (c + 1) * CH)
                    if isrw:
                        for h in range(4):
                            hp, r = h // 2, (h % 2) * 64
                            hs = slice(h * 64, (h + 1) * 64)
                            OP('pe', lambda e, hp=hp, r=r, hs=hs, cs=cs: e.matmul(PS[6][0:64, hs], lhsT=rw['BE'][hp][0][r:r + 64, cs], rhs=Sb[hp][r:r + 64, :],
                                                                         start=True, stop=False),
                               reads=[rw['BE'][hp][1], SbB[hp]], writes=[PSB[6]], rg=r)
                            OP('pe', lambda e, hs=hs, c=c: e.matmul(PS[6][0:64, hs], lhsT=LKs[c][:, hs], rhs=Vt[c][:, hs], start=False, stop=True),
                               reads=[LKsB[c], VtB[c]], writes=[PSB[6]], rg=0)
                        OP('act', lambda e: e.activation(out=Ysb[:], in_=PS[6][0:64, 0:256], func=AF.Copy), reads=[PSB[6]], writes=[YsbB])
                        for h in range(4):
                            hs = slice(h * 64, (h + 1) * 64)
                            hs2 = slice(256 + h * 64, 256 + (h + 1) * 64)
                            OP('pe', lambda e, hs=hs, hs2=hs2, c=c: e.matmul(PS[6][0:64, hs2], lhsT=TT[c][:, hs], rhs=Ysb[:, hs], start=True, stop=True),
                               reads=[TTB[c], YsbB], writes=[PSB[6]], rg=0)
                        OP('act', lambda e: e.activation(out=Usb[:], in_=PS[6][0:64, 256:512], func=AF.Copy, scale=-1.0),
                           reads=[PSB[6]], writes=[UsbB])
                    for h in range(4):
                        hp, r = h // 2, (h % 2) * 64
                        hs = slice(h * 64, (h + 1) * 64)
                        ob = PS[4 + hp][r:r + 64, cs]
                        OP('pe', lambda e, ob=ob, hs=hs, c=c: e.matmul(ob, lhsT=Vt[c][:, hs], rhs=ATs[c][:, hs], start=True, stop=False),
                           reads=[VtB[c], ATsB[c]], writes=[PSB[4 + hp]], rg=0)
                        if isrw:
                            OP('pe', lambda e, ob=ob, hs=hs, c=c: e.matmul(ob, lhsT=Usb[:, hs], rhs=ARs[c][:, hs], start=False, stop=False),
                               reads=[UsbB, ARsB[c]], writes=[PSB[4 + hp]], rg=0)
                        OP('pe', lambda e, ob=ob, hp=hp, r=r, cs=cs: e.matmul(ob, lhsT=Sb[hp][r:r + 64, :], rhs=QE[hp][0][r:r + 64, cs], start=False, stop=True),
                           reads=[SbB[hp], QE[hp][1]], writes=[PSB[4 + hp]], rg=r)
                    for h in range(4):
                        hp, r = h // 2, (h % 2) * 64
                        hs = slice(h * 64, (h + 1) * 64)
                        sp_ = PS[7][r:r + 64, 256 + hp * 64:256 + (hp + 1) * 64]
                        OP('pe', lambda e, sp_=sp_, hs=hs, c=c: e.matmul(sp_, lhsT=KEt[c][:, hs], rhs=Vt[c][:, hs], start=True, stop=(not isrw)),
                           reads=[KEtB[c], VtB[c]], writes=[PSB[7]], rg=0)
                        if isrw:
                            OP('pe', lambda e, sp_=sp_, hs=hs, c=c: e.matmul(sp_, lhsT=AEt[c][:, hs], rhs=Usb[:, hs], start=False, stop=True),
                               reads=[AEtB[c], UsbB], writes=[PSB[7]], rg=0)
                    for hp in range(2):
                        pc = PC[hp][0][:, (c + 1) * CH - 1:(c + 1) * CH]
                        OP('dve', lambda e, hp=hp: e.tensor_tensor(out=stmp[hp][:], in0=PS[7][:, 256 + hp * 64:256 + (hp + 1) * 64], in1=S3[hp][:], op=ALU.add),
                           reads=[PSB[7], S3B[hp]], writes=[stmpB[hp]])
                        OP('dve', lambda e, hp=hp, pc=pc: e.tensor_scalar(out=S3[hp][:], in0=stmp[hp][:], scalar1=pc, scalar2=None, op0=ALU.mult),
                           reads=[stmpB[hp], PC[hp][1]], writes=[S3B[hp]])
                        OP('dve', lambda e, hp=hp, pc=pc: e.tensor_scalar(out=Sb[hp][:], in0=stmp[hp][:], scalar1=pc, scalar2=None, op0=ALU.mult),
                           reads=[stmpB[hp], PC[hp][1]], writes=[SbB[hp]])

            def evac_fm(bk, func=AF.Copy, scale=1.0, bias=None, dt='f', rows=128, dst=None):
                t, b = dst if dst is not None else (fa() if dt == 'f' else ba())
                kw = {}
                if bias is not None:
                    kw['bias'] = bias
                OP('act', lambda e: e.activation(out=t[0:rows, :], in_=PS[bk][0:rows, 0:TBM], func=func, scale=scale, **kw),
                   reads=[PSB[bk]] + ([pvB[0]] if bias is not None else []), writes=[b])
                return t, b

            def mixer_block(l, s, bi):
                first = (bi == 0)
                c0 = bi * TBM
                ti = c0 // TB
                plan = []
                if 'pool' in mixers:
                    plan += [('fm', 0), ('fm', 1)]
                if 'hgrn' in mixers:
                    for hp_ in range(2):
                        plan += [('fm', 2 + hp_), ('fm', 4 + hp_), ('fm', 6 + hp_)]
                    plan += [('tm', 0)]
                if 'gla' in mixers:
                    plan += [('fm', 22)]
                    for hp_ in range(2):
                        plan += [('fm', 16 + hp_), ('fm', 18 + hp_), ('fm', 20 + hp_)]
                    plan += [('tm', 2)]
                if 'rwkv' in mixers:
                    plan += [('fm', 14), ('fm', 15)]
                    for hp_ in range(2):
                        plan += [('fm', 8 + hp_), ('fm', 10 + hp_), ('fm', 12 + hp_)]
                    plan += [('tm', 1)]
                plan += [('wo', jp) for jp in range(KD // 2)]
                wplan['plan'] = plan
                wplan['pos'] = 0
                wplan['issued'] = {}
                wplan['loaded'] = {}
                if plan:
                    ensure_load(l, 0)
                    ensure_load(l, 1)
                    ensure_cast(l, 0)
                pv = pvec[l]
                if 'rwkv' in mixers:
                    if first:
                        OP('pool', lambda e: e.memset(xns[:, :, 0:2], 0.0), writes=[xnsB])
                    else:
                        OP('pool', lambda e: e.tensor_copy(out=xns[:, :, 0:1], in_=xn[:, :, TBM - 1:TBM]), reads=xnB, writes=[xnsB])
                rmsnorm_to_xn(l, PV['nmx'], c0, TBM, 0)
                if 'rwkv' in mixers:
                    OP('pool', lambda e: e.tensor_copy(out=xns[:, :, 1:TBM], in_=xn[:, :, 0:TBM - 1]), reads=xnB, writes=[xnsB])
                if 'pool' in mixers:
                    if first:
                        OP('pool', lambda e: e.memset(pext[:, :, 0:16], 0.0), writes=[pextB])
                    else:
                        OP('pool', lambda e: e.tensor_copy(out=pext[:, :, 0:16], in_=pext[:, :, TBM:TBM + 16]), reads=[pextB], writes=[pextB])
                    for ck in range(2):
                        bk = proj_fm(l, ck, first)
                        OP('act', lambda e, ck=ck, bk=bk: e.activation(out=pext[:, ck, 16:16 + TBM], in_=PS[bk][:, 0:TBM], func=AF.Copy),
                           reads=[PSB[bk]], writes=[pextB])
                    W_ = 16 + TBM
                    OP('dve', lambda e: e.tensor_tensor(out=pw[0][:, :, 1:W_], in0=pext[:, :, 1:W_], in1=pext[:, :, 0:W_ - 1], op=ALU.add),
                       reads=[pextB], writes=[pwB[0]])
                    def poolfin(src, ck, r0, wdw, first=first):
                        yt, yb = PB16[ck], PB16B[ck]
                        OP('dve', lambda e: e.scalar_tensor_tensor(out=yt[r0:r0 + 64, :], in0=src[r0:r0 + 64, ck, 16:16 + TBM], scalar=1.0 / wdw,
                                                                   in1=pext[r0:r0 + 64, ck, 16:16 + TBM], op0=ALU.mult, op1=ALU.subtract),
                           reads=[pwB[0], pwB[1], pextB], writes=[yb])
                        if first:
                            t2, b2 = FA[0], FAB[0]
                            OP('dve', lambda e: e.tensor_tensor(out=t2[r0:r0 + 64, 0:16], in0=src[r0:r0 + 64, ck, 16:32],
                                                                in1=cst[r0:r0 + 64, ICN + ck * 16:ICN + ck * 16 + 16], op=ALU.mult),
                               reads=[pwB[0], pwB[1], cstB], writes=[b2])
                            OP('dve', lambda e: e.tensor_tensor(out=yt[r0:r0 + 64, 0:16], in0=t2[r0:r0 + 64, 0:16],
                                                                in1=pext[r0:r0 + 64, ck, 16:32], op=ALU.subtract),
                               reads=[b2, pextB], writes=[yb])
                    poolfin(pw[0], 0, 0, 2)
                    OP('dve', lambda e: e.tensor_tensor(out=pw[1][:, :, 3:W_], in0=pw[0][:, :, 3:W_], in1=pw[0][:, :, 1:W_ - 2], op=ALU.add),
                       reads=[pwB[0]], writes=[pwB[1]])
                    poolfin(pw[1], 0, 64, 4)
                    OP('dve', lambda e: e.tensor_tensor(out=pw[0][:, :, 7:W_], in0=pw[1][:, :, 7:W_], in1=pw[1][:, :, 3:W_ - 4], op=ALU.add),
                       reads=[pwB[1]], writes=[pwB[0]])
                    poolfin(pw[0], 1, 0, 8)
                    OP('dve', lambda e: e.tensor_tensor(out=pw[1][:, :, 15:W_], in0=pw[0][:, :, 15:W_], in1=pw[0][:, :, 7:W_ - 8], op=ALU.add),
                       reads=[pwB[0]], writes=[pwB[1]])
                    poolfin(pw[1], 1, 64, 16)
                    for ck in range(2):
                        bk = pbank()
                        OP('pe', lambda e, ck=ck, bk=bk: e.matmul(PS[bk][:, 0:TBM], lhsT=smatb[:, ck * 128:(ck + 1) * 128], rhs=PB16[ck][:], start=True, stop=True),
                           reads=[PB16B[ck], smatB], writes=[PSB[bk]])
                        OP('dve', lambda e, ck=ck, bk=bk: e.tensor_scalar(out=YT[:, ck, :], in0=PS[bk][:, 0:TBM], scalar1=pv[:, PV['pool_b'] + ck:PV['pool_b'] + ck + 1],
                                                                      scalar2=pv[:, PV['pool_s'] + ck:PV['pool_s'] + ck + 1], op0=ALU.add, op1=ALU.mult),
                           reads=[PSB[bk], pvB[l]], writes=[YB[ck]])
                else:
                    for ck in range(2):
                        OP('pool', lambda e, ck=ck: e.memset(YT[:, ck, :], 0.0), writes=[YB[ck]])

                def out_rstd(Ot, lhs_ones, n_ch, eps):
                    bk = pbank()
                    for hp in range(2):
                        st, sbb = ba()
                        OP('act', lambda e, hp=hp, st=st, Ot=Ot: e.activation(out=st[:], in_=Ot[hp][0][:], func=AF.Square), reads=[Ot[hp][1]], writes=[sbb])
                        if lhs_ones is ones:
                            OP('pe', lambda e, hp=hp, st=st: e.matmul(PS[bk][:, 0:TBM], lhsT=ones[:], rhs=st[:], start=(hp == 0), stop=(hp == 1)),
                               reads=[sbb, onesB], writes=[PSB[bk]])
                        else:
                            bk2 = bk if hp == 0 else pbank()
                            OP('pe', lambda e, hp=hp, st=st, bk2=bk2: e.matmul(PS[bk2][:, 0:TBM], lhsT=bones[:], rhs=st[:], start=True, stop=True),
                               reads=[sbb, bonesB], writes=[PSB[bk2]])
                            if hp == 0:
                                bk0 = bk2
                            else:
                                bk1 = bk2
                    if lhs_ones is ones:
                        rt, rb = fa()
                        rstd_from(PS[bk], PSB[bk], TBM, 1.0 / n_ch, eps, rt, rb)
                        return [(rt, rb), (rt, rb)]
                    res = []
                    for bkx in (bk0, bk1):
                        rt, rb = fa()
                        rstd_from(PS[bkx], PSB[bkx], TBM, 1.0 / n_ch, eps, rt, rb)
                        res.append((rt, rb))
                    return res

                def evac_O():
                    Ot = []
                    for hp in range(2):
                        t, b = fa()
                        OP('act', lambda e, hp=hp, t=t: e.activation(out=t[:], in_=PS[4 + hp][:, 0:TBM], func=AF.Copy), reads=[PSB[4 + hp]], writes=[b])
                        Ot.append((t, b))
                    return Ot

                if 'hgrn' in mixers:
                    QE, KE, PCx, GT = [], [], [], []
                    for hp in range(2):
                        bq = proj_fm(l, 2 + hp, first)
                        qt, qb = evac_fm(bq, AF.Silu)
                        bf_ = proj_fm(l, 4 + hp, first)
                        st_, sb_ = evac_fm(bf_, AF.Sigmoid)
                        ft, fb = fa()
                        OP('dve', lambda e, hp=hp, st_=st_, ft=ft: e.tensor_scalar(out=ft[:], in0=st_[:], scalar1=lbt[:, 2 * l + hp:2 * l + hp + 1],
                                                                             scalar2=lbt[:, 4 + 2 * l + hp:4 + 2 * l + hp + 1], op0=ALU.mult, op1=ALU.add),
                           reads=[sb_, lbtB], writes=[fb])
                        lt, lb_ = fa()
                        OP('dve', lambda e, ft=ft, lt=lt: e.tensor_scalar_max(out=lt[:], in0=ft[:], scalar1=1e-30), reads=[fb], writes=[lb_])
                        OP('act', lambda e, lt=lt: e.activation(out=lt[:], in_=lt[:], func=AF.Ln), reads=[lb_], writes=[lb_])
                        bt, bb = scan_decay(lt, lb_)
                        ebt, ebb = LL[hp][0], LLB_[hp][0]
                        OP('act', lambda e, bt=bt, ebt=ebt: e.activation(out=ebt[:], in_=bt[:], func=AF.Exp), reads=[bb], writes=[ebb])
                        OP('act', lambda e, bt=bt: e.activation(out=bt[:], in_=bt[:], func=AF.Exp, scale=-1.0), reads=[bb], writes=[bb])
                        qe, qeb = LB[hp][0], LBB[hp][0]
                        OP('dve', lambda e, qt=qt, ebt=ebt, qe=qe: e.scalar_tensor_tensor(out=qe[:], in0=qt[:], scalar=QK, in1=ebt[:], op0=ALU.mult, op1=ALU.mult),
                           reads=[qb, ebb], writes=[qeb])
                        OP('dve', lambda e, ft=ft: e.tensor_scalar(out=ft[:], in0=ft[:], scalar1=-1.0, scalar2=1.0, op0=ALU.mult, op1=ALU.add),
                           reads=[fb], writes=[fb])
                        ke, keb = LB[hp][1], LBB[hp][1]
                        OP('dve', lambda e, ft=ft, bt=bt, ke=ke: e.tensor_tensor(out=ke[:], in0=ft[:], in1=bt[:], op=ALU.mult), reads=[fb, bb], writes=[keb])
                        bg_ = proj_fm(l, 6 + hp, first)
                        gt, gb = evac_fm(bg_, AF.Sigmoid, dst=(LL[hp][1], LLB_[hp][1]))
                        QE.append((qe, qeb)); KE.append((ke, keb)); PCx.append((ebt, ebb)); GT.append((gt, gb))
                    proj_tm(l, 0)
                    chunk_engine('hgrn', QE, KE, PCx)
                    Ot = evac_O()
                    rs = out_rstd(Ot, ones, 256, NORM_EPS)
                    for hp in range(2):
                        t1, b1 = fa()
                        OP('dve', lambda e, hp=hp, t1=t1, Ot=Ot, rs=rs: e.scalar_tensor_tensor(out=t1[:], in0=Ot[hp][0][:], scalar=pv[:, PV['hnorm'] + hp:PV['hnorm'] + hp + 1],
                                                                           in1=rs[hp][0][:], op0=ALU.mult, op1=ALU.mult),
                           reads=[Ot[hp][1], rs[hp][1], pvB[l]], writes=[b1])
                        OP('dve', lambda e, hp=hp, t1=t1, GT=GT: e.tensor_tensor(out=YT[:, 2 + hp, :], in0=t1[:], in1=GT[hp][0][:], op=ALU.mult),
                           reads=[b1, GT[hp][1]], writes=[YB[2 + hp]])
                else:
                    for ck in (2, 3):
                        OP('pool', lambda e, ck=ck: e.memset(YT[:, ck, :], 0.0), writes=[YB[ck]])

                if 'gla' in mixers:
                    bga = proj_fm(l, 22, first)
                    gat, gab = LLX[0], LLXB[0]
                    OP('act', lambda e: e.activation(out=gat[:], in_=PS[bga][:, 0:TBM], func=AF.Copy), reads=[PSB[bga]], writes=[gab])
                    QE, KE, PCx, GT = [], [], [], []
                    for hp in range(2):
                        bk = pbank()
                        OP('pe', lambda e, hp=hp, bk=bk: e.matmul(PS[bk][:, 0:TBM], lhsT=smat[:, 1024 + hp * 128:1024 + (hp + 1) * 128], rhs=gat[:], start=True, stop=True),
                           reads=[gab, smatB], writes=[PSB[bk]])
                        lt, lb_ = evac_fm(bk, AF.Sigmoid, bias=pv[:, PV['glab'] + hp:PV['glab'] + hp + 1])
                        OP('act', lambda e, lt=lt: e.activation(out=lt[:], in_=lt[:], func=AF.Ln), reads=[lb_], writes=[lb_])
                        bt, bb = scan_decay(lt, lb_)
                        ebt, ebb = LL[hp][0], LLB_[hp][0]
                        OP('act', lambda e, bt=bt, ebt=ebt: e.activation(out=ebt[:], in_=bt[:], func=AF.Exp, scale=1.0 / 16), reads=[bb], writes=[ebb])
                        OP('act', lambda e, bt=bt: e.activation(out=bt[:], in_=bt[:], func=AF.Exp, scale=-1.0 / 16), reads=[bb], writes=[bb])
                        bq = proj_fm(l, 16 + hp, first)
                        qe, qeb = LB[hp][0], LBB[hp][0]
                        OP('dve', lambda e, bq=bq, ebt=ebt, qe=qe: e.scalar_tensor_tensor(out=qe[:], in0=PS[bq][:, 0:TBM], scalar=QK, in1=ebt[:], op0=ALU.mult, op1=ALU.mult),
                           reads=[PSB[bq], ebb], writes=[qeb])
                        bkk = proj_fm(l, 18 + hp, first)
                        ke, keb = LB[hp][1], LBB[hp][1]
                        OP('dve', lambda e, bkk=bkk, bt=bt, ke=ke: e.tensor_tensor(out=ke[:], in0=PS[bkk][:, 0:TBM], in1=bt[:], op=ALU.mult), reads=[PSB[bkk], bb], writes=[keb])
                        bg_ = proj_fm(l, 20 + hp, first)
                        gt, gb = evac_fm(bg_, AF.Silu, dst=(LL[hp][1], LLB_[hp][1]))
                        QE.append((qe, qeb)); KE.append((ke, keb)); PCx.append((ebt, ebb)); GT.append((gt, gb))
                    proj_tm(l, 2)
                    chunk_engine('gla', QE, KE, PCx)
                    Ot = evac_O()
                    rs = out_rstd(Ot, bones, 64, NORM_EPS)
                    for hp in range(2):
                        t1, b1 = fa()
                        OP('dve', lambda e, hp=hp, t1=t1, Ot=Ot, rs=rs: e.scalar_tensor_tensor(out=t1[:], in0=Ot[hp][0][:], scalar=pv[:, PV['gnorm'] + hp:PV['gnorm'] + hp + 1],
                                                                           in1=rs[hp][0][:], op0=ALU.mult, op1=ALU.mult),
                           reads=[Ot[hp][1], rs[hp][1], pvB[l]], writes=[b1])
                        OP('dve', lambda e, hp=hp, t1=t1, GT=GT: e.tensor_tensor(out=YT[:, 6 + hp, :], in0=t1[:], in1=GT[hp][0][:], op=ALU.mult),
                           reads=[b1, GT[hp][1]], writes=[YB[6 + hp]])
                else:
                    for ck in (6, 7):
                        OP('pool', lambda e, ck=ck: e.memset(YT[:, ck, :], 0.0), writes=[YB[ck]])

                if 'rwkv' in mixers:
                    bwa = proj_fm(l, 14, first)
                    twa, twab = LLX[0], LLXB[0]
                    OP('act', lambda e: e.activation(out=twa[0:64, :], in_=PS[bwa][0:64, 0:TBM], func=AF.Tanh), reads=[PSB[bwa]], writes=[twab])
                    OP('act', lambda e: e.activation(out=twa[64:128, :], in_=PS[bwa][64:128, 0:TBM], func=AF.Copy), reads=[PSB[bwa]], writes=[twab])
                    bxg = proj_fm(l, 15, first)
                    sxg, sxgb = evac_fm(bxg, AF.Sigmoid, dst=(LLX[1], LLXB[1]))
                    QE, KE, AE, BE, PCx, GR, RKR, VF = [], [], [], [], [], [], [], []
                    for hp in range(2):
                        cs_ = slice(hp * 128, (hp + 1) * 128)
                        bk = pbank()
                        OP('pe', lambda e, bk=bk, hp=hp: e.matmul(PS[bk][:, 0:TBM], lhsT=smat[:, 256 + hp * 128:256 + (hp + 1) * 128], rhs=twa[:], start=True, stop=True),
                           reads=[twab, smatB], writes=[PSB[bk]])
                        lw, lwb = evac_fm(bk, AF.Sigmoid, bias=pv[:, PV['w0'] + hp:PV['w0'] + hp + 1])
                        bk = pbank()
                        OP('pe', lambda e, bk=bk, hp=hp: e.matmul(PS[bk][:, 0:TBM], lhsT=smat[:, 768 + hp * 128:768 + (hp + 1) * 128], rhs=twa[:], start=True, stop=True),
                           reads=[twab, smatB], writes=[PSB[bk]])
                        at, ab_ = evac_fm(bk, AF.Sigmoid, bias=pv[:, PV['a0'] + hp:PV['a0'] + hp + 1])
                        bk = pbank()
                        OP('pe', lambda e, bk=bk, hp=hp: e.matmul(PS[bk][:, 0:TBM], lhsT=smat[:, 512 + hp * 128:512 + (hp + 1) * 128], rhs=sxg[:], start=True, stop=True),
                           reads=[sxgb, smatB], writes=[PSB[bk]])
                        grt, grb = evac_fm(bk, AF.Copy, dst=(LL[hp][1], LLB_[hp][1]))
                        bt, bb = scan_decay(lw, lwb)
                        CW = -float(np.exp(-0.5))
                        ebt, ebb = LL[hp][0], LLB_[hp][0]
                        OP('act', lambda e, bt=bt, ebt=ebt: e.activation(out=ebt[:], in_=bt[:], func=AF.Exp, scale=CW), reads=[bb], writes=[ebb])
                        enb, enbb = fa()
                        OP('act', lambda e, bt=bt, enb=enb: e.activation(out=enb[:], in_=bt[:], func=AF.Exp, scale=-CW), reads=[bb], writes=[enbb])
                        OP('dve', lambda e, bt=bt, lw=lw: e.tensor_tensor(out=bt[:], in0=bt[:], in1=lw[:], op=ALU.subtract), reads=[bb, lwb], writes=[bb])
                        OP('act', lambda e, bt=bt: e.activation(out=bt[:], in_=bt[:], func=AF.Exp, scale=CW), reads=[bb], writes=[bb])
                        br = proj_fm(l, 8 + hp, first)
                        rt, rb = evac_fm(br, AF.Copy)
                        bkr = proj_fm(l, 10 + hp, first)
                        kt, kb = evac_fm(bkr, AF.Copy)
                        bv = proj_fm(l, 12 + hp, first)
                        vt, vb = evac_fm(bv, AF.Copy, dst=(LL[hp][2], LLB_[hp][2]))
                        kkt, kkb = fa()
                        OP('dve', lambda e, kt=kt, kkt=kkt, hp=hp: e.tensor_scalar(out=kkt[:], in0=kt[:], scalar1=pv[:, PV['kk'] + hp:PV['kk'] + hp + 1], scalar2=None, op0=ALU.mult),
                           reads=[kb, pvB[l]], writes=[kkb])
                        sq_, sqb_ = ba()
                        OP('act', lambda e, kkt=kkt, sq_=sq_: e.activation(out=sq_[:], in_=kkt[:], func=AF.Square), reads=[kkb], writes=[sqb_])
                        bk = pbank()
                        OP('pe', lambda e, bk=bk, sq_=sq_: e.matmul(PS[bk][:, 0:TBM], lhsT=bones[:], rhs=sq_[:], start=True, stop=True), reads=[sqb_, bonesB], writes=[PSB[bk]])
                        rn, rnb = fa()
                        rstd_from(PS[bk], PSB[bk], TBM, 1.0, 1e-24, rn, rnb)
                        OP('dve', lambda e, kkt=kkt, rn=rn: e.tensor_tensor(out=kkt[:], in0=kkt[:], in1=rn[:], op=ALU.mult), reads=[kkb, rnb], writes=[kkb])
                        fac, facb = fa()
                        OP('dve', lambda e, at=at, fac=fac, hp=hp: e.tensor_scalar(out=fac[:], in0=at[:], scalar1=-1.0, scalar2=pv[:, PV['ka'] + hp:PV['ka'] + hp + 1], op0=ALU.add, op1=ALU.mult),
                           reads=[ab_, pvB[l]], writes=[facb])
                        OP('dve', lambda e, fac=fac, kt=kt: e.scalar_tensor_tensor(out=kt[:], in0=fac[:], scalar=1.0, in1=kt[:], op0=ALU.add, op1=ALU.mult),
                           reads=[facb, kb], writes=[kb])
                        rk_, rkb_ = LB[hp][4], LBB[hp][4]
                        OP('dve', lambda e, rt=rt, kt=kt, rk_=rk_, hp=hp: e.scalar_tensor_tensor(out=rk_[:], in0=rt[:], scalar=pv[:, PV['rk'] + hp:PV['rk'] + hp + 1], in1=kt[:], op0=ALU.mult, op1=ALU.mult),
                           reads=[rb, kb, pvB[l]], writes=[rkb_])
                        qe, qeb = LB[hp][0], LBB[hp][0]
                        OP('dve', lambda e, rt=rt, ebt=ebt, qe=qe: e.tensor_tensor(out=qe[:], in0=rt[:], in1=ebt[:], op=ALU.mult), reads=[rb, ebb], writes=[qeb])
                        ke, keb = LB[hp][1], LBB[hp][1]
                        OP('dve', lambda e, kt=kt, enb=enb, ke=ke: e.tensor_tensor(out=ke[:], in0=kt[:], in1=enb[:], op=ALU.mult), reads=[kb, enbb], writes=[keb])
                        be, beb = LB[hp][3], LBB[hp][3]
                        OP('dve', lambda e, kkt=kkt, bt=bt, be=be: e.tensor_tensor(out=be[:], in0=kkt[:], in1=bt[:], op=ALU.mult), reads=[kkb, bb], writes=[beb])
                        OP('dve', lambda e, kkt=kkt, at=at: e.tensor_tensor(out=kkt[:], in0=kkt[:], in1=at[:], op=ALU.mult), reads=[kkb, ab_], writes=[kkb])
                        ae, aeb = LB[hp][2], LBB[hp][2]
                        OP('dve', lambda e, kkt=kkt, enb=enb, ae=ae: e.tensor_tensor(out=ae[:], in0=kkt[:], in1=enb[:], op=ALU.mult), reads=[kkb, enbb], writes=[aeb])
                        QE.append((qe, qeb)); KE.append((ke, keb)); AE.append((ae, aeb)); BE.append((be, beb)); PCx.append((ebt, ebb))
                        GR.append((grt, grb)); RKR.append((rk_, rkb_)); VF.append((vt, vb))
                    proj_tm(l, 1)
                    chunk_engine('rwkv', QE, KE, PCx, rw={'AE': AE, 'BE': BE})
                    Ot = evac_O()
                    for hp in range(2):
                        ob16, ob16b = ba()
                        OP('dve', lambda e, hp=hp, ob16=ob16, Ot=Ot: e.tensor_copy(out=ob16[:], in_=Ot[hp][0][:]), reads=[Ot[hp][1]], writes=[ob16b])
                        bk = pbank()
                        OP('pe', lambda e, bk=bk, ob16=ob16: e.matmul(PS[bk][:, 0:TBM], lhsT=bones[:], rhs=ob16[:], start=True, stop=True), reads=[ob16b, bonesB], writes=[PSB[bk]])
                        ct, cb = fa()
                        OP('dve', lambda e, hp=hp, bk=bk, ct=ct, Ot=Ot: e.scalar_tensor_tensor(out=ct[:], in0=PS[bk][:, 0:TBM], scalar=-1.0 / 64, in1=Ot[hp][0][:], op0=ALU.mult, op1=ALU.add),
                           reads=[PSB[bk], Ot[hp][1]], writes=[cb])
                        s2, s2b = ba()
                        OP('act', lambda e, ct=ct, s2=s2: e.activation(out=s2[:], in_=ct[:], func=AF.Square), reads=[cb], writes=[s2b])
                        bk = pbank()
                        OP('pe', lambda e, bk=bk, s2=s2: e.matmul(PS[bk][:, 0:TBM], lhsT=bones[:], rhs=s2[:], start=True, stop=True), reads=[s2b, bonesB], writes=[PSB[bk]])
                        rn, rnb = fa()
                        rstd_from(PS[bk], PSB[bk], TBM, 1.0 / 64, GN_EPS, rn, rnb)
                        OP('dve', lambda e, hp=hp, ct=ct, rn=rn: e.scalar_tensor_tensor(out=ct[:], in0=ct[:], scalar=pv[:, PV['lnw'] + hp:PV['lnw'] + hp + 1], in1=rn[:], op0=ALU.mult, op1=ALU.mult),
                           reads=[cb, rnb, pvB[l]], writes=[cb])
                        bk = pbank()
                        OP('pe', lambda e, bk=bk, hp=hp, RKR=RKR: e.matmul(PS[bk][:, 0:TBM], lhsT=bones[:], rhs=RKR[hp][0][:], start=True, stop=True), reads=[RKR[hp][1], bonesB], writes=[PSB[bk]])
                        bo, bob = fa()
                        OP('dve', lambda e, bk=bk, hp=hp, bo=bo, VF=VF: e.tensor_tensor(out=bo[:], in0=PS[bk][:, 0:TBM], in1=VF[hp][0][:], op=ALU.mult), reads=[PSB[bk], VF[hp][1]], writes=[bob])
                        OP('dve', lambda e, hp=hp, ct=ct, bo=bo: e.scalar_tensor_tensor(out=ct[:], in0=ct[:], scalar=pv[:, PV['lnb'] + hp:PV['lnb'] + hp + 1], in1=bo[:], op0=ALU.add, op1=ALU.add),
                           reads=[cb, bob, pvB[l]], writes=[cb])
                        OP('dve', lambda e, hp=hp, ct=ct, GR=GR: e.tensor_tensor(out=YT[:, 4 + hp, :], in0=ct[:], in1=GR[hp][0][:], op=ALU.mult),
                           reads=[cb, GR[hp][1]], writes=[YB[4 + hp]])
                else:
                    for ck in (4, 5):
                        OP('pool', lambda e, ck=ck: e.memset(YT[:, ck, :], 0.0), writes=[YB[ck]])

                for jp in range(KD // 2):
                    (i,) = acq(l, ('wo', jp))
                    for q in range(2):
                        j = 2 * jp + q
                        for m in range(KD):
                            OP('pe', lambda e, m=m, j=j, q=q, i=i: e.matmul(PS[m][:, 0:TBM], lhsT=wbf[i][:, q * D + m * 128:q * D + (m + 1) * 128], rhs=YT[:, j, :],
                                                                   start=(j == 0), stop=(j == KD - 1)),
                               reads=[wbfB[i], wbfB2[i][0], wbfB2[i][1], YB[j]], writes=[PSB[m]], sig=(m == KD - 1 or j == KD - 1))
                for m in range(KD):
                    OP('dve', lambda e, m=m: e.tensor_tensor(out=X[:, m, c0:c0 + TBM], in0=PS[m][:, 0:TBM], in1=X[:, m, c0:c0 + TBM], op=ALU.add),
                       reads=[PSB[m], XB[m][ti]], writes=[XB[m][ti]])

            def mixer_setup(l, s):
                OP('sp', lambda e: e.dma_start(out=mub[:], in_=mub_d[l]), writes=[mubB], dsem=muS)
                OP('dve', lambda e: e.tensor_scalar(out=omub[:], in0=mub[:], scalar1=-1.0, scalar2=1.0, op0=ALU.mult, op1=ALU.add), reads=[mubB], writes=[omubB])
                OP('sp', lambda e: e.dma_start(out=smat[:], in_=smat_d[l]), writes=[smatB], dsem=smS)
                OP('pool', lambda e: e.tensor_copy(out=smatb[:], in_=smat[:, 0:256]), reads=[smatB], writes=[smatB])
                for m in ('hgrn', 'gla', 'rwkv'):
                    for hp in range(2):
                        OP('pool', lambda e, m=m, hp=hp: e.memset(S32[m][hp][:], 0.0), writes=[S32B[m][hp]])
                        OP('pool', lambda e, m=m, hp=hp: e.memset(Sbf[m][hp][:], 0.0), writes=[SbfB[m][hp]])

        if do_mix:
            OP('act', lambda e: e.activation(out=lbt[:, 0:2], in_=pvec[0][:, PV['lb0']:PV['lb0'] + 2], func=AF.Exp), reads=[pvB[0]], writes=[lbtB])
            OP('act', lambda e: e.activation(out=lbt[:, 2:4], in_=pvec[L - 1][:, PV['lbl']:PV['lbl'] + 2], func=AF.Exp), reads=[pvB[L - 1], lbtB], writes=[lbtB])
            OP('dve', lambda e: e.tensor_tensor(out=lbt[:, 4:6], in0=lbt[:, 0:2], in1=lbt[:, 2:4], op=ALU.add), reads=[lbtB], writes=[lbtB])
            OP('dve', lambda e: e.reciprocal(out=lbt[:, 4:6], in_=lbt[:, 4:6]), reads=[lbtB], writes=[lbtB])
            OP('dve', lambda e: e.tensor_tensor(out=lbt[:, 6:8], in0=lbt[:, 2:4], in1=lbt[:, 4:6], op=ALU.mult), reads=[lbtB], writes=[lbtB])
            OP('dve', lambda e: e.memset(lbt[:, 4:6], 0.0), reads=[lbtB], writes=[lbtB])
            OP('dve', lambda e: e.tensor_scalar(out=lbt[:, 0:4], in0=lbt[:, 4:8], scalar1=-1.0, scalar2=1.0, op0=ALU.mult, op1=ALU.add), reads=[lbtB], writes=[lbtB])

        for s in range(NS):
            for k in range(KD):
                OP('sp', lambda e, k=k, s=s: e.dma_start(out=X[:, k, :], in_=xT[s, :, k, :]), writes=XB[k], dsem=xS[k])
            for l in range(L):
                if do_ffn:
                    for ti in range(NT):
                        ffn(l, 0, ti)
                if do_mix:
                    mixer_setup(l, s)
                    for bi in range(T // TBM):
                        mixer_block(l, s, bi)
                if do_ffn:
                    for ti in range(NT):
                        ffn(l, 1, ti)
            for ti in range(NT):
                c0 = ti * TB
                for k in range(KD):
                    i = counters['sq'] % 2
                    counters['sq'] += 1
                    OP('act', lambda e, k=k, i=i, c0=c0: e.activation(out=sq[i][:], in_=X[:, k, c0:c0 + TB], func=AF.Square),
                       reads=[XB[k][ti]], writes=[sqB[i]])
                    OP('pe', lambda e, k=k, i=i: e.matmul(PS[0][:, :], lhsT=ones[:], rhs=sq[i][:], start=(k == 0), stop=(k == KD - 1)),
                       reads=[sqB[i], onesB], writes=[PSB[0]])
                rstd_from(PS[0], PSB[0], TB, 1.0 / D, NORM_EPS, rstd, rstdB)
                for k in range(KD):
                    OP('dve', lambda e, k=k, c0=c0: e.scalar_tensor_tensor(out=X[:, k, c0:c0 + TB], in0=X[:, k, c0:c0 + TB],
                                                                        scalar=pvec[0][:, PV['nfin'] + k:PV['nfin'] + k + 1], in1=rstd[:],
                                                                        op0=ALU.mult, op1=ALU.mult),
                       reads=[XB[k][ti], rstdB, pvB[0]], writes=[XB[k][ti]])
            for k in range(KD):
                OP('sp', lambda e, k=k, s=s: e.dma_start(out=outT[s, :, k, :], in_=X[:, k, :]), reads=XB[k], writes=XB[k], dsem=oS[k])
        pg.ops['sp'].append((lambda e: e.nop(), {o_: o_.count for o_ in oS}, False, None))
        pg.emit(block, esems)
    return nc, pg


def _col(v, n):
    return np.ascontiguousarray(np.asarray(v, np.float32).reshape(n, 128).T)


def make_consts():
    cst = np.zeros((128, 1600), np.float32)
    p = np.arange(64)[:, None]
    f = np.arange(64)[None, :]
    for h in range(4):
        cst[0:64, 0 + h * 64:0 + (h + 1) * 64] = (p <= f)
        cst[0:64, 256 + h * 64:256 + (h + 1) * 64] = (p < f)
        cst[0:64, 512 + h * 64:512 + (h + 1) * 64] = (f < p)
        cst[0:64, 768 + h * 64:768 + (h + 1) * 64] = (p == f)
    scm = np.ones(512, np.float32)
    scm[::64] = 0.0
    cst[:, 1024:1536] = scm[None, :]
    wins = {(0, 0): 2, (0, 1): 4, (1, 0): 8, (1, 1): 16}
    for ck in range(2):
        for half in range(2):
            w = wins[(ck, half)]
            t = np.arange(16)
            cst[half * 64:(half + 1) * 64, 1536 + ck * 16:1536 + ck * 16 + 16] = (1.0 / np.minimum(t + 1, w))[None, :]
    return cst


def prep_weights(inp, L):
    out = {}
    f32 = np.float32
    for l in range(L):
        for w, (wi, wo) in enumerate((('ffn1_w_in', 'ffn1_w_out'), ('ffn2_w_in', 'ffn2_w_out'))):
            W = np.asarray(inp[wi][l], f32)
            Wk = W.reshape(KD, 128, 2 * FF)
            g = Wk[:, :, :FF].reshape(KD, 128, NJ, 128)
            u = Wk[:, :, FF:].reshape(KD, 128, NJ, 128)
            blk = np.concatenate([g, u], axis=3)
            out[f"f{w + 1}_win{l}"] = np.ascontiguousarray(blk.transpose(2, 1, 0, 3)).reshape(NJ, 128, KD * 256)
            out[f"f{w + 1}_wout{l}"] = np.ascontiguousarray(np.asarray(inp[wo][l], f32).reshape(NJ, 128, D))
        W = np.asarray(inp['w_in'][l], f32).reshape(KD, 128, DIN)
        fm = np.zeros((len(FMG), 128, KD, 128), f32)
        for gi, (c0, nc_, _) in enumerate(FMG):
            nc_ = min(nc_, DIN - c0)
            fm[gi, :, :, :nc_] = W[:, :, c0:c0 + nc_].transpose(1, 0, 2)
        out[f"m_fm{l}"] = fm.reshape(len(FMG), 128, KD * 128)
        tm = np.zeros((len(TMG), 128, KD, 256), f32)
        for gi, (c0, nc_, _) in enumerate(TMG):
            tm[gi] = W[:, :, c0:c0 + nc_].transpose(1, 0, 2)
        out[f"m_tm{l}"] = tm.reshape(len(TMG), 128, KD * 256)
        out[f"m_wout{l}"] = np.ascontiguousarray(np.asarray(inp['w_out'][l], f32).reshape(KD, 128, D))
        pv = np.zeros((128, NPV), f32)
        pv[:, PV['nf1']:PV['nf1'] + 8] = _col(inp['norm_ffn1'][l], 8)
        pv[:, PV['nmx']:PV['nmx'] + 8] = _col(inp['norm_mix'][l], 8)
        pv[:, PV['nf2']:PV['nf2'] + 8] = _col(inp['norm_ffn2'][l], 8)
        pv[:, PV['nfin']:PV['nfin'] + 8] = _col(inp['norm_final'], 8)
        pv[:, PV['lb0']:PV['lb0'] + 2] = _col(inp['hgrn_lb_logits'][0], 2)
        pv[:, PV['lbl']:PV['lbl'] + 2] = _col(inp['hgrn_lb_logits'][l], 2)
        for nm, key in (('pool_b', 'pool_b'), ('pool_s', 'pool_scale'), ('hnorm', 'hgrn_norm'), ('w0', 'rwkv_w0'), ('a0', 'rwkv_a0'),
                        ('kk', 'rwkv_k_k'), ('ka', 'rwkv_k_a'), ('rk', 'rwkv_r_k'), ('lnw', 'rwkv_ln_w'), ('lnb', 'rwkv_ln_b'),
                        ('glab', 'gla_b'), ('gnorm', 'gla_norm')):
            pv[:, PV[nm]:PV[nm] + 2] = _col(inp[key][l], 2)
        out[f"pvec{l}"] = pv
        out[f"mub{l}"] = np.ascontiguousarray(np.broadcast_to(np.asarray(inp['rwkv_mu'][l], f32)[None, :], (128, 1024)))
        sm = np.zeros((128, 5 * 256), f32)
        pw_ = np.asarray(inp['pool_w'][l], f32)
        for ck in range(2):
            sm[0:64, ck * 128:ck * 128 + 64] = pw_[2 * ck]
            sm[64:128, ck * 128 + 64:ck * 128 + 128] = pw_[2 * ck + 1]
        sm[0:64, 256:512] = np.asarray(inp['rwkv_w2'][l], f32)
        sm[64:128, 768:1024] = np.asarray(inp['rwkv_a2'][l], f32)
        sm[:, 512:768] = np.asarray(inp['rwkv_g2'][l], f32)
        sm[0:16, 1024:1280] = np.asarray(inp['gla_w2'][l], f32)
        out[f"smat{l}"] = sm
    out["cst"] = make_consts()
    return out


def prep_x(xc):
    NS, T, _ = xc.shape
    return np.ascontiguousarray(xc.reshape(NS, T, KD, 128).transpose(0, 3, 2, 1))


def unprep_out(o):
    NS, _, _, T = o.shape
    return np.ascontiguousarray(o.transpose(0, 3, 2, 1)).reshape(NS, T, D)


def kernel(**inputs):
    x = np.asarray(inputs['x'], np.float32)
    B, T, _ = x.shape
    NCORES = 8
    NS = B // NCORES
    L = 2
    nc, pg = build_program(T, NS, L)
    wts = prep_weights(inputs, L)
    in_maps = []
    for c in range(NCORES):
        m = dict(wts)
        m["xT"] = prep_x(x[c * NS:(c + 1) * NS])
        in_maps.append(m)
    res = run_bass_kernel_spmd(nc, in_maps, core_ids=list(range(NCORES)))
    outs = [unprep_out(np.asarray(r["outT"])) for r in res.results]
    return np.concatenate(outs, axis=0).astype(np.float32)
```
